# Optimizing a Trainium2 kernel written in Bass

```python
import math
import jax, jax.numpy as jnp
from jax import lax
import numpy as np

D_MODEL = 2048
BATCH = 4
SEQ = 2048
DEPTH = 4

HEAD_DIM = 128
H_MOBA = 4
H_MLSTM = 4
H_DIL = 4
DK_MLSTM = 128
DV_MLSTM = 256
D_MIX = H_MOBA * HEAD_DIM + H_MLSTM * DV_MLSTM + H_DIL * HEAD_DIM
D_FF = 5632
D_PLE = 256
MOBA_BLOCK = 256
MOBA_TOPK = 3
MOBA_Q_CHUNK = 64
DIL_PAIRS = ((128, 1), (512, 4), (2048, 16))
DIL_BLOCK = 128
MLSTM_CHUNK = 64
CONV_WIDTH = 4
RMS_EPS = 1e-6
NEG_INF = -1e30
N_ALIBI = H_MOBA + H_DIL
IN_SIZES = (3 * H_MOBA * HEAD_DIM,
            3 * H_DIL * HEAD_DIM,
            2 * H_MLSTM * DK_MLSTM,
            H_MLSTM * DV_MLSTM,
            H_MLSTM * DV_MLSTM,
            H_MLSTM,
            H_MLSTM)
D_IN = (3 * H_MOBA * HEAD_DIM + 3 * H_DIL * HEAD_DIM + 2 * H_MLSTM * DK_MLSTM
        + 2 * H_MLSTM * DV_MLSTM + 2 * H_MLSTM)

kernel_name = 'hymba_moba_mlstm_dilated_macaron_ple'


def rms_norm(x, g):
    xf = x.astype(jnp.float32)
    y = xf * lax.rsqrt(jnp.mean(xf * xf, axis=-1, keepdims=True) + RMS_EPS)
    return (y * g.astype(jnp.float32)).astype(x.dtype)


def swiglu(h, w_up, w_down):
    gate, up = jnp.split(h @ w_up, 2, axis=-1)
    return (jax.nn.silu(gate) * up) @ w_down


def alibi_slopes():
    return 2.0 ** (-8.0 * jnp.arange(1, N_ALIBI + 1, dtype=jnp.float32) / N_ALIBI)


def split_heads(x, n):
    b, t, _ = x.shape
    return x.reshape(b, t, n, -1).transpose(0, 2, 1, 3)


def merge_heads(x):
    b, h, t, d = x.shape
    return x.transpose(0, 2, 1, 3).reshape(b, t, h * d)


def moba_attention(q, k, v, slopes):
    B, H, T, hd = q.shape
    f32 = jnp.float32
    scale = hd ** -0.5
    nb = -(-T // MOBA_BLOCK)
    Tp = nb * MOBA_BLOCK
    pad = ((0, 0), (0, 0), (0, Tp - T), (0, 0))
    qp, kp, vp = jnp.pad(q, pad), jnp.pad(k, pad), jnp.pad(v, pad)
    kb = kp.reshape(B, H, nb, MOBA_BLOCK, hd)
    vb = vp.reshape(B, H, nb, MOBA_BLOCK, hd)
    k_mean = jnp.mean(kb.astype(f32), axis=3)
    n_cols = max(nb, MOBA_TOPK)
    nq = Tp // MOBA_Q_CHUNK
    q_chunks = jnp.moveaxis(qp.reshape(B, H, nq, MOBA_Q_CHUNK, hd), 2, 0)
    b_idx = jnp.arange(B)[:, None, None, None]
    h_idx = jnp.arange(H)[None, :, None, None]
    KM = MOBA_TOPK * MOBA_BLOCK

    def one_chunk(args):
        qi, ci = args
        start = ci * MOBA_Q_CHUNK
        cur = start // MOBA_BLOCK
        q_pos = start + jnp.arange(MOBA_Q_CHUNK)
        gate = jnp.einsum('bhqd,bhnd->bhqn', qi.astype(f32), k_mean)
        gate = jnp.where(jnp.arange(nb) < cur, gate, NEG_INF)
        gate = jnp.pad(gate, ((0, 0), (0, 0), (0, 0), (0, n_cols - nb)), constant_values=NEG_INF)
        _, sel = lax.top_k(gate, MOBA_TOPK)
        sel = jnp.minimum(sel, nb - 1)
        valid = jnp.arange(MOBA_TOPK) < cur
        k_sel = kb[b_idx, h_idx, sel]
        v_sel = vb[b_idx, h_idx, sel]
        s_sel = jnp.einsum('bhqd,bhqjkd->bhqjk', qi, k_sel, preferred_element_type=f32) * scale
        dist_sel = (q_pos[:, None, None] - (sel[..., None] * MOBA_BLOCK + jnp.arange(MOBA_BLOCK))).astype(f32)
        s_sel = jnp.where(valid[:, None], s_sel - slopes[:, None, None, None] * dist_sel, NEG_INF)
        k_own = lax.dynamic_slice_in_dim(kp, cur * MOBA_BLOCK, MOBA_BLOCK, axis=2)
        v_own = lax.dynamic_slice_in_dim(vp, cur * MOBA_BLOCK, MOBA_BLOCK, axis=2)
        s_own = jnp.einsum('bhqd,bhkd->bhqk', qi, k_own, preferred_element_type=f32) * scale
        dist_own = q_pos[:, None] - (cur * MOBA_BLOCK + jnp.arange(MOBA_BLOCK))[None, :]
        s_own = jnp.where(dist_own >= 0, s_own - slopes[:, None, None] * dist_own.astype(f32), NEG_INF)
        s = jnp.concatenate([s_sel.reshape(B, H, MOBA_Q_CHUNK, KM), s_own], axis=-1)
        prob = jax.nn.softmax(s, axis=-1).astype(v.dtype)
        p_sel = prob[..., :KM].reshape(B, H, MOBA_Q_CHUNK, MOBA_TOPK, MOBA_BLOCK)
        p_own = prob[..., KM:]
        return (jnp.einsum('bhqjk,bhqjkd->bhqd', p_sel, v_sel)
                + jnp.einsum('bhqk,bhkd->bhqd', p_own, v_own))

    out = lax.map(one_chunk, (q_chunks, jnp.arange(nq)))
    out = jnp.moveaxis(out, 0, 2).reshape(B, H, Tp, hd)
    return out[:, :, :T]


def dilated_branch(q, k, v, slopes, window, dil):
    B, H, T, hd = q.shape
    f32 = jnp.float32
    scale = hd ** -0.5
    L = T // dil
    band = window // dil
    nbl = -(-L // DIL_BLOCK)
    Lp = nbl * DIL_BLOCK

    def to_blocks(a):
        a = a.reshape(B, H, L, dil, hd).transpose(0, 1, 3, 2, 4)
        a = jnp.pad(a, ((0, 0), (0, 0), (0, 0), (0, Lp - L), (0, 0)))
        return a.reshape(B, H, dil, nbl, DIL_BLOCK, hd)

    def with_prev(a):
        prev = jnp.pad(a, ((0, 0), (0, 0), (0, 0), (1, 0), (0, 0), (0, 0)))[:, :, :, :-1]
        return jnp.concatenate([prev, a], axis=4)

    qb = to_blocks(q)
    kc, vc = with_prev(to_blocks(k)), with_prev(to_blocks(v))
    s = jnp.einsum('bhrnqd,bhrnkd->bhrnqk', qb, kc, preferred_element_type=f32) * scale
    k_loc = jnp.arange(2 * DIL_BLOCK)
    delta = (DIL_BLOCK + jnp.arange(DIL_BLOCK))[:, None] - k_loc[None, :]
    k_sub = (jnp.arange(nbl)[:, None] - 1) * DIL_BLOCK + k_loc[None, :]
    mask = (delta >= 0) & (delta <= band) & (k_sub[:, None, :] >= 0)
    s = s - slopes[:, None, None, None, None] * (delta * dil).astype(f32)
    s = jnp.where(mask, s, NEG_INF)
    lse = jax.nn.logsumexp(s, axis=-1)
    prob = jnp.exp(s - lse[..., None]).astype(v.dtype)
    o = jnp.einsum('bhrnqk,bhrnkd->bhrnqd', prob, vc)

    def from_blocks(a):
        a = a.reshape(B, H, dil, Lp, *a.shape[5:])[:, :, :, :L]
        a = jnp.swapaxes(a, 2, 3)
        return a.reshape(B, H, T, *a.shape[4:])

    return from_blocks(o), from_blocks(lse)


def dilated_attention(q, k, v, slopes):
    outs, lses = [], []
    for window, dil in DIL_PAIRS:
        o, lse = dilated_branch(q, k, v, slopes, window, dil)
        outs.append(o.astype(jnp.float32))
        lses.append(lse)
    wts = jax.nn.softmax(jnp.stack(lses, axis=0), axis=0)
    return jnp.sum(wts[..., None] * jnp.stack(outs, axis=0), axis=0).astype(q.dtype)


def mlstm(q, k, v, i_pre, f_pre):
    B, H, T, dk = q.shape
    dv = v.shape[-1]
    f32 = jnp.float32
    nc = T // MLSTM_CHUNK
    q = q.astype(f32)
    k = k.astype(f32) * dk ** -0.5
    v = v.astype(f32)
    log_f = jax.nn.log_sigmoid(f_pre.astype(f32))
    log_i = i_pre.astype(f32)
    causal = jnp.tril(jnp.ones((MLSTM_CHUNK, MLSTM_CHUNK), dtype=bool))

    def chunks(a):
        return jnp.moveaxis(a.reshape(B, H, nc, MLSTM_CHUNK, *a.shape[3:]), 2, 0)

    def step(carry, xs):
        c_st, n_st, m_st = carry
        qc, kc, vc, lf, li = xs
        b = jnp.cumsum(lf, axis=-1)
        d_log = jnp.where(causal, b[..., :, None] - b[..., None, :] + li[..., None, :], NEG_INF)
        m_inter = b + m_st[..., None]
        m_t = jnp.maximum(m_inter, jnp.max(d_log, axis=-1))
        w_inter = jnp.exp(m_inter - m_t)
        s = jnp.einsum('bhtd,bhsd->bhts', qc, kc) * jnp.exp(d_log - m_t[..., None])
        num = (w_inter[..., None] * jnp.einsum('bhtd,bhde->bhte', qc, c_st)
               + jnp.einsum('bhts,bhse->bhte', s, vc))
        den = w_inter * jnp.einsum('bhtd,bhd->bht', qc, n_st) + jnp.sum(s, axis=-1)
        h = num / jnp.maximum(jnp.abs(den), jnp.exp(-m_t))[..., None]
        b_last = b[..., -1]
        g = b_last[..., None] - b + li
        m_new = jnp.maximum(b_last + m_st, jnp.max(g, axis=-1))
        w_old = jnp.exp(b_last + m_st - m_new)
        w_k = jnp.exp(g - m_new[..., None])
        c_new = w_old[..., None, None] * c_st + jnp.einsum('bhs,bhsd,bhse->bhde', w_k, kc, vc)
        n_new = w_old[..., None] * n_st + jnp.einsum('bhs,bhsd->bhd', w_k, kc)
        return (c_new, n_new, m_new), h

    init = (jnp.zeros((B, H, dk, dv), f32), jnp.zeros((B, H, dk), f32), jnp.zeros((B, H), f32))
    _, h = lax.scan(step, init, (chunks(q), chunks(k), chunks(v), chunks(log_f), chunks(log_i)))
    return jnp.moveaxis(h, 0, 2).reshape(B, H, T, dv)


def causal_conv(u, w, b):
    c = u.shape[-1]
    y = lax.conv_general_dilated(u, w.astype(u.dtype)[:, None, :], window_strides=(1,),
                                 padding=((CONV_WIDTH - 1, 0),),
                                 dimension_numbers=('NWC', 'WIO', 'NWC'), feature_group_count=c)
    return y + b.astype(u.dtype)


def token_mix(hn, w_in, conv_w, conv_b, b_igate, b_fgate, g_head, w_out, slopes_moba, slopes_dil):
    z = hn @ w_in
    offs = np.cumsum(IN_SIZES)[:-1].tolist()
    qkv_a, qkv_c, qk_m, v_m, o_m, i_m, f_m = jnp.split(z, offs, axis=-1)
    qa, ka, va = (split_heads(t, H_MOBA) for t in jnp.split(qkv_a, 3, axis=-1))
    out_a = moba_attention(qa, ka, va, slopes_moba)
    qc, kc, vc = (split_heads(t, H_DIL) for t in jnp.split(qkv_c, 3, axis=-1))
    out_c = dilated_attention(qc, kc, vc, slopes_dil)
    qk_m = jax.nn.silu(causal_conv(qk_m, conv_w, conv_b))
    qm, km = jnp.split(qk_m, 2, axis=-1)
    i_pre = jnp.swapaxes(i_m + b_igate, 1, 2)
    f_pre = jnp.swapaxes(f_m + b_fgate, 1, 2)
    h_m = mlstm(split_heads(qm, H_MLSTM), split_heads(km, H_MLSTM), split_heads(v_m, H_MLSTM), i_pre, f_pre)
    h_m = h_m * lax.rsqrt(jnp.mean(h_m * h_m, axis=-1, keepdims=True) + RMS_EPS)
    h_m = h_m * g_head.astype(jnp.float32).reshape(H_MLSTM, 1, DV_MLSTM)
    out_m = (jax.nn.sigmoid(o_m.astype(jnp.float32)) * merge_heads(h_m)).astype(hn.dtype)
    mixed = jnp.concatenate([merge_heads(out_a), out_m, merge_heads(out_c)], axis=-1)
    return mixed @ w_out


def setup_inputs(seed: int = 0) -> dict:
    key = jax.random.key(seed)
    ks = jax.random.split(key, 24)
    f32 = jnp.float32

    def normal(k, shape, scale):
        return jax.random.normal(k, shape, f32) * scale

    def gain(k, shape):
        return 1.0 + normal(k, shape, 0.02)

    return {
        'x': normal(ks[0], (BATCH, SEQ, D_MODEL), 1.0),
        'p': normal(ks[1], (DEPTH, BATCH, SEQ, D_PLE), 1.0),
        'g_ffn1': gain(ks[2], (DEPTH, D_MODEL)),
        'w_up1': normal(ks[3], (DEPTH, D_MODEL, 2 * D_FF), D_MODEL ** -0.5),
        'w_down1': normal(ks[4], (DEPTH, D_FF, D_MODEL), D_FF ** -0.5),
        'g_mix': gain(ks[5], (DEPTH, D_MODEL)),
        'w_in': normal(ks[6], (DEPTH, D_MODEL, D_IN), D_MODEL ** -0.5),
        'conv_w': normal(ks[7], (DEPTH, CONV_WIDTH, 2 * H_MLSTM * DK_MLSTM), CONV_WIDTH ** -0.5),
        'conv_b': normal(ks[8], (DEPTH, 2 * H_MLSTM * DK_MLSTM), 0.01),
        'b_igate': normal(ks[9], (DEPTH, H_MLSTM), 0.1),
        'b_fgate': jnp.linspace(3.0, 6.0, H_MLSTM, dtype=f32)[None, :] + normal(ks[10], (DEPTH, H_MLSTM), 0.1),
        'g_head': gain(ks[11], (DEPTH, H_MLSTM * DV_MLSTM)),
        'w_out': normal(ks[12], (DEPTH, D_MIX, D_MODEL), D_MIX ** -0.5),
        'g_ffn2': gain(ks[13], (DEPTH, D_MODEL)),
        'w_up2': normal(ks[14], (DEPTH, D_MODEL, 2 * D_FF), D_MODEL ** -0.5),
        'w_down2': normal(ks[15], (DEPTH, D_FF, D_MODEL), D_FF ** -0.5),
        'g_ple': gain(ks[16], (DEPTH, D_MODEL)),
        'w_ple_gate': normal(ks[17], (DEPTH, D_MODEL, D_MODEL), D_MODEL ** -0.5),
        'w_ple_proj': normal(ks[18], (DEPTH, D_PLE, D_MODEL), D_PLE ** -0.5),
        'g_final': gain(ks[19], (D_MODEL,)),
    }


def reference(x, p, g_ffn1, w_up1, w_down1, g_mix, w_in, conv_w, conv_b, b_igate, b_fgate,
              g_head, w_out, g_ffn2, w_up2, w_down2, g_ple, w_ple_gate, w_ple_proj, g_final):
    slopes = alibi_slopes()
    slopes_moba, slopes_dil = slopes[0::2], slopes[1::2]
    h = x
    for i in range(DEPTH):
        h = h + 0.5 * swiglu(rms_norm(h, g_ffn1[i]), w_up1[i], w_down1[i])
        h = h + token_mix(rms_norm(h, g_mix[i]), w_in[i], conv_w[i], conv_b[i], b_igate[i],
                          b_fgate[i], g_head[i], w_out[i], slopes_moba, slopes_dil)
        h = h + 0.5 * swiglu(rms_norm(h, g_ffn2[i]), w_up2[i], w_down2[i])
        gate = jax.nn.sigmoid(rms_norm(h, g_ple[i]) @ w_ple_gate[i])
        h = h + gate * (p[i] @ w_ple_proj[i])
    return rms_norm(h, g_final)
```

```python
from contextlib import ExitStack

COMPUTE = ("pe", "act", "dve", "pool")
EPOCH = 30000
NDMA_SLOTS = 24


class Prog:
    def __init__(self, nc):
        self.nc = nc
        self.ops = []
        self.last_w = {}
        self.readers = {}
        self.cnt = {e: 0 for e in COMPUTE}
        self.epoch = {e: 0 for e in COMPUTE}
        self.dma_n = {"sp": 0, "pool": 0, "act": 0}
        self.dma_val = {}
        self.sem_ids = set()
        self._bank = 0

    def barrier(self):
        fin = {}
        for (_e, _f, _d, tok, _i) in self.ops:
            sid = tok[0]
            if sid[0] in COMPUTE:
                k = sid[0]
                if k not in fin or (fin[k][0][1], fin[k][1]) < (sid[1], tok[1]):
                    fin[k] = tok
            else:
                if sid not in fin or fin[sid][1] < tok[1]:
                    fin[sid] = tok
        self.bar = list(fin.values())

    def _deps(self, reads, writes):
        deps = list(getattr(self, "bar", ()))
        for r in reads:
            t = self.last_w.get(r)
            if t is not None:
                deps.append(t)
            if r[0] == "ps":
                deps.extend(self.readers.get(r, ()))
        for w in writes:
            t = self.last_w.get(w)
            if t is not None:
                deps.append(t)
            deps.extend(self.readers.get(w, ()))
        return deps

    def _commit(self, tok, reads, writes):
        for r in reads:
            self.readers.setdefault(r, []).append(tok)
        for w in writes:
            self.last_w[w] = tok
            self.readers[w] = []

    def op(self, eng, fn, reads=(), writes=()):
        deps = self._deps(reads, writes)
        if self.cnt[eng] >= EPOCH:
            self.epoch[eng] += 1
            self.cnt[eng] = 0
        self.cnt[eng] += 1
        sid = (eng, self.epoch[eng])
        self.sem_ids.add(sid)
        tok = (sid, self.cnt[eng], eng)
        self.ops.append((eng, fn, deps, tok, 1))
        self._commit(tok, reads, writes)
        return tok

    def dma(self, q, fn, reads=(), writes=()):
        deps = self._deps(reads, writes)
        slot = self.dma_n[q] % NDMA_SLOTS
        self.dma_n[q] += 1
        sid = ("dma", q, slot)
        self.sem_ids.add(sid)
        prev = self.dma_val.get(sid, 0)
        if prev:
            deps.append((sid, prev, "dma"))
        val = prev + 16
        self.dma_val[sid] = val
        tok = (sid, val, "dma")
        self.ops.append((q, fn, deps, tok, 16))
        self._commit(tok, reads, writes)
        return tok

    def cc(self, fn, reads=(), writes=()):
        deps = self._deps(reads, writes)
        n = self.__dict__.setdefault("_ncc", 0)
        self._ncc = n + 1
        sid = ("cc", n % 8)
        self.sem_ids.add(sid)
        prev = self.dma_val.get(sid, 0)
        if prev:
            deps.append((sid, prev, "dma"))
        val = prev + 1
        self.dma_val[sid] = val
        tok = (sid, val, "dma")
        self.ops.append(("pool", fn, deps, tok, 1))
        self._commit(tok, reads, writes)
        return tok

    def bank(self, lo=0, hi=8):
        if (lo, hi) == (0, 8):
            b = self._bank
            self._bank = (b + 1) % 8
            return b
        st = self.__dict__.setdefault("_rb", {})
        i = st.get((lo, hi), 0)
        st[(lo, hi)] = i + 1
        return lo + i % (hi - lo)

    def build(self, es: ExitStack):
        nc = self.nc
        sems = {}
        for sid in sorted(self.sem_ids, key=str):
            nm = "s_" + "_".join(str(x) for x in sid)
            sems[sid] = es.enter_context(nc.semaphore(nm))
        final = {}
        for (_e, _f, _d, tok, _i) in self.ops:
            final[tok[0]] = max(final.get(tok[0], 0), tok[1])
        ops = self.ops
        block = es.enter_context(nc.Block())

        def emit(eng_name, e):
            waited = {}
            for (en, fn, deps, tok, inc) in ops:
                if en != eng_name:
                    continue
                need = {}
                for (sid, val, deng) in deps:
                    if deng == "pe" and eng_name == "pe":
                        continue
                    if waited.get(sid, 0) >= val:
                        continue
                    if need.get(sid, 0) < val:
                        need[sid] = val
                for sid, val in need.items():
                    e.wait_ge(sems[sid], val)
                    waited[sid] = val
                ins = fn(e)
                ins.then_inc(sems[tok[0]], inc)
            if eng_name == "sp":
                for sid, val in final.items():
                    if waited.get(sid, 0) < val:
                        e.wait_ge(sems[sid], val)

        @block.tensor
        def _(e):
            emit("pe", e)

        @block.scalar
        def _(e):
            emit("act", e)

        @block.vector
        def _(e):
            emit("dve", e)

        @block.gpsimd
        def _(e):
            emit("pool", e)

        @block.sync
        def _(e):
            emit("sp", e)


import numpy as np
import concourse.bass as bass
import concourse.mybir as mybir
from concourse.bass_utils import run_bass_kernel_spmd

F32 = mybir.dt.float32
BF16 = mybir.dt.bfloat16
AF = mybir.ActivationFunctionType
ALU = mybir.AluOpType
AX = mybir.AxisListType

NTOK = 1024
D = 2048
KC_D = 16
DFF = 5632
NFF = 44
WSLOTS = 4
WSLOT_KC = 22


class Ctx:
    def __init__(self, nc, es):
        self.nc = nc
        self.es = es
        self.P = Prog(nc)
        self.n_w = 0
        self.ps = [es.enter_context(nc.psum_tensor("ps%d" % i, [128, 512], F32)) for i in range(8)]

    def sb(self, name, shape, dt):
        return self.es.enter_context(self.nc.sbuf_tensor(name, shape, dt))

    def dram(self, name, shape, dt, kind):
        return self.nc.dram_tensor(name, shape, dt, kind=kind).ap()


def alloc_token_local(C):
    C.hT = C.sb("hT", [128, KC_D, NTOK], F32)
    C.hnT = C.sb("hnT", [128, KC_D, NTOK], BF16)
    C.actT = C.sb("actT", [128, WSLOT_KC, NTOK], BF16)
    C.wbuf = [C.sb("wbuf%d" % i, [128, WSLOT_KC, 128], BF16) for i in range(WSLOTS)]
    C.sq = [C.sb("sq%d" % i, [128, NTOK], BF16) for i in range(2)]
    C.rstd = C.sb("rstd", [128, NTOK], F32)
    C.tmpf = [C.sb("tmpf%d" % i, [128, 512], F32) for i in range(3)]
    C.ones_bf = C.sb("ones_bf", [128, 128], BF16)
    C.gvec = C.sb("gvec", [128, 8, KC_D], F32)
    C.n_sq = 0
    C.n_tmpf = 0
    C.epsv = C.sb("epsv", [128, 1], F32)
    C.P.op("dve", lambda e: e.memset(C.ones_bf[:], 1.0), writes=[("ones_bf",)])
    C.P.op("dve", lambda e: e.memset(C.epsv[:], 1e-6), writes=[("epsv",)])


def load_gain(C, idx, g_dram):
    C.P.dma("sp", lambda e: e.dma_start(out=C.gvec[:, idx, :], in_=g_dram), writes=[("gvec", idx)])


def load_h(C, h_dram):
    for kc in range(KC_D):
        C.P.dma("sp", lambda e, kc=kc: e.dma_start(out=C.hT[:, kc, :], in_=h_dram[kc]),
                writes=[("hT", kc)])


def store_h(C, h_dram):
    for kc in range(KC_D):
        C.P.dma("sp", lambda e, kc=kc: e.dma_start(out=h_dram[kc], in_=C.hT[:, kc, :]),
                reads=[("hT", kc)])


def rmsnorm(C, gidx, out_fn=None):
    P = C.P
    banks = [P.bank(), P.bank()]
    for kc in range(KC_D):
        s = C.n_sq % 2
        C.n_sq += 1
        P.op("act", lambda e, kc=kc, s=s: e.activation(out=C.sq[s][:], in_=C.hT[:, kc, :], func=AF.Square),
             reads=[("hT", kc)], writes=[("sq", s)])
        for th in range(2):
            P.op("pe", lambda e, kc=kc, s=s, th=th: e.matmul(
                C.ps[banks[th]][:], C.ones_bf[:], C.sq[s][:, th * 512:(th + 1) * 512],
                start=(kc == 0), stop=(kc == KC_D - 1)),
                reads=[("sq", s), ("ones_bf",)], writes=[("ps", banks[th])])
    for th in range(2):
        sl = slice(th * 512, (th + 1) * 512)
        P.op("act", lambda e, th=th, sl=sl: e.activation(
            out=C.rstd[:, sl], in_=C.ps[banks[th]][:], func=AF.Ln, bias=C.epsv[:, 0:1], scale=1.0 / D),
            reads=[("ps", banks[th]), ("epsv",)], writes=[("rstd", th)])
        P.op("act", lambda e, sl=sl: e.activation(out=C.rstd[:, sl], in_=C.rstd[:, sl], func=AF.Exp, scale=-0.5),
            reads=[("rstd", th)], writes=[("rstd", th)])
    for kc in range(KC_D):
        if out_fn is None:
            P.op("dve", lambda e, kc=kc: e.scalar_tensor_tensor(
                out=C.hnT[:, kc, :], in0=C.hT[:, kc, :], scalar=C.gvec[:, gidx, kc:kc + 1],
                in1=C.rstd[:], op0=ALU.mult, op1=ALU.mult),
                reads=[("hT", kc), ("rstd", 0), ("rstd", 1), ("gvec", gidx)], writes=[("hnT", kc)])
        else:
            out_fn(kc)


def wload(C, src_ap, kc_n):
    slot = C.n_w % WSLOTS
    C.n_w += 1
    C.P.dma("pool", lambda e: e.dma_start(out=C.wbuf[slot][:, 0:kc_n, :], in_=src_ap),
            writes=[("w", slot)])
    return slot


def mm_fm(C, slot, kc_n, x, xkey, x_kc0, th, bank):
    for kc in range(kc_n):
        C.P.op("pe", lambda e, kc=kc: e.matmul(
            C.ps[bank][:], C.wbuf[slot][:, kc, :], x[:, x_kc0 + kc, th * 512:(th + 1) * 512],
            start=(kc == 0), stop=(kc == kc_n - 1)),
            reads=[("w", slot), (xkey, x_kc0 + kc)], writes=[("ps", bank)])


def ffn(C, gidx, wup, wdown):
    P = C.P
    rmsnorm(C, gidx)
    for half in range(2):
        for cl in range(WSLOT_KC):
            c = half * WSLOT_KC + cl
            sg_ = wload(C, wup[c], KC_D)
            su_ = wload(C, wup[NFF + c], KC_D)
            for th in range(2):
                bg, bu = P.bank(), P.bank()
                mm_fm(C, sg_, KC_D, C.hnT, "hnT", 0, th, bg)
                mm_fm(C, su_, KC_D, C.hnT, "hnT", 0, th, bu)
                t = C.n_tmpf % len(C.tmpf)
                C.n_tmpf += 1
                P.op("act", lambda e, t=t, bg=bg: e.activation(out=C.tmpf[t][:], in_=C.ps[bg][:], func=AF.Silu),
                     reads=[("ps", bg)], writes=[("tmpf", t)])
                P.op("dve", lambda e, t=t, bu=bu, cl=cl, th=th: e.tensor_tensor(
                    out=C.actT[:, cl, th * 512:(th + 1) * 512], in0=C.ps[bu][:], in1=C.tmpf[t][:], op=ALU.mult),
                    reads=[("ps", bu), ("tmpf", t)], writes=[("actT", cl, th)])
        for j in range(KC_D):
            sd = wload(C, wdown[j][:, half * WSLOT_KC:(half + 1) * WSLOT_KC, :], WSLOT_KC)
            for th in range(2):
                b = P.bank()
                for kc in range(WSLOT_KC):
                    P.op("pe", lambda e, kc=kc, th=th, b=b, sd=sd: e.matmul(
                        C.ps[b][:], C.wbuf[sd][:, kc, :], C.actT[:, kc, th * 512:(th + 1) * 512],
                        start=(kc == 0), stop=(kc == WSLOT_KC - 1)),
                        reads=[("w", sd), ("actT", kc, th)], writes=[("ps", b)])
                sl = slice(th * 512, (th + 1) * 512)
                P.op("dve", lambda e, j=j, b=b, sl=sl: e.scalar_tensor_tensor(
                    out=C.hT[:, j, sl], in0=C.ps[b][:], scalar=0.5, in1=C.hT[:, j, sl],
                    op0=ALU.mult, op1=ALU.add),
                    reads=[("ps", b), ("hT", j)], writes=[("hT", j)])


HD_SCALE = 128.0 ** -0.5


def alloc_inproj(C):
    C.stf = [C.sb("stf%d" % i, [128, 512], F32) for i in range(4)]
    C.stb = [C.sb("stb%d" % i, [128, 512], BF16) for i in range(4)]
    C.n_stf = 0
    C.n_stb = 0
    C.kms = C.sb("kms_sb", [128, 16], F32)
    C.wg = C.sb("wg", [128, KC_D, 8], BF16)
    C.gst = C.sb("gst", [128, 8, 8], F32)
    C.vst = [C.sb("vst%d" % i, [128, 128], BF16) for i in range(4)]
    C.n_vst = 0


def _stage_out(C, kind, b, dst_ap, func=None, scale=1.0, eng="act", grp=None):
    P = C.P
    if kind == "f":
        i = C.n_stf % len(C.stf)
        C.n_stf += 1
        buf, key = C.stf[i], ("stf", i)
    else:
        i = C.n_stb % 4
        C.n_stb += 1
        buf, key = C.stb[i], ("stb", i)
    if eng == "act":
        P.op("act", lambda e: e.activation(out=buf[:], in_=C.ps[b][:], func=func or AF.Copy, scale=scale),
             reads=[("ps", b)], writes=[key])
    else:
        P.op("dve", lambda e: e.tensor_copy(out=buf[:], in_=C.ps[b][:]), reads=[("ps", b)], writes=[key])
    P.dma("sp", lambda e: e.dma_start(out=dst_ap, in_=buf[:]), reads=[key], writes=_gk(C, grp))


def _gk(C, grp):
    if grp is None:
        return []
    lst = C.grp_keys.setdefault(grp, [])
    k = ("xw", grp, len(lst), C.grp_epoch)
    lst.append(k)
    return [k]


def inproj(C, gidx, win, win_g, o, group_cb=None):
    P = C.P
    C.grp_keys = {}
    C.grp_epoch = getattr(C, "grp_epoch", 0) + 1

    def done(g):
        if group_cb is not None:
            group_cb(g, C.grp_keys.get(g, []))
    rmsnorm(C, gidx)
    P.dma("pool", lambda e: e.dma_start(out=C.wg[:], in_=win_g), writes=[("wg",)])

    def fm_chunks(lst):
        for (wj, kind, j) in lst:
            slot = wload(C, win[wj], KC_D)
            for th in range(2):
                b = P.bank()
                sl = slice(th * 512, (th + 1) * 512)
                mm_fm(C, slot, KC_D, C.hnT, "hnT", 0, th, b)
                if kind == "qa":
                    _stage_out(C, "b", b, o["qa_bf"][j][:, sl], scale=HD_SCALE)
                    _stage_out(C, "f", b, o["qa_f"][j][:, sl], eng="dve")
                elif kind == "ka":
                    _stage_out(C, "b", b, o["ka_bf"][j][:, sl], grp="kk")
                    P.op("dve", lambda e, j=j, th=th, b=b: e.tensor_reduce(
                        out=C.kms[:, j * 4 + th * 2:j * 4 + th * 2 + 2],
                        in_=C.ps[b][:].rearrange("p (a c) -> p a c", a=2), axis=AX.X, op=ALU.add),
                        reads=[("ps", b)], writes=[("kms", j, th)])
                elif kind == "qc":
                    _stage_out(C, "b", b, o["qc_bf"][j][:, sl], scale=HD_SCALE)
                elif kind == "kc":
                    _stage_out(C, "b", b, o["kc_bf"][j][:, sl], grp="kk")
                elif kind == "uq":
                    _stage_out(C, "f", b, o["uq"][j][:, sl], grp="uq")
                elif kind == "uk":
                    _stage_out(C, "f", b, o["uk"][j][:, sl], grp="uk")
                elif kind == "om":
                    _stage_out(C, "f", b, o["om"][j][:, sl], func=AF.Sigmoid)
    fm_chunks([(4 + j, "ka", j) for j in range(4)] + [(16 + j, "kc", j) for j in range(4)])
    done("kk")
    vt = [(8 + j, j) for j in range(4)] + [(20 + j, 4 + j) for j in range(4)] + [(32 + j, 8 + j) for j in range(8)]
    for (wj, cg) in vt:
        slot = wload(C, win[wj], KC_D)
        for tt in range(8):
            b = P.bank()
            for kc in range(KC_D):
                P.op("pe", lambda e, kc=kc, tt=tt, b=b, slot=slot: e.matmul(
                    C.ps[b][:, 0:128], C.hnT[:, kc, tt * 128:(tt + 1) * 128], C.wbuf[slot][:, kc, :],
                    start=(kc == 0), stop=(kc == KC_D - 1)),
                    reads=[("w", slot), ("hnT", kc)], writes=[("ps", b)])
            i = C.n_vst % 4
            C.n_vst += 1
            P.op("act" if tt % 2 else "dve",
                 (lambda e, i=i, b=b: e.activation(out=C.vst[i][:], in_=C.ps[b][:, 0:128], func=AF.Copy)) if tt % 2 else
                 (lambda e, i=i, b=b: e.tensor_copy(out=C.vst[i][:], in_=C.ps[b][:, 0:128])),
                 reads=[("ps", b)], writes=[("vst", i)])
            P.dma("sp", lambda e, i=i, tt=tt, cg=cg: e.dma_start(
                out=o["v_tm"][tt][:, cg * 128:(cg + 1) * 128], in_=C.vst[i][:]), reads=[("vst", i)],
                writes=_gk(C, "v0" if tt < 4 else "v1"))
    done("v0")
    done("v1")
    fm_chunks([(28 + j, "uk", j) for j in range(4)])
    done("uk")
    fm_chunks([(24 + j, "uq", j) for j in range(4)])
    done("uq")
    for tt in range(8):
        b = P.bank()
        for kc in range(KC_D):
            P.op("pe", lambda e, kc=kc, tt=tt, b=b: e.matmul(
                C.ps[b][:, 0:8], C.hnT[:, kc, tt * 128:(tt + 1) * 128], C.wg[:, kc, :],
                start=(kc == 0), stop=(kc == KC_D - 1)),
                reads=[("wg",), ("hnT", kc)], writes=[("ps", b)])
        P.op("dve", lambda e, tt=tt, b=b: e.tensor_copy(out=C.gst[:, tt, :], in_=C.ps[b][:, 0:8]),
             reads=[("ps", b)], writes=[("gst", tt)])
    P.dma("sp", lambda e: e.dma_start(out=o["gates"].rearrange("t p c -> p t c"), in_=C.gst[:]),
          reads=[("gst", tt) for tt in range(8)], writes=_gk(C, "gk"))
    P.dma("sp", lambda e: e.dma_start(out=o["kms"], in_=C.kms[:]),
          reads=[("kms", j, th) for j in range(4) for th in range(2)], writes=_gk(C, "gk"))
    done("gk")
    fm_chunks([(j, "qa", j) for j in range(4)] + [(12 + j, "qc", j) for j in range(4)] + [(40 + j, "om", j) for j in range(8)])


def outproj(C, mixT_dram, wout):
    P = C.P
    for kc in range(KC_D):
        P.dma("sp", lambda e, kc=kc: e.dma_start(out=C.hnT[:, kc, :], in_=mixT_dram[kc]), writes=[("hnT", kc)])
    for j in range(KC_D):
        slot = wload(C, wout[j], KC_D)
        for th in range(2):
            b = P.bank()
            sl = slice(th * 512, (th + 1) * 512)
            mm_fm(C, slot, KC_D, C.hnT, "hnT", 0, th, b)
            P.op("dve", lambda e, j=j, b=b, sl=sl: e.tensor_tensor(
                out=C.hT[:, j, sl], in0=C.ps[b][:], in1=C.hT[:, j, sl], op=ALU.add),
                reads=[("ps", b), ("hT", j)], writes=[("hT", j)])


def ple(C, gidx, wpg, wpp, pT_dram):
    P = C.P
    rmsnorm(C, gidx)
    for kc in range(2):
        P.dma("pool", lambda e, kc=kc: e.dma_start(out=C.actT[:, kc, :], in_=pT_dram[kc]), writes=[("actT", kc, 0), ("actT", kc, 1)])
    for j in range(KC_D):
        sg = wload(C, wpg[j], KC_D)
        sp_ = wload(C, wpp[j], 2)
        for th in range(2):
            bg, bp = P.bank(), P.bank()
            sl = slice(th * 512, (th + 1) * 512)
            mm_fm(C, sg, KC_D, C.hnT, "hnT", 0, th, bg)
            for kc in range(2):
                P.op("pe", lambda e, kc=kc, th=th, bp=bp, sp_=sp_: e.matmul(
                    C.ps[bp][:], C.wbuf[sp_][:, kc, :], C.actT[:, kc, th * 512:(th + 1) * 512],
                    start=(kc == 0), stop=(kc == 1)),
                    reads=[("w", sp_), ("actT", kc, th)], writes=[("ps", bp)])
            t = C.n_tmpf % len(C.tmpf)
            C.n_tmpf += 1
            P.op("act", lambda e, t=t, bg=bg: e.activation(out=C.tmpf[t][:], in_=C.ps[bg][:], func=AF.Sigmoid),
                 reads=[("ps", bg)], writes=[("tmpf", t)])
            P.op("dve", lambda e, t=t, bp=bp: e.tensor_tensor(
                out=C.tmpf[t][:], in0=C.ps[bp][:], in1=C.tmpf[t][:], op=ALU.mult),
                reads=[("ps", bp), ("tmpf", t)], writes=[("tmpf", t)])
            P.op("dve", lambda e, t=t, j=j, sl=sl: e.tensor_tensor(
                out=C.hT[:, j, sl], in0=C.hT[:, j, sl], in1=C.tmpf[t][:], op=ALU.add),
                reads=[("hT", j), ("tmpf", t)], writes=[("hT", j)])


def final_norm(C, gidx, out_dram):
    P = C.P

    def out_fn(kc):
        for th in range(2):
            sl = slice(th * 512, (th + 1) * 512)
            i = C.n_stf % len(C.stf)
            C.n_stf += 1
            P.op("dve", lambda e, kc=kc, sl=sl, i=i: e.scalar_tensor_tensor(
                out=C.stf[i][:], in0=C.hT[:, kc, sl], scalar=C.gvec[:, gidx, kc:kc + 1],
                in1=C.rstd[:, sl], op0=ALU.mult, op1=ALU.mult),
                reads=[("hT", kc), ("rstd", 0), ("rstd", 1), ("gvec", gidx)], writes=[("stf", i)])
            P.dma("sp", lambda e, kc=kc, sl=sl, i=i: e.dma_start(out=out_dram[kc][:, sl], in_=C.stf[i][:]),
                  reads=[("stf", i)])
    rmsnorm(C, gidx, out_fn=out_fn)


NEG = -30000.0
SLOPES_MOBA = [2.0 ** -1, 2.0 ** -3, 2.0 ** -5, 2.0 ** -7]
SLOPES_DIL = [2.0 ** -2, 2.0 ** -4, 2.0 ** -6, 2.0 ** -8]


def mixer_consts(is_b):
    kk = np.arange(128)[:, None]
    qq = np.arange(512)[None, :]

    def lnmult(dist):
        m = ((dist >= 0) & (dist <= 128)).astype(np.float64)
        m += ((dist >= 0) & (dist % 4 == 0) & (dist <= 512))
        m += ((dist >= 0) & (dist % 16 == 0) & (dist <= 2048))
        return np.where(m > 0, np.log(np.maximum(m, 1)), NEG).astype(np.float32)

    dil_loc = np.stack([lnmult(128 * d + qq - kk) for d in range(-3, 5)], axis=1)
    dil_rem = np.stack([lnmult(128 * d + qq - kk) for d in range(1, 6)], axis=1)
    if not is_b:
        dil_rem = np.full_like(dil_rem, NEG)
    cau = np.stack([np.where(128 * d + qq - kk >= 0, 0.0, NEG) for d in range(-3, 1)], axis=1).astype(np.float32)
    step = (cau == 0).astype(np.float32)
    pk = np.arange(2048)
    posK = np.stack([128.0 * (pk // 128), pk % 128, np.ones(2048), np.ones(2048)]).astype(np.float32)
    pq = 1024 + np.arange(1024)
    posQ = np.zeros((4, 8, 1024), np.float32)
    for i, s in enumerate(SLOPES_MOBA + SLOPES_DIL):
        posQ[0, i] = s
        posQ[1, i] = s
        posQ[2, i] = -s * 128.0 * (pq // 128)
        posQ[3, i] = -s * (pq % 128)
    E8 = np.zeros((8, 8 * 128), np.float32)
    for n in range(8):
        E8[n, n * 128:(n + 1) * 128] = 1.0
    G = np.zeros((1024, 8), np.float32)
    t = np.arange(1024)
    own = 4 + t // 256
    for n in range(8):
        G[:, n] = np.where(n == own, 1e30, np.where(n < own, 0.0, -1e30))
    if not is_b:
        G[:, 0:4] = -1e30
    G = np.ascontiguousarray(G.reshape(8, 128, 8).transpose(1, 0, 2))
    ident = np.eye(128, dtype=np.float32)
    U = np.triu(np.ones((128, 128), np.float32))
    rbias = np.full((128, 1), 0.0 if is_b else NEG, np.float32)
    return dict(dil_loc=dil_loc, dil_rem=dil_rem, cau=cau, step=step, posK=posK, posQ=posQ, E8=E8, G=G,
                ident=ident, U=U, rbias=rbias)


CONST_SHAPES = dict(dil_loc=[128, 8, 512], dil_rem=[128, 5, 512], cau=[128, 4, 512], step=[128, 4, 512],
                    posK=[4, 2048], posQ=[4, 8, 1024], E8=[8, 1024], G=[128, 8, 8], ident=[128, 128],
                    U=[128, 128], rbias=[128, 1])


def alloc_mixer(C):
    sb = C.sb
    C.m_dil_loc = sb("m_dil_loc", [128, 8, 512], F32)
    C.m_dil_rem = sb("m_dil_rem", [128, 5, 512], F32)
    C.m_cau = sb("m_cau", [128, 4, 512], F32)
    C.m_step = sb("m_step", [128, 4, 512], F32)
    C.m_posK = sb("m_posK", [4, 2048], BF16)
    C.m_posQ = sb("m_posQ", [4, 1024], BF16)
    C.m_E8 = sb("m_E8", [8, 1024], BF16)
    C.m_G = sb("m_G", [128, 8, 8], F32)
    C.m_ident = sb("m_ident", [128, 128], F32)
    C.m_U = sb("m_U", [128, 128], F32)
    C.m_rbias = sb("m_rbias", [128, 1], F32)
    C.m_onesf = sb("m_onesf", [128, 512], F32)
    C.m_cw = sb("m_cw", [128, 8, 4], F32)
    C.m_cb = sb("m_cb", [128, 8], F32)
    C.m_gb = sb("m_gb", [128, 8], F32)
    C.m_gh = sb("m_gh", [128, 8], F32)
    C.m_eps = sb("m_eps", [128, 1], F32)
    C.m_qT = sb("m_qT", [128, 1024], BF16)
    C.m_kT = sb("m_kT", [128, 2048], BF16)
    C.m_V = sb("m_V", [128, 16, 256], BF16)
    C.m_qf = sb("m_qf", [128, 1024], F32)
    C.m_kms = sb("m_kms", [128, 8], F32)
    C.m_gs = sb("m_gs", [128, 8], F32)
    C.m_m8 = sb("m_m8", [128, 8], F32)
    C.m_sel = sb("m_sel", [128, 8], F32)
    C.m_val = sb("m_val", [128, 8], F32)
    C.m_gs4 = [C.m_gs] + [sb("m_gs_%d" % i, [128, 8], F32) for i in range(3)]
    C.m_m84 = [C.m_m8] + [sb("m_m8_%d" % i, [128, 8], F32) for i in range(3)]
    C.m_sel4 = [C.m_sel] + [sb("m_sel_%d" % i, [128, 8], F32) for i in range(3)]
    C.m_val4 = [C.m_val] + [sb("m_val_%d" % i, [128, 8], F32) for i in range(3)]
    C.m_selbT = sb("m_selbT", [8, 1024], BF16)
    C.m_pT = [sb("m_pT%d" % i, [128, 512], BF16) for i in range(3)]
    C.m_tmp = [sb("m_tmp%d" % i, [128, 512], F32) for i in range(3)]
    C.m_rl = sb("m_rl", [128, 512], F32)
    C.m_ost = [sb("m_ost%d" % i, [128, 512], BF16) for i in range(2)]
    C.m_gl = sb("m_gl", [128, 16, 8], F32)
    C.m_lf = sb("m_lf", [128, 16, 4], F32)
    C.m_a = sb("m_a", [128, 16, 4], F32)
    C.m_lfrep = sb("m_lfrep", [128, 16, 128], F32)
    C.m_Fbc = sb("m_Fbc", [128, 1024], F32)
    C.m_uqb = sb("m_uqb", [128, 1027], F32)
    C.m_ukb = sb("m_ukb", [128, 2051], F32)
    C.m_acc = sb("m_acc", [128, 2048], F32)
    C.m_hm = [sb("m_hm%d" % i, [128, 512], F32) for i in range(2)]
    C.m_sqb = sb("m_sqb", [128, 512], BF16)
    C.m_omb = [sb("m_omb%d" % i, [128, 512], F32) for i in range(2)]
    C.m_onesb = sb("m_onesb", [128, 128], BF16)
    C.n_pT = 0
    C.n_tmp = 0
    C.n_ost = 0
    C.n_omb = 0
    C.m_tmpx = C.m_tmp
    C.m_pTx = C.m_pT + [sb("m_pT3", [128, 512], BF16)]


def load_mixer_consts(C, cd, conv_w, conv_b, gbias, ghead):
    P = C.P
    C.cd = cd
    for nm, q in (("dil_loc", "sp"), ("dil_rem", "sp"), ("cau", "sp"), ("step", "sp"), ("posK", "pool"),
                  ("E8", "pool"), ("G", "sp"), ("ident", "sp"), ("U", "sp"), ("rbias", "sp")):
        dst = getattr(C, "m_" + nm)
        P.dma(q, lambda e, dst=dst, nm=nm: e.dma_start(out=dst[:], in_=cd[nm]), writes=[("c_" + nm,)])
    P.dma("sp", lambda e: e.dma_start(out=C.m_cw[:], in_=conv_w), writes=[("c_cw",)])
    P.dma("sp", lambda e: e.dma_start(out=C.m_cb[:], in_=conv_b), writes=[("c_cb",)])
    P.dma("sp", lambda e: e.dma_start(out=C.m_gb[:], in_=gbias), writes=[("c_gb",)])
    P.dma("sp", lambda e: e.dma_start(out=C.m_gh[:], in_=ghead), writes=[("c_gh",)])
    P.op("dve", lambda e: e.memset(C.m_onesf[:], 1.0), writes=[("c_onesf",)])
    P.op("dve", lambda e: e.memset(C.m_onesb[:], 1.0), writes=[("c_onesb",)])
    P.op("dve", lambda e: e.memset(C.m_eps[:], 1e-6), writes=[("c_eps",)])
    P.op("dve", lambda e: e.memset(C.m_ukb[:, 0:3], 0.0), writes=[("ukb_pad",)])


def load_V(C, parts, tile0, c0, w, key):
    if not isinstance(parts, (list, tuple)):
        parts = [parts]
    t0 = tile0
    for ap in parts:
        n = ap.shape[0]
        C.P.dma("sp", lambda e, ap=ap, t0=t0, n=n: e.dma_start(
            out=C.m_V[:, t0:t0 + n, 0:w], in_=ap.rearrange("t p c -> p t c")[:, :, c0:c0 + w]), writes=[key])
        t0 += n


def _ktiles(qh):
    lst = [(kt, True, qh * 4 + 8 - kt) for kt in range(8)]
    lst += [(8 + lt, False, qh * 4 - lt) for lt in range(4 * (qh + 1))]
    return lst


def attn_group(C, kind, I, mixT):
    P = C.P
    moba = kind == "moba"
    if getattr(C, "fused", False):
        load_tabs(C, kind)
    qb, kL, kR = (I["qa_bf"], I["ka_bf_L"], I["ka_bf_R"]) if moba else (I["qc_bf"], I["kc_bf_L"], I["kc_bf_R"])
    vcol0 = 0 if moba else 512
    def head(h):
        hh = h if moba else 4 + h
        P.dma("pool", lambda e: e.dma_start(out=C.m_posQ[:], in_=C.cd["posQ"][:, hh, :]), writes=[("c_posQ",)])
        P.dma("sp", lambda e: e.dma_start(out=C.m_qT[:], in_=qb[h]), writes=[("m_qT",)])
        P.dma("sp", lambda e: e.dma_start(out=C.m_kT[:, 0:1024], in_=kR[h]), writes=[("m_kT", 0)])
        P.dma("sp", lambda e: e.dma_start(out=C.m_kT[:, 1024:2048], in_=kL[h]), writes=[("m_kT", 1)])
        c0 = vcol0 + h * 128
        load_V(C, I["v_tm_R"], 0, c0, 128, ("m_V", 0))
        load_V(C, I["v_tm_L"], 8, c0, 128, ("m_V", 1))
        if moba:
            P.dma("sp", lambda e: e.dma_start(out=C.m_qf[:], in_=I["qa_f"][h]), writes=[("m_qf",)])
            P.dma("sp", lambda e: e.dma_start(out=C.m_kms[:, 0:4], in_=I["kms_R"][:, h * 4:h * 4 + 4]), writes=[("m_kms", 0)])
            P.dma("sp", lambda e: e.dma_start(out=C.m_kms[:, 4:8], in_=I["kms_L"][:, h * 4:h * 4 + 4]), writes=[("m_kms", 1)])
            for qt in range(8):
                b = P.bank()
                g_ = qt % 4
                gs, m8, sel, val = C.m_gs4[g_], C.m_m84[g_], C.m_sel4[g_], C.m_val4[g_]
                kg = lambda n, g_=g_: (n, g_)
                P.op("pe", lambda e, qt=qt, b=b: e.matmul(C.ps[b][:, 0:8], C.m_qf[:, qt * 128:(qt + 1) * 128], C.m_kms[:],
                                                          start=True, stop=True),
                     reads=[("m_qf",), ("m_kms", 0), ("m_kms", 1)], writes=[("ps", b)])
                P.op("dve", lambda e, qt=qt, b=b, gs=gs: e.tensor_tensor(out=gs[:], in0=C.ps[b][:, 0:8], in1=C.m_G[:, qt, :], op=ALU.add),
                     reads=[("ps", b), ("c_G",)], writes=[kg("m_gs")])
                P.op("dve", lambda e, gs=gs, m8=m8: e.max(out=m8[:], in_=gs[:]), reads=[kg("m_gs")], writes=[kg("m_m8")])
                P.op("dve", lambda e, gs=gs, m8=m8, sel=sel: e.tensor_scalar(out=sel[:], in0=gs[:], scalar1=m8[:, 3:4], scalar2=None, op0=ALU.is_ge),
                     reads=[kg("m_gs"), kg("m_m8")], writes=[kg("m_sel")])
                P.op("dve", lambda e, gs=gs, val=val: e.tensor_scalar(out=val[:], in0=gs[:], scalar1=-1e29, scalar2=None, op0=ALU.is_gt),
                     reads=[kg("m_gs")], writes=[kg("m_val")])
                P.op("dve", lambda e, sel=sel, val=val: e.tensor_tensor(out=sel[:], in0=sel[:], in1=val[:], op=ALU.mult),
                     reads=[kg("m_sel"), kg("m_val")], writes=[kg("m_sel")])
                P.op("dve", lambda e, sel=sel: e.tensor_scalar(out=sel[:], in0=sel[:], scalar1=1.0, scalar2=-NEG,
                                                               op0=ALU.subtract, op1=ALU.mult),
                     reads=[kg("m_sel")], writes=[kg("m_sel")])
                b2 = P.bank()
                P.op("pe", lambda e, b2=b2, sel=sel: e.matmul(C.ps[b2][0:8, 0:128], sel[:], C.m_ident[:], start=True, stop=True),
                     reads=[kg("m_sel"), ("c_ident",)], writes=[("ps", b2)])
                P.op("act", lambda e, qt=qt, b2=b2: e.activation(out=C.m_selbT[:, qt * 128:(qt + 1) * 128], in_=C.ps[b2][0:8, 0:128], func=AF.Copy),
                     reads=[("ps", b2)], writes=[("m_selbT", qt)])

        def qhalf(qh):
            qsl = slice(qh * 512, (qh + 1) * 512)
            bo = P.bank(0, 4)
            bl = P.bank(0, 4)
            tiles = _ktiles(qh)
            def stage_S(ti):
                kt, rem, dl = tiles[ti]
                bs = P.bank(4, 8)
                ksl = slice(kt * 128, (kt + 1) * 128)
                P.op("pe", lambda e, bs=bs, ksl=ksl: e.matmul(C.ps[bs][:], C.m_kT[:, ksl], C.m_qT[:, qsl], start=True, stop=False),
                     reads=[("m_kT", 0 if rem else 1), ("m_qT",)], writes=[("ps", bs)])
                P.op("pe", lambda e, bs=bs, ksl=ksl: e.matmul(C.ps[bs][:], C.m_posK[:, ksl], C.m_posQ[:, qsl], start=False, stop=(not moba)),
                     reads=[("c_posK",), ("c_posQ",)], writes=[("ps", bs)])
                if moba:
                    n = kt // 2
                    P.op("pe", lambda e, bs=bs, n=n: e.matmul(C.ps[bs][:], C.m_E8[:, n * 128:(n + 1) * 128], C.m_selbT[:, qsl], start=False, stop=True),
                         reads=[("c_E8",)] + [("m_selbT", qh * 4 + i) for i in range(4)], writes=[("ps", bs)])
                tab = None
                if not moba:
                    tab = C.m_dil_rem[:, min(dl, 5) - 1, :] if rem else C.m_dil_loc[:, dl + 3, :]
                elif (not rem) and dl <= 0:
                    tab = C.m_cau[:, dl + 3, :]
                pi = C.n_pT % len(C.m_pT)
                C.n_pT += 1
                if tab is not None:
                    t = C.n_tmp % 3
                    C.n_tmp += 1
                    P.op("dve", lambda e, bs=bs, t=t, tab=tab: e.tensor_tensor(out=C.m_tmp[t][:], in0=C.ps[bs][:], in1=tab, op=ALU.add),
                         reads=[("ps", bs), ("c_dil_loc",), ("c_dil_rem",), ("c_cau",)], writes=[("m_tmp", t)])
                    P.op("act", lambda e, t=t, pi=pi: e.activation(out=C.m_pT[pi][:], in_=C.m_tmp[t][:], func=AF.Exp),
                         reads=[("m_tmp", t)], writes=[("m_pT", pi)])
                else:
                    P.op("act", lambda e, bs=bs, pi=pi: e.activation(out=C.m_pT[pi][:], in_=C.ps[bs][:], func=AF.Exp),
                         reads=[("ps", bs)], writes=[("m_pT", pi)])
                return pi

            def stage_V(ti, pi):
                kt, rem, dl = tiles[ti]
                first, last = ti == 0, ti == len(tiles) - 1
                P.op("pe", lambda e, kt=kt, pi=pi, first=first, last=last: e.matmul(
                    C.ps[bo][:], C.m_V[:, kt, 0:128], C.m_pT[pi][:], start=first, stop=last),
                    reads=[("m_V", 0 if rem else 1), ("m_pT", pi)], writes=[("ps", bo)])
                P.op("pe", lambda e, pi=pi, first=first, last=last: e.matmul(
                    C.ps[bl][:], C.m_onesb[:], C.m_pT[pi][:], start=first, stop=last),
                    reads=[("c_onesb",), ("m_pT", pi)], writes=[("ps", bl)])
            LA = 3 if len(C.m_pT) >= 4 else 2
            pis = {}
            for step in range(len(tiles) + LA):
                if step < len(tiles):
                    pis[step] = stage_S(step)
                if step - LA >= 0:
                    stage_V(step - LA, pis[step - LA])
            P.op("act", lambda e: e.activation(out=C.m_rl[:], in_=C.ps[bl][:], func=AF.Ln), reads=[("ps", bl)], writes=[("m_rl",)])
            P.op("act", lambda e: e.activation(out=C.m_rl[:], in_=C.m_rl[:], func=AF.Exp, scale=-1.0), reads=[("m_rl",)], writes=[("m_rl",)])
            oi = C.n_ost % 2
            C.n_ost += 1
            P.op("dve", lambda e, oi=oi: e.tensor_tensor(out=C.m_ost[oi][:], in0=C.ps[bo][:], in1=C.m_rl[:], op=ALU.mult),
                 reads=[("ps", bo), ("m_rl",)], writes=[("m_ost", oi)])
            ch = h if moba else 12 + h
            P.dma("sp", lambda e, oi=oi, ch=ch: e.dma_start(out=mixT[ch][:, qsl], in_=C.m_ost[oi][:]), reads=[("m_ost", oi)])
        for qh in range(2):
            qhalf(qh)
    for h in range(4):
        head(h)


DK_SCALE = 128.0 ** -0.5


def mlstm_group(C, I, mixT):
    P = C.P
    if getattr(C, "fused", False):
        load_tabs(C, "mlstm")
        l_ = C.m_layer
        cw_, cb_, gb_, gh_ = C.m_cw[:, l_], C.m_cb[:, l_], C.m_gb[:, l_], C.m_gh[:, l_]
    else:
        cw_, cb_, gb_, gh_ = C.m_cw, C.m_cb, C.m_gb, C.m_gh
    P.dma("sp", lambda e: e.dma_start(out=C.m_gl[:, 0:8, :], in_=I["gates_R"].rearrange("t p c -> p t c")), writes=[("m_gl", 0)])
    P.dma("sp", lambda e: e.dma_start(out=C.m_gl[:, 8:16, :], in_=I["gates_L"].rearrange("t p c -> p t c")), writes=[("m_gl", 1)])
    for t in range(16):
        P.op("dve", lambda e, t=t: e.tensor_tensor(out=C.m_gl[:, t, :], in0=C.m_gl[:, t, :], in1=gb_[:], op=ALU.add),
             reads=[("m_gl", 0), ("m_gl", 1), ("c_gb",)], writes=[("m_gl", 0), ("m_gl", 1)])
    P.op("act", lambda e: e.activation(out=C.m_lf[:], in_=C.m_gl[:, :, 4:8], func=AF.Exp, scale=-1.0),
         reads=[("m_gl", 0), ("m_gl", 1)], writes=[("m_lf",)])
    P.op("act", lambda e: e.activation(out=C.m_lf[:], in_=C.m_lf[:], func=AF.Ln, bias=C.m_onesf[:, 0:1], scale=1.0),
         reads=[("m_lf",), ("c_onesf",)], writes=[("m_lf",)])
    P.op("dve", lambda e: e.tensor_scalar(out=C.m_lf[:], in0=C.m_lf[:], scalar1=-1.0, scalar2=None, op0=ALU.mult),
         reads=[("m_lf",)], writes=[("m_lf",)])
    P.op("dve", lambda e: e.tensor_scalar(out=C.m_gl[:, 0:8, 0:4], in0=C.m_gl[:, 0:8, 0:4], scalar1=C.m_rbias[:, 0:1],
                                          scalar2=None, op0=ALU.add),
         reads=[("m_gl", 0), ("c_rbias",)], writes=[("m_gl", 0)])
    for i in range(16):
        b = P.bank()
        for r in range(i + 1):
            lhs = C.m_U if r == i else C.m_onesf
            P.op("pe", lambda e, r=r, b=b, lhs=lhs, i=i: e.matmul(C.ps[b][:, 0:4], lhs[:, 0:128], C.m_lf[:, r, :],
                                                             start=(r == 0), stop=(r == i)),
                 reads=[("m_lf",), ("c_U",), ("c_onesf",)], writes=[("ps", b)])
        P.op("dve", lambda e, i=i, b=b: e.tensor_tensor(out=C.m_a[:, i, :], in0=C.m_gl[:, i, 0:4], in1=C.ps[b][:, 0:4], op=ALU.subtract),
             reads=[("ps", b), ("m_gl", 0), ("m_gl", 1)], writes=[("m_a",)])
    def head(h):
        for t in range(16):
            P.op("dve", lambda e, t=t: e.tensor_scalar(out=C.m_lfrep[:, t, :], in0=C.m_onesf[:, 0:128], scalar1=C.m_lf[:, t, h:h + 1],
                                                       scalar2=None, op0=ALU.mult),
                 reads=[("m_lf",), ("c_onesf",)], writes=[("m_lfrep",)])
        for qh in range(2):
            b = P.bank()
            tl = _ktiles(qh)
            for ti, (kt, rem, dl) in enumerate(tl):
                rhs = C.m_onesf[:, :] if (rem or dl >= 1) else C.m_step[:, dl + 3, :]
                P.op("pe", lambda e, kt=kt, rhs=rhs, ti=ti, b=b, n=len(tl): e.matmul(
                    C.ps[b][:], C.m_lfrep[:, kt, :], rhs, start=(ti == 0), stop=(ti == n - 1)),
                    reads=[("m_lfrep",), ("c_onesf",), ("c_step",)], writes=[("ps", b)])
            P.op("act", lambda e, b=b, qh=qh: e.activation(out=C.m_Fbc[:, qh * 512:(qh + 1) * 512], in_=C.ps[b][:], func=AF.Copy),
                 reads=[("ps", b)], writes=[("m_Fbc", qh)])
        P.dma("sp", lambda e: e.dma_start(out=C.m_uqb[:, 0:3], in_=I["uq_R"][h][:, 1021:1024]), writes=[("m_uqb", 0)])
        P.dma("sp", lambda e: e.dma_start(out=C.m_uqb[:, 3:1027], in_=I["uq_L"][h]), writes=[("m_uqb", 1)])
        P.dma("sp", lambda e: e.dma_start(out=C.m_ukb[:, 3:1027], in_=I["uk_R"][h]), writes=[("m_ukb", 0)])
        P.dma("sp", lambda e: e.dma_start(out=C.m_ukb[:, 1027:2051], in_=I["uk_L"][h]), writes=[("m_ukb", 1)])
        if getattr(C, "fused", False):
            P.op("dve", lambda e: e.tensor_scalar(out=C.m_uqb[:, 0:3], in0=C.m_uqb[:, 0:3], scalar1=C.m_flagB[:, 0:1], scalar2=None, op0=ALU.mult),
                 reads=[("m_uqb", 0), ("c_flagB",)], writes=[("m_uqb", 0)])
            P.op("dve", lambda e: e.tensor_scalar(out=C.m_ukb[:, 1024:1027], in0=C.m_ukb[:, 1024:1027], scalar1=C.m_flagB[:, 0:1], scalar2=None, op0=ALU.mult),
                 reads=[("m_ukb", 0), ("c_flagB",)], writes=[("m_ukb", 0)])
        for (src, skeys, n, c, dst, dkey, scale) in ((C.m_uqb, [("m_uqb", 0), ("m_uqb", 1)], 1024, h, C.m_qT, ("m_qT",), DK_SCALE),
                                                     (C.m_ukb, [("m_ukb", 0), ("m_ukb", 1), ("ukb_pad",)], 2048, 4 + h, C.m_kT, None, None)):
            P.op("dve", lambda e, src=src, n=n, c=c: e.tensor_scalar(out=C.m_acc[:, 0:n], in0=src[:, 3:3 + n], scalar1=cw_[:, c, 3:4],
                                                                 scalar2=None, op0=ALU.mult),
                 reads=skeys + [("c_cw",)], writes=[("m_acc",)])
            for j in (2, 1, 0):
                P.op("dve", lambda e, src=src, n=n, c=c, j=j: e.scalar_tensor_tensor(
                    out=C.m_acc[:, 0:n], in0=src[:, j:j + n], scalar=cw_[:, c, j:j + 1], in1=C.m_acc[:, 0:n],
                    op0=ALU.mult, op1=ALU.add),
                    reads=skeys + [("c_cw",), ("m_acc",)], writes=[("m_acc",)])
            if scale is None:
                P.op("act", lambda e, n=n, c=c, dst=dst: e.activation(out=dst[:, 0:n], in_=C.m_acc[:, 0:n], func=AF.Silu, bias=cb_[:, c:c + 1]),
                     reads=[("m_acc",), ("c_cb",)], writes=[("m_kT", 0), ("m_kT", 1)])
            else:
                P.op("act", lambda e, n=n, c=c: e.activation(out=C.m_acc[:, 0:n], in_=C.m_acc[:, 0:n], func=AF.Silu, bias=cb_[:, c:c + 1]),
                     reads=[("m_acc",), ("c_cb",)], writes=[("m_acc",)])
                P.op("dve", lambda e, n=n, dst=dst, scale=scale: e.tensor_scalar(out=dst[:, 0:n], in0=C.m_acc[:, 0:n], scalar1=scale, scalar2=None, op0=ALU.mult),
                     reads=[("m_acc",)], writes=[dkey])
        c0 = 1024 + h * 256
        load_V(C, I["v_tm_R"], 0, c0, 256, ("m_V", 0))
        load_V(C, I["v_tm_L"], 8, c0, 256, ("m_V", 1))

        def qhalf(qh):
            qsl = slice(qh * 512, (qh + 1) * 512)
            bn = [0, 1]
            bd = 2
            tiles = _ktiles(qh)
            def stage_S(ti):
                kt, rem, dl = tiles[ti]
                bs = P.bank(4, 8)
                ksl = slice(kt * 128, (kt + 1) * 128)
                P.op("pe", lambda e, bs=bs, ksl=ksl: e.matmul(C.ps[bs][:], C.m_kT[:, ksl], C.m_qT[:, qsl], start=True, stop=True),
                     reads=[("m_kT", 0 if rem else 1), ("m_qT",)], writes=[("ps", bs)])
                NT = len(C.m_tmpx)
                t = C.n_tmp % NT
                C.n_tmp += 1
                if (not rem) and dl <= 0:
                    t2 = C.n_tmp % NT
                    C.n_tmp += 1
                    P.op("dve", lambda e, t2=t2, dl=dl: e.tensor_tensor(out=C.m_tmpx[t2][:], in0=C.m_Fbc[:, qsl], in1=C.m_cau[:, dl + 3, :], op=ALU.add),
                         reads=[("m_Fbc", qh), ("c_cau",)], writes=[("m_tmp", t2)])
                    P.op("act", lambda e, t=t, t2=t2, kt=kt: e.activation(out=C.m_tmpx[t][:], in_=C.m_tmpx[t2][:], func=AF.Exp, bias=C.m_a[:, kt, h:h + 1]),
                         reads=[("m_tmp", t2), ("m_a",)], writes=[("m_tmp", t)])
                else:
                    P.op("act", lambda e, t=t, kt=kt: e.activation(out=C.m_tmpx[t][:], in_=C.m_Fbc[:, qsl], func=AF.Exp, bias=C.m_a[:, kt, h:h + 1]),
                         reads=[("m_Fbc", qh), ("m_a",)], writes=[("m_tmp", t)])
                pi = C.n_pT % len(C.m_pTx)
                C.n_pT += 1
                P.op("dve", lambda e, bs=bs, t=t, pi=pi: e.tensor_tensor(out=C.m_pTx[pi][:], in0=C.ps[bs][:], in1=C.m_tmpx[t][:], op=ALU.mult),
                     reads=[("ps", bs), ("m_tmp", t)], writes=[("m_pT", pi)])
                return pi

            def stage_V(ti, pi):
                kt, rem, dl = tiles[ti]
                first, last = ti == 0, ti == len(tiles) - 1
                for dvc in range(2):
                    P.op("pe", lambda e, kt=kt, pi=pi, dvc=dvc, first=first, last=last: e.matmul(
                        C.ps[bn[dvc]][:], C.m_V[:, kt, dvc * 128:(dvc + 1) * 128], C.m_pTx[pi][:], start=first, stop=last),
                        reads=[("m_V", 0 if rem else 1), ("m_pT", pi)], writes=[("ps", bn[dvc])])
                P.op("pe", lambda e, pi=pi, first=first, last=last: e.matmul(C.ps[bd][:], C.m_onesb[:], C.m_pTx[pi][:], start=first, stop=last),
                     reads=[("c_onesb",), ("m_pT", pi)], writes=[("ps", bd)])
            LA = 3
            pis = {}
            for step in range(len(tiles) + LA):
                if step < len(tiles):
                    pis[step] = stage_S(step)
                if step - LA >= 0:
                    stage_V(step - LA, pis[step - LA])
            P.op("act", lambda e: e.activation(out=C.m_rl[:], in_=C.ps[bd][:], func=AF.Abs),
                 reads=[("ps", bd)], writes=[("m_rl",)])
            P.op("dve", lambda e: e.tensor_scalar(out=C.m_rl[:], in0=C.m_rl[:], scalar1=1.0, scalar2=None, op0=ALU.max),
                 reads=[("m_rl",)], writes=[("m_rl",)])
            P.op("act", lambda e: e.activation(out=C.m_rl[:], in_=C.m_rl[:], func=AF.Ln), reads=[("m_rl",)], writes=[("m_rl",)])
            P.op("act", lambda e: e.activation(out=C.m_rl[:], in_=C.m_rl[:], func=AF.Exp, scale=-1.0), reads=[("m_rl",)], writes=[("m_rl",)])
            bq = 3
            for dvc in range(2):
                P.op("dve", lambda e, dvc=dvc: e.tensor_tensor(out=C.m_hm[dvc][:], in0=C.ps[bn[dvc]][:], in1=C.m_rl[:], op=ALU.mult),
                     reads=[("ps", bn[dvc]), ("m_rl",)], writes=[("m_hm", dvc)])
                P.op("act", lambda e, dvc=dvc: e.activation(out=C.m_sqb[:], in_=C.m_hm[dvc][:], func=AF.Square),
                     reads=[("m_hm", dvc)], writes=[("m_sqb",)])
                P.op("pe", lambda e, dvc=dvc: e.matmul(C.ps[bq][:], C.m_onesb[:], C.m_sqb[:], start=(dvc == 0), stop=(dvc == 1)),
                     reads=[("m_sqb",), ("c_onesb",)], writes=[("ps", bq)])
            P.op("act", lambda e: e.activation(out=C.m_rl[:], in_=C.ps[bq][:], func=AF.Ln, bias=C.m_eps[:, 0:1], scale=1.0 / 256),
                 reads=[("ps", bq), ("c_eps",), ("m_hm", 0), ("m_hm", 1)], writes=[("m_rl",)])
            P.op("act", lambda e: e.activation(out=C.m_rl[:], in_=C.m_rl[:], func=AF.Exp, scale=-0.5), reads=[("m_rl",)], writes=[("m_rl",)])
            for dvc in range(2):
                cc = 2 * h + dvc
                oi = C.n_omb % 2
                C.n_omb += 1
                P.dma("sp", lambda e, cc=cc, oi=oi: e.dma_start(out=C.m_omb[oi][:], in_=I["om"][cc][:, qsl]), writes=[("m_omb", oi)])
                P.op("dve", lambda e, dvc=dvc, cc=cc: e.scalar_tensor_tensor(
                    out=C.m_hm[dvc][:], in0=C.m_hm[dvc][:], scalar=gh_[:, cc:cc + 1], in1=C.m_rl[:], op0=ALU.mult, op1=ALU.mult),
                    reads=[("m_hm", dvc), ("c_gh",), ("m_rl",)], writes=[("m_hm", dvc)])
                si = C.n_ost % 2
                C.n_ost += 1
                P.op("dve", lambda e, dvc=dvc, oi=oi, si=si: e.tensor_tensor(out=C.m_ost[si][:], in0=C.m_hm[dvc][:], in1=C.m_omb[oi][:], op=ALU.mult),
                     reads=[("m_hm", dvc), ("m_omb", oi)], writes=[("m_ost", si)])
                P.dma("sp", lambda e, si=si, cc=cc: e.dma_start(out=mixT[4 + cc][:, qsl], in_=C.m_ost[si][:]), reads=[("m_ost", si)])
        for qh in range(2):
            qhalf(qh)
    for h in range(4):
        head(h)


from contextlib import ExitStack as _ES

Z_OUT = dict(qa_bf=([4, 128, 1024], BF16), qa_f=([4, 128, 1024], F32), ka_bf=([4, 128, 1024], BF16),
             kms=([128, 16], F32), qc_bf=([4, 128, 1024], BF16), kc_bf=([4, 128, 1024], BF16),
             uq=([4, 128, 1024], F32), uk=([4, 128, 1024], F32), om=([8, 128, 1024], F32),
             v_tm=([8, 128, 2048], BF16), gates=([8, 128, 8], F32))


def build_A():
    nc = bass.Bass("TRN2", target_bir_lowering=False)
    with _ES() as es:
        C = Ctx(nc, es)
        h_in = C.dram("h_in", [16, 128, 1024], F32, "ExternalInput")
        g1 = C.dram("g1", [128, 16], F32, "ExternalInput")
        gm = C.dram("gm", [128, 16], F32, "ExternalInput")
        wup = C.dram("wup", [88, 128, 16, 128], F32, "ExternalInput")
        wdown = C.dram("wdown", [16, 128, 44, 128], F32, "ExternalInput")
        win = C.dram("win", [48, 128, 16, 128], F32, "ExternalInput")
        win_g = C.dram("win_g", [128, 16, 8], F32, "ExternalInput")
        h_out = C.dram("h_out", [16, 128, 1024], F32, "ExternalOutput")
        o = {k: C.dram(k, sh, dt, "ExternalOutput") for k, (sh, dt) in Z_OUT.items()}
        alloc_token_local(C)
        alloc_inproj(C)
        load_gain(C, 0, g1)
        load_gain(C, 1, gm)
        load_h(C, h_in)
        ffn(C, 0, wup, wdown)
        store_h(C, h_out)
        inproj(C, 1, win, win_g, o)
        C.P.build(es)
    return nc


M_IN = dict(qa_bf=([4, 128, 1024], BF16), qa_f=([4, 128, 1024], F32), ka_bf_L=([4, 128, 1024], BF16),
            ka_bf_R=([4, 128, 1024], BF16), kms_L=([128, 16], F32), kms_R=([128, 16], F32),
            qc_bf=([4, 128, 1024], BF16), kc_bf_L=([4, 128, 1024], BF16), kc_bf_R=([4, 128, 1024], BF16),
            uq_L=([4, 128, 1024], F32), uq_R=([4, 128, 1024], F32), uk_L=([4, 128, 1024], F32),
            uk_R=([4, 128, 1024], F32), om=([8, 128, 1024], F32), v_tm_L=([8, 128, 2048], BF16),
            v_tm_R=([8, 128, 2048], BF16), gates_L=([8, 128, 8], F32), gates_R=([8, 128, 8], F32))


def build_M():
    nc = bass.Bass("TRN2", target_bir_lowering=False)
    with _ES() as es:
        C = Ctx(nc, es)
        I = {k: C.dram(k, sh, dt, "ExternalInput") for k, (sh, dt) in M_IN.items()}
        cd = {k: C.dram("c_" + k, sh, F32, "ExternalInput") for k, sh in CONST_SHAPES.items()}
        conv_w = C.dram("conv_w", [128, 8, 4], F32, "ExternalInput")
        conv_b = C.dram("conv_b", [128, 8], F32, "ExternalInput")
        gbias = C.dram("gbias", [128, 8], F32, "ExternalInput")
        ghead = C.dram("ghead", [128, 8], F32, "ExternalInput")
        mixT = C.dram("mixT", [16, 128, 1024], BF16, "ExternalOutput")
        alloc_mixer(C)
        load_mixer_consts(C, cd, conv_w, conv_b, gbias, ghead)
        attn_group(C, "moba", I, mixT)
        attn_group(C, "dil", I, mixT)
        mlstm_group(C, I, mixT)
        C.P.build(es)
    return nc


def build_C(final):
    nc = bass.Bass("TRN2", target_bir_lowering=False)
    with _ES() as es:
        C = Ctx(nc, es)
        h_in = C.dram("h_in", [16, 128, 1024], F32, "ExternalInput")
        mixT = C.dram("mixT", [16, 128, 1024], BF16, "ExternalInput")
        wout = C.dram("wout", [16, 128, 16, 128], F32, "ExternalInput")
        g2 = C.dram("g2", [128, 16], F32, "ExternalInput")
        gp = C.dram("gp", [128, 16], F32, "ExternalInput")
        gf = C.dram("gf", [128, 16], F32, "ExternalInput")
        wup = C.dram("wup", [88, 128, 16, 128], F32, "ExternalInput")
        wdown = C.dram("wdown", [16, 128, 44, 128], F32, "ExternalInput")
        wpg = C.dram("wpg", [16, 128, 16, 128], F32, "ExternalInput")
        wpp = C.dram("wpp", [16, 128, 2, 128], F32, "ExternalInput")
        pT = C.dram("pT", [2, 128, 1024], F32, "ExternalInput")
        h_out = C.dram("h_out", [16, 128, 1024], F32, "ExternalOutput")
        alloc_token_local(C)
        alloc_inproj(C)
        load_gain(C, 0, g2)
        load_gain(C, 1, gp)
        load_gain(C, 2, gf)
        load_h(C, h_in)
        outproj(C, mixT, wout)
        ffn(C, 0, wup, wdown)
        ple(C, 1, wpg, wpp, pT)
        if final:
            final_norm(C, 2, h_out)
        else:
            store_h(C, h_out)
        C.P.build(es)
    return nc


def tiles_fm(W):
    Kd, M = W.shape
    return np.ascontiguousarray(W.reshape(Kd // 128, 128, M // 128, 128).transpose(2, 1, 0, 3))


def vec_fm(g):
    return np.ascontiguousarray(np.asarray(g, np.float32).reshape(-1, 128).T)


def _run(nc, in_maps):
    res = run_bass_kernel_spmd(nc, in_maps, core_ids=list(range(8)))
    return res.results


def kernel_unfused(x, p, g_ffn1, w_up1, w_down1, g_mix, w_in, conv_w, conv_b, b_igate, b_fgate,
           g_head, w_out, g_ffn2, w_up2, w_down2, g_ple, w_ple_gate, w_ple_proj, g_final):
    f = lambda a: np.asarray(a, dtype=np.float32)
    x, p = f(x), f(p)
    ncA, ncM, ncC, ncF = build_A(), build_M(), build_C(False), build_C(True)
    consts = [mixer_consts(c % 2 == 1) for c in range(8)]
    hs = []
    for c in range(8):
        b, s = c // 2, c % 2
        hs.append(np.ascontiguousarray(x[b, s * 1024:(s + 1) * 1024, :].T.reshape(16, 128, 1024)))
    out = None
    for l in range(4):
        wi = f(w_in[l])
        a_in = dict(g1=vec_fm(g_ffn1[l]), gm=vec_fm(g_mix[l]), wup=tiles_fm(f(w_up1[l])), wdown=tiles_fm(f(w_down1[l])),
                    win=tiles_fm(wi[:, :6144]),
                    win_g=np.ascontiguousarray(wi[:, 6144:6152].reshape(16, 128, 8).transpose(1, 0, 2)))
        ra = _run(ncA, [dict(a_in, h_in=hs[c]) for c in range(8)])
        hs = [ra[c]["h_out"] for c in range(8)]
        cw = np.ascontiguousarray(f(conv_w[l]).reshape(4, 8, 128).transpose(2, 1, 0))
        m_common = dict(conv_w=cw, conv_b=vec_fm(conv_b[l]),
                        gbias=np.ascontiguousarray(np.broadcast_to(np.concatenate([f(b_igate[l]), f(b_fgate[l])])[None, :], (128, 8))),
                        ghead=vec_fm(g_head[l]))
        m_in = []
        for c in range(8):
            d = dict(m_common)
            for k, v in consts[c].items():
                d["c_" + k] = v
            me, other = ra[c], ra[c - 1] if c % 2 == 1 else None
            for k in ("qa_bf", "qa_f", "qc_bf", "om"):
                d[k] = me[k]
            for k in ("ka_bf", "kms", "kc_bf", "uq", "uk", "v_tm", "gates"):
                d[k + "_L"] = me[k]
                d[k + "_R"] = other[k] if other is not None else np.zeros_like(me[k])
            m_in.append(d)
        rm = _run(ncM, m_in)
        c_common = dict(wout=tiles_fm(f(w_out[l])), g2=vec_fm(g_ffn2[l]), gp=vec_fm(g_ple[l]), gf=vec_fm(g_final),
                        wup=tiles_fm(f(w_up2[l])), wdown=tiles_fm(f(w_down2[l])), wpg=tiles_fm(f(w_ple_gate[l])),
                        wpp=tiles_fm(f(w_ple_proj[l])))
        c_in = []
        for c in range(8):
            b, s = c // 2, c % 2
            pT = np.ascontiguousarray(p[l, b, s * 1024:(s + 1) * 1024, :].T.reshape(2, 128, 1024))
            c_in.append(dict(c_common, h_in=hs[c], mixT=rm[c]["mixT"], pT=pT))
        rc = _run(ncF if l == 3 else ncC, c_in)
        hs = [rc[c]["h_out"] for c in range(8)]
    out = np.empty((4, 2048, 2048), np.float32)
    for c in range(8):
        b, s = c // 2, c % 2
        out[b, s * 1024:(s + 1) * 1024, :] = hs[c].reshape(2048, 1024).T
    return out


ARENA_BYTES = 65536 + 106496
NL = 4


def alloc_fused(C):
    sb = C.sb
    C.fused = True
    arena = sb("arena", [128, ARENA_BYTES // 4], F32)

    def view(off, shape, dt, parts=128):
        n = int(np.prod(shape[1:]))
        nb = n * (4 if dt == F32 else 2)
        assert off % 4 == 0 and off + nb <= ARENA_BYTES, (off, nb)
        ap = arena[0:parts, off // 4:(off + nb + 3) // 4]
        if dt != F32:
            ap = ap.bitcast(dt)
        if len(shape) == 3:
            ap = ap.rearrange("p (a b) -> p a b", b=shape[2])
        return ap

    K = 1024
    C.hT = view(0, [128, KC_D, NTOK], F32)
    base = 65536
    C.hnT = view(base, [128, KC_D, NTOK], BF16)
    C.actT = view(base + 32 * K, [128, WSLOT_KC, NTOK], BF16)
    C.wbuf = [view(base + 76 * K + i * 5632, [128, WSLOT_KC, 128], BF16) for i in range(WSLOTS)]
    o = base
    C.m_tab = view(o, [128, 13, 512], F32); o += 26 * K
    C.m_dil_loc = C.m_tab[:, 0:8, :]
    C.m_dil_rem = C.m_tab[:, 8:13, :]
    C.m_cau = C.m_tab[:, 0:4, :]
    C.m_step = C.m_tab[:, 4:8, :]
    C.m_posQ = view(o, [4, 1024], BF16, parts=4); o += 2 * K
    C.m_qT = view(o, [128, 1024], BF16); o += 2 * K
    C.m_kT = view(o, [128, 2048], BF16); o += 4 * K
    C.m_V = view(o, [128, 16, 256], BF16); o += 8 * K
    C.m_qf = view(o, [128, 1024], F32); o += 4 * K
    C.m_selbT = view(o, [8, 1024], BF16, parts=8); o += 2 * K
    C.m_pT = []
    for i in range(3):
        C.m_pT.append(view(o, [128, 512], BF16)); o += K
    C.m_tmp = []
    for i in range(3):
        C.m_tmp.append(view(o, [128, 512], F32)); o += 2 * K
    C.m_tmpx = C.m_tmp + [C.m_tab[:, 8 + i, :] for i in range(4)]
    C.m_pT = C.m_pT + [sb("m_pT3", [128, 512], BF16)]
    C.m_pTx = C.m_pT
    C.m_rl = view(o, [128, 512], F32); o += 2 * K
    C.m_ost = []
    for i in range(2):
        C.m_ost.append(view(o, [128, 512], BF16)); o += K
    C.m_lfrep = view(o, [128, 16, 128], F32); o += 8 * K
    C.m_Fbc = view(o, [128, 1024], F32); o += 4 * K
    C.m_uqb = view(o, [128, 1027], F32); o += 5 * K
    C.m_ukb = view(o, [128, 2051], F32); o += 9 * K
    C.m_acc = view(o, [128, 2048], F32); o += 8 * K
    C.m_hm = []
    for i in range(2):
        C.m_hm.append(view(o, [128, 512], F32)); o += 2 * K
    C.m_sqb = view(o, [128, 512], BF16); o += K
    C.m_omb = []
    for i in range(2):
        C.m_omb.append(view(o, [128, 512], F32)); o += 2 * K
    assert o <= ARENA_BYTES, o
    C.sq = [sb("sq%d" % i, [128, NTOK], BF16) for i in range(2)]
    C.rstd = sb("rstd", [128, NTOK], F32)
    C.tmpf = [sb("tmpf%d" % i, [128, 512], F32) for i in range(2)]
    C.ones_bf = sb("ones_bf", [128, 128], BF16)
    C.epsv = sb("epsv", [128, 1], F32)
    C.gvec = sb("gvec", [128, 4 * NL + 1, KC_D], F32)
    C.stf = [sb("stf%d" % i, [128, 512], F32) for i in range(3)]
    C.stb = [sb("stb%d" % i, [128, 512], BF16) for i in range(4)]
    C.vst = [sb("vst%d" % i, [128, 128], BF16) for i in range(4)]
    C.kms = sb("kms_sb", [128, 16], F32)
    C.wg = sb("wg", [128, KC_D, 8], BF16)
    C.gst = sb("gst", [128, 8, 8], F32)
    C.m_posK = sb("m_posK", [4, 2048], BF16)
    C.m_E8 = sb("m_E8", [8, 1024], BF16)
    C.m_G = sb("m_G", [128, 8, 8], F32)
    C.m_ident = sb("m_ident", [128, 128], F32)
    C.m_U = sb("m_U", [128, 128], F32)
    C.m_rbias = sb("m_rbias", [128, 1], F32)
    C.m_flagB = sb("m_flagB", [128, 1], F32)
    C.m_onesf = sb("m_onesf", [128, 512], F32)
    C.m_cw = sb("m_cw", [128, NL, 8, 4], F32)
    C.m_cb = sb("m_cb", [128, NL, 8], F32)
    C.m_gb = sb("m_gb", [128, NL, 8], F32)
    C.m_gh = sb("m_gh", [128, NL, 8], F32)
    C.m_eps = C.epsv
    C.m_onesb = C.ones_bf
    C.m_kms = sb("m_kms", [128, 8], F32)
    C.m_gs = sb("m_gs", [128, 8], F32)
    C.m_m8 = sb("m_m8", [128, 8], F32)
    C.m_sel = sb("m_sel", [128, 8], F32)
    C.m_val = sb("m_val", [128, 8], F32)
    C.m_gs4 = [C.m_gs] + [sb("m_gs_%d" % i, [128, 8], F32) for i in range(3)]
    C.m_m84 = [C.m_m8] + [sb("m_m8_%d" % i, [128, 8], F32) for i in range(3)]
    C.m_sel4 = [C.m_sel] + [sb("m_sel_%d" % i, [128, 8], F32) for i in range(3)]
    C.m_val4 = [C.m_val] + [sb("m_val_%d" % i, [128, 8], F32) for i in range(3)]
    C.m_gl = sb("m_gl", [128, 16, 8], F32)
    C.m_lf = sb("m_lf", [128, 16, 4], F32)
    C.m_a = sb("m_a", [128, 16, 4], F32)
    for nm in ("n_sq", "n_tmpf", "n_stf", "n_stb", "n_vst", "n_pT", "n_tmp", "n_ost", "n_omb"):
        setattr(C, nm, 0)
    P = C.P
    P.op("dve", lambda e: e.memset(C.ones_bf[:], 1.0), writes=[("ones_bf",), ("c_onesb",)])
    P.op("dve", lambda e: e.memset(C.epsv[:], 1e-6), writes=[("epsv",), ("c_eps",)])
    P.op("dve", lambda e: e.memset(C.m_onesf[:], 1.0), writes=[("c_onesf",)])


def load_small_consts(C, cd, D_):
    P = C.P
    C.cd = cd
    for nm, q in (("posK", "pool"), ("E8", "pool"), ("G", "sp"), ("ident", "sp"), ("U", "sp"), ("rbias", "sp"), ("flagB", "sp")):
        dst = getattr(C, "m_" + nm)
        P.dma(q, lambda e, dst=dst, nm=nm: e.dma_start(out=dst[:], in_=cd[nm]), writes=[("c_" + nm,)])
    P.dma("sp", lambda e: e.dma_start(out=C.m_cw[:], in_=D_["conv_w"]), writes=[("c_cw",)])
    P.dma("sp", lambda e: e.dma_start(out=C.m_cb[:], in_=D_["conv_b"]), writes=[("c_cb",)])
    P.dma("sp", lambda e: e.dma_start(out=C.m_gb[:], in_=D_["gbias"]), writes=[("c_gb",)])
    P.dma("sp", lambda e: e.dma_start(out=C.m_gh[:], in_=D_["ghead"]), writes=[("c_gh",)])
    P.dma("sp", lambda e: e.dma_start(out=C.gvec[:], in_=D_["gains"]), writes=[("gvec", i) for i in range(4 * NL + 1)])


def load_tabs(C, kind):
    P = C.P
    P.barrier()
    if kind == "dil":
        P.dma("sp", lambda e: e.dma_start(out=C.m_tab[:, 0:8, :], in_=C.cd["dil_loc"]), writes=[("c_dil_loc",)])
        P.dma("sp", lambda e: e.dma_start(out=C.m_tab[:, 8:13, :], in_=C.cd["dil_rem"]), writes=[("c_dil_rem",)])
    else:
        P.dma("sp", lambda e: e.dma_start(out=C.m_tab[:, 0:4, :], in_=C.cd["cau"]), writes=[("c_cau",)])
        P.dma("sp", lambda e: e.dma_start(out=C.m_tab[:, 4:8, :], in_=C.cd["step"]), writes=[("c_step",)])
    if kind == "mlstm":
        P.op("dve", lambda e: e.memset(C.m_ukb[:, 0:3], 0.0), writes=[("ukb_pad",)])


XB_ROWS = 3072
XF_ROWS = 1034
PAIRS = [[0, 1], [2, 3], [4, 5], [6, 7]]


def build_fused(nl=NL, phases="AXMC"):
    nc = bass.Bass("TRN2", target_bir_lowering=False)
    with _ES() as es:
        C = Ctx(nc, es)
        P = C.P
        ext = lambda n, sh: C.dram(n, sh, F32, "ExternalInput")
        x_in = ext("x_in", [16, 128, 1024])
        pT = ext("pT", [NL, 2, 128, 1024])
        W = dict(wup1=ext("wup1", [NL, 88, 128, 16, 128]), wdown1=ext("wdown1", [NL, 16, 128, 44, 128]),
                 win=ext("win", [NL, 48, 128, 16, 128]), win_g=ext("win_g", [NL, 128, 16, 8]),
                 wout=ext("wout", [NL, 16, 128, 16, 128]),
                 wup2=ext("wup2", [NL, 88, 128, 16, 128]), wdown2=ext("wdown2", [NL, 16, 128, 44, 128]),
                 wpg=ext("wpg", [NL, 16, 128, 16, 128]), wpp=ext("wpp", [NL, 16, 128, 2, 128]))
        D_ = dict(conv_w=ext("conv_w", [128, NL, 8, 4]), conv_b=ext("conv_b", [128, NL, 8]),
                  gbias=ext("gbias", [128, NL, 8]), ghead=ext("ghead", [128, NL, 8]),
                  gains=ext("gains", [128, 4 * NL + 1, 16]))
        shapes = dict(CONST_SHAPES, flagB=[128, 1])
        cd = {k: ext("c_" + k, sh) for k, sh in shapes.items()}
        out = C.dram("out", [16, 128, 1024], F32, "ExternalOutput")
        I32 = lambda n, sh: nc.dram_tensor(n, sh, F32, kind="Internal").ap()
        I16 = lambda n, sh: nc.dram_tensor(n, sh, BF16, kind="Internal").ap()
        exch = []
        exd = {}

        def mk(n, rows, mkfn):
            a, g = mkfn("x_" + n, [rows, 1024]), mkfn("g_" + n, [2 * rows, 1024])
            exch.append((a, g))
            exd[n] = (a, g)
            return a, g[0:rows, :]
        loc = dict(qa_bf=I16("z_qa_bf", [4, 128, 1024]), qa_f=I32("z_qa_f", [4, 128, 1024]),
                   qc_bf=I16("z_qc_bf", [4, 128, 1024]), om=I32("z_om", [8, 128, 1024]))
        mixT = I16("z_mixT", [16, 128, 1024])
        kk = mk("kk", 1024, I16)
        v0 = mk("v0", 1024, I16)
        v1 = mk("v1", 1024, I16)
        uk = mk("uk", 512, I32)
        uq = mk("uq", 512, I32)
        gk = mk("gk", 128, I32)
        ch = lambda t: t.rearrange("(j p) t -> j p t", p=128)
        vt = lambda t: t.rearrange("(t p a) b -> t p (a b)", t=4, p=128, a=2)
        Lv, Rv = {}, {}
        for i, dst in ((0, Lv), (1, Rv)):
            dst["ka_bf"] = ch(kk[i][0:512, :])
            dst["kc_bf"] = ch(kk[i][512:1024, :])
            dst["v_tm"] = [vt(v0[i]), vt(v1[i])]
            dst["uk"] = ch(uk[i])
            dst["uq"] = ch(uq[i])
            dst["gates"] = gk[i][0:8, :].rearrange("t (p c) -> t p c", c=8)
            dst["kms"] = gk[i][8:10, :].rearrange("a (p c) -> (a p) c", c=16)
        o_A = dict(loc, **Lv)
        v8 = [Lv["v_tm"][0][t] for t in range(4)] + [Lv["v_tm"][1][t] for t in range(4)]
        o_A["v_tm"] = v8
        I_M = dict(loc)
        for k in ("ka_bf", "kms", "kc_bf", "uq", "uk", "v_tm", "gates"):
            I_M[k + "_L"] = Lv[k]
            I_M[k + "_R"] = Rv[k]

        alloc_fused(C)
        load_small_consts(C, cd, D_)
        load_h(C, x_in)
        for l in range(nl):
            if "A" in phases:
                ffn(C, 4 * l + 0, W["wup1"][l], W["wdown1"][l])
                inproj(C, 4 * l + 1, W["win"][l], W["win_g"][l], o_A, None)
            P.barrier()
            if "X" in phases:
                for (a_, g_) in exch:
                    P.cc(lambda e, a_=a_, g_=g_: e.collective_compute("AllGather", ALU.bypass, replica_groups=PAIRS,
                                                                      ins=[a_.opt()], outs=[g_.opt()]))
            P.barrier()
            C.m_layer = l
            if "M" in phases:
                attn_group(C, "moba", I_M, mixT)
                attn_group(C, "dil", I_M, mixT)
                mlstm_group(C, I_M, mixT)
            P.barrier()
            if "C" in phases:
                outproj(C, mixT, W["wout"][l])
                ffn(C, 4 * l + 2, W["wup2"][l], W["wdown2"][l])
                ple(C, 4 * l + 3, W["wpg"][l], W["wpp"][l], pT[l])
            P.barrier()
        final_norm(C, 4 * NL, out)
        C.P.build(es)
    return nc


def kernel(x, p, g_ffn1, w_up1, w_down1, g_mix, w_in, conv_w, conv_b, b_igate, b_fgate,
           g_head, w_out, g_ffn2, w_up2, w_down2, g_ple, w_ple_gate, w_ple_proj, g_final):
    f = lambda a: np.asarray(a, dtype=np.float32)
    x, p = f(x), f(p)
    nc = build_fused()
    st = lambda w, fn=tiles_fm: np.stack([fn(f(w[l])) for l in range(NL)])
    wi = f(w_in)
    gains = np.stack([vec_fm(g[l]) for l in range(NL) for g in (g_ffn1, g_mix, g_ffn2, g_ple)] + [vec_fm(g_final)], axis=1)
    common = dict(
        wup1=st(w_up1), wdown1=st(w_down1), win=np.stack([tiles_fm(wi[l][:, :6144]) for l in range(NL)]),
        win_g=np.stack([np.ascontiguousarray(wi[l][:, 6144:6152].reshape(16, 128, 8).transpose(1, 0, 2)) for l in range(NL)]),
        wout=st(w_out), wup2=st(w_up2), wdown2=st(w_down2), wpg=st(w_ple_gate), wpp=st(w_ple_proj),
        conv_w=np.ascontiguousarray(np.stack([f(conv_w[l]).reshape(4, 8, 128).transpose(2, 1, 0) for l in range(NL)], axis=1)),
        conv_b=np.ascontiguousarray(np.stack([vec_fm(conv_b[l]) for l in range(NL)], axis=1)),
        gbias=np.ascontiguousarray(np.stack([np.broadcast_to(np.concatenate([f(b_igate[l]), f(b_fgate[l])])[None, :], (128, 8))
                                             for l in range(NL)], axis=1)),
        ghead=np.ascontiguousarray(np.stack([vec_fm(g_head[l]) for l in range(NL)], axis=1)),
        gains=np.ascontiguousarray(gains))
    in_maps = []
    for c in range(8):
        b, s_ = c // 2, c % 2
        d = dict(common)
        d["x_in"] = np.ascontiguousarray(x[b, s_ * 1024:(s_ + 1) * 1024, :].T.reshape(16, 128, 1024))
        d["pT"] = np.ascontiguousarray(np.stack([p[l, b, s_ * 1024:(s_ + 1) * 1024, :].T.reshape(2, 128, 1024) for l in range(NL)]))
        for k, v in mixer_consts(s_ == 1).items():
            d["c_" + k] = v
        d["c_flagB"] = np.full((128, 1), float(s_), np.float32)
        in_maps.append(d)
    res = run_bass_kernel_spmd(nc, in_maps, core_ids=list(range(8))).results
    out = np.empty((4, 2048, 2048), np.float32)
    for c in range(8):
        b, s_ = c // 2, c % 2
        out[b, s_ * 1024:(s_ + 1) * 1024, :] = res[c]["out"].reshape(2048, 1024).T
    return out
```

```python
from contextlib import ExitStack

COMPUTE = ("pe", "act", "dve", "pool")
EPOCH = 30000
NDMA_SLOTS = 24


class Prog:
    def __init__(self, nc):
        self.nc = nc
        self.ops = []
        self.last_w = {}
        self.readers = {}
        self.cnt = {e: 0 for e in COMPUTE}
        self.epoch = {e: 0 for e in COMPUTE}
        self.dma_n = {"sp": 0, "pool": 0, "act": 0}
        self.dma_val = {}
        self.sem_ids = set()
        self._bank = 0

    def barrier(self):
        fin = {}
        for (_e, _f, _d, tok, _i) in self.ops:
            sid = tok[0]
            if sid[0] in COMPUTE:
                k = sid[0]
                if k not in fin or (fin[k][0][1], fin[k][1]) < (sid[1], tok[1]):
                    fin[k] = tok
            else:
                if sid not in fin or fin[sid][1] < tok[1]:
                    fin[sid] = tok
        self.bar = list(fin.values())

    def _deps(self, reads, writes):
        deps = list(getattr(self, "bar", ()))
        for r in reads:
            t = self.last_w.get(r)
            if t is not None:
                deps.append(t)
            if r[0] == "ps":
                deps.extend(self.readers.get(r, ()))
        for w in writes:
            t = self.last_w.get(w)
            if t is not None:
                deps.append(t)
            deps.extend(self.readers.get(w, ()))
        return deps

    def _commit(self, tok, reads, writes):
        for r in reads:
            self.readers.setdefault(r, []).append(tok)
        for w in writes:
            self.last_w[w] = tok
            self.readers[w] = []

    def op(self, eng, fn, reads=(), writes=()):
        deps = self._deps(reads, writes)
        if self.cnt[eng] >= EPOCH:
            self.epoch[eng] += 1
            self.cnt[eng] = 0
        self.cnt[eng] += 1
        sid = (eng, self.epoch[eng])
        self.sem_ids.add(sid)
        tok = (sid, self.cnt[eng], eng)
        self.ops.append((eng, fn, deps, tok, 1))
        self._commit(tok, reads, writes)
        return tok

    def dma(self, q, fn, reads=(), writes=()):
        deps = self._deps(reads, writes)
        slot = self.dma_n[q] % NDMA_SLOTS
        self.dma_n[q] += 1
        sid = ("dma", q, slot)
        self.sem_ids.add(sid)
        prev = self.dma_val.get(sid, 0)
        if prev:
            deps.append((sid, prev, "dma"))
        val = prev + 16
        self.dma_val[sid] = val
        tok = (sid, val, "dma")
        self.ops.append((q, fn, deps, tok, 16))
        self._commit(tok, reads, writes)
        return tok

    def cc(self, fn, reads=(), writes=()):
        deps = self._deps(reads, writes)
        n = self.__dict__.setdefault("_ncc", 0)
        self._ncc = n + 1
        sid = ("cc", n % 8)
        self.sem_ids.add(sid)
        prev = self.dma_val.get(sid, 0)
        if prev:
            deps.append((sid, prev, "dma"))
        val = prev + 1
        self.dma_val[sid] = val
        tok = (sid, val, "dma")
        self.ops.append(("pool", fn, deps, tok, 1))
        self._commit(tok, reads, writes)
        return tok

    def bank(self, lo=0, hi=8):
        if (lo, hi) == (0, 8):
            b = self._bank
            self._bank = (b + 1) % 8
            return b
        st = self.__dict__.setdefault("_rb", {})
        i = st.get((lo, hi), 0)
        st[(lo, hi)] = i + 1
        return lo + i % (hi - lo)

    def build(self, es: ExitStack):
        nc = self.nc
        sems = {}
        for sid in sorted(self.sem_ids, key=str):
            nm = "s_" + "_".join(str(x) for x in sid)
            sems[sid] = es.enter_context(nc.semaphore(nm))
        final = {}
        for (_e, _f, _d, tok, _i) in self.ops:
            final[tok[0]] = max(final.get(tok[0], 0), tok[1])
        ops = self.ops
        block = es.enter_context(nc.Block())

        def emit(eng_name, e):
            waited = {}
            for (en, fn, deps, tok, inc) in ops:
                if en != eng_name:
                    continue
                need = {}
                for (sid, val, deng) in deps:
                    if deng == "pe" and eng_name == "pe":
                        continue
                    if waited.get(sid, 0) >= val:
                        continue
                    if need.get(sid, 0) < val:
                        need[sid] = val
                for sid, val in need.items():
                    e.wait_ge(sems[sid], val)
                    waited[sid] = val
                ins = fn(e)
                ins.then_inc(sems[tok[0]], inc)
            if eng_name == "sp":
                for sid, val in final.items():
                    if waited.get(sid, 0) < val:
                        e.wait_ge(sems[sid], val)

        @block.tensor
        def _(e):
            emit("pe", e)

        @block.scalar
        def _(e):
            emit("act", e)

        @block.vector
        def _(e):
            emit("dve", e)

        @block.gpsimd
        def _(e):
            emit("pool", e)

        @block.sync
        def _(e):
            emit("sp", e)


import numpy as np
import concourse.bass as bass
import concourse.mybir as mybir
from concourse.bass_utils import run_bass_kernel_spmd

F32 = mybir.dt.float32
BF16 = mybir.dt.bfloat16
AF = mybir.ActivationFunctionType
ALU = mybir.AluOpType
AX = mybir.AxisListType

NTOK = 1024
D = 2048
KC_D = 16
DFF = 5632
NFF = 44
WSLOTS = 4
WSLOT_KC = 22


class Ctx:
    def __init__(self, nc, es):
        self.nc = nc
        self.es = es
        self.P = Prog(nc)
        self.n_w = 0
        self.ps = [es.enter_context(nc.psum_tensor("ps%d" % i, [128, 512], F32)) for i in range(8)]

    def sb(self, name, shape, dt):
        return self.es.enter_context(self.nc.sbuf_tensor(name, shape, dt))

    def dram(self, name, shape, dt, kind):
        return self.nc.dram_tensor(name, shape, dt, kind=kind).ap()


def alloc_token_local(C):
    C.hT = C.sb("hT", [128, KC_D, NTOK], F32)
    C.hnT = C.sb("hnT", [128, KC_D, NTOK], BF16)
    C.actT = C.sb("actT", [128, WSLOT_KC, NTOK], BF16)
    C.wbuf = [C.sb("wbuf%d" % i, [128, WSLOT_KC, 128], BF16) for i in range(WSLOTS)]
    C.sq = [C.sb("sq%d" % i, [128, NTOK], BF16) for i in range(2)]
    C.rstd = C.sb("rstd", [128, NTOK], F32)
    C.tmpf = [C.sb("tmpf%d" % i, [128, 512], F32) for i in range(3)]
    C.ones_bf = C.sb("ones_bf", [128, 128], BF16)
    C.gvec = C.sb("gvec", [128, 8, KC_D], F32)
    C.n_sq = 0
    C.n_tmpf = 0
    C.epsv = C.sb("epsv", [128, 1], F32)
    C.P.op("dve", lambda e: e.memset(C.ones_bf[:], 1.0), writes=[("ones_bf",)])
    C.P.op("dve", lambda e: e.memset(C.epsv[:], 1e-6), writes=[("epsv",)])


def load_gain(C, idx, g_dram):
    C.P.dma("sp", lambda e: e.dma_start(out=C.gvec[:, idx, :], in_=g_dram), writes=[("gvec", idx)])


def load_h(C, h_dram):
    for kc in range(KC_D):
        C.P.dma("sp", lambda e, kc=kc: e.dma_start(out=C.hT[:, kc, :], in_=h_dram[kc]),
                writes=[("hT", kc)])


def store_h(C, h_dram):
    for kc in range(KC_D):
        C.P.dma("sp", lambda e, kc=kc: e.dma_start(out=h_dram[kc], in_=C.hT[:, kc, :]),
                reads=[("hT", kc)])


def rmsnorm(C, gidx, out_fn=None):
    P = C.P
    banks = [P.bank(), P.bank()]
    for kc in range(KC_D):
        s = C.n_sq % 2
        C.n_sq += 1
        P.op("act", lambda e, kc=kc, s=s: e.activation(out=C.sq[s][:], in_=C.hT[:, kc, :], func=AF.Square),
             reads=[("hT", kc)], writes=[("sq", s)])
        for th in range(2):
            P.op("pe", lambda e, kc=kc, s=s, th=th: e.matmul(
                C.ps[banks[th]][:], C.ones_bf[:], C.sq[s][:, th * 512:(th + 1) * 512],
                start=(kc == 0), stop=(kc == KC_D - 1)),
                reads=[("sq", s), ("ones_bf",)], writes=[("ps", banks[th])])
    for th in range(2):
        sl = slice(th * 512, (th + 1) * 512)
        P.op("act", lambda e, th=th, sl=sl: e.activation(
            out=C.rstd[:, sl], in_=C.ps[banks[th]][:], func=AF.Ln, bias=C.epsv[:, 0:1], scale=1.0 / D),
            reads=[("ps", banks[th]), ("epsv",)], writes=[("rstd", th)])
        P.op("act", lambda e, sl=sl: e.activation(out=C.rstd[:, sl], in_=C.rstd[:, sl], func=AF.Exp, scale=-0.5),
            reads=[("rstd", th)], writes=[("rstd", th)])
    for kc in range(KC_D):
        if out_fn is None:
            P.op("dve", lambda e, kc=kc: e.scalar_tensor_tensor(
                out=C.hnT[:, kc, :], in0=C.hT[:, kc, :], scalar=C.gvec[:, gidx, kc:kc + 1],
                in1=C.rstd[:], op0=ALU.mult, op1=ALU.mult),
                reads=[("hT", kc), ("rstd", 0), ("rstd", 1), ("gvec", gidx)], writes=[("hnT", kc)])
        else:
            out_fn(kc)


def wload(C, src_ap, kc_n):
    slot = C.n_w % WSLOTS
    C.n_w += 1
    C.P.dma("pool", lambda e: e.dma_start(out=C.wbuf[slot][:, 0:kc_n, :], in_=src_ap),
            writes=[("w", slot)])
    return slot


def mm_fm(C, slot, kc_n, x, xkey, x_kc0, th, bank):
    for kc in range(kc_n):
        C.P.op("pe", lambda e, kc=kc: e.matmul(
            C.ps[bank][:], C.wbuf[slot][:, kc, :], x[:, x_kc0 + kc, th * 512:(th + 1) * 512],
            start=(kc == 0), stop=(kc == kc_n - 1)),
            reads=[("w", slot), (xkey, x_kc0 + kc)], writes=[("ps", bank)])


def ffn(C, gidx, wup, wdown):
    P = C.P
    rmsnorm(C, gidx)
    for half in range(2):
        for cl in range(WSLOT_KC):
            c = half * WSLOT_KC + cl
            sg_ = wload(C, wup[c], KC_D)
            su_ = wload(C, wup[NFF + c], KC_D)
            for th in range(2):
                bg, bu = P.bank(), P.bank()
                mm_fm(C, sg_, KC_D, C.hnT, "hnT", 0, th, bg)
                mm_fm(C, su_, KC_D, C.hnT, "hnT", 0, th, bu)
                t = C.n_tmpf % len(C.tmpf)
                C.n_tmpf += 1
                P.op("act", lambda e, t=t, bg=bg: e.activation(out=C.tmpf[t][:], in_=C.ps[bg][:], func=AF.Silu),
                     reads=[("ps", bg)], writes=[("tmpf", t)])
                P.op("dve", lambda e, t=t, bu=bu, cl=cl, th=th: e.tensor_tensor(
                    out=C.actT[:, cl, th * 512:(th + 1) * 512], in0=C.ps[bu][:], in1=C.tmpf[t][:], op=ALU.mult),
                    reads=[("ps", bu), ("tmpf", t)], writes=[("actT", cl, th)])
        for j in range(KC_D):
            sd = wload(C, wdown[j][:, half * WSLOT_KC:(half + 1) * WSLOT_KC, :], WSLOT_KC)
            for th in range(2):
                b = P.bank()
                for kc in range(WSLOT_KC):
                    P.op("pe", lambda e, kc=kc, th=th, b=b, sd=sd: e.matmul(
                        C.ps[b][:], C.wbuf[sd][:, kc, :], C.actT[:, kc, th * 512:(th + 1) * 512],
                        start=(kc == 0), stop=(kc == WSLOT_KC - 1)),
                        reads=[("w", sd), ("actT", kc, th)], writes=[("ps", b)])
                sl = slice(th * 512, (th + 1) * 512)
                P.op("dve", lambda e, j=j, b=b, sl=sl: e.scalar_tensor_tensor(
                    out=C.hT[:, j, sl], in0=C.ps[b][:], scalar=0.5, in1=C.hT[:, j, sl],
                    op0=ALU.mult, op1=ALU.add),
                    reads=[("ps", b), ("hT", j)], writes=[("hT", j)])


HD_SCALE = 128.0 ** -0.5


def alloc_inproj(C):
    C.stf = [C.sb("stf%d" % i, [128, 512], F32) for i in range(4)]
    C.stb = [C.sb("stb%d" % i, [128, 512], BF16) for i in range(4)]
    C.n_stf = 0
    C.n_stb = 0
    C.kms = C.sb("kms_sb", [128, 16], F32)
    C.wg = C.sb("wg", [128, KC_D, 8], BF16)
    C.gst = C.sb("gst", [128, 8, 8], F32)
    C.vst = [C.sb("vst%d" % i, [128, 128], BF16) for i in range(4)]
    C.n_vst = 0


def _stage_out(C, kind, b, dst_ap, func=None, scale=1.0, eng="act", grp=None):
    P = C.P
    if kind == "f":
        i = C.n_stf % len(C.stf)
        C.n_stf += 1
        buf, key = C.stf[i], ("stf", i)
    else:
        i = C.n_stb % 4
        C.n_stb += 1
        buf, key = C.stb[i], ("stb", i)
    if eng == "act":
        P.op("act", lambda e: e.activation(out=buf[:], in_=C.ps[b][:], func=func or AF.Copy, scale=scale),
             reads=[("ps", b)], writes=[key])
    else:
        P.op("dve", lambda e: e.tensor_copy(out=buf[:], in_=C.ps[b][:]), reads=[("ps", b)], writes=[key])
    P.dma("sp", lambda e: e.dma_start(out=dst_ap, in_=buf[:]), reads=[key], writes=_gk(C, grp))


def _gk(C, grp):
    if grp is None:
        return []
    lst = C.grp_keys.setdefault(grp, [])
    k = ("xw", grp, len(lst), C.grp_epoch)
    lst.append(k)
    return [k]


def inproj(C, gidx, win, win_g, o, group_cb=None):
    P = C.P
    C.grp_keys = {}
    C.grp_epoch = getattr(C, "grp_epoch", 0) + 1

    def done(g):
        if group_cb is not None:
            group_cb(g, C.grp_keys.get(g, []))
    rmsnorm(C, gidx)
    P.dma("pool", lambda e: e.dma_start(out=C.wg[:], in_=win_g), writes=[("wg",)])

    def fm_chunks(lst):
        for (wj, kind, j) in lst:
            slot = wload(C, win[wj], KC_D)
            for th in range(2):
                b = P.bank()
                sl = slice(th * 512, (th + 1) * 512)
                mm_fm(C, slot, KC_D, C.hnT, "hnT", 0, th, b)
                if kind == "qa":
                    _stage_out(C, "b", b, o["qa_bf"][j][:, sl], scale=HD_SCALE)
                    _stage_out(C, "f", b, o["qa_f"][j][:, sl], eng="dve")
                elif kind == "ka":
                    _stage_out(C, "b", b, o["ka_bf"][j][:, sl], grp="kk")
                    P.op("dve", lambda e, j=j, th=th, b=b: e.tensor_reduce(
                        out=C.kms[:, j * 4 + th * 2:j * 4 + th * 2 + 2],
                        in_=C.ps[b][:].rearrange("p (a c) -> p a c", a=2), axis=AX.X, op=ALU.add),
                        reads=[("ps", b)], writes=[("kms", j, th)])
                elif kind == "qc":
                    _stage_out(C, "b", b, o["qc_bf"][j][:, sl], scale=HD_SCALE)
                elif kind == "kc":
                    _stage_out(C, "b", b, o["kc_bf"][j][:, sl], grp="kk")
                elif kind == "uq":
                    _stage_out(C, "f", b, o["uq"][j][:, sl], grp="uq")
                elif kind == "uk":
                    _stage_out(C, "f", b, o["uk"][j][:, sl], grp="uk")
                elif kind == "om":
                    _stage_out(C, "f", b, o["om"][j][:, sl], func=AF.Sigmoid)
    fm_chunks([(4 + j, "ka", j) for j in range(4)] + [(16 + j, "kc", j) for j in range(4)])
    done("kk")
    vt = [(8 + j, j) for j in range(4)] + [(20 + j, 4 + j) for j in range(4)] + [(32 + j, 8 + j) for j in range(8)]
    for (wj, cg) in vt:
        slot = wload(C, win[wj], KC_D)
        for tt in range(8):
            b = P.bank()
            for kc in range(KC_D):
                P.op("pe", lambda e, kc=kc, tt=tt, b=b, slot=slot: e.matmul(
                    C.ps[b][:, 0:128], C.hnT[:, kc, tt * 128:(tt + 1) * 128], C.wbuf[slot][:, kc, :],
                    start=(kc == 0), stop=(kc == KC_D - 1)),
                    reads=[("w", slot), ("hnT", kc)], writes=[("ps", b)])
            i = C.n_vst % 4
            C.n_vst += 1
            P.op("act" if tt % 2 else "dve",
                 (lambda e, i=i, b=b: e.activation(out=C.vst[i][:], in_=C.ps[b][:, 0:128], func=AF.Copy)) if tt % 2 else
                 (lambda e, i=i, b=b: e.tensor_copy(out=C.vst[i][:], in_=C.ps[b][:, 0:128])),
                 reads=[("ps", b)], writes=[("vst", i)])
            P.dma("sp", lambda e, i=i, tt=tt, cg=cg: e.dma_start(
                out=o["v_tm"][tt][:, cg * 128:(cg + 1) * 128], in_=C.vst[i][:]), reads=[("vst", i)],
                writes=_gk(C, "v0" if tt < 4 else "v1"))
    done("v0")
    done("v1")
    fm_chunks([(28 + j, "uk", j) for j in range(4)])
    done("uk")
    fm_chunks([(24 + j, "uq", j) for j in range(4)])
    done("uq")
    for tt in range(8):
        b = P.bank()
        for kc in range(KC_D):
            P.op("pe", lambda e, kc=kc, tt=tt, b=b: e.matmul(
                C.ps[b][:, 0:8], C.hnT[:, kc, tt * 128:(tt + 1) * 128], C.wg[:, kc, :],
                start=(kc == 0), stop=(kc == KC_D - 1)),
                reads=[("wg",), ("hnT", kc)], writes=[("ps", b)])
        P.op("dve", lambda e, tt=tt, b=b: e.tensor_copy(out=C.gst[:, tt, :], in_=C.ps[b][:, 0:8]),
             reads=[("ps", b)], writes=[("gst", tt)])
    P.dma("sp", lambda e: e.dma_start(out=o["gates"].rearrange("t p c -> p t c"), in_=C.gst[:]),
          reads=[("gst", tt) for tt in range(8)], writes=_gk(C, "gk"))
    P.dma("sp", lambda e: e.dma_start(out=o["kms"], in_=C.kms[:]),
          reads=[("kms", j, th) for j in range(4) for th in range(2)], writes=_gk(C, "gk"))
    done("gk")
    fm_chunks([(j, "qa", j) for j in range(4)] + [(12 + j, "qc", j) for j in range(4)] + [(40 + j, "om", j) for j in range(8)])


def outproj(C, mixT_dram, wout):
    P = C.P
    for kc in range(KC_D):
        P.dma("sp", lambda e, kc=kc: e.dma_start(out=C.hnT[:, kc, :], in_=mixT_dram[kc]), writes=[("hnT", kc)])
    for j in range(KC_D):
        slot = wload(C, wout[j], KC_D)
        for th in range(2):
            b = P.bank()
            sl = slice(th * 512, (th + 1) * 512)
            mm_fm(C, slot, KC_D, C.hnT, "hnT", 0, th, b)
            P.op("dve", lambda e, j=j, b=b, sl=sl: e.tensor_tensor(
                out=C.hT[:, j, sl], in0=C.ps[b][:], in1=C.hT[:, j, sl], op=ALU.add),
                reads=[("ps", b), ("hT", j)], writes=[("hT", j)])


def ple(C, gidx, wpg, wpp, pT_dram):
    P = C.P
    rmsnorm(C, gidx)
    for kc in range(2):
        P.dma("pool", lambda e, kc=kc: e.dma_start(out=C.actT[:, kc, :], in_=pT_dram[kc]), writes=[("actT", kc, 0), ("actT", kc, 1)])
    for j in range(KC_D):
        sg = wload(C, wpg[j], KC_D)
        sp_ = wload(C, wpp[j], 2)
        for th in range(2):
            bg, bp = P.bank(), P.bank()
            sl = slice(th * 512, (th + 1) * 512)
            mm_fm(C, sg, KC_D, C.hnT, "hnT", 0, th, bg)
            for kc in range(2):
                P.op("pe", lambda e, kc=kc, th=th, bp=bp, sp_=sp_: e.matmul(
                    C.ps[bp][:], C.wbuf[sp_][:, kc, :], C.actT[:, kc, th * 512:(th + 1) * 512],
                    start=(kc == 0), stop=(kc == 1)),
                    reads=[("w", sp_), ("actT", kc, th)], writes=[("ps", bp)])
            t = C.n_tmpf % len(C.tmpf)
            C.n_tmpf += 1
            P.op("act", lambda e, t=t, bg=bg: e.activation(out=C.tmpf[t][:], in_=C.ps[bg][:], func=AF.Sigmoid),
                 reads=[("ps", bg)], writes=[("tmpf", t)])
            P.op("dve", lambda e, t=t, bp=bp: e.tensor_tensor(
                out=C.tmpf[t][:], in0=C.ps[bp][:], in1=C.tmpf[t][:], op=ALU.mult),
                reads=[("ps", bp), ("tmpf", t)], writes=[("tmpf", t)])
            P.op("dve", lambda e, t=t, j=j, sl=sl: e.tensor_tensor(
                out=C.hT[:, j, sl], in0=C.hT[:, j, sl], in1=C.tmpf[t][:], op=ALU.add),
                reads=[("hT", j), ("tmpf", t)], writes=[("hT", j)])


def final_norm(C, gidx, out_dram):
    P = C.P

    def out_fn(kc):
        for th in range(2):
            sl = slice(th * 512, (th + 1) * 512)
            i = C.n_stf % len(C.stf)
            C.n_stf += 1
            P.op("dve", lambda e, kc=kc, sl=sl, i=i: e.scalar_tensor_tensor(
                out=C.stf[i][:], in0=C.hT[:, kc, sl], scalar=C.gvec[:, gidx, kc:kc + 1],
                in1=C.rstd[:, sl], op0=ALU.mult, op1=ALU.mult),
                reads=[("hT", kc), ("rstd", 0), ("rstd", 1), ("gvec", gidx)], writes=[("stf", i)])
            P.dma("sp", lambda e, kc=kc, sl=sl, i=i: e.dma_start(out=out_dram[kc][:, sl], in_=C.stf[i][:]),
                  reads=[("stf", i)])
    rmsnorm(C, gidx, out_fn=out_fn)


NEG = -30000.0
SLOPES_MOBA = [2.0 ** -1, 2.0 ** -3, 2.0 ** -5, 2.0 ** -7]
SLOPES_DIL = [2.0 ** -2, 2.0 ** -4, 2.0 ** -6, 2.0 ** -8]


def mixer_consts(is_b):
    kk = np.arange(128)[:, None]
    qq = np.arange(512)[None, :]

    def lnmult(dist):
        m = ((dist >= 0) & (dist <= 128)).astype(np.float64)
        m += ((dist >= 0) & (dist % 4 == 0) & (dist <= 512))
        m += ((dist >= 0) & (dist % 16 == 0) & (dist <= 2048))
        return np.where(m > 0, np.log(np.maximum(m, 1)), NEG).astype(np.float32)

    dil_loc = np.stack([lnmult(128 * d + qq - kk) for d in range(-3, 5)], axis=1)
    dil_rem = np.stack([lnmult(128 * d + qq - kk) for d in range(1, 6)], axis=1)
    if not is_b:
        dil_rem = np.full_like(dil_rem, NEG)
    cau = np.stack([np.where(128 * d + qq - kk >= 0, 0.0, NEG) for d in range(-3, 1)], axis=1).astype(np.float32)
    step = (cau == 0).astype(np.float32)
    pk = np.arange(2048)
    posK = np.stack([128.0 * (pk // 128), pk % 128, np.ones(2048), np.ones(2048)]).astype(np.float32)
    pq = 1024 + np.arange(1024)
    posQ = np.zeros((4, 8, 1024), np.float32)
    for i, s in enumerate(SLOPES_MOBA + SLOPES_DIL):
        posQ[0, i] = s
        posQ[1, i] = s
        posQ[2, i] = -s * 128.0 * (pq // 128)
        posQ[3, i] = -s * (pq % 128)
    E8 = np.zeros((8, 8 * 128), np.float32)
    for n in range(8):
        E8[n, n * 128:(n + 1) * 128] = 1.0
    G = np.zeros((1024, 8), np.float32)
    t = np.arange(1024)
    own = 4 + t // 256
    for n in range(8):
        G[:, n] = np.where(n == own, 1e30, np.where(n < own, 0.0, -1e30))
    if not is_b:
        G[:, 0:4] = -1e30
    G = np.ascontiguousarray(G.reshape(8, 128, 8).transpose(1, 0, 2))
    ident = np.eye(128, dtype=np.float32)
    U = np.triu(np.ones((128, 128), np.float32))
    rbias = np.full((128, 1), 0.0 if is_b else NEG, np.float32)
    return dict(dil_loc=dil_loc, dil_rem=dil_rem, cau=cau, step=step, posK=posK, posQ=posQ, E8=E8, G=G,
                ident=ident, U=U, rbias=rbias)


CONST_SHAPES = dict(dil_loc=[128, 8, 512], dil_rem=[128, 5, 512], cau=[128, 4, 512], step=[128, 4, 512],
                    posK=[4, 2048], posQ=[4, 8, 1024], E8=[8, 1024], G=[128, 8, 8], ident=[128, 128],
                    U=[128, 128], rbias=[128, 1])


def alloc_mixer(C):
    sb = C.sb
    C.m_dil_loc = sb("m_dil_loc", [128, 8, 512], F32)
    C.m_dil_rem = sb("m_dil_rem", [128, 5, 512], F32)
    C.m_cau = sb("m_cau", [128, 4, 512], F32)
    C.m_step = sb("m_step", [128, 4, 512], F32)
    C.m_posK = sb("m_posK", [4, 2048], BF16)
    C.m_posQ = sb("m_posQ", [4, 1024], BF16)
    C.m_E8 = sb("m_E8", [8, 1024], BF16)
    C.m_G = sb("m_G", [128, 8, 8], F32)
    C.m_ident = sb("m_ident", [128, 128], F32)
    C.m_U = sb("m_U", [128, 128], F32)
    C.m_rbias = sb("m_rbias", [128, 1], F32)
    C.m_onesf = sb("m_onesf", [128, 512], F32)
    C.m_cw = sb("m_cw", [128, 8, 4], F32)
    C.m_cb = sb("m_cb", [128, 8], F32)
    C.m_gb = sb("m_gb", [128, 8], F32)
    C.m_gh = sb("m_gh", [128, 8], F32)
    C.m_eps = sb("m_eps", [128, 1], F32)
    C.m_qT = sb("m_qT", [128, 1024], BF16)
    C.m_kT = sb("m_kT", [128, 2048], BF16)
    C.m_V = sb("m_V", [128, 16, 256], BF16)
    C.m_qf = sb("m_qf", [128, 1024], F32)
    C.m_kms = sb("m_kms", [128, 8], F32)
    C.m_gs = sb("m_gs", [128, 8], F32)
    C.m_m8 = sb("m_m8", [128, 8], F32)
    C.m_sel = sb("m_sel", [128, 8], F32)
    C.m_val = sb("m_val", [128, 8], F32)
    C.m_gs4 = [C.m_gs] + [sb("m_gs_%d" % i, [128, 8], F32) for i in range(3)]
    C.m_m84 = [C.m_m8] + [sb("m_m8_%d" % i, [128, 8], F32) for i in range(3)]
    C.m_sel4 = [C.m_sel] + [sb("m_sel_%d" % i, [128, 8], F32) for i in range(3)]
    C.m_val4 = [C.m_val] + [sb("m_val_%d" % i, [128, 8], F32) for i in range(3)]
    C.m_selbT = sb("m_selbT", [8, 1024], BF16)
    C.m_pT = [sb("m_pT%d" % i, [128, 512], BF16) for i in range(3)]
    C.m_tmp = [sb("m_tmp%d" % i, [128, 512], F32) for i in range(3)]
    C.m_rl = sb("m_rl", [128, 512], F32)
    C.m_ost = [sb("m_ost%d" % i, [128, 512], BF16) for i in range(2)]
    C.m_gl = sb("m_gl", [128, 16, 8], F32)
    C.m_lf = sb("m_lf", [128, 16, 4], F32)
    C.m_a = sb("m_a", [128, 16, 4], F32)
    C.m_lfrep = sb("m_lfrep", [128, 16, 128], F32)
    C.m_Fbc = sb("m_Fbc", [128, 1024], F32)
    C.m_uqb = sb("m_uqb", [128, 1027], F32)
    C.m_ukb = sb("m_ukb", [128, 2051], F32)
    C.m_acc = sb("m_acc", [128, 2048], F32)
    C.m_hm = [sb("m_hm%d" % i, [128, 512], F32) for i in range(2)]
    C.m_sqb = sb("m_sqb", [128, 512], BF16)
    C.m_omb = [sb("m_omb%d" % i, [128, 512], F32) for i in range(2)]
    C.m_onesb = sb("m_onesb", [128, 128], BF16)
    C.n_pT = 0
    C.n_tmp = 0
    C.n_ost = 0
    C.n_omb = 0
    C.m_tmpx = C.m_tmp
    C.m_pTx = C.m_pT + [sb("m_pT3", [128, 512], BF16)]


def load_mixer_consts(C, cd, conv_w, conv_b, gbias, ghead):
    P = C.P
    C.cd = cd
    for nm, q in (("dil_loc", "sp"), ("dil_rem", "sp"), ("cau", "sp"), ("step", "sp"), ("posK", "pool"),
                  ("E8", "pool"), ("G", "sp"), ("ident", "sp"), ("U", "sp"), ("rbias", "sp")):
        dst = getattr(C, "m_" + nm)
        P.dma(q, lambda e, dst=dst, nm=nm: e.dma_start(out=dst[:], in_=cd[nm]), writes=[("c_" + nm,)])
    P.dma("sp", lambda e: e.dma_start(out=C.m_cw[:], in_=conv_w), writes=[("c_cw",)])
    P.dma("sp", lambda e: e.dma_start(out=C.m_cb[:], in_=conv_b), writes=[("c_cb",)])
    P.dma("sp", lambda e: e.dma_start(out=C.m_gb[:], in_=gbias), writes=[("c_gb",)])
    P.dma("sp", lambda e: e.dma_start(out=C.m_gh[:], in_=ghead), writes=[("c_gh",)])
    P.op("dve", lambda e: e.memset(C.m_onesf[:], 1.0), writes=[("c_onesf",)])
    P.op("dve", lambda e: e.memset(C.m_onesb[:], 1.0), writes=[("c_onesb",)])
    P.op("dve", lambda e: e.memset(C.m_eps[:], 1e-6), writes=[("c_eps",)])
    P.op("dve", lambda e: e.memset(C.m_ukb[:, 0:3], 0.0), writes=[("ukb_pad",)])


def load_V(C, parts, tile0, c0, w, key):
    if not isinstance(parts, (list, tuple)):
        parts = [parts]
    t0 = tile0
    for ap in parts:
        n = ap.shape[0]
        C.P.dma("sp", lambda e, ap=ap, t0=t0, n=n: e.dma_start(
            out=C.m_V[:, t0:t0 + n, 0:w], in_=ap.rearrange("t p c -> p t c")[:, :, c0:c0 + w]), writes=[key])
        t0 += n


def _ktiles(qh):
    lst = [(kt, True, qh * 4 + 8 - kt) for kt in range(8)]
    lst += [(8 + lt, False, qh * 4 - lt) for lt in range(4 * (qh + 1))]
    return lst


def attn_group(C, kind, I, mixT):
    P = C.P
    moba = kind == "moba"
    if getattr(C, "fused", False):
        load_tabs(C, kind)
    qb, kL, kR = (I["qa_bf"], I["ka_bf_L"], I["ka_bf_R"]) if moba else (I["qc_bf"], I["kc_bf_L"], I["kc_bf_R"])
    vcol0 = 0 if moba else 512
    def head(h):
        hh = h if moba else 4 + h
        P.dma("pool", lambda e: e.dma_start(out=C.m_posQ[:], in_=C.cd["posQ"][:, hh, :]), writes=[("c_posQ",)])
        P.dma("sp", lambda e: e.dma_start(out=C.m_qT[:], in_=qb[h]), writes=[("m_qT",)])
        P.dma("sp", lambda e: e.dma_start(out=C.m_kT[:, 0:1024], in_=kR[h]), writes=[("m_kT", 0)])
        P.dma("sp", lambda e: e.dma_start(out=C.m_kT[:, 1024:2048], in_=kL[h]), writes=[("m_kT", 1)])
        c0 = vcol0 + h * 128
        load_V(C, I["v_tm_R"], 0, c0, 128, ("m_V", 0))
        load_V(C, I["v_tm_L"], 8, c0, 128, ("m_V", 1))
        if moba:
            P.dma("sp", lambda e: e.dma_start(out=C.m_qf[:], in_=I["qa_f"][h]), writes=[("m_qf",)])
            P.dma("sp", lambda e: e.dma_start(out=C.m_kms[:, 0:4], in_=I["kms_R"][:, h * 4:h * 4 + 4]), writes=[("m_kms", 0)])
            P.dma("sp", lambda e: e.dma_start(out=C.m_kms[:, 4:8], in_=I["kms_L"][:, h * 4:h * 4 + 4]), writes=[("m_kms", 1)])
            for qt in range(8):
                b = P.bank()
                g_ = qt % 4
                gs, m8, sel, val = C.m_gs4[g_], C.m_m84[g_], C.m_sel4[g_], C.m_val4[g_]
                kg = lambda n, g_=g_: (n, g_)
                P.op("pe", lambda e, qt=qt, b=b: e.matmul(C.ps[b][:, 0:8], C.m_qf[:, qt * 128:(qt + 1) * 128], C.m_kms[:],
                                                          start=True, stop=True),
                     reads=[("m_qf",), ("m_kms", 0), ("m_kms", 1)], writes=[("ps", b)])
                P.op("dve", lambda e, qt=qt, b=b, gs=gs: e.tensor_tensor(out=gs[:], in0=C.ps[b][:, 0:8], in1=C.m_G[:, qt, :], op=ALU.add),
                     reads=[("ps", b), ("c_G",)], writes=[kg("m_gs")])
                P.op("dve", lambda e, gs=gs, m8=m8: e.max(out=m8[:], in_=gs[:]), reads=[kg("m_gs")], writes=[kg("m_m8")])
                P.op("dve", lambda e, gs=gs, m8=m8, sel=sel: e.tensor_scalar(out=sel[:], in0=gs[:], scalar1=m8[:, 3:4], scalar2=None, op0=ALU.is_ge),
                     reads=[kg("m_gs"), kg("m_m8")], writes=[kg("m_sel")])
                P.op("dve", lambda e, gs=gs, val=val: e.tensor_scalar(out=val[:], in0=gs[:], scalar1=-1e29, scalar2=None, op0=ALU.is_gt),
                     reads=[kg("m_gs")], writes=[kg("m_val")])
                P.op("dve", lambda e, sel=sel, val=val: e.tensor_tensor(out=sel[:], in0=sel[:], in1=val[:], op=ALU.mult),
                     reads=[kg("m_sel"), kg("m_val")], writes=[kg("m_sel")])
                P.op("dve", lambda e, sel=sel: e.tensor_scalar(out=sel[:], in0=sel[:], scalar1=1.0, scalar2=-NEG,
                                                               op0=ALU.subtract, op1=ALU.mult),
                     reads=[kg("m_sel")], writes=[kg("m_sel")])
                b2 = P.bank()
                P.op("pe", lambda e, b2=b2, sel=sel: e.matmul(C.ps[b2][0:8, 0:128], sel[:], C.m_ident[:], start=True, stop=True),
                     reads=[kg("m_sel"), ("c_ident",)], writes=[("ps", b2)])
                P.op("act", lambda e, qt=qt, b2=b2: e.activation(out=C.m_selbT[:, qt * 128:(qt + 1) * 128], in_=C.ps[b2][0:8, 0:128], func=AF.Copy),
                     reads=[("ps", b2)], writes=[("m_selbT", qt)])

        def qhalf(qh):
            qsl = slice(qh * 512, (qh + 1) * 512)
            bo = P.bank(0, 4)
            bl = P.bank(0, 4)
            tiles = _ktiles(qh)
            def stage_S(ti):
                kt, rem, dl = tiles[ti]
                bs = P.bank(4, 8)
                ksl = slice(kt * 128, (kt + 1) * 128)
                P.op("pe", lambda e, bs=bs, ksl=ksl: e.matmul(C.ps[bs][:], C.m_kT[:, ksl], C.m_qT[:, qsl], start=True, stop=False),
                     reads=[("m_kT", 0 if rem else 1), ("m_qT",)], writes=[("ps", bs)])
                P.op("pe", lambda e, bs=bs, ksl=ksl: e.matmul(C.ps[bs][:], C.m_posK[:, ksl], C.m_posQ[:, qsl], start=False, stop=(not moba)),
                     reads=[("c_posK",), ("c_posQ",)], writes=[("ps", bs)])
                if moba:
                    n = kt // 2
                    P.op("pe", lambda e, bs=bs, n=n: e.matmul(C.ps[bs][:], C.m_E8[:, n * 128:(n + 1) * 128], C.m_selbT[:, qsl], start=False, stop=True),
                         reads=[("c_E8",)] + [("m_selbT", qh * 4 + i) for i in range(4)], writes=[("ps", bs)])
                tab = None
                if not moba:
                    tab = C.m_dil_rem[:, min(dl, 5) - 1, :] if rem else C.m_dil_loc[:, dl + 3, :]
                elif (not rem) and dl <= 0:
                    tab = C.m_cau[:, dl + 3, :]
                pi = C.n_pT % len(C.m_pT)
                C.n_pT += 1
                if tab is not None:
                    t = C.n_tmp % 3
                    C.n_tmp += 1
                    P.op("dve", lambda e, bs=bs, t=t, tab=tab: e.tensor_tensor(out=C.m_tmp[t][:], in0=C.ps[bs][:], in1=tab, op=ALU.add),
                         reads=[("ps", bs), ("c_dil_loc",), ("c_dil_rem",), ("c_cau",)], writes=[("m_tmp", t)])
                    P.op("act", lambda e, t=t, pi=pi: e.activation(out=C.m_pT[pi][:], in_=C.m_tmp[t][:], func=AF.Exp),
                         reads=[("m_tmp", t)], writes=[("m_pT", pi)])
                else:
                    P.op("act", lambda e, bs=bs, pi=pi: e.activation(out=C.m_pT[pi][:], in_=C.ps[bs][:], func=AF.Exp),
                         reads=[("ps", bs)], writes=[("m_pT", pi)])
                return pi

            def stage_V(ti, pi):
                kt, rem, dl = tiles[ti]
                first, last = ti == 0, ti == len(tiles) - 1
                P.op("pe", lambda e, kt=kt, pi=pi, first=first, last=last: e.matmul(
                    C.ps[bo][:], C.m_V[:, kt, 0:128], C.m_pT[pi][:], start=first, stop=last),
                    reads=[("m_V", 0 if rem else 1), ("m_pT", pi)], writes=[("ps", bo)])
                P.op("pe", lambda e, pi=pi, first=first, last=last: e.matmul(
                    C.ps[bl][:], C.m_onesb[:], C.m_pT[pi][:], start=first, stop=last),
                    reads=[("c_onesb",), ("m_pT", pi)], writes=[("ps", bl)])
            LA = 3 if len(C.m_pT) >= 4 else 2
            pis = {}
            for step in range(len(tiles) + LA):
                if step < len(tiles):
                    pis[step] = stage_S(step)
                if step - LA >= 0:
                    stage_V(step - LA, pis[step - LA])
            P.op("act", lambda e: e.activation(out=C.m_rl[:], in_=C.ps[bl][:], func=AF.Ln), reads=[("ps", bl)], writes=[("m_rl",)])
            P.op("act", lambda e: e.activation(out=C.m_rl[:], in_=C.m_rl[:], func=AF.Exp, scale=-1.0), reads=[("m_rl",)], writes=[("m_rl",)])
            oi = C.n_ost % 2
            C.n_ost += 1
            P.op("dve", lambda e, oi=oi: e.tensor_tensor(out=C.m_ost[oi][:], in0=C.ps[bo][:], in1=C.m_rl[:], op=ALU.mult),
                 reads=[("ps", bo), ("m_rl",)], writes=[("m_ost", oi)])
            ch = h if moba else 12 + h
            P.dma("sp", lambda e, oi=oi, ch=ch: e.dma_start(out=mixT[ch][:, qsl], in_=C.m_ost[oi][:]), reads=[("m_ost", oi)])
        for qh in range(2):
            qhalf(qh)
    for h in range(4):
        head(h)


DK_SCALE = 128.0 ** -0.5


def mlstm_group(C, I, mixT):
    P = C.P
    if getattr(C, "fused", False):
        load_tabs(C, "mlstm")
        l_ = C.m_layer
        cw_, cb_, gb_, gh_ = C.m_cw[:, l_], C.m_cb[:, l_], C.m_gb[:, l_], C.m_gh[:, l_]
    else:
        cw_, cb_, gb_, gh_ = C.m_cw, C.m_cb, C.m_gb, C.m_gh
    P.dma("sp", lambda e: e.dma_start(out=C.m_gl[:, 0:8, :], in_=I["gates_R"].rearrange("t p c -> p t c")), writes=[("m_gl", 0)])
    P.dma("sp", lambda e: e.dma_start(out=C.m_gl[:, 8:16, :], in_=I["gates_L"].rearrange("t p c -> p t c")), writes=[("m_gl", 1)])
    for t in range(16):
        P.op("dve", lambda e, t=t: e.tensor_tensor(out=C.m_gl[:, t, :], in0=C.m_gl[:, t, :], in1=gb_[:], op=ALU.add),
             reads=[("m_gl", 0), ("m_gl", 1), ("c_gb",)], writes=[("m_gl", 0), ("m_gl", 1)])
    P.op("act", lambda e: e.activation(out=C.m_lf[:], in_=C.m_gl[:, :, 4:8], func=AF.Exp, scale=-1.0),
         reads=[("m_gl", 0), ("m_gl", 1)], writes=[("m_lf",)])
    P.op("act", lambda e: e.activation(out=C.m_lf[:], in_=C.m_lf[:], func=AF.Ln, bias=C.m_onesf[:, 0:1], scale=1.0),
         reads=[("m_lf",), ("c_onesf",)], writes=[("m_lf",)])
    P.op("dve", lambda e: e.tensor_scalar(out=C.m_lf[:], in0=C.m_lf[:], scalar1=-1.0, scalar2=None, op0=ALU.mult),
         reads=[("m_lf",)], writes=[("m_lf",)])
    P.op("dve", lambda e: e.tensor_scalar(out=C.m_gl[:, 0:8, 0:4], in0=C.m_gl[:, 0:8, 0:4], scalar1=C.m_rbias[:, 0:1],
                                          scalar2=None, op0=ALU.add),
         reads=[("m_gl", 0), ("c_rbias",)], writes=[("m_gl", 0)])
    for i in range(16):
        b = P.bank()
        for r in range(i + 1):
            lhs = C.m_U if r == i else C.m_onesf
            P.op("pe", lambda e, r=r, b=b, lhs=lhs, i=i: e.matmul(C.ps[b][:, 0:4], lhs[:, 0:128], C.m_lf[:, r, :],
                                                             start=(r == 0), stop=(r == i)),
                 reads=[("m_lf",), ("c_U",), ("c_onesf",)], writes=[("ps", b)])
        P.op("dve", lambda e, i=i, b=b: e.tensor_tensor(out=C.m_a[:, i, :], in0=C.m_gl[:, i, 0:4], in1=C.ps[b][:, 0:4], op=ALU.subtract),
             reads=[("ps", b), ("m_gl", 0), ("m_gl", 1)], writes=[("m_a",)])
    def head(h):
        for t in range(16):
            P.op("dve", lambda e, t=t: e.tensor_scalar(out=C.m_lfrep[:, t, :], in0=C.m_onesf[:, 0:128], scalar1=C.m_lf[:, t, h:h + 1],
                                                       scalar2=None, op0=ALU.mult),
                 reads=[("m_lf",), ("c_onesf",)], writes=[("m_lfrep",)])
        for qh in range(2):
            b = P.bank()
            tl = _ktiles(qh)
            for ti, (kt, rem, dl) in enumerate(tl):
                rhs = C.m_onesf[:, :] if (rem or dl >= 1) else C.m_step[:, dl + 3, :]
                P.op("pe", lambda e, kt=kt, rhs=rhs, ti=ti, b=b, n=len(tl): e.matmul(
                    C.ps[b][:], C.m_lfrep[:, kt, :], rhs, start=(ti == 0), stop=(ti == n - 1)),
                    reads=[("m_lfrep",), ("c_onesf",), ("c_step",)], writes=[("ps", b)])
            P.op("act", lambda e, b=b, qh=qh: e.activation(out=C.m_Fbc[:, qh * 512:(qh + 1) * 512], in_=C.ps[b][:], func=AF.Copy),
                 reads=[("ps", b)], writes=[("m_Fbc", qh)])
        P.dma("sp", lambda e: e.dma_start(out=C.m_uqb[:, 0:3], in_=I["uq_R"][h][:, 1021:1024]), writes=[("m_uqb", 0)])
        P.dma("sp", lambda e: e.dma_start(out=C.m_uqb[:, 3:1027], in_=I["uq_L"][h]), writes=[("m_uqb", 1)])
        P.dma("sp", lambda e: e.dma_start(out=C.m_ukb[:, 3:1027], in_=I["uk_R"][h]), writes=[("m_ukb", 0)])
        P.dma("sp", lambda e: e.dma_start(out=C.m_ukb[:, 1027:2051], in_=I["uk_L"][h]), writes=[("m_ukb", 1)])
        if getattr(C, "fused", False):
            P.op("dve", lambda e: e.tensor_scalar(out=C.m_uqb[:, 0:3], in0=C.m_uqb[:, 0:3], scalar1=C.m_flagB[:, 0:1], scalar2=None, op0=ALU.mult),
                 reads=[("m_uqb", 0), ("c_flagB",)], writes=[("m_uqb", 0)])
            P.op("dve", lambda e: e.tensor_scalar(out=C.m_ukb[:, 1024:1027], in0=C.m_ukb[:, 1024:1027], scalar1=C.m_flagB[:, 0:1], scalar2=None, op0=ALU.mult),
                 reads=[("m_ukb", 0), ("c_flagB",)], writes=[("m_ukb", 0)])
        for (src, skeys, n, c, dst, dkey, scale) in ((C.m_uqb, [("m_uqb", 0), ("m_uqb", 1)], 1024, h, C.m_qT, ("m_qT",), DK_SCALE),
                                                     (C.m_ukb, [("m_ukb", 0), ("m_ukb", 1), ("ukb_pad",)], 2048, 4 + h, C.m_kT, None, None)):
            P.op("dve", lambda e, src=src, n=n, c=c: e.tensor_scalar(out=C.m_acc[:, 0:n], in0=src[:, 3:3 + n], scalar1=cw_[:, c, 3:4],
                                                                 scalar2=None, op0=ALU.mult),
                 reads=skeys + [("c_cw",)], writes=[("m_acc",)])
            for j in (2, 1, 0):
                P.op("dve", lambda e, src=src, n=n, c=c, j=j: e.scalar_tensor_tensor(
                    out=C.m_acc[:, 0:n], in0=src[:, j:j + n], scalar=cw_[:, c, j:j + 1], in1=C.m_acc[:, 0:n],
                    op0=ALU.mult, op1=ALU.add),
                    reads=skeys + [("c_cw",), ("m_acc",)], writes=[("m_acc",)])
            if scale is None:
                P.op("act", lambda e, n=n, c=c, dst=dst: e.activation(out=dst[:, 0:n], in_=C.m_acc[:, 0:n], func=AF.Silu, bias=cb_[:, c:c + 1]),
                     reads=[("m_acc",), ("c_cb",)], writes=[("m_kT", 0), ("m_kT", 1)])
            else:
                P.op("act", lambda e, n=n, c=c: e.activation(out=C.m_acc[:, 0:n], in_=C.m_acc[:, 0:n], func=AF.Silu, bias=cb_[:, c:c + 1]),
                     reads=[("m_acc",), ("c_cb",)], writes=[("m_acc",)])
                P.op("dve", lambda e, n=n, dst=dst, scale=scale: e.tensor_scalar(out=dst[:, 0:n], in0=C.m_acc[:, 0:n], scalar1=scale, scalar2=None, op0=ALU.mult),
                     reads=[("m_acc",)], writes=[dkey])
        c0 = 1024 + h * 256
        load_V(C, I["v_tm_R"], 0, c0, 256, ("m_V", 0))
        load_V(C, I["v_tm_L"], 8, c0, 256, ("m_V", 1))

        def qhalf(qh):
            qsl = slice(qh * 512, (qh + 1) * 512)
            bn = [0, 1]
            bd = 2
            tiles = _ktiles(qh)
            def stage_S(ti):
                kt, rem, dl = tiles[ti]
                bs = P.bank(4, 8)
                ksl = slice(kt * 128, (kt + 1) * 128)
                P.op("pe", lambda e, bs=bs, ksl=ksl: e.matmul(C.ps[bs][:], C.m_kT[:, ksl], C.m_qT[:, qsl], start=True, stop=True),
                     reads=[("m_kT", 0 if rem else 1), ("m_qT",)], writes=[("ps", bs)])
                NT = len(C.m_tmpx)
                t = C.n_tmp % NT
                C.n_tmp += 1
                if (not rem) and dl <= 0:
                    t2 = C.n_tmp % NT
                    C.n_tmp += 1
                    P.op("dve", lambda e, t2=t2, dl=dl: e.tensor_tensor(out=C.m_tmpx[t2][:], in0=C.m_Fbc[:, qsl], in1=C.m_cau[:, dl + 3, :], op=ALU.add),
                         reads=[("m_Fbc", qh), ("c_cau",)], writes=[("m_tmp", t2)])
                    P.op("act", lambda e, t=t, t2=t2, kt=kt: e.activation(out=C.m_tmpx[t][:], in_=C.m_tmpx[t2][:], func=AF.Exp, bias=C.m_a[:, kt, h:h + 1]),
                         reads=[("m_tmp", t2), ("m_a",)], writes=[("m_tmp", t)])
                else:
                    P.op("act", lambda e, t=t, kt=kt: e.activation(out=C.m_tmpx[t][:], in_=C.m_Fbc[:, qsl], func=AF.Exp, bias=C.m_a[:, kt, h:h + 1]),
                         reads=[("m_Fbc", qh), ("m_a",)], writes=[("m_tmp", t)])
                pi = C.n_pT % len(C.m_pTx)
                C.n_pT += 1
                P.op("dve", lambda e, bs=bs, t=t, pi=pi: e.tensor_tensor(out=C.m_pTx[pi][:], in0=C.ps[bs][:], in1=C.m_tmpx[t][:], op=ALU.mult),
                     reads=[("ps", bs), ("m_tmp", t)], writes=[("m_pT", pi)])
                return pi

            def stage_V(ti, pi):
                kt, rem, dl = tiles[ti]
                first, last = ti == 0, ti == len(tiles) - 1
                for dvc in range(2):
                    P.op("pe", lambda e, kt=kt, pi=pi, dvc=dvc, first=first, last=last: e.matmul(
                        C.ps[bn[dvc]][:], C.m_V[:, kt, dvc * 128:(dvc + 1) * 128], C.m_pTx[pi][:], start=first, stop=last),
                        reads=[("m_V", 0 if rem else 1), ("m_pT", pi)], writes=[("ps", bn[dvc])])
                P.op("pe", lambda e, pi=pi, first=first, last=last: e.matmul(C.ps[bd][:], C.m_onesb[:], C.m_pTx[pi][:], start=first, stop=last),
                     reads=[("c_onesb",), ("m_pT", pi)], writes=[("ps", bd)])
            LA = 3
            pis = {}
            for step in range(len(tiles) + LA):
                if step < len(tiles):
                    pis[step] = stage_S(step)
                if step - LA >= 0:
                    stage_V(step - LA, pis[step - LA])
            P.op("act", lambda e: e.activation(out=C.m_rl[:], in_=C.ps[bd][:], func=AF.Abs),
                 reads=[("ps", bd)], writes=[("m_rl",)])
            P.op("dve", lambda e: e.tensor_scalar(out=C.m_rl[:], in0=C.m_rl[:], scalar1=1.0, scalar2=None, op0=ALU.max),
                 reads=[("m_rl",)], writes=[("m_rl",)])
            P.op("act", lambda e: e.activation(out=C.m_rl[:], in_=C.m_rl[:], func=AF.Ln), reads=[("m_rl",)], writes=[("m_rl",)])
            P.op("act", lambda e: e.activation(out=C.m_rl[:], in_=C.m_rl[:], func=AF.Exp, scale=-1.0), reads=[("m_rl",)], writes=[("m_rl",)])
            bq = 3
            for dvc in range(2):
                P.op("dve", lambda e, dvc=dvc: e.tensor_tensor(out=C.m_hm[dvc][:], in0=C.ps[bn[dvc]][:], in1=C.m_rl[:], op=ALU.mult),
                     reads=[("ps", bn[dvc]), ("m_rl",)], writes=[("m_hm", dvc)])
                P.op("act", lambda e, dvc=dvc: e.activation(out=C.m_sqb[:], in_=C.m_hm[dvc][:], func=AF.Square),
                     reads=[("m_hm", dvc)], writes=[("m_sqb",)])
                P.op("pe", lambda e, dvc=dvc: e.matmul(C.ps[bq][:], C.m_onesb[:], C.m_sqb[:], start=(dvc == 0), stop=(dvc == 1)),
                     reads=[("m_sqb",), ("c_onesb",)], writes=[("ps", bq)])
            P.op("act", lambda e: e.activation(out=C.m_rl[:], in_=C.ps[bq][:], func=AF.Ln, bias=C.m_eps[:, 0:1], scale=1.0 / 256),
                 reads=[("ps", bq), ("c_eps",), ("m_hm", 0), ("m_hm", 1)], writes=[("m_rl",)])
            P.op("act", lambda e: e.activation(out=C.m_rl[:], in_=C.m_rl[:], func=AF.Exp, scale=-0.5), reads=[("m_rl",)], writes=[("m_rl",)])
            for dvc in range(2):
                cc = 2 * h + dvc
                oi = C.n_omb % 2
                C.n_omb += 1
                P.dma("sp", lambda e, cc=cc, oi=oi: e.dma_start(out=C.m_omb[oi][:], in_=I["om"][cc][:, qsl]), writes=[("m_omb", oi)])
                P.op("dve", lambda e, dvc=dvc, cc=cc: e.scalar_tensor_tensor(
                    out=C.m_hm[dvc][:], in0=C.m_hm[dvc][:], scalar=gh_[:, cc:cc + 1], in1=C.m_rl[:], op0=ALU.mult, op1=ALU.mult),
                    reads=[("m_hm", dvc), ("c_gh",), ("m_rl",)], writes=[("m_hm", dvc)])
                si = C.n_ost % 2
                C.n_ost += 1
                P.op("dve", lambda e, dvc=dvc, oi=oi, si=si: e.tensor_tensor(out=C.m_ost[si][:], in0=C.m_hm[dvc][:], in1=C.m_omb[oi][:], op=ALU.mult),
                     reads=[("m_hm", dvc), ("m_omb", oi)], writes=[("m_ost", si)])
                P.dma("sp", lambda e, si=si, cc=cc: e.dma_start(out=mixT[4 + cc][:, qsl], in_=C.m_ost[si][:]), reads=[("m_ost", si)])
        for qh in range(2):
            qhalf(qh)
    for h in range(4):
        head(h)


from contextlib import ExitStack as _ES

Z_OUT = dict(qa_bf=([4, 128, 1024], BF16), qa_f=([4, 128, 1024], F32), ka_bf=([4, 128, 1024], BF16),
             kms=([128, 16], F32), qc_bf=([4, 128, 1024], BF16), kc_bf=([4, 128, 1024], BF16),
             uq=([4, 128, 1024], F32), uk=([4, 128, 1024], F32), om=([8, 128, 1024], F32),
             v_tm=([8, 128, 2048], BF16), gates=([8, 128, 8], F32))


def build_A():
    nc = bass.Bass("TRN2", target_bir_lowering=False)
    with _ES() as es:
        C = Ctx(nc, es)
        h_in = C.dram("h_in", [16, 128, 1024], F32, "ExternalInput")
        g1 = C.dram("g1", [128, 16], F32, "ExternalInput")
        gm = C.dram("gm", [128, 16], F32, "ExternalInput")
        wup = C.dram("wup", [88, 128, 16, 128], F32, "ExternalInput")
        wdown = C.dram("wdown", [16, 128, 44, 128], F32, "ExternalInput")
        win = C.dram("win", [48, 128, 16, 128], F32, "ExternalInput")
        win_g = C.dram("win_g", [128, 16, 8], F32, "ExternalInput")
        h_out = C.dram("h_out", [16, 128, 1024], F32, "ExternalOutput")
        o = {k: C.dram(k, sh, dt, "ExternalOutput") for k, (sh, dt) in Z_OUT.items()}
        alloc_token_local(C)
        alloc_inproj(C)
        load_gain(C, 0, g1)
        load_gain(C, 1, gm)
        load_h(C, h_in)
        ffn(C, 0, wup, wdown)
        store_h(C, h_out)
        inproj(C, 1, win, win_g, o)
        C.P.build(es)
    return nc


M_IN = dict(qa_bf=([4, 128, 1024], BF16), qa_f=([4, 128, 1024], F32), ka_bf_L=([4, 128, 1024], BF16),
            ka_bf_R=([4, 128, 1024], BF16), kms_L=([128, 16], F32), kms_R=([128, 16], F32),
            qc_bf=([4, 128, 1024], BF16), kc_bf_L=([4, 128, 1024], BF16), kc_bf_R=([4, 128, 1024], BF16),
            uq_L=([4, 128, 1024], F32), uq_R=([4, 128, 1024], F32), uk_L=([4, 128, 1024], F32),
            uk_R=([4, 128, 1024], F32), om=([8, 128, 1024], F32), v_tm_L=([8, 128, 2048], BF16),
            v_tm_R=([8, 128, 2048], BF16), gates_L=([8, 128, 8], F32), gates_R=([8, 128, 8], F32))


def build_M():
    nc = bass.Bass("TRN2", target_bir_lowering=False)
    with _ES() as es:
        C = Ctx(nc, es)
        I = {k: C.dram(k, sh, dt, "ExternalInput") for k, (sh, dt) in M_IN.items()}
        cd = {k: C.dram("c_" + k, sh, F32, "ExternalInput") for k, sh in CONST_SHAPES.items()}
        conv_w = C.dram("conv_w", [128, 8, 4], F32, "ExternalInput")
        conv_b = C.dram("conv_b", [128, 8], F32, "ExternalInput")
        gbias = C.dram("gbias", [128, 8], F32, "ExternalInput")
        ghead = C.dram("ghead", [128, 8], F32, "ExternalInput")
        mixT = C.dram("mixT", [16, 128, 1024], BF16, "ExternalOutput")
        alloc_mixer(C)
        load_mixer_consts(C, cd, conv_w, conv_b, gbias, ghead)
        attn_group(C, "moba", I, mixT)
        attn_group(C, "dil", I, mixT)
        mlstm_group(C, I, mixT)
        C.P.build(es)
    return nc


def build_C(final):
    nc = bass.Bass("TRN2", target_bir_lowering=False)
    with _ES() as es:
        C = Ctx(nc, es)
        h_in = C.dram("h_in", [16, 128, 1024], F32, "ExternalInput")
        mixT = C.dram("mixT", [16, 128, 1024], BF16, "ExternalInput")
        wout = C.dram("wout", [16, 128, 16, 128], F32, "ExternalInput")
        g2 = C.dram("g2", [128, 16], F32, "ExternalInput")
        gp = C.dram("gp", [128, 16], F32, "ExternalInput")
        gf = C.dram("gf", [128, 16], F32, "ExternalInput")
        wup = C.dram("wup", [88, 128, 16, 128], F32, "ExternalInput")
        wdown = C.dram("wdown", [16, 128, 44, 128], F32, "ExternalInput")
        wpg = C.dram("wpg", [16, 128, 16, 128], F32, "ExternalInput")
        wpp = C.dram("wpp", [16, 128, 2, 128], F32, "ExternalInput")
        pT = C.dram("pT", [2, 128, 1024], F32, "ExternalInput")
        h_out = C.dram("h_out", [16, 128, 1024], F32, "ExternalOutput")
        alloc_token_local(C)
        alloc_inproj(C)
        load_gain(C, 0, g2)
        load_gain(C, 1, gp)
        load_gain(C, 2, gf)
        load_h(C, h_in)
        outproj(C, mixT, wout)
        ffn(C, 0, wup, wdown)
        ple(C, 1, wpg, wpp, pT)
        if final:
            final_norm(C, 2, h_out)
        else:
            store_h(C, h_out)
        C.P.build(es)
    return nc


def tiles_fm(W):
    Kd, M = W.shape
    return np.ascontiguousarray(W.reshape(Kd // 128, 128, M // 128, 128).transpose(2, 1, 0, 3))


def vec_fm(g):
    return np.ascontiguousarray(np.asarray(g, np.float32).reshape(-1, 128).T)


def _run(nc, in_maps):
    res = run_bass_kernel_spmd(nc, in_maps, core_ids=list(range(8)))
    return res.results


def kernel_unfused(x, p, g_ffn1, w_up1, w_down1, g_mix, w_in, conv_w, conv_b, b_igate, b_fgate,
           g_head, w_out, g_ffn2, w_up2, w_down2, g_ple, w_ple_gate, w_ple_proj, g_final):
    f = lambda a: np.asarray(a, dtype=np.float32)
    x, p = f(x), f(p)
    ncA, ncM, ncC, ncF = build_A(), build_M(), build_C(False), build_C(True)
    consts = [mixer_consts(c % 2 == 1) for c in range(8)]
    hs = []
    for c in range(8):
        b, s = c // 2, c % 2
        hs.append(np.ascontiguousarray(x[b, s * 1024:(s + 1) * 1024, :].T.reshape(16, 128, 1024)))
    out = None
    for l in range(4):
        wi = f(w_in[l])
        a_in = dict(g1=vec_fm(g_ffn1[l]), gm=vec_fm(g_mix[l]), wup=tiles_fm(f(w_up1[l])), wdown=tiles_fm(f(w_down1[l])),
                    win=tiles_fm(wi[:, :6144]),
                    win_g=np.ascontiguousarray(wi[:, 6144:6152].reshape(16, 128, 8).transpose(1, 0, 2)))
        ra = _run(ncA, [dict(a_in, h_in=hs[c]) for c in range(8)])
        hs = [ra[c]["h_out"] for c in range(8)]
        cw = np.ascontiguousarray(f(conv_w[l]).reshape(4, 8, 128).transpose(2, 1, 0))
        m_common = dict(conv_w=cw, conv_b=vec_fm(conv_b[l]),
                        gbias=np.ascontiguousarray(np.broadcast_to(np.concatenate([f(b_igate[l]), f(b_fgate[l])])[None, :], (128, 8))),
                        ghead=vec_fm(g_head[l]))
        m_in = []
        for c in range(8):
            d = dict(m_common)
            for k, v in consts[c].items():
                d["c_" + k] = v
            me, other = ra[c], ra[c - 1] if c % 2 == 1 else None
            for k in ("qa_bf", "qa_f", "qc_bf", "om"):
                d[k] = me[k]
            for k in ("ka_bf", "kms", "kc_bf", "uq", "uk", "v_tm", "gates"):
                d[k + "_L"] = me[k]
                d[k + "_R"] = other[k] if other is not None else np.zeros_like(me[k])
            m_in.append(d)
        rm = _run(ncM, m_in)
        c_common = dict(wout=tiles_fm(f(w_out[l])), g2=vec_fm(g_ffn2[l]), gp=vec_fm(g_ple[l]), gf=vec_fm(g_final),
                        wup=tiles_fm(f(w_up2[l])), wdown=tiles_fm(f(w_down2[l])), wpg=tiles_fm(f(w_ple_gate[l])),
                        wpp=tiles_fm(f(w_ple_proj[l])))
        c_in = []
        for c in range(8):
            b, s = c // 2, c % 2
            pT = np.ascontiguousarray(p[l, b, s * 1024:(s + 1) * 1024, :].T.reshape(2, 128, 1024))
            c_in.append(dict(c_common, h_in=hs[c], mixT=rm[c]["mixT"], pT=pT))
        rc = _run(ncF if l == 3 else ncC, c_in)
        hs = [rc[c]["h_out"] for c in range(8)]
    out = np.empty((4, 2048, 2048), np.float32)
    for c in range(8):
        b, s = c // 2, c % 2
        out[b, s * 1024:(s + 1) * 1024, :] = hs[c].reshape(2048, 1024).T
    return out


ARENA_BYTES = 65536 + 106496
NL = 4


def alloc_fused(C):
    sb = C.sb
    C.fused = True
    arena = sb("arena", [128, ARENA_BYTES // 4], F32)

    def view(off, shape, dt, parts=128):
        n = int(np.prod(shape[1:]))
        nb = n * (4 if dt == F32 else 2)
        assert off % 4 == 0 and off + nb <= ARENA_BYTES, (off, nb)
        ap = arena[0:parts, off // 4:(off + nb + 3) // 4]
        if dt != F32:
            ap = ap.bitcast(dt)
        if len(shape) == 3:
            ap = ap.rearrange("p (a b) -> p a b", b=shape[2])
        return ap

    K = 1024
    C.hT = view(0, [128, KC_D, NTOK], F32)
    base = 65536
    C.hnT = view(base, [128, KC_D, NTOK], BF16)
    C.actT = view(base + 32 * K, [128, WSLOT_KC, NTOK], BF16)
    C.wbuf = [view(base + 76 * K + i * 5632, [128, WSLOT_KC, 128], BF16) for i in range(WSLOTS)]
    o = base
    C.m_tab = view(o, [128, 13, 512], F32); o += 26 * K
    C.m_dil_loc = C.m_tab[:, 0:8, :]
    C.m_dil_rem = C.m_tab[:, 8:13, :]
    C.m_cau = C.m_tab[:, 0:4, :]
    C.m_step = C.m_tab[:, 4:8, :]
    C.m_posQ = view(o, [4, 1024], BF16, parts=4); o += 2 * K
    C.m_qT = view(o, [128, 1024], BF16); o += 2 * K
    C.m_kT = view(o, [128, 2048], BF16); o += 4 * K
    C.m_V = view(o, [128, 16, 256], BF16); o += 8 * K
    C.m_qf = view(o, [128, 1024], F32); o += 4 * K
    C.m_selbT = view(o, [8, 1024], BF16, parts=8); o += 2 * K
    C.m_pT = []
    for i in range(3):
        C.m_pT.append(view(o, [128, 512], BF16)); o += K
    C.m_tmp = []
    for i in range(3):
        C.m_tmp.append(view(o, [128, 512], F32)); o += 2 * K
    C.m_tmpx = C.m_tmp + [C.m_tab[:, 8 + i, :] for i in range(4)]
    C.m_pT = C.m_pT + [sb("m_pT3", [128, 512], BF16)]
    C.m_pTx = C.m_pT
    C.m_rl = view(o, [128, 512], F32); o += 2 * K
    C.m_ost = []
    for i in range(2):
        C.m_ost.append(view(o, [128, 512], BF16)); o += K
    C.m_lfrep = view(o, [128, 16, 128], F32); o += 8 * K
    C.m_Fbc = view(o, [128, 1024], F32); o += 4 * K
    C.m_uqb = view(o, [128, 1027], F32); o += 5 * K
    C.m_ukb = view(o, [128, 2051], F32); o += 9 * K
    C.m_acc = view(o, [128, 2048], F32); o += 8 * K
    C.m_hm = []
    for i in range(2):
        C.m_hm.append(view(o, [128, 512], F32)); o += 2 * K
    C.m_sqb = view(o, [128, 512], BF16); o += K
    C.m_omb = []
    for i in range(2):
        C.m_omb.append(view(o, [128, 512], F32)); o += 2 * K
    assert o <= ARENA_BYTES, o
    C.sq = [sb("sq%d" % i, [128, NTOK], BF16) for i in range(2)]
    C.rstd = sb("rstd", [128, NTOK], F32)
    C.tmpf = [sb("tmpf%d" % i, [128, 512], F32) for i in range(2)]
    C.ones_bf = sb("ones_bf", [128, 128], BF16)
    C.epsv = sb("epsv", [128, 1], F32)
    C.gvec = sb("gvec", [128, 4 * NL + 1, KC_D], F32)
    C.stf = [sb("stf%d" % i, [128, 512], F32) for i in range(3)]
    C.stb = [sb("stb%d" % i, [128, 512], BF16) for i in range(4)]
    C.vst = [sb("vst%d" % i, [128, 128], BF16) for i in range(4)]
    C.kms = sb("kms_sb", [128, 16], F32)
    C.wg = sb("wg", [128, KC_D, 8], BF16)
    C.gst = sb("gst", [128, 8, 8], F32)
    C.m_posK = sb("m_posK", [4, 2048], BF16)
    C.m_E8 = sb("m_E8", [8, 1024], BF16)
    C.m_G = sb("m_G", [128, 8, 8], F32)
    C.m_ident = sb("m_ident", [128, 128], F32)
    C.m_U = sb("m_U", [128, 128], F32)
    C.m_rbias = sb("m_rbias", [128, 1], F32)
    C.m_flagB = sb("m_flagB", [128, 1], F32)
    C.m_onesf = sb("m_onesf", [128, 512], F32)
    C.m_cw = sb("m_cw", [128, NL, 8, 4], F32)
    C.m_cb = sb("m_cb", [128, NL, 8], F32)
    C.m_gb = sb("m_gb", [128, NL, 8], F32)
    C.m_gh = sb("m_gh", [128, NL, 8], F32)
    C.m_eps = C.epsv
    C.m_onesb = C.ones_bf
    C.m_kms = sb("m_kms", [128, 8], F32)
    C.m_gs = sb("m_gs", [128, 8], F32)
    C.m_m8 = sb("m_m8", [128, 8], F32)
    C.m_sel = sb("m_sel", [128, 8], F32)
    C.m_val = sb("m_val", [128, 8], F32)
    C.m_gs4 = [C.m_gs] + [sb("m_gs_%d" % i, [128, 8], F32) for i in range(3)]
    C.m_m84 = [C.m_m8] + [sb("m_m8_%d" % i, [128, 8], F32) for i in range(3)]
    C.m_sel4 = [C.m_sel] + [sb("m_sel_%d" % i, [128, 8], F32) for i in range(3)]
    C.m_val4 = [C.m_val] + [sb("m_val_%d" % i, [128, 8], F32) for i in range(3)]
    C.m_gl = sb("m_gl", [128, 16, 8], F32)
    C.m_lf = sb("m_lf", [128, 16, 4], F32)
    C.m_a = sb("m_a", [128, 16, 4], F32)
    for nm in ("n_sq", "n_tmpf", "n_stf", "n_stb", "n_vst", "n_pT", "n_tmp", "n_ost", "n_omb"):
        setattr(C, nm, 0)
    P = C.P
    P.op("dve", lambda e: e.memset(C.ones_bf[:], 1.0), writes=[("ones_bf",), ("c_onesb",)])
    P.op("dve", lambda e: e.memset(C.epsv[:], 1e-6), writes=[("epsv",), ("c_eps",)])
    P.op("dve", lambda e: e.memset(C.m_onesf[:], 1.0), writes=[("c_onesf",)])


def load_small_consts(C, cd, D_):
    P = C.P
    C.cd = cd
    for nm, q in (("posK", "pool"), ("E8", "pool"), ("G", "sp"), ("ident", "sp"), ("U", "sp"), ("rbias", "sp"), ("flagB", "sp")):
        dst = getattr(C, "m_" + nm)
        P.dma(q, lambda e, dst=dst, nm=nm: e.dma_start(out=dst[:], in_=cd[nm]), writes=[("c_" + nm,)])
    P.dma("sp", lambda e: e.dma_start(out=C.m_cw[:], in_=D_["conv_w"]), writes=[("c_cw",)])
    P.dma("sp", lambda e: e.dma_start(out=C.m_cb[:], in_=D_["conv_b"]), writes=[("c_cb",)])
    P.dma("sp", lambda e: e.dma_start(out=C.m_gb[:], in_=D_["gbias"]), writes=[("c_gb",)])
    P.dma("sp", lambda e: e.dma_start(out=C.m_gh[:], in_=D_["ghead"]), writes=[("c_gh",)])
    P.dma("sp", lambda e: e.dma_start(out=C.gvec[:], in_=D_["gains"]), writes=[("gvec", i) for i in range(4 * NL + 1)])


def load_tabs(C, kind):
    P = C.P
    if kind == "mlstm":
        P.op("dve", lambda e: e.memset(C.m_ukb[:, 0:3], 0.0), writes=[("ukb_pad",)])
        return
    P.barrier()
    if kind == "dil":
        P.dma("sp", lambda e: e.dma_start(out=C.m_tab[:, 0:8, :], in_=C.cd["dil_loc"]), writes=[("c_dil_loc",)])
        P.dma("sp", lambda e: e.dma_start(out=C.m_tab[:, 8:13, :], in_=C.cd["dil_rem"]), writes=[("c_dil_rem",)])
    else:
        P.dma("sp", lambda e: e.dma_start(out=C.m_tab[:, 0:4, :], in_=C.cd["cau"]), writes=[("c_cau",)])
        P.dma("sp", lambda e: e.dma_start(out=C.m_tab[:, 4:8, :], in_=C.cd["step"]), writes=[("c_step",)])


XB_ROWS = 3072
XF_ROWS = 1034
PAIRS = [[0, 1], [2, 3], [4, 5], [6, 7]]


def build_fused(nl=NL, phases="AXMC"):
    nc = bass.Bass("TRN2", target_bir_lowering=False)
    with _ES() as es:
        C = Ctx(nc, es)
        P = C.P
        ext = lambda n, sh: C.dram(n, sh, F32, "ExternalInput")
        x_in = ext("x_in", [16, 128, 1024])
        pT = ext("pT", [NL, 2, 128, 1024])
        W = dict(wup1=ext("wup1", [NL, 88, 128, 16, 128]), wdown1=ext("wdown1", [NL, 16, 128, 44, 128]),
                 win=ext("win", [NL, 48, 128, 16, 128]), win_g=ext("win_g", [NL, 128, 16, 8]),
                 wout=ext("wout", [NL, 16, 128, 16, 128]),
                 wup2=ext("wup2", [NL, 88, 128, 16, 128]), wdown2=ext("wdown2", [NL, 16, 128, 44, 128]),
                 wpg=ext("wpg", [NL, 16, 128, 16, 128]), wpp=ext("wpp", [NL, 16, 128, 2, 128]))
        D_ = dict(conv_w=ext("conv_w", [128, NL, 8, 4]), conv_b=ext("conv_b", [128, NL, 8]),
                  gbias=ext("gbias", [128, NL, 8]), ghead=ext("ghead", [128, NL, 8]),
                  gains=ext("gains", [128, 4 * NL + 1, 16]))
        shapes = dict(CONST_SHAPES, flagB=[128, 1])
        cd = {k: ext("c_" + k, sh) for k, sh in shapes.items()}
        out = C.dram("out", [16, 128, 1024], F32, "ExternalOutput")
        I32 = lambda n, sh: nc.dram_tensor(n, sh, F32, kind="Internal").ap()
        I16 = lambda n, sh: nc.dram_tensor(n, sh, BF16, kind="Internal").ap()
        exch = []
        exd = {}

        def mk(n, rows, mkfn):
            a, g = mkfn("x_" + n, [rows, 1024]), mkfn("g_" + n, [2 * rows, 1024])
            exch.append((a, g))
            exd[n] = (a, g)
            return a, g[0:rows, :]
        loc = dict(qa_bf=I16("z_qa_bf", [4, 128, 1024]), qa_f=I32("z_qa_f", [4, 128, 1024]),
                   qc_bf=I16("z_qc_bf", [4, 128, 1024]), om=I32("z_om", [8, 128, 1024]))
        mixT = I16("z_mixT", [16, 128, 1024])
        kk = mk("kk", 1024, I16)
        v0 = mk("v0", 1024, I16)
        v1 = mk("v1", 1024, I16)
        uk = mk("uk", 512, I32)
        uq = mk("uq", 512, I32)
        gk = mk("gk", 128, I32)
        ch = lambda t: t.rearrange("(j p) t -> j p t", p=128)
        vt = lambda t: t.rearrange("(t p a) b -> t p (a b)", t=4, p=128, a=2)
        Lv, Rv = {}, {}
        for i, dst in ((0, Lv), (1, Rv)):
            dst["ka_bf"] = ch(kk[i][0:512, :])
            dst["kc_bf"] = ch(kk[i][512:1024, :])
            dst["v_tm"] = [vt(v0[i]), vt(v1[i])]
            dst["uk"] = ch(uk[i])
            dst["uq"] = ch(uq[i])
            dst["gates"] = gk[i][0:8, :].rearrange("t (p c) -> t p c", c=8)
            dst["kms"] = gk[i][8:10, :].rearrange("a (p c) -> (a p) c", c=16)
        o_A = dict(loc, **Lv)
        v8 = [Lv["v_tm"][0][t] for t in range(4)] + [Lv["v_tm"][1][t] for t in range(4)]
        o_A["v_tm"] = v8
        I_M = dict(loc)
        for k in ("ka_bf", "kms", "kc_bf", "uq", "uk", "v_tm", "gates"):
            I_M[k + "_L"] = Lv[k]
            I_M[k + "_R"] = Rv[k]

        alloc_fused(C)
        load_small_consts(C, cd, D_)
        load_h(C, x_in)
        for l in range(nl):
            if "A" in phases:
                ffn(C, 4 * l + 0, W["wup1"][l], W["wdown1"][l])
                def group_cb(g, keys):
                    if g != "gk" or "X" not in phases:
                        return
                    P.barrier()
                    for (a_, g_) in exch:
                        P.cc(lambda e, a_=a_, g_=g_: e.collective_compute("AllGather", ALU.bypass, replica_groups=PAIRS,
                                                                          ins=[a_.opt()], outs=[g_.opt()]))
                inproj(C, 4 * l + 1, W["win"][l], W["win_g"][l], o_A, group_cb)
            P.barrier()
            C.m_layer = l
            if "M" in phases:
                attn_group(C, "moba", I_M, mixT)
                mlstm_group(C, I_M, mixT)
                attn_group(C, "dil", I_M, mixT)
            P.barrier()
            if "C" in phases:
                outproj(C, mixT, W["wout"][l])
                ffn(C, 4 * l + 2, W["wup2"][l], W["wdown2"][l])
                ple(C, 4 * l + 3, W["wpg"][l], W["wpp"][l], pT[l])
            P.bar = []
        final_norm(C, 4 * NL, out)
        C.P.build(es)
    return nc


def kernel(x, p, g_ffn1, w_up1, w_down1, g_mix, w_in, conv_w, conv_b, b_igate, b_fgate,
           g_head, w_out, g_ffn2, w_up2, w_down2, g_ple, w_ple_gate, w_ple_proj, g_final):
    f = lambda a: np.asarray(a, dtype=np.float32)
    x, p = f(x), f(p)
    nc = build_fused()
    st = lambda w, fn=tiles_fm: np.stack([fn(f(w[l])) for l in range(NL)])
    wi = f(w_in)
    gains = np.stack([vec_fm(g[l]) for l in range(NL) for g in (g_ffn1, g_mix, g_ffn2, g_ple)] + [vec_fm(g_final)], axis=1)
    common = dict(
        wup1=st(w_up1), wdown1=st(w_down1), win=np.stack([tiles_fm(wi[l][:, :6144]) for l in range(NL)]),
        win_g=np.stack([np.ascontiguousarray(wi[l][:, 6144:6152].reshape(16, 128, 8).transpose(1, 0, 2)) for l in range(NL)]),
        wout=st(w_out), wup2=st(w_up2), wdown2=st(w_down2), wpg=st(w_ple_gate), wpp=st(w_ple_proj),
        conv_w=np.ascontiguousarray(np.stack([f(conv_w[l]).reshape(4, 8, 128).transpose(2, 1, 0) for l in range(NL)], axis=1)),
        conv_b=np.ascontiguousarray(np.stack([vec_fm(conv_b[l]) for l in range(NL)], axis=1)),
        gbias=np.ascontiguousarray(np.stack([np.broadcast_to(np.concatenate([f(b_igate[l]), f(b_fgate[l])])[None, :], (128, 8))
                                             for l in range(NL)], axis=1)),
        ghead=np.ascontiguousarray(np.stack([vec_fm(g_head[l]) for l in range(NL)], axis=1)),
        gains=np.ascontiguousarray(gains))
    in_maps = []
    for c in range(8):
        b, s_ = c // 2, c % 2
        d = dict(common)
        d["x_in"] = np.ascontiguousarray(x[b, s_ * 1024:(s_ + 1) * 1024, :].T.reshape(16, 128, 1024))
        d["pT"] = np.ascontiguousarray(np.stack([p[l, b, s_ * 1024:(s_ + 1) * 1024, :].T.reshape(2, 128, 1024) for l in range(NL)]))
        for k, v in mixer_consts(s_ == 1).items():
            d["c_" + k] = v
        d["c_flagB"] = np.full((128, 1), float(s_), np.float32)
        in_maps.append(d)
    res = run_bass_kernel_spmd(nc, in_maps, core_ids=list(range(8))).results
    out = np.empty((4, 2048, 2048), np.float32)
    for c in range(8):
        b, s_ = c // 2, c % 2
        out[b, s_ * 1024:(s_ + 1) * 1024, :] = res[c]["out"].reshape(2048, 1024).T
    return out
```

```python
from contextlib import ExitStack

COMPUTE = ("pe", "act", "dve", "pool")
EPOCH = 30000
NDMA_SLOTS = 24


class Prog:
    def __init__(self, nc):
        self.nc = nc
        self.ops = []
        self.last_w = {}
        self.readers = {}
        self.cnt = {e: 0 for e in COMPUTE}
        self.epoch = {e: 0 for e in COMPUTE}
        self.dma_n = {"sp": 0, "pool": 0, "act": 0}
        self.dma_val = {}
        self.sem_ids = set()
        self._bank = 0

    def barrier(self):
        fin = {}
        for (_e, _f, _d, tok, _i) in self.ops:
            sid = tok[0]
            if sid[0] in COMPUTE:
                k = sid[0]
                if k not in fin or (fin[k][0][1], fin[k][1]) < (sid[1], tok[1]):
                    fin[k] = tok
            else:
                if sid not in fin or fin[sid][1] < tok[1]:
                    fin[sid] = tok
        self.bar = list(fin.values())

    def _deps(self, reads, writes):
        deps = list(getattr(self, "bar", ()))
        for r in reads:
            t = self.last_w.get(r)
            if t is not None:
                deps.append(t)
            if r[0] == "ps":
                deps.extend(self.readers.get(r, ()))
        for w in writes:
            t = self.last_w.get(w)
            if t is not None:
                deps.append(t)
            deps.extend(self.readers.get(w, ()))
        return deps

    def _commit(self, tok, reads, writes):
        for r in reads:
            self.readers.setdefault(r, []).append(tok)
        for w in writes:
            self.last_w[w] = tok
            self.readers[w] = []

    def op(self, eng, fn, reads=(), writes=()):
        deps = self._deps(reads, writes)
        if self.cnt[eng] >= EPOCH:
            self.epoch[eng] += 1
            self.cnt[eng] = 0
        self.cnt[eng] += 1
        sid = (eng, self.epoch[eng])
        self.sem_ids.add(sid)
        tok = (sid, self.cnt[eng], eng)
        self.ops.append((eng, fn, deps, tok, 1))
        self._commit(tok, reads, writes)
        return tok

    def dma(self, q, fn, reads=(), writes=()):
        deps = self._deps(reads, writes)
        slot = self.dma_n[q] % NDMA_SLOTS
        self.dma_n[q] += 1
        sid = ("dma", q, slot)
        self.sem_ids.add(sid)
        prev = self.dma_val.get(sid, 0)
        if prev:
            deps.append((sid, prev, "dma"))
        val = prev + 16
        self.dma_val[sid] = val
        tok = (sid, val, "dma")
        self.ops.append((q, fn, deps, tok, 16))
        self._commit(tok, reads, writes)
        return tok

    def cc(self, fn, reads=(), writes=()):
        deps = self._deps(reads, writes)
        n = self.__dict__.setdefault("_ncc", 0)
        self._ncc = n + 1
        sid = ("cc", n % 8)
        self.sem_ids.add(sid)
        prev = self.dma_val.get(sid, 0)
        if prev:
            deps.append((sid, prev, "dma"))
        val = prev + 1
        self.dma_val[sid] = val
        tok = (sid, val, "dma")
        self.ops.append(("pool", fn, deps, tok, 1))
        self._commit(tok, reads, writes)
        return tok

    def bank(self, lo=0, hi=8):
        if (lo, hi) == (0, 8):
            b = self._bank
            self._bank = (b + 1) % 8
            return b
        st = self.__dict__.setdefault("_rb", {})
        i = st.get((lo, hi), 0)
        st[(lo, hi)] = i + 1
        return lo + i % (hi - lo)

    def build(self, es: ExitStack):
        nc = self.nc
        sems = {}
        for sid in sorted(self.sem_ids, key=str):
            nm = "s_" + "_".join(str(x) for x in sid)
            sems[sid] = es.enter_context(nc.semaphore(nm))
        final = {}
        for (_e, _f, _d, tok, _i) in self.ops:
            final[tok[0]] = max(final.get(tok[0], 0), tok[1])
        ops = self.ops
        block = es.enter_context(nc.Block())

        def emit(eng_name, e):
            waited = {}
            for (en, fn, deps, tok, inc) in ops:
                if en != eng_name:
                    continue
                need = {}
                for (sid, val, deng) in deps:
                    if deng == "pe" and eng_name == "pe":
                        continue
                    if waited.get(sid, 0) >= val:
                        continue
                    if need.get(sid, 0) < val:
                        need[sid] = val
                for sid, val in need.items():
                    e.wait_ge(sems[sid], val)
                    waited[sid] = val
                ins = fn(e)
                ins.then_inc(sems[tok[0]], inc)
            if eng_name == "sp":
                for sid, val in final.items():
                    if waited.get(sid, 0) < val:
                        e.wait_ge(sems[sid], val)

        @block.tensor
        def _(e):
            emit("pe", e)

        @block.scalar
        def _(e):
            emit("act", e)

        @block.vector
        def _(e):
            emit("dve", e)

        @block.gpsimd
        def _(e):
            emit("pool", e)

        @block.sync
        def _(e):
            emit("sp", e)


import numpy as np
import concourse.bass as bass
import concourse.mybir as mybir
from concourse.bass_utils import run_bass_kernel_spmd

F32 = mybir.dt.float32
BF16 = mybir.dt.bfloat16
AF = mybir.ActivationFunctionType
ALU = mybir.AluOpType
AX = mybir.AxisListType

NTOK = 1024
D = 2048
KC_D = 16
DFF = 5632
NFF = 44
WSLOTS = 4
WSLOT_KC = 22


class Ctx:
    def __init__(self, nc, es):
        self.nc = nc
        self.es = es
        self.P = Prog(nc)
        self.n_w = 0
        self.ps = [es.enter_context(nc.psum_tensor("ps%d" % i, [128, 512], F32)) for i in range(8)]

    def sb(self, name, shape, dt):
        return self.es.enter_context(self.nc.sbuf_tensor(name, shape, dt))

    def dram(self, name, shape, dt, kind):
        return self.nc.dram_tensor(name, shape, dt, kind=kind).ap()


def alloc_token_local(C):
    C.hT = C.sb("hT", [128, KC_D, NTOK], F32)
    C.hnT = C.sb("hnT", [128, KC_D, NTOK], BF16)
    C.actT = C.sb("actT", [128, WSLOT_KC, NTOK], BF16)
    C.wbuf = [C.sb("wbuf%d" % i, [128, WSLOT_KC, 128], BF16) for i in range(WSLOTS)]
    C.sq = [C.sb("sq%d" % i, [128, NTOK], BF16) for i in range(2)]
    C.rstd = C.sb("rstd", [128, NTOK], F32)
    C.tmpf = [C.sb("tmpf%d" % i, [128, 512], F32) for i in range(3)]
    C.ones_bf = C.sb("ones_bf", [128, 128], BF16)
    C.gvec = C.sb("gvec", [128, 8, KC_D], F32)
    C.n_sq = 0
    C.n_tmpf = 0
    C.epsv = C.sb("epsv", [128, 1], F32)
    C.P.op("dve", lambda e: e.memset(C.ones_bf[:], 1.0), writes=[("ones_bf",)])
    C.P.op("dve", lambda e: e.memset(C.epsv[:], 1e-6), writes=[("epsv",)])


def load_gain(C, idx, g_dram):
    C.P.dma("sp", lambda e: e.dma_start(out=C.gvec[:, idx, :], in_=g_dram), writes=[("gvec", idx)])


def load_h(C, h_dram):
    for kc in range(KC_D):
        C.P.dma("sp", lambda e, kc=kc: e.dma_start(out=C.hT[:, kc, :], in_=h_dram[kc]),
                writes=[("hT", kc)])


def store_h(C, h_dram):
    for kc in range(KC_D):
        C.P.dma("sp", lambda e, kc=kc: e.dma_start(out=h_dram[kc], in_=C.hT[:, kc, :]),
                reads=[("hT", kc)])


def rmsnorm(C, gidx, out_fn=None):
    P = C.P
    banks = [P.bank(), P.bank()]
    for kc in range(KC_D):
        s = C.n_sq % 2
        C.n_sq += 1
        P.op("act", lambda e, kc=kc, s=s: e.activation(out=C.sq[s][:], in_=C.hT[:, kc, :], func=AF.Square),
             reads=[("hT", kc)], writes=[("sq", s)])
        for th in range(2):
            P.op("pe", lambda e, kc=kc, s=s, th=th: e.matmul(
                C.ps[banks[th]][:], C.ones_bf[:], C.sq[s][:, th * 512:(th + 1) * 512],
                start=(kc == 0), stop=(kc == KC_D - 1)),
                reads=[("sq", s), ("ones_bf",)], writes=[("ps", banks[th])])
    for th in range(2):
        sl = slice(th * 512, (th + 1) * 512)
        P.op("act", lambda e, th=th, sl=sl: e.activation(
            out=C.rstd[:, sl], in_=C.ps[banks[th]][:], func=AF.Ln, bias=C.epsv[:, 0:1], scale=1.0 / D),
            reads=[("ps", banks[th]), ("epsv",)], writes=[("rstd", th)])
        P.op("act", lambda e, sl=sl: e.activation(out=C.rstd[:, sl], in_=C.rstd[:, sl], func=AF.Exp, scale=-0.5),
            reads=[("rstd", th)], writes=[("rstd", th)])
    for kc in range(KC_D):
        if out_fn is None:
            P.op("dve", lambda e, kc=kc: e.scalar_tensor_tensor(
                out=C.hnT[:, kc, :], in0=C.hT[:, kc, :], scalar=C.gvec[:, gidx, kc:kc + 1],
                in1=C.rstd[:], op0=ALU.mult, op1=ALU.mult),
                reads=[("hT", kc), ("rstd", 0), ("rstd", 1), ("gvec", gidx)], writes=[("hnT", kc)])
        else:
            out_fn(kc)


def wload(C, src_ap, kc_n):
    slot = C.n_w % WSLOTS
    C.n_w += 1
    C.P.dma("pool", lambda e: e.dma_start(out=C.wbuf[slot][:, 0:kc_n, :], in_=src_ap),
            writes=[("w", slot)])
    return slot


def mm_fm(C, slot, kc_n, x, xkey, x_kc0, th, bank):
    for kc in range(kc_n):
        C.P.op("pe", lambda e, kc=kc: e.matmul(
            C.ps[bank][:], C.wbuf[slot][:, kc, :], x[:, x_kc0 + kc, th * 512:(th + 1) * 512],
            start=(kc == 0), stop=(kc == kc_n - 1)),
            reads=[("w", slot), (xkey, x_kc0 + kc)], writes=[("ps", bank)])


def ffn(C, gidx, wup, wdown):
    P = C.P
    rmsnorm(C, gidx)
    for half in range(2):
        for cl in range(WSLOT_KC):
            c = half * WSLOT_KC + cl
            sg_ = wload(C, wup[c], KC_D)
            su_ = wload(C, wup[NFF + c], KC_D)
            for th in range(2):
                bg, bu = P.bank(), P.bank()
                mm_fm(C, sg_, KC_D, C.hnT, "hnT", 0, th, bg)
                mm_fm(C, su_, KC_D, C.hnT, "hnT", 0, th, bu)
                t = C.n_tmpf % len(C.tmpf)
                C.n_tmpf += 1
                P.op("act", lambda e, t=t, bg=bg: e.activation(out=C.tmpf[t][:], in_=C.ps[bg][:], func=AF.Silu),
                     reads=[("ps", bg)], writes=[("tmpf", t)])
                P.op("dve", lambda e, t=t, bu=bu, cl=cl, th=th: e.tensor_tensor(
                    out=C.actT[:, cl, th * 512:(th + 1) * 512], in0=C.ps[bu][:], in1=C.tmpf[t][:], op=ALU.mult),
                    reads=[("ps", bu), ("tmpf", t)], writes=[("actT", cl, th)])
        for j in range(KC_D):
            sd = wload(C, wdown[j][:, half * WSLOT_KC:(half + 1) * WSLOT_KC, :], WSLOT_KC)
            for th in range(2):
                b = P.bank()
                for kc in range(WSLOT_KC):
                    P.op("pe", lambda e, kc=kc, th=th, b=b, sd=sd: e.matmul(
                        C.ps[b][:], C.wbuf[sd][:, kc, :], C.actT[:, kc, th * 512:(th + 1) * 512],
                        start=(kc == 0), stop=(kc == WSLOT_KC - 1)),
                        reads=[("w", sd), ("actT", kc, th)], writes=[("ps", b)])
                sl = slice(th * 512, (th + 1) * 512)
                P.op("dve", lambda e, j=j, b=b, sl=sl: e.scalar_tensor_tensor(
                    out=C.hT[:, j, sl], in0=C.ps[b][:], scalar=0.5, in1=C.hT[:, j, sl],
                    op0=ALU.mult, op1=ALU.add),
                    reads=[("ps", b), ("hT", j)], writes=[("hT", j)])


HD_SCALE = 128.0 ** -0.5


def alloc_inproj(C):
    C.stf = [C.sb("stf%d" % i, [128, 512], F32) for i in range(4)]
    C.stb = [C.sb("stb%d" % i, [128, 512], BF16) for i in range(4)]
    C.n_stf = 0
    C.n_stb = 0
    C.kms = C.sb("kms_sb", [128, 16], F32)
    C.wg = C.sb("wg", [128, KC_D, 8], BF16)
    C.gst = C.sb("gst", [128, 8, 8], F32)
    C.vst = [C.sb("vst%d" % i, [128, 128], BF16) for i in range(4)]
    C.n_vst = 0


def _stage_out(C, kind, b, dst_ap, func=None, scale=1.0, eng="act", grp=None):
    P = C.P
    if kind == "f":
        i = C.n_stf % len(C.stf)
        C.n_stf += 1
        buf, key = C.stf[i], ("stf", i)
    else:
        i = C.n_stb % 4
        C.n_stb += 1
        buf, key = C.stb[i], ("stb", i)
    if eng == "act":
        P.op("act", lambda e: e.activation(out=buf[:], in_=C.ps[b][:], func=func or AF.Copy, scale=scale),
             reads=[("ps", b)], writes=[key])
    else:
        P.op("dve", lambda e: e.tensor_copy(out=buf[:], in_=C.ps[b][:]), reads=[("ps", b)], writes=[key])
    P.dma("sp", lambda e: e.dma_start(out=dst_ap, in_=buf[:]), reads=[key], writes=_gk(C, grp))


def _gk(C, grp):
    if grp is None:
        return []
    lst = C.grp_keys.setdefault(grp, [])
    k = ("xw", grp, len(lst), C.grp_epoch)
    lst.append(k)
    return [k]


def inproj(C, gidx, win, win_g, o, group_cb=None):
    P = C.P
    C.grp_keys = {}
    C.grp_epoch = getattr(C, "grp_epoch", 0) + 1

    def done(g):
        if group_cb is not None:
            group_cb(g, C.grp_keys.get(g, []))
    rmsnorm(C, gidx)
    P.dma("pool", lambda e: e.dma_start(out=C.wg[:], in_=win_g), writes=[("wg",)])

    def fm_chunks(lst):
        for (wj, kind, j) in lst:
            slot = wload(C, win[wj], KC_D)
            for th in range(2):
                b = P.bank()
                sl = slice(th * 512, (th + 1) * 512)
                mm_fm(C, slot, KC_D, C.hnT, "hnT", 0, th, b)
                if kind == "qa":
                    _stage_out(C, "b", b, o["qa_bf"][j][:, sl], scale=HD_SCALE)
                    _stage_out(C, "f", b, o["qa_f"][j][:, sl], eng="dve")
                elif kind == "ka":
                    _stage_out(C, "b", b, o["ka_bf"][j][:, sl], grp="kk")
                    P.op("dve", lambda e, j=j, th=th, b=b: e.tensor_reduce(
                        out=C.kms[:, j * 4 + th * 2:j * 4 + th * 2 + 2],
                        in_=C.ps[b][:].rearrange("p (a c) -> p a c", a=2), axis=AX.X, op=ALU.add),
                        reads=[("ps", b)], writes=[("kms", j, th)])
                elif kind == "qc":
                    _stage_out(C, "b", b, o["qc_bf"][j][:, sl], scale=HD_SCALE)
                elif kind == "kc":
                    _stage_out(C, "b", b, o["kc_bf"][j][:, sl], grp="kk")
                elif kind == "uq":
                    _stage_out(C, "f", b, o["uq"][j][:, sl], grp="uq")
                elif kind == "uk":
                    _stage_out(C, "f", b, o["uk"][j][:, sl], grp="uk")
                elif kind == "om":
                    _stage_out(C, "f", b, o["om"][j][:, sl], func=AF.Sigmoid)
    fm_chunks([(4 + j, "ka", j) for j in range(4)] + [(16 + j, "kc", j) for j in range(4)])
    done("kk")
    vt = [(8 + j, j) for j in range(4)] + [(20 + j, 4 + j) for j in range(4)] + [(32 + j, 8 + j) for j in range(8)]
    for (wj, cg) in vt:
        slot = wload(C, win[wj], KC_D)
        for tt in range(8):
            b = P.bank()
            for kc in range(KC_D):
                P.op("pe", lambda e, kc=kc, tt=tt, b=b, slot=slot: e.matmul(
                    C.ps[b][:, 0:128], C.hnT[:, kc, tt * 128:(tt + 1) * 128], C.wbuf[slot][:, kc, :],
                    start=(kc == 0), stop=(kc == KC_D - 1)),
                    reads=[("w", slot), ("hnT", kc)], writes=[("ps", b)])
            i = C.n_vst % 4
            C.n_vst += 1
            P.op("act" if tt % 2 else "dve",
                 (lambda e, i=i, b=b: e.activation(out=C.vst[i][:], in_=C.ps[b][:, 0:128], func=AF.Copy)) if tt % 2 else
                 (lambda e, i=i, b=b: e.tensor_copy(out=C.vst[i][:], in_=C.ps[b][:, 0:128])),
                 reads=[("ps", b)], writes=[("vst", i)])
            P.dma("sp", lambda e, i=i, tt=tt, cg=cg: e.dma_start(
                out=o["v_tm"][tt][:, cg * 128:(cg + 1) * 128], in_=C.vst[i][:]), reads=[("vst", i)],
                writes=_gk(C, "v0" if tt < 4 else "v1"))
    done("v0")
    done("v1")
    fm_chunks([(28 + j, "uk", j) for j in range(4)])
    done("uk")
    fm_chunks([(24 + j, "uq", j) for j in range(4)])
    done("uq")
    for tt in range(8):
        b = P.bank()
        for kc in range(KC_D):
            P.op("pe", lambda e, kc=kc, tt=tt, b=b: e.matmul(
                C.ps[b][:, 0:8], C.hnT[:, kc, tt * 128:(tt + 1) * 128], C.wg[:, kc, :],
                start=(kc == 0), stop=(kc == KC_D - 1)),
                reads=[("wg",), ("hnT", kc)], writes=[("ps", b)])
        P.op("dve", lambda e, tt=tt, b=b: e.tensor_copy(out=C.gst[:, tt, :], in_=C.ps[b][:, 0:8]),
             reads=[("ps", b)], writes=[("gst", tt)])
    P.dma("sp", lambda e: e.dma_start(out=o["gates"].rearrange("t p c -> p t c"), in_=C.gst[:]),
          reads=[("gst", tt) for tt in range(8)], writes=_gk(C, "gk"))
    P.dma("sp", lambda e: e.dma_start(out=o["kms"], in_=C.kms[:]),
          reads=[("kms", j, th) for j in range(4) for th in range(2)], writes=_gk(C, "gk"))
    done("gk")
    fm_chunks([(j, "qa", j) for j in range(4)] + [(12 + j, "qc", j) for j in range(4)] + [(40 + j, "om", j) for j in range(8)])


def outproj(C, mixT_dram, wout):
    P = C.P
    for kc in range(KC_D):
        P.dma("sp", lambda e, kc=kc: e.dma_start(out=C.hnT[:, kc, :], in_=mixT_dram[kc]), writes=[("hnT", kc)])
    for j in range(KC_D):
        slot = wload(C, wout[j], KC_D)
        for th in range(2):
            b = P.bank()
            sl = slice(th * 512, (th + 1) * 512)
            mm_fm(C, slot, KC_D, C.hnT, "hnT", 0, th, b)
            P.op("dve", lambda e, j=j, b=b, sl=sl: e.tensor_tensor(
                out=C.hT[:, j, sl], in0=C.ps[b][:], in1=C.hT[:, j, sl], op=ALU.add),
                reads=[("ps", b), ("hT", j)], writes=[("hT", j)])


def ple(C, gidx, wpg, wpp, pT_dram):
    P = C.P
    rmsnorm(C, gidx)
    for kc in range(2):
        P.dma("pool", lambda e, kc=kc: e.dma_start(out=C.actT[:, kc, :], in_=pT_dram[kc]), writes=[("actT", kc, 0), ("actT", kc, 1)])
    for j in range(KC_D):
        sg = wload(C, wpg[j], KC_D)
        sp_ = wload(C, wpp[j], 2)
        for th in range(2):
            bg, bp = P.bank(), P.bank()
            sl = slice(th * 512, (th + 1) * 512)
            mm_fm(C, sg, KC_D, C.hnT, "hnT", 0, th, bg)
            for kc in range(2):
                P.op("pe", lambda e, kc=kc, th=th, bp=bp, sp_=sp_: e.matmul(
                    C.ps[bp][:], C.wbuf[sp_][:, kc, :], C.actT[:, kc, th * 512:(th + 1) * 512],
                    start=(kc == 0), stop=(kc == 1)),
                    reads=[("w", sp_), ("actT", kc, th)], writes=[("ps", bp)])
            t = C.n_tmpf % len(C.tmpf)
            C.n_tmpf += 1
            P.op("act", lambda e, t=t, bg=bg: e.activation(out=C.tmpf[t][:], in_=C.ps[bg][:], func=AF.Sigmoid),
                 reads=[("ps", bg)], writes=[("tmpf", t)])
            P.op("dve", lambda e, t=t, bp=bp: e.tensor_tensor(
                out=C.tmpf[t][:], in0=C.ps[bp][:], in1=C.tmpf[t][:], op=ALU.mult),
                reads=[("ps", bp), ("tmpf", t)], writes=[("tmpf", t)])
            P.op("dve", lambda e, t=t, j=j, sl=sl: e.tensor_tensor(
                out=C.hT[:, j, sl], in0=C.hT[:, j, sl], in1=C.tmpf[t][:], op=ALU.add),
                reads=[("hT", j), ("tmpf", t)], writes=[("hT", j)])


def final_norm(C, gidx, out_dram):
    P = C.P

    def out_fn(kc):
        for th in range(2):
            sl = slice(th * 512, (th + 1) * 512)
            i = C.n_stf % len(C.stf)
            C.n_stf += 1
            P.op("dve", lambda e, kc=kc, sl=sl, i=i: e.scalar_tensor_tensor(
                out=C.stf[i][:], in0=C.hT[:, kc, sl], scalar=C.gvec[:, gidx, kc:kc + 1],
                in1=C.rstd[:, sl], op0=ALU.mult, op1=ALU.mult),
                reads=[("hT", kc), ("rstd", 0), ("rstd", 1), ("gvec", gidx)], writes=[("stf", i)])
            P.dma("sp", lambda e, kc=kc, sl=sl, i=i: e.dma_start(out=out_dram[kc][:, sl], in_=C.stf[i][:]),
                  reads=[("stf", i)])
    rmsnorm(C, gidx, out_fn=out_fn)


NEG = -30000.0
SLOPES_MOBA = [2.0 ** -1, 2.0 ** -3, 2.0 ** -5, 2.0 ** -7]
SLOPES_DIL = [2.0 ** -2, 2.0 ** -4, 2.0 ** -6, 2.0 ** -8]


def mixer_consts(is_b):
    kk = np.arange(128)[:, None]
    qq = np.arange(512)[None, :]

    def lnmult(dist):
        m = ((dist >= 0) & (dist <= 128)).astype(np.float64)
        m += ((dist >= 0) & (dist % 4 == 0) & (dist <= 512))
        m += ((dist >= 0) & (dist % 16 == 0) & (dist <= 2048))
        return np.where(m > 0, np.log(np.maximum(m, 1)), NEG).astype(np.float32)

    dil_loc = np.stack([lnmult(128 * d + qq - kk) for d in range(-3, 5)], axis=1)
    dil_rem = np.stack([lnmult(128 * d + qq - kk) for d in range(1, 6)], axis=1)
    if not is_b:
        dil_rem = np.full_like(dil_rem, NEG)
    cau = np.stack([np.where(128 * d + qq - kk >= 0, 0.0, NEG) for d in range(-3, 1)], axis=1).astype(np.float32)
    step = (cau == 0).astype(np.float32)
    pk = np.arange(2048)
    posK = np.stack([128.0 * (pk // 128), pk % 128, np.ones(2048), np.ones(2048)]).astype(np.float32)
    pq = 1024 + np.arange(1024)
    posQ = np.zeros((4, 8, 1024), np.float32)
    for i, s in enumerate(SLOPES_MOBA + SLOPES_DIL):
        posQ[0, i] = s
        posQ[1, i] = s
        posQ[2, i] = -s * 128.0 * (pq // 128)
        posQ[3, i] = -s * (pq % 128)
    E8 = np.zeros((8, 8 * 128), np.float32)
    for n in range(8):
        E8[n, n * 128:(n + 1) * 128] = 1.0
    G = np.zeros((1024, 8), np.float32)
    t = np.arange(1024)
    own = 4 + t // 256
    for n in range(8):
        G[:, n] = np.where(n == own, 1e30, np.where(n < own, 0.0, -1e30))
    if not is_b:
        G[:, 0:4] = -1e30
    G = np.ascontiguousarray(G.reshape(8, 128, 8).transpose(1, 0, 2))
    ident = np.eye(128, dtype=np.float32)
    U = np.triu(np.ones((128, 128), np.float32))
    rbias = np.full((128, 1), 0.0 if is_b else NEG, np.float32)
    return dict(dil_loc=dil_loc, dil_rem=dil_rem, cau=cau, step=step, posK=posK, posQ=posQ, E8=E8, G=G,
                ident=ident, U=U, rbias=rbias)


CONST_SHAPES = dict(dil_loc=[128, 8, 512], dil_rem=[128, 5, 512], cau=[128, 4, 512], step=[128, 4, 512],
                    posK=[4, 2048], posQ=[4, 8, 1024], E8=[8, 1024], G=[128, 8, 8], ident=[128, 128],
                    U=[128, 128], rbias=[128, 1])


def alloc_mixer(C):
    sb = C.sb
    C.m_dil_loc = sb("m_dil_loc", [128, 8, 512], F32)
    C.m_dil_rem = sb("m_dil_rem", [128, 5, 512], F32)
    C.m_cau = sb("m_cau", [128, 4, 512], F32)
    C.m_step = sb("m_step", [128, 4, 512], F32)
    C.m_posK = sb("m_posK", [4, 2048], BF16)
    C.m_posQ = sb("m_posQ", [4, 1024], BF16)
    C.m_E8 = sb("m_E8", [8, 1024], BF16)
    C.m_G = sb("m_G", [128, 8, 8], F32)
    C.m_ident = sb("m_ident", [128, 128], F32)
    C.m_U = sb("m_U", [128, 128], F32)
    C.m_rbias = sb("m_rbias", [128, 1], F32)
    C.m_onesf = sb("m_onesf", [128, 512], F32)
    C.m_cw = sb("m_cw", [128, 8, 4], F32)
    C.m_cb = sb("m_cb", [128, 8], F32)
    C.m_gb = sb("m_gb", [128, 8], F32)
    C.m_gh = sb("m_gh", [128, 8], F32)
    C.m_eps = sb("m_eps", [128, 1], F32)
    C.m_qT = sb("m_qT", [128, 1024], BF16)
    C.m_kT = sb("m_kT", [128, 2048], BF16)
    C.m_V = sb("m_V", [128, 16, 256], BF16)
    C.m_qf = sb("m_qf", [128, 1024], F32)
    C.m_kms = sb("m_kms", [128, 8], F32)
    C.m_gs = sb("m_gs", [128, 8], F32)
    C.m_m8 = sb("m_m8", [128, 8], F32)
    C.m_sel = sb("m_sel", [128, 8], F32)
    C.m_val = sb("m_val", [128, 8], F32)
    C.m_gs4 = [C.m_gs] + [sb("m_gs_%d" % i, [128, 8], F32) for i in range(3)]
    C.m_m84 = [C.m_m8] + [sb("m_m8_%d" % i, [128, 8], F32) for i in range(3)]
    C.m_sel4 = [C.m_sel] + [sb("m_sel_%d" % i, [128, 8], F32) for i in range(3)]
    C.m_val4 = [C.m_val] + [sb("m_val_%d" % i, [128, 8], F32) for i in range(3)]
    C.m_selbT = sb("m_selbT", [8, 1024], BF16)
    C.m_pT = [sb("m_pT%d" % i, [128, 512], BF16) for i in range(3)]
    C.m_tmp = [sb("m_tmp%d" % i, [128, 512], F32) for i in range(3)]
    C.m_rl = sb("m_rl", [128, 512], F32)
    C.m_ost = [sb("m_ost%d" % i, [128, 512], BF16) for i in range(2)]
    C.m_gl = sb("m_gl", [128, 16, 8], F32)
    C.m_lf = sb("m_lf", [128, 16, 4], F32)
    C.m_a = sb("m_a", [128, 16, 4], F32)
    C.m_Rrem = sb("m_Rrem", [128, 1], F32)
    C.m_lfrep = sb("m_lfrep", [128, 16, 128], F32)
    C.m_Fbc = sb("m_Fbc", [128, 1024], F32)
    C.m_uqb = sb("m_uqb", [128, 1027], F32)
    C.m_ukb = sb("m_ukb", [128, 2051], F32)
    C.m_acc = sb("m_acc", [128, 2048], F32)
    C.m_hm = [sb("m_hm%d" % i, [128, 512], F32) for i in range(2)]
    C.m_sqb = sb("m_sqb", [128, 512], BF16)
    C.m_omb = [sb("m_omb%d" % i, [128, 512], F32) for i in range(2)]
    C.m_onesb = sb("m_onesb", [128, 128], BF16)
    C.n_pT = 0
    C.n_tmp = 0
    C.n_ost = 0
    C.n_omb = 0
    C.m_tmpx = C.m_tmp
    C.m_pTx = C.m_pT + [sb("m_pT3", [128, 512], BF16)]


def load_mixer_consts(C, cd, conv_w, conv_b, gbias, ghead):
    P = C.P
    C.cd = cd
    for nm, q in (("dil_loc", "sp"), ("dil_rem", "sp"), ("cau", "sp"), ("step", "sp"), ("posK", "pool"),
                  ("E8", "pool"), ("G", "sp"), ("ident", "sp"), ("U", "sp"), ("rbias", "sp")):
        dst = getattr(C, "m_" + nm)
        P.dma(q, lambda e, dst=dst, nm=nm: e.dma_start(out=dst[:], in_=cd[nm]), writes=[("c_" + nm,)])
    P.dma("sp", lambda e: e.dma_start(out=C.m_cw[:], in_=conv_w), writes=[("c_cw",)])
    P.dma("sp", lambda e: e.dma_start(out=C.m_cb[:], in_=conv_b), writes=[("c_cb",)])
    P.dma("sp", lambda e: e.dma_start(out=C.m_gb[:], in_=gbias), writes=[("c_gb",)])
    P.dma("sp", lambda e: e.dma_start(out=C.m_gh[:], in_=ghead), writes=[("c_gh",)])
    P.op("dve", lambda e: e.memset(C.m_onesf[:], 1.0), writes=[("c_onesf",)])
    P.op("dve", lambda e: e.memset(C.m_onesb[:], 1.0), writes=[("c_onesb",)])
    P.op("dve", lambda e: e.memset(C.m_eps[:], 1e-6), writes=[("c_eps",)])
    P.op("dve", lambda e: e.memset(C.m_ukb[:, 0:3], 0.0), writes=[("ukb_pad",)])


def load_V(C, parts, tile0, c0, w, key):
    if not isinstance(parts, (list, tuple)):
        parts = [parts]
    t0 = tile0
    for ap in parts:
        n = ap.shape[0]
        C.P.dma("sp", lambda e, ap=ap, t0=t0, n=n: e.dma_start(
            out=C.m_V[:, t0:t0 + n, 0:w], in_=ap.rearrange("t p c -> p t c")[:, :, c0:c0 + w]), writes=[key])
        t0 += n


def _ktiles(qh):
    lst = [(kt, True, qh * 4 + 8 - kt) for kt in range(8)]
    lst += [(8 + lt, False, qh * 4 - lt) for lt in range(4 * (qh + 1))]
    return lst


def attn_group(C, kind, I, mixT):
    P = C.P
    moba = kind == "moba"
    if getattr(C, "fused", False):
        load_tabs(C, kind)
    qb, kL, kR = (I["qa_bf"], I["ka_bf_L"], I["ka_bf_R"]) if moba else (I["qc_bf"], I["kc_bf_L"], I["kc_bf_R"])
    vcol0 = 0 if moba else 512
    def head(h):
        hh = h if moba else 4 + h
        P.dma("pool", lambda e: e.dma_start(out=C.m_posQ[:], in_=C.cd["posQ"][:, hh, :]), writes=[("c_posQ",)])
        P.dma("sp", lambda e: e.dma_start(out=C.m_qT[:], in_=qb[h]), writes=[("m_qT",)])
        P.dma("sp", lambda e: e.dma_start(out=C.m_kT[:, 0:1024], in_=kR[h]), writes=[("m_kT", 0)])
        P.dma("sp", lambda e: e.dma_start(out=C.m_kT[:, 1024:2048], in_=kL[h]), writes=[("m_kT", 1)])
        c0 = vcol0 + h * 128
        load_V(C, I["v_tm_R"], 0, c0, 128, ("m_V", 0))
        load_V(C, I["v_tm_L"], 8, c0, 128, ("m_V", 1))
        if moba:
            P.dma("sp", lambda e: e.dma_start(out=C.m_qf[:], in_=I["qa_f"][h]), writes=[("m_qf",)])
            P.dma("sp", lambda e: e.dma_start(out=C.m_kms[:, 0:4], in_=I["kms_R"][:, h * 4:h * 4 + 4]), writes=[("m_kms", 0)])
            P.dma("sp", lambda e: e.dma_start(out=C.m_kms[:, 4:8], in_=I["kms_L"][:, h * 4:h * 4 + 4]), writes=[("m_kms", 1)])
            for qt in range(8):
                b = P.bank()
                g_ = qt % 4
                gs, m8, sel, val = C.m_gs4[g_], C.m_m84[g_], C.m_sel4[g_], C.m_val4[g_]
                kg = lambda n, g_=g_: (n, g_)
                P.op("pe", lambda e, qt=qt, b=b: e.matmul(C.ps[b][:, 0:8], C.m_qf[:, qt * 128:(qt + 1) * 128], C.m_kms[:],
                                                          start=True, stop=True),
                     reads=[("m_qf",), ("m_kms", 0), ("m_kms", 1)], writes=[("ps", b)])
                P.op("dve", lambda e, qt=qt, b=b, gs=gs: e.tensor_tensor(out=gs[:], in0=C.ps[b][:, 0:8], in1=C.m_G[:, qt, :], op=ALU.add),
                     reads=[("ps", b), ("c_G",)], writes=[kg("m_gs")])
                P.op("dve", lambda e, gs=gs, m8=m8: e.max(out=m8[:], in_=gs[:]), reads=[kg("m_gs")], writes=[kg("m_m8")])
                P.op("dve", lambda e, gs=gs, m8=m8, sel=sel: e.tensor_scalar(out=sel[:], in0=gs[:], scalar1=m8[:, 3:4], scalar2=None, op0=ALU.is_ge),
                     reads=[kg("m_gs"), kg("m_m8")], writes=[kg("m_sel")])
                P.op("dve", lambda e, gs=gs, val=val: e.tensor_scalar(out=val[:], in0=gs[:], scalar1=-1e29, scalar2=None, op0=ALU.is_gt),
                     reads=[kg("m_gs")], writes=[kg("m_val")])
                P.op("dve", lambda e, sel=sel, val=val: e.tensor_tensor(out=sel[:], in0=sel[:], in1=val[:], op=ALU.mult),
                     reads=[kg("m_sel"), kg("m_val")], writes=[kg("m_sel")])
                P.op("dve", lambda e, sel=sel: e.tensor_scalar(out=sel[:], in0=sel[:], scalar1=1.0, scalar2=-NEG,
                                                               op0=ALU.subtract, op1=ALU.mult),
                     reads=[kg("m_sel")], writes=[kg("m_sel")])
                b2 = P.bank()
                P.op("pe", lambda e, b2=b2, sel=sel: e.matmul(C.ps[b2][0:8, 0:128], sel[:], C.m_ident[:], start=True, stop=True),
                     reads=[kg("m_sel"), ("c_ident",)], writes=[("ps", b2)])
                P.op("act", lambda e, qt=qt, b2=b2: e.activation(out=C.m_selbT[:, qt * 128:(qt + 1) * 128], in_=C.ps[b2][0:8, 0:128], func=AF.Copy),
                     reads=[("ps", b2)], writes=[("m_selbT", qt)])

        def qhalf(qh):
            qsl = slice(qh * 512, (qh + 1) * 512)
            bo = P.bank(0, 4)
            bl = P.bank(0, 4)
            tiles = _ktiles(qh)
            def stage_S(ti):
                kt, rem, dl = tiles[ti]
                bs = P.bank(4, 8)
                ksl = slice(kt * 128, (kt + 1) * 128)
                P.op("pe", lambda e, bs=bs, ksl=ksl: e.matmul(C.ps[bs][:], C.m_kT[:, ksl], C.m_qT[:, qsl], start=True, stop=False),
                     reads=[("m_kT", 0 if rem else 1), ("m_qT",)], writes=[("ps", bs)])
                P.op("pe", lambda e, bs=bs, ksl=ksl: e.matmul(C.ps[bs][:], C.m_posK[:, ksl], C.m_posQ[:, qsl], start=False, stop=(not moba)),
                     reads=[("c_posK",), ("c_posQ",)], writes=[("ps", bs)])
                if moba:
                    n = kt // 2
                    P.op("pe", lambda e, bs=bs, n=n: e.matmul(C.ps[bs][:], C.m_E8[:, n * 128:(n + 1) * 128], C.m_selbT[:, qsl], start=False, stop=True),
                         reads=[("c_E8",)] + [("m_selbT", qh * 4 + i) for i in range(4)], writes=[("ps", bs)])
                tab = None
                if not moba:
                    tab = C.m_dil_rem[:, min(dl, 5) - 1, :] if rem else C.m_dil_loc[:, dl + 3, :]
                elif (not rem) and dl <= 0:
                    tab = C.m_cau[:, dl + 3, :]
                pi = C.n_pT % len(C.m_pT)
                C.n_pT += 1
                if tab is not None:
                    t = C.n_tmp % 3
                    C.n_tmp += 1
                    P.op("dve", lambda e, bs=bs, t=t, tab=tab: e.tensor_tensor(out=C.m_tmp[t][:], in0=C.ps[bs][:], in1=tab, op=ALU.add),
                         reads=[("ps", bs), ("c_dil_loc",), ("c_dil_rem",), ("c_cau",)], writes=[("m_tmp", t)])
                    P.op("act", lambda e, t=t, pi=pi: e.activation(out=C.m_pT[pi][:], in_=C.m_tmp[t][:], func=AF.Exp),
                         reads=[("m_tmp", t)], writes=[("m_pT", pi)])
                else:
                    P.op("act", lambda e, bs=bs, pi=pi: e.activation(out=C.m_pT[pi][:], in_=C.ps[bs][:], func=AF.Exp),
                         reads=[("ps", bs)], writes=[("m_pT", pi)])
                return pi

            def stage_V(ti, pi):
                kt, rem, dl = tiles[ti]
                first, last = ti == 0, ti == len(tiles) - 1
                P.op("pe", lambda e, kt=kt, pi=pi, first=first, last=last: e.matmul(
                    C.ps[bo][:], C.m_V[:, kt, 0:128], C.m_pT[pi][:], start=first, stop=last),
                    reads=[("m_V", 0 if rem else 1), ("m_pT", pi)], writes=[("ps", bo)])
                P.op("pe", lambda e, pi=pi, first=first, last=last: e.matmul(
                    C.ps[bl][:], C.m_onesb[:], C.m_pT[pi][:], start=first, stop=last),
                    reads=[("c_onesb",), ("m_pT", pi)], writes=[("ps", bl)])
            LA = 3 if len(C.m_pT) >= 4 else 2
            pis = {}
            for step in range(len(tiles) + LA):
                if step < len(tiles):
                    pis[step] = stage_S(step)
                if step - LA >= 0:
                    stage_V(step - LA, pis[step - LA])
            P.op("act", lambda e: e.activation(out=C.m_rl[:], in_=C.ps[bl][:], func=AF.Ln), reads=[("ps", bl)], writes=[("m_rl",)])
            P.op("act", lambda e: e.activation(out=C.m_rl[:], in_=C.m_rl[:], func=AF.Exp, scale=-1.0), reads=[("m_rl",)], writes=[("m_rl",)])
            oi = C.n_ost % 2
            C.n_ost += 1
            P.op("dve", lambda e, oi=oi: e.tensor_tensor(out=C.m_ost[oi][:], in0=C.ps[bo][:], in1=C.m_rl[:], op=ALU.mult),
                 reads=[("ps", bo), ("m_rl",)], writes=[("m_ost", oi)])
            ch = h if moba else 12 + h
            P.dma("sp", lambda e, oi=oi, ch=ch: e.dma_start(out=mixT[ch][:, qsl], in_=C.m_ost[oi][:]), reads=[("m_ost", oi)])
        for qh in range(2):
            qhalf(qh)
    for h in range(4):
        head(h)


DK_SCALE = 128.0 ** -0.5


def mlstm_group(C, I, mixT):
    P = C.P
    if getattr(C, "fused", False):
        load_tabs(C, "mlstm")
        l_ = C.m_layer
        cw_, cb_, gb_, gh_ = C.m_cw[:, l_], C.m_cb[:, l_], C.m_gb[:, l_], C.m_gh[:, l_]
    else:
        cw_, cb_, gb_, gh_ = C.m_cw, C.m_cb, C.m_gb, C.m_gh
    P.dma("sp", lambda e: e.dma_start(out=C.m_gl[:, 0:8, :], in_=I["gates_R"].rearrange("t p c -> p t c")), writes=[("m_gl", 0)])
    P.dma("sp", lambda e: e.dma_start(out=C.m_gl[:, 8:16, :], in_=I["gates_L"].rearrange("t p c -> p t c")), writes=[("m_gl", 1)])
    for t in range(16):
        P.op("dve", lambda e, t=t: e.tensor_tensor(out=C.m_gl[:, t, :], in0=C.m_gl[:, t, :], in1=gb_[:], op=ALU.add),
             reads=[("m_gl", 0), ("m_gl", 1), ("c_gb",)], writes=[("m_gl", 0), ("m_gl", 1)])
    P.op("act", lambda e: e.activation(out=C.m_lf[:], in_=C.m_gl[:, :, 4:8], func=AF.Exp, scale=-1.0),
         reads=[("m_gl", 0), ("m_gl", 1)], writes=[("m_lf",)])
    P.op("act", lambda e: e.activation(out=C.m_lf[:], in_=C.m_lf[:], func=AF.Ln, bias=C.m_onesf[:, 0:1], scale=1.0),
         reads=[("m_lf",), ("c_onesf",)], writes=[("m_lf",)])
    P.op("dve", lambda e: e.tensor_scalar(out=C.m_lf[:], in0=C.m_lf[:], scalar1=-1.0, scalar2=None, op0=ALU.mult),
         reads=[("m_lf",)], writes=[("m_lf",)])
    P.op("dve", lambda e: e.tensor_scalar(out=C.m_gl[:, 0:8, 0:4], in0=C.m_gl[:, 0:8, 0:4], scalar1=C.m_rbias[:, 0:1],
                                          scalar2=None, op0=ALU.add),
         reads=[("m_gl", 0), ("c_rbias",)], writes=[("m_gl", 0)])
    for i in range(16):
        b = P.bank()
        for r in range(i + 1):
            lhs = C.m_U if r == i else C.m_onesf
            P.op("pe", lambda e, r=r, b=b, lhs=lhs, i=i: e.matmul(C.ps[b][:, 0:4], lhs[:, 0:128], C.m_lf[:, r, :],
                                                             start=(r == 0), stop=(r == i)),
                 reads=[("m_lf",), ("c_U",), ("c_onesf",)], writes=[("ps", b)])
        P.op("dve", lambda e, i=i, b=b: e.tensor_tensor(out=C.m_a[:, i, :], in0=C.m_gl[:, i, 0:4], in1=C.ps[b][:, 0:4], op=ALU.subtract),
             reads=[("ps", b), ("m_gl", 0), ("m_gl", 1)], writes=[("m_a",)])
    def head(h):
        for t in range(8, 16):
            P.op("dve", lambda e, t=t: e.tensor_scalar(out=C.m_lfrep[:, t, :], in0=C.m_onesf[:, 0:128], scalar1=C.m_lf[:, t, h:h + 1],
                                                       scalar2=None, op0=ALU.mult),
                 reads=[("m_lf",), ("c_onesf",)], writes=[("m_lfrep",)])
        bR = P.bank()
        for t in range(8):
            P.op("pe", lambda e, t=t, bR=bR: e.matmul(C.ps[bR][:, 0:1], C.m_onesf[:, 0:128], C.m_lf[:, t, h:h + 1],
                                                      start=(t == 0), stop=(t == 7)),
                 reads=[("m_lf",), ("c_onesf",)], writes=[("ps", bR)])
        P.op("dve", lambda e, bR=bR: e.tensor_copy(out=C.m_Rrem[:], in_=C.ps[bR][:, 0:1]), reads=[("ps", bR)], writes=[("m_Rrem",)])
        for qh in range(2):
            b = P.bank()
            tl = [x for x in _ktiles(qh) if not x[1]]
            for ti, (kt, rem, dl) in enumerate(tl):
                rhs = C.m_onesf[:, :] if dl >= 1 else C.m_step[:, dl + 3, :]
                P.op("pe", lambda e, kt=kt, rhs=rhs, ti=ti, b=b, n=len(tl): e.matmul(
                    C.ps[b][:], C.m_lfrep[:, kt, :], rhs, start=(ti == 0), stop=(ti == n - 1)),
                    reads=[("m_lfrep",), ("c_onesf",), ("c_step",)], writes=[("ps", b)])
            P.op("dve", lambda e, b=b, qh=qh: e.tensor_scalar(out=C.m_Fbc[:, qh * 512:(qh + 1) * 512], in0=C.ps[b][:],
                                                              scalar1=C.m_Rrem[:, 0:1], scalar2=None, op0=ALU.add),
                 reads=[("ps", b), ("m_Rrem",)], writes=[("m_Fbc", qh)])
        P.dma("sp", lambda e: e.dma_start(out=C.m_uqb[:, 0:3], in_=I["uq_R"][h][:, 1021:1024]), writes=[("m_uqb", 0)])
        P.dma("sp", lambda e: e.dma_start(out=C.m_uqb[:, 3:1027], in_=I["uq_L"][h]), writes=[("m_uqb", 1)])
        P.dma("sp", lambda e: e.dma_start(out=C.m_ukb[:, 3:1027], in_=I["uk_R"][h]), writes=[("m_ukb", 0)])
        P.dma("sp", lambda e: e.dma_start(out=C.m_ukb[:, 1027:2051], in_=I["uk_L"][h]), writes=[("m_ukb", 1)])
        if getattr(C, "fused", False):
            P.op("dve", lambda e: e.tensor_scalar(out=C.m_uqb[:, 0:3], in0=C.m_uqb[:, 0:3], scalar1=C.m_flagB[:, 0:1], scalar2=None, op0=ALU.mult),
                 reads=[("m_uqb", 0), ("c_flagB",)], writes=[("m_uqb", 0)])
            P.op("dve", lambda e: e.tensor_scalar(out=C.m_ukb[:, 1024:1027], in0=C.m_ukb[:, 1024:1027], scalar1=C.m_flagB[:, 0:1], scalar2=None, op0=ALU.mult),
                 reads=[("m_ukb", 0), ("c_flagB",)], writes=[("m_ukb", 0)])
        for (src, skeys, n, c, dst, dkey, scale) in ((C.m_uqb, [("m_uqb", 0), ("m_uqb", 1)], 1024, h, C.m_qT, ("m_qT",), DK_SCALE),
                                                     (C.m_ukb, [("m_ukb", 0), ("m_ukb", 1), ("ukb_pad",)], 2048, 4 + h, C.m_kT, None, None)):
            P.op("dve", lambda e, src=src, n=n, c=c: e.tensor_scalar(out=C.m_acc[:, 0:n], in0=src[:, 3:3 + n], scalar1=cw_[:, c, 3:4],
                                                                 scalar2=None, op0=ALU.mult),
                 reads=skeys + [("c_cw",)], writes=[("m_acc",)])
            for j in (2, 1, 0):
                P.op("dve", lambda e, src=src, n=n, c=c, j=j: e.scalar_tensor_tensor(
                    out=C.m_acc[:, 0:n], in0=src[:, j:j + n], scalar=cw_[:, c, j:j + 1], in1=C.m_acc[:, 0:n],
                    op0=ALU.mult, op1=ALU.add),
                    reads=skeys + [("c_cw",), ("m_acc",)], writes=[("m_acc",)])
            if scale is None:
                P.op("act", lambda e, n=n, c=c, dst=dst: e.activation(out=dst[:, 0:n], in_=C.m_acc[:, 0:n], func=AF.Silu, bias=cb_[:, c:c + 1]),
                     reads=[("m_acc",), ("c_cb",)], writes=[("m_kT", 0), ("m_kT", 1)])
            else:
                P.op("act", lambda e, n=n, c=c: e.activation(out=C.m_acc[:, 0:n], in_=C.m_acc[:, 0:n], func=AF.Silu, bias=cb_[:, c:c + 1]),
                     reads=[("m_acc",), ("c_cb",)], writes=[("m_acc",)])
                P.op("dve", lambda e, n=n, dst=dst, scale=scale: e.tensor_scalar(out=dst[:, 0:n], in0=C.m_acc[:, 0:n], scalar1=scale, scalar2=None, op0=ALU.mult),
                     reads=[("m_acc",)], writes=[dkey])
        c0 = 1024 + h * 256
        load_V(C, I["v_tm_R"], 0, c0, 256, ("m_V", 0))
        load_V(C, I["v_tm_L"], 8, c0, 256, ("m_V", 1))

        def qhalf(qh):
            qsl = slice(qh * 512, (qh + 1) * 512)
            bn = [0, 1]
            bd = 2
            tiles = _ktiles(qh)
            def stage_S(ti):
                kt, rem, dl = tiles[ti]
                bs = P.bank(4, 8)
                ksl = slice(kt * 128, (kt + 1) * 128)
                P.op("pe", lambda e, bs=bs, ksl=ksl: e.matmul(C.ps[bs][:], C.m_kT[:, ksl], C.m_qT[:, qsl], start=True, stop=True),
                     reads=[("m_kT", 0 if rem else 1), ("m_qT",)], writes=[("ps", bs)])
                NT = len(C.m_tmpx)
                t = C.n_tmp % NT
                C.n_tmp += 1
                if (not rem) and dl <= 0:
                    t2 = C.n_tmp % NT
                    C.n_tmp += 1
                    P.op("dve", lambda e, t2=t2, dl=dl: e.tensor_tensor(out=C.m_tmpx[t2][:], in0=C.m_Fbc[:, qsl], in1=C.m_cau[:, dl + 3, :], op=ALU.add),
                         reads=[("m_Fbc", qh), ("c_cau",)], writes=[("m_tmp", t2)])
                    P.op("act", lambda e, t=t, t2=t2, kt=kt: e.activation(out=C.m_tmpx[t][:], in_=C.m_tmpx[t2][:], func=AF.Exp, bias=C.m_a[:, kt, h:h + 1]),
                         reads=[("m_tmp", t2), ("m_a",)], writes=[("m_tmp", t)])
                else:
                    P.op("act", lambda e, t=t, kt=kt: e.activation(out=C.m_tmpx[t][:], in_=C.m_Fbc[:, qsl], func=AF.Exp, bias=C.m_a[:, kt, h:h + 1]),
                         reads=[("m_Fbc", qh), ("m_a",)], writes=[("m_tmp", t)])
                pi = C.n_pT % len(C.m_pTx)
                C.n_pT += 1
                P.op("dve", lambda e, bs=bs, t=t, pi=pi: e.tensor_tensor(out=C.m_pTx[pi][:], in0=C.ps[bs][:], in1=C.m_tmpx[t][:], op=ALU.mult),
                     reads=[("ps", bs), ("m_tmp", t)], writes=[("m_pT", pi)])
                return pi

            def stage_V(ti, pi):
                kt, rem, dl = tiles[ti]
                first, last = ti == 0, ti == len(tiles) - 1
                for dvc in range(2):
                    P.op("pe", lambda e, kt=kt, pi=pi, dvc=dvc, first=first, last=last: e.matmul(
                        C.ps[bn[dvc]][:], C.m_V[:, kt, dvc * 128:(dvc + 1) * 128], C.m_pTx[pi][:], start=first, stop=last),
                        reads=[("m_V", 0 if rem else 1), ("m_pT", pi)], writes=[("ps", bn[dvc])])
                P.op("pe", lambda e, pi=pi, first=first, last=last: e.matmul(C.ps[bd][:], C.m_onesb[:], C.m_pTx[pi][:], start=first, stop=last),
                     reads=[("c_onesb",), ("m_pT", pi)], writes=[("ps", bd)])
            LA = 3
            pis = {}
            for step in range(len(tiles) + LA):
                if step < len(tiles):
                    pis[step] = stage_S(step)
                if step - LA >= 0:
                    stage_V(step - LA, pis[step - LA])
            P.op("act", lambda e: e.activation(out=C.m_rl[:], in_=C.ps[bd][:], func=AF.Abs),
                 reads=[("ps", bd)], writes=[("m_rl",)])
            P.op("dve", lambda e: e.tensor_scalar(out=C.m_rl[:], in0=C.m_rl[:], scalar1=1.0, scalar2=None, op0=ALU.max),
                 reads=[("m_rl",)], writes=[("m_rl",)])
            P.op("act", lambda e: e.activation(out=C.m_rl[:], in_=C.m_rl[:], func=AF.Ln), reads=[("m_rl",)], writes=[("m_rl",)])
            P.op("act", lambda e: e.activation(out=C.m_rl[:], in_=C.m_rl[:], func=AF.Exp, scale=-1.0), reads=[("m_rl",)], writes=[("m_rl",)])
            bq = 3
            for dvc in range(2):
                P.op("dve", lambda e, dvc=dvc: e.tensor_tensor(out=C.m_hm[dvc][:], in0=C.ps[bn[dvc]][:], in1=C.m_rl[:], op=ALU.mult),
                     reads=[("ps", bn[dvc]), ("m_rl",)], writes=[("m_hm", dvc)])
                P.op("act", lambda e, dvc=dvc: e.activation(out=C.m_sqb[:], in_=C.m_hm[dvc][:], func=AF.Square),
                     reads=[("m_hm", dvc)], writes=[("m_sqb",)])
                P.op("pe", lambda e, dvc=dvc: e.matmul(C.ps[bq][:], C.m_onesb[:], C.m_sqb[:], start=(dvc == 0), stop=(dvc == 1)),
                     reads=[("m_sqb",), ("c_onesb",)], writes=[("ps", bq)])
            P.op("act", lambda e: e.activation(out=C.m_rl[:], in_=C.ps[bq][:], func=AF.Ln, bias=C.m_eps[:, 0:1], scale=1.0 / 256),
                 reads=[("ps", bq), ("c_eps",), ("m_hm", 0), ("m_hm", 1)], writes=[("m_rl",)])
            P.op("act", lambda e: e.activation(out=C.m_rl[:], in_=C.m_rl[:], func=AF.Exp, scale=-0.5), reads=[("m_rl",)], writes=[("m_rl",)])
            for dvc in range(2):
                cc = 2 * h + dvc
                oi = C.n_omb % 2
                C.n_omb += 1
                P.dma("sp", lambda e, cc=cc, oi=oi: e.dma_start(out=C.m_omb[oi][:], in_=I["om"][cc][:, qsl]), writes=[("m_omb", oi)])
                P.op("dve", lambda e, dvc=dvc, cc=cc: e.scalar_tensor_tensor(
                    out=C.m_hm[dvc][:], in0=C.m_hm[dvc][:], scalar=gh_[:, cc:cc + 1], in1=C.m_rl[:], op0=ALU.mult, op1=ALU.mult),
                    reads=[("m_hm", dvc), ("c_gh",), ("m_rl",)], writes=[("m_hm", dvc)])
                si = C.n_ost % 2
                C.n_ost += 1
                P.op("dve", lambda e, dvc=dvc, oi=oi, si=si: e.tensor_tensor(out=C.m_ost[si][:], in0=C.m_hm[dvc][:], in1=C.m_omb[oi][:], op=ALU.mult),
                     reads=[("m_hm", dvc), ("m_omb", oi)], writes=[("m_ost", si)])
                P.dma("sp", lambda e, si=si, cc=cc: e.dma_start(out=mixT[4 + cc][:, qsl], in_=C.m_ost[si][:]), reads=[("m_ost", si)])
        for qh in range(2):
            qhalf(qh)
    for h in range(4):
        head(h)


from contextlib import ExitStack as _ES

Z_OUT = dict(qa_bf=([4, 128, 1024], BF16), qa_f=([4, 128, 1024], F32), ka_bf=([4, 128, 1024], BF16),
             kms=([128, 16], F32), qc_bf=([4, 128, 1024], BF16), kc_bf=([4, 128, 1024], BF16),
             uq=([4, 128, 1024], F32), uk=([4, 128, 1024], F32), om=([8, 128, 1024], F32),
             v_tm=([8, 128, 2048], BF16), gates=([8, 128, 8], F32))


def build_A():
    nc = bass.Bass("TRN2", target_bir_lowering=False)
    with _ES() as es:
        C = Ctx(nc, es)
        h_in = C.dram("h_in", [16, 128, 1024], F32, "ExternalInput")
        g1 = C.dram("g1", [128, 16], F32, "ExternalInput")
        gm = C.dram("gm", [128, 16], F32, "ExternalInput")
        wup = C.dram("wup", [88, 128, 16, 128], F32, "ExternalInput")
        wdown = C.dram("wdown", [16, 128, 44, 128], F32, "ExternalInput")
        win = C.dram("win", [48, 128, 16, 128], F32, "ExternalInput")
        win_g = C.dram("win_g", [128, 16, 8], F32, "ExternalInput")
        h_out = C.dram("h_out", [16, 128, 1024], F32, "ExternalOutput")
        o = {k: C.dram(k, sh, dt, "ExternalOutput") for k, (sh, dt) in Z_OUT.items()}
        alloc_token_local(C)
        alloc_inproj(C)
        load_gain(C, 0, g1)
        load_gain(C, 1, gm)
        load_h(C, h_in)
        ffn(C, 0, wup, wdown)
        store_h(C, h_out)
        inproj(C, 1, win, win_g, o)
        C.P.build(es)
    return nc


M_IN = dict(qa_bf=([4, 128, 1024], BF16), qa_f=([4, 128, 1024], F32), ka_bf_L=([4, 128, 1024], BF16),
            ka_bf_R=([4, 128, 1024], BF16), kms_L=([128, 16], F32), kms_R=([128, 16], F32),
            qc_bf=([4, 128, 1024], BF16), kc_bf_L=([4, 128, 1024], BF16), kc_bf_R=([4, 128, 1024], BF16),
            uq_L=([4, 128, 1024], F32), uq_R=([4, 128, 1024], F32), uk_L=([4, 128, 1024], F32),
            uk_R=([4, 128, 1024], F32), om=([8, 128, 1024], F32), v_tm_L=([8, 128, 2048], BF16),
            v_tm_R=([8, 128, 2048], BF16), gates_L=([8, 128, 8], F32), gates_R=([8, 128, 8], F32))


def build_M():
    nc = bass.Bass("TRN2", target_bir_lowering=False)
    with _ES() as es:
        C = Ctx(nc, es)
        I = {k: C.dram(k, sh, dt, "ExternalInput") for k, (sh, dt) in M_IN.items()}
        cd = {k: C.dram("c_" + k, sh, F32, "ExternalInput") for k, sh in CONST_SHAPES.items()}
        conv_w = C.dram("conv_w", [128, 8, 4], F32, "ExternalInput")
        conv_b = C.dram("conv_b", [128, 8], F32, "ExternalInput")
        gbias = C.dram("gbias", [128, 8], F32, "ExternalInput")
        ghead = C.dram("ghead", [128, 8], F32, "ExternalInput")
        mixT = C.dram("mixT", [16, 128, 1024], BF16, "ExternalOutput")
        alloc_mixer(C)
        load_mixer_consts(C, cd, conv_w, conv_b, gbias, ghead)
        attn_group(C, "moba", I, mixT)
        attn_group(C, "dil", I, mixT)
        mlstm_group(C, I, mixT)
        C.P.build(es)
    return nc


def build_C(final):
    nc = bass.Bass("TRN2", target_bir_lowering=False)
    with _ES() as es:
        C = Ctx(nc, es)
        h_in = C.dram("h_in", [16, 128, 1024], F32, "ExternalInput")
        mixT = C.dram("mixT", [16, 128, 1024], BF16, "ExternalInput")
        wout = C.dram("wout", [16, 128, 16, 128], F32, "ExternalInput")
        g2 = C.dram("g2", [128, 16], F32, "ExternalInput")
        gp = C.dram("gp", [128, 16], F32, "ExternalInput")
        gf = C.dram("gf", [128, 16], F32, "ExternalInput")
        wup = C.dram("wup", [88, 128, 16, 128], F32, "ExternalInput")
        wdown = C.dram("wdown", [16, 128, 44, 128], F32, "ExternalInput")
        wpg = C.dram("wpg", [16, 128, 16, 128], F32, "ExternalInput")
        wpp = C.dram("wpp", [16, 128, 2, 128], F32, "ExternalInput")
        pT = C.dram("pT", [2, 128, 1024], F32, "ExternalInput")
        h_out = C.dram("h_out", [16, 128, 1024], F32, "ExternalOutput")
        alloc_token_local(C)
        alloc_inproj(C)
        load_gain(C, 0, g2)
        load_gain(C, 1, gp)
        load_gain(C, 2, gf)
        load_h(C, h_in)
        outproj(C, mixT, wout)
        ffn(C, 0, wup, wdown)
        ple(C, 1, wpg, wpp, pT)
        if final:
            final_norm(C, 2, h_out)
        else:
            store_h(C, h_out)
        C.P.build(es)
    return nc


def tiles_fm(W):
    Kd, M = W.shape
    return np.ascontiguousarray(W.reshape(Kd // 128, 128, M // 128, 128).transpose(2, 1, 0, 3))


def vec_fm(g):
    return np.ascontiguousarray(np.asarray(g, np.float32).reshape(-1, 128).T)


def _run(nc, in_maps):
    res = run_bass_kernel_spmd(nc, in_maps, core_ids=list(range(8)))
    return res.results


def kernel_unfused(x, p, g_ffn1, w_up1, w_down1, g_mix, w_in, conv_w, conv_b, b_igate, b_fgate,
           g_head, w_out, g_ffn2, w_up2, w_down2, g_ple, w_ple_gate, w_ple_proj, g_final):
    f = lambda a: np.asarray(a, dtype=np.float32)
    x, p = f(x), f(p)
    ncA, ncM, ncC, ncF = build_A(), build_M(), build_C(False), build_C(True)
    consts = [mixer_consts(c % 2 == 1) for c in range(8)]
    hs = []
    for c in range(8):
        b, s = c // 2, c % 2
        hs.append(np.ascontiguousarray(x[b, s * 1024:(s + 1) * 1024, :].T.reshape(16, 128, 1024)))
    out = None
    for l in range(4):
        wi = f(w_in[l])
        a_in = dict(g1=vec_fm(g_ffn1[l]), gm=vec_fm(g_mix[l]), wup=tiles_fm(f(w_up1[l])), wdown=tiles_fm(f(w_down1[l])),
                    win=tiles_fm(wi[:, :6144]),
                    win_g=np.ascontiguousarray(wi[:, 6144:6152].reshape(16, 128, 8).transpose(1, 0, 2)))
        ra = _run(ncA, [dict(a_in, h_in=hs[c]) for c in range(8)])
        hs = [ra[c]["h_out"] for c in range(8)]
        cw = np.ascontiguousarray(f(conv_w[l]).reshape(4, 8, 128).transpose(2, 1, 0))
        m_common = dict(conv_w=cw, conv_b=vec_fm(conv_b[l]),
                        gbias=np.ascontiguousarray(np.broadcast_to(np.concatenate([f(b_igate[l]), f(b_fgate[l])])[None, :], (128, 8))),
                        ghead=vec_fm(g_head[l]))
        m_in = []
        for c in range(8):
            d = dict(m_common)
            for k, v in consts[c].items():
                d["c_" + k] = v
            me, other = ra[c], ra[c - 1] if c % 2 == 1 else None
            for k in ("qa_bf", "qa_f", "qc_bf", "om"):
                d[k] = me[k]
            for k in ("ka_bf", "kms", "kc_bf", "uq", "uk", "v_tm", "gates"):
                d[k + "_L"] = me[k]
                d[k + "_R"] = other[k] if other is not None else np.zeros_like(me[k])
            m_in.append(d)
        rm = _run(ncM, m_in)
        c_common = dict(wout=tiles_fm(f(w_out[l])), g2=vec_fm(g_ffn2[l]), gp=vec_fm(g_ple[l]), gf=vec_fm(g_final),
                        wup=tiles_fm(f(w_up2[l])), wdown=tiles_fm(f(w_down2[l])), wpg=tiles_fm(f(w_ple_gate[l])),
                        wpp=tiles_fm(f(w_ple_proj[l])))
        c_in = []
        for c in range(8):
            b, s = c // 2, c % 2
            pT = np.ascontiguousarray(p[l, b, s * 1024:(s + 1) * 1024, :].T.reshape(2, 128, 1024))
            c_in.append(dict(c_common, h_in=hs[c], mixT=rm[c]["mixT"], pT=pT))
        rc = _run(ncF if l == 3 else ncC, c_in)
        hs = [rc[c]["h_out"] for c in range(8)]
    out = np.empty((4, 2048, 2048), np.float32)
    for c in range(8):
        b, s = c // 2, c % 2
        out[b, s * 1024:(s + 1) * 1024, :] = hs[c].reshape(2048, 1024).T
    return out


ARENA_BYTES = 65536 + 106496
NL = 4


def alloc_fused(C):
    sb = C.sb
    C.fused = True
    arena = sb("arena", [128, ARENA_BYTES // 4], F32)

    def view(off, shape, dt, parts=128):
        n = int(np.prod(shape[1:]))
        nb = n * (4 if dt == F32 else 2)
        assert off % 4 == 0 and off + nb <= ARENA_BYTES, (off, nb)
        ap = arena[0:parts, off // 4:(off + nb + 3) // 4]
        if dt != F32:
            ap = ap.bitcast(dt)
        if len(shape) == 3:
            ap = ap.rearrange("p (a b) -> p a b", b=shape[2])
        return ap

    K = 1024
    C.hT = view(0, [128, KC_D, NTOK], F32)
    base = 65536
    C.hnT = view(base, [128, KC_D, NTOK], BF16)
    C.actT = view(base + 32 * K, [128, WSLOT_KC, NTOK], BF16)
    C.wbuf = [view(base + 76 * K + i * 5632, [128, WSLOT_KC, 128], BF16) for i in range(WSLOTS)]
    o = base
    C.m_tab = view(o, [128, 13, 512], F32); o += 26 * K
    C.m_dil_loc = C.m_tab[:, 0:8, :]
    C.m_dil_rem = C.m_tab[:, 8:13, :]
    C.m_cau = C.m_tab[:, 0:4, :]
    C.m_step = C.m_tab[:, 4:8, :]
    C.m_posQ = view(o, [4, 1024], BF16, parts=4); o += 2 * K
    C.m_qT = view(o, [128, 1024], BF16); o += 2 * K
    C.m_kT = view(o, [128, 2048], BF16); o += 4 * K
    C.m_V = view(o, [128, 16, 256], BF16); o += 8 * K
    C.m_qf = view(o, [128, 1024], F32); o += 4 * K
    C.m_selbT = view(o, [8, 1024], BF16, parts=8); o += 2 * K
    C.m_pT = []
    for i in range(3):
        C.m_pT.append(view(o, [128, 512], BF16)); o += K
    C.m_tmp = []
    for i in range(3):
        C.m_tmp.append(view(o, [128, 512], F32)); o += 2 * K
    C.m_tmpx = C.m_tmp + [C.m_tab[:, 8 + i, :] for i in range(4)]
    C.m_pT = C.m_pT + [sb("m_pT3", [128, 512], BF16)]
    C.m_pTx = C.m_pT
    C.m_rl = view(o, [128, 512], F32); o += 2 * K
    C.m_ost = []
    for i in range(2):
        C.m_ost.append(view(o, [128, 512], BF16)); o += K
    C.m_lfrep = view(o, [128, 16, 128], F32); o += 8 * K
    C.m_Fbc = view(o, [128, 1024], F32); o += 4 * K
    C.m_uqb = view(o, [128, 1027], F32); o += 5 * K
    C.m_ukb = view(o, [128, 2051], F32); o += 9 * K
    C.m_acc = view(o, [128, 2048], F32); o += 8 * K
    C.m_hm = []
    for i in range(2):
        C.m_hm.append(view(o, [128, 512], F32)); o += 2 * K
    C.m_sqb = view(o, [128, 512], BF16); o += K
    C.m_omb = []
    for i in range(2):
        C.m_omb.append(view(o, [128, 512], F32)); o += 2 * K
    assert o <= ARENA_BYTES, o
    C.sq = [sb("sq%d" % i, [128, NTOK], BF16) for i in range(2)]
    C.rstd = sb("rstd", [128, NTOK], F32)
    C.tmpf = [sb("tmpf%d" % i, [128, 512], F32) for i in range(2)]
    C.ones_bf = sb("ones_bf", [128, 128], BF16)
    C.epsv = sb("epsv", [128, 1], F32)
    C.gvec = sb("gvec", [128, 4 * NL + 1, KC_D], F32)
    C.stf = [sb("stf%d" % i, [128, 512], F32) for i in range(3)]
    C.stb = [sb("stb%d" % i, [128, 512], BF16) for i in range(4)]
    C.vst = [sb("vst%d" % i, [128, 128], BF16) for i in range(4)]
    C.kms = sb("kms_sb", [128, 16], F32)
    C.wg = sb("wg", [128, KC_D, 8], BF16)
    C.gst = sb("gst", [128, 8, 8], F32)
    C.m_posK = sb("m_posK", [4, 2048], BF16)
    C.m_E8 = sb("m_E8", [8, 1024], BF16)
    C.m_G = sb("m_G", [128, 8, 8], F32)
    C.m_ident = sb("m_ident", [128, 128], F32)
    C.m_U = sb("m_U", [128, 128], F32)
    C.m_rbias = sb("m_rbias", [128, 1], F32)
    C.m_flagB = sb("m_flagB", [128, 1], F32)
    C.m_onesf = sb("m_onesf", [128, 512], F32)
    C.m_cw = sb("m_cw", [128, NL, 8, 4], F32)
    C.m_cb = sb("m_cb", [128, NL, 8], F32)
    C.m_gb = sb("m_gb", [128, NL, 8], F32)
    C.m_gh = sb("m_gh", [128, NL, 8], F32)
    C.m_eps = C.epsv
    C.m_onesb = C.ones_bf
    C.m_kms = sb("m_kms", [128, 8], F32)
    C.m_gs = sb("m_gs", [128, 8], F32)
    C.m_m8 = sb("m_m8", [128, 8], F32)
    C.m_sel = sb("m_sel", [128, 8], F32)
    C.m_val = sb("m_val", [128, 8], F32)
    C.m_gs4 = [C.m_gs] + [sb("m_gs_%d" % i, [128, 8], F32) for i in range(3)]
    C.m_m84 = [C.m_m8] + [sb("m_m8_%d" % i, [128, 8], F32) for i in range(3)]
    C.m_sel4 = [C.m_sel] + [sb("m_sel_%d" % i, [128, 8], F32) for i in range(3)]
    C.m_val4 = [C.m_val] + [sb("m_val_%d" % i, [128, 8], F32) for i in range(3)]
    C.m_gl = sb("m_gl", [128, 16, 8], F32)
    C.m_lf = sb("m_lf", [128, 16, 4], F32)
    C.m_a = sb("m_a", [128, 16, 4], F32)
    C.m_Rrem = sb("m_Rrem", [128, 1], F32)
    for nm in ("n_sq", "n_tmpf", "n_stf", "n_stb", "n_vst", "n_pT", "n_tmp", "n_ost", "n_omb"):
        setattr(C, nm, 0)
    P = C.P
    P.op("dve", lambda e: e.memset(C.ones_bf[:], 1.0), writes=[("ones_bf",), ("c_onesb",)])
    P.op("dve", lambda e: e.memset(C.epsv[:], 1e-6), writes=[("epsv",), ("c_eps",)])
    P.op("dve", lambda e: e.memset(C.m_onesf[:], 1.0), writes=[("c_onesf",)])


def load_small_consts(C, cd, D_):
    P = C.P
    C.cd = cd
    for nm, q in (("posK", "pool"), ("E8", "pool"), ("G", "sp"), ("ident", "sp"), ("U", "sp"), ("rbias", "sp"), ("flagB", "sp")):
        dst = getattr(C, "m_" + nm)
        P.dma(q, lambda e, dst=dst, nm=nm: e.dma_start(out=dst[:], in_=cd[nm]), writes=[("c_" + nm,)])
    P.dma("sp", lambda e: e.dma_start(out=C.m_cw[:], in_=D_["conv_w"]), writes=[("c_cw",)])
    P.dma("sp", lambda e: e.dma_start(out=C.m_cb[:], in_=D_["conv_b"]), writes=[("c_cb",)])
    P.dma("sp", lambda e: e.dma_start(out=C.m_gb[:], in_=D_["gbias"]), writes=[("c_gb",)])
    P.dma("sp", lambda e: e.dma_start(out=C.m_gh[:], in_=D_["ghead"]), writes=[("c_gh",)])
    P.dma("sp", lambda e: e.dma_start(out=C.gvec[:], in_=D_["gains"]), writes=[("gvec", i) for i in range(4 * NL + 1)])


def load_tabs(C, kind):
    P = C.P
    if kind == "mlstm":
        P.op("dve", lambda e: e.memset(C.m_ukb[:, 0:3], 0.0), writes=[("ukb_pad",)])
        return
    P.barrier()
    if kind == "dil":
        P.dma("sp", lambda e: e.dma_start(out=C.m_tab[:, 0:8, :], in_=C.cd["dil_loc"]), writes=[("c_dil_loc",)])
        P.dma("sp", lambda e: e.dma_start(out=C.m_tab[:, 8:13, :], in_=C.cd["dil_rem"]), writes=[("c_dil_rem",)])
    else:
        P.dma("sp", lambda e: e.dma_start(out=C.m_tab[:, 0:4, :], in_=C.cd["cau"]), writes=[("c_cau",)])
        P.dma("sp", lambda e: e.dma_start(out=C.m_tab[:, 4:8, :], in_=C.cd["step"]), writes=[("c_step",)])


XB_ROWS = 3072
XF_ROWS = 1034
PAIRS = [[0, 1], [2, 3], [4, 5], [6, 7]]


def build_fused(nl=NL, phases="AXMC"):
    nc = bass.Bass("TRN2", target_bir_lowering=False)
    with _ES() as es:
        C = Ctx(nc, es)
        P = C.P
        ext = lambda n, sh: C.dram(n, sh, F32, "ExternalInput")
        x_in = ext("x_in", [16, 128, 1024])
        pT = ext("pT", [NL, 2, 128, 1024])
        W = dict(wup1=ext("wup1", [NL, 88, 128, 16, 128]), wdown1=ext("wdown1", [NL, 16, 128, 44, 128]),
                 win=ext("win", [NL, 48, 128, 16, 128]), win_g=ext("win_g", [NL, 128, 16, 8]),
                 wout=ext("wout", [NL, 16, 128, 16, 128]),
                 wup2=ext("wup2", [NL, 88, 128, 16, 128]), wdown2=ext("wdown2", [NL, 16, 128, 44, 128]),
                 wpg=ext("wpg", [NL, 16, 128, 16, 128]), wpp=ext("wpp", [NL, 16, 128, 2, 128]))
        D_ = dict(conv_w=ext("conv_w", [128, NL, 8, 4]), conv_b=ext("conv_b", [128, NL, 8]),
                  gbias=ext("gbias", [128, NL, 8]), ghead=ext("ghead", [128, NL, 8]),
                  gains=ext("gains", [128, 4 * NL + 1, 16]))
        shapes = dict(CONST_SHAPES, flagB=[128, 1])
        cd = {k: ext("c_" + k, sh) for k, sh in shapes.items()}
        out = C.dram("out", [16, 128, 1024], F32, "ExternalOutput")
        I32 = lambda n, sh: nc.dram_tensor(n, sh, F32, kind="Internal").ap()
        I16 = lambda n, sh: nc.dram_tensor(n, sh, BF16, kind="Internal").ap()
        exch = []
        exd = {}

        def mk(n, rows, mkfn):
            a, g = mkfn("x_" + n, [rows, 1024]), mkfn("g_" + n, [2 * rows, 1024])
            exch.append((a, g))
            exd[n] = (a, g)
            return a, g[0:rows, :]
        loc = dict(qa_bf=I16("z_qa_bf", [4, 128, 1024]), qa_f=I32("z_qa_f", [4, 128, 1024]),
                   qc_bf=I16("z_qc_bf", [4, 128, 1024]), om=I32("z_om", [8, 128, 1024]))
        mixT = I16("z_mixT", [16, 128, 1024])
        kk = mk("kk", 1024, I16)
        v0 = mk("v0", 1024, I16)
        v1 = mk("v1", 1024, I16)
        uk = mk("uk", 512, I32)
        uq = mk("uq", 512, I32)
        gk = mk("gk", 128, I32)
        ch = lambda t: t.rearrange("(j p) t -> j p t", p=128)
        vt = lambda t: t.rearrange("(t p a) b -> t p (a b)", t=4, p=128, a=2)
        Lv, Rv = {}, {}
        for i, dst in ((0, Lv), (1, Rv)):
            dst["ka_bf"] = ch(kk[i][0:512, :])
            dst["kc_bf"] = ch(kk[i][512:1024, :])
            dst["v_tm"] = [vt(v0[i]), vt(v1[i])]
            dst["uk"] = ch(uk[i])
            dst["uq"] = ch(uq[i])
            dst["gates"] = gk[i][0:8, :].rearrange("t (p c) -> t p c", c=8)
            dst["kms"] = gk[i][8:10, :].rearrange("a (p c) -> (a p) c", c=16)
        o_A = dict(loc, **Lv)
        v8 = [Lv["v_tm"][0][t] for t in range(4)] + [Lv["v_tm"][1][t] for t in range(4)]
        o_A["v_tm"] = v8
        I_M = dict(loc)
        for k in ("ka_bf", "kms", "kc_bf", "uq", "uk", "v_tm", "gates"):
            I_M[k + "_L"] = Lv[k]
            I_M[k + "_R"] = Rv[k]

        alloc_fused(C)
        load_small_consts(C, cd, D_)
        load_h(C, x_in)
        for l in range(nl):
            if "A" in phases:
                ffn(C, 4 * l + 0, W["wup1"][l], W["wdown1"][l])
                def group_cb(g, keys):
                    if g != "gk" or "X" not in phases:
                        return
                    P.barrier()
                    for (a_, g_) in exch:
                        P.cc(lambda e, a_=a_, g_=g_: e.collective_compute("AllGather", ALU.bypass, replica_groups=PAIRS,
                                                                          ins=[a_.opt()], outs=[g_.opt()]))
                inproj(C, 4 * l + 1, W["win"][l], W["win_g"][l], o_A, group_cb)
            P.barrier()
            C.m_layer = l
            if "M" in phases:
                attn_group(C, "moba", I_M, mixT)
                mlstm_group(C, I_M, mixT)
                attn_group(C, "dil", I_M, mixT)
            P.barrier()
            if "C" in phases:
                outproj(C, mixT, W["wout"][l])
                ffn(C, 4 * l + 2, W["wup2"][l], W["wdown2"][l])
                ple(C, 4 * l + 3, W["wpg"][l], W["wpp"][l], pT[l])
            P.bar = []
        final_norm(C, 4 * NL, out)
        C.P.build(es)
    return nc


def kernel(x, p, g_ffn1, w_up1, w_down1, g_mix, w_in, conv_w, conv_b, b_igate, b_fgate,
           g_head, w_out, g_ffn2, w_up2, w_down2, g_ple, w_ple_gate, w_ple_proj, g_final):
    f = lambda a: np.asarray(a, dtype=np.float32)
    x, p = f(x), f(p)
    nc = build_fused()
    st = lambda w, fn=tiles_fm: np.stack([fn(f(w[l])) for l in range(NL)])
    wi = f(w_in)
    gains = np.stack([vec_fm(g[l]) for l in range(NL) for g in (g_ffn1, g_mix, g_ffn2, g_ple)] + [vec_fm(g_final)], axis=1)
    common = dict(
        wup1=st(w_up1), wdown1=st(w_down1), win=np.stack([tiles_fm(wi[l][:, :6144]) for l in range(NL)]),
        win_g=np.stack([np.ascontiguousarray(wi[l][:, 6144:6152].reshape(16, 128, 8).transpose(1, 0, 2)) for l in range(NL)]),
        wout=st(w_out), wup2=st(w_up2), wdown2=st(w_down2), wpg=st(w_ple_gate), wpp=st(w_ple_proj),
        conv_w=np.ascontiguousarray(np.stack([f(conv_w[l]).reshape(4, 8, 128).transpose(2, 1, 0) for l in range(NL)], axis=1)),
        conv_b=np.ascontiguousarray(np.stack([vec_fm(conv_b[l]) for l in range(NL)], axis=1)),
        gbias=np.ascontiguousarray(np.stack([np.broadcast_to(np.concatenate([f(b_igate[l]), f(b_fgate[l])])[None, :], (128, 8))
                                             for l in range(NL)], axis=1)),
        ghead=np.ascontiguousarray(np.stack([vec_fm(g_head[l]) for l in range(NL)], axis=1)),
        gains=np.ascontiguousarray(gains))
    in_maps = []
    for c in range(8):
        b, s_ = c // 2, c % 2
        d = dict(common)
        d["x_in"] = np.ascontiguousarray(x[b, s_ * 1024:(s_ + 1) * 1024, :].T.reshape(16, 128, 1024))
        d["pT"] = np.ascontiguousarray(np.stack([p[l, b, s_ * 1024:(s_ + 1) * 1024, :].T.reshape(2, 128, 1024) for l in range(NL)]))
        for k, v in mixer_consts(s_ == 1).items():
            d["c_" + k] = v
        d["c_flagB"] = np.full((128, 1), float(s_), np.float32)
        in_maps.append(d)
    res = run_bass_kernel_spmd(nc, in_maps, core_ids=list(range(8))).results
    out = np.empty((4, 2048, 2048), np.float32)
    for c in range(8):
        b, s_ = c // 2, c % 2
        out[b, s_ * 1024:(s_ + 1) * 1024, :] = res[c]["out"].reshape(2048, 1024).T
    return out
```

```python
from contextlib import ExitStack

COMPUTE = ("pe", "act", "dve", "pool")
EPOCH = 30000
NDMA_SLOTS = 24


class Prog:
    def __init__(self, nc):
        self.nc = nc
        self.ops = []
        self.last_w = {}
        self.readers = {}
        self.cnt = {e: 0 for e in COMPUTE}
        self.epoch = {e: 0 for e in COMPUTE}
        self.dma_n = {"sp": 0, "pool": 0, "act": 0}
        self.dma_val = {}
        self.sem_ids = set()
        self._bank = 0

    def barrier(self):
        fin = {}
        for (_e, _f, _d, tok, _i) in self.ops:
            sid = tok[0]
            if sid[0] in COMPUTE:
                k = sid[0]
                if k not in fin or (fin[k][0][1], fin[k][1]) < (sid[1], tok[1]):
                    fin[k] = tok
            else:
                if sid not in fin or fin[sid][1] < tok[1]:
                    fin[sid] = tok
        self.bar = list(fin.values())

    def _deps(self, reads, writes):
        deps = list(getattr(self, "bar", ()))
        for r in reads:
            t = self.last_w.get(r)
            if t is not None:
                deps.append(t)
            if r[0] == "ps":
                deps.extend(self.readers.get(r, ()))
        for w in writes:
            t = self.last_w.get(w)
            if t is not None:
                deps.append(t)
            deps.extend(self.readers.get(w, ()))
        return deps

    def _commit(self, tok, reads, writes):
        for r in reads:
            self.readers.setdefault(r, []).append(tok)
        for w in writes:
            self.last_w[w] = tok
            self.readers[w] = []

    def op(self, eng, fn, reads=(), writes=()):
        deps = self._deps(reads, writes)
        if self.cnt[eng] >= EPOCH:
            self.epoch[eng] += 1
            self.cnt[eng] = 0
        self.cnt[eng] += 1
        sid = (eng, self.epoch[eng])
        self.sem_ids.add(sid)
        tok = (sid, self.cnt[eng], eng)
        self.ops.append((eng, fn, deps, tok, 1))
        self._commit(tok, reads, writes)
        return tok

    def dma(self, q, fn, reads=(), writes=()):
        deps = self._deps(reads, writes)
        slot = self.dma_n[q] % NDMA_SLOTS
        self.dma_n[q] += 1
        sid = ("dma", q, slot)
        self.sem_ids.add(sid)
        prev = self.dma_val.get(sid, 0)
        if prev:
            deps.append((sid, prev, "dma"))
        val = prev + 16
        self.dma_val[sid] = val
        tok = (sid, val, "dma")
        self.ops.append((q, fn, deps, tok, 16))
        self._commit(tok, reads, writes)
        return tok

    def cc(self, fn, reads=(), writes=()):
        deps = self._deps(reads, writes)
        n = self.__dict__.setdefault("_ncc", 0)
        self._ncc = n + 1
        sid = ("cc", n % 8)
        self.sem_ids.add(sid)
        prev = self.dma_val.get(sid, 0)
        if prev:
            deps.append((sid, prev, "dma"))
        val = prev + 1
        self.dma_val[sid] = val
        tok = (sid, val, "dma")
        self.ops.append(("pool", fn, deps, tok, 1))
        self._commit(tok, reads, writes)
        return tok

    def bank(self, lo=0, hi=8):
        if (lo, hi) == (0, 8):
            b = self._bank
            self._bank = (b + 1) % 8
            return b
        st = self.__dict__.setdefault("_rb", {})
        i = st.get((lo, hi), 0)
        st[(lo, hi)] = i + 1
        return lo + i % (hi - lo)

    def build(self, es: ExitStack):
        nc = self.nc
        sems = {}
        for sid in sorted(self.sem_ids, key=str):
            nm = "s_" + "_".join(str(x) for x in sid)
            sems[sid] = es.enter_context(nc.semaphore(nm))
        final = {}
        for (_e, _f, _d, tok, _i) in self.ops:
            final[tok[0]] = max(final.get(tok[0], 0), tok[1])
        ops = self.ops
        block = es.enter_context(nc.Block())

        def emit(eng_name, e):
            waited = {}
            for (en, fn, deps, tok, inc) in ops:
                if en != eng_name:
                    continue
                need = {}
                for (sid, val, deng) in deps:
                    if deng == "pe" and eng_name == "pe":
                        continue
                    if waited.get(sid, 0) >= val:
                        continue
                    if need.get(sid, 0) < val:
                        need[sid] = val
                for sid, val in need.items():
                    e.wait_ge(sems[sid], val)
                    waited[sid] = val
                ins = fn(e)
                ins.then_inc(sems[tok[0]], inc)
            if eng_name == "sp":
                for sid, val in final.items():
                    if waited.get(sid, 0) < val:
                        e.wait_ge(sems[sid], val)

        @block.tensor
        def _(e):
            emit("pe", e)

        @block.scalar
        def _(e):
            emit("act", e)

        @block.vector
        def _(e):
            emit("dve", e)

        @block.gpsimd
        def _(e):
            emit("pool", e)

        @block.sync
        def _(e):
            emit("sp", e)


import numpy as np
import concourse.bass as bass
import concourse.mybir as mybir
from concourse.bass_utils import run_bass_kernel_spmd

F32 = mybir.dt.float32
BF16 = mybir.dt.bfloat16
AF = mybir.ActivationFunctionType
ALU = mybir.AluOpType
AX = mybir.AxisListType

NTOK = 1024
D = 2048
KC_D = 16
DFF = 5632
NFF = 44
WSLOTS = 4
WSLOT_KC = 22


class Ctx:
    def __init__(self, nc, es):
        self.nc = nc
        self.es = es
        self.P = Prog(nc)
        self.n_w = 0
        self.ps = [es.enter_context(nc.psum_tensor("ps%d" % i, [128, 512], F32)) for i in range(8)]

    def sb(self, name, shape, dt):
        return self.es.enter_context(self.nc.sbuf_tensor(name, shape, dt))

    def dram(self, name, shape, dt, kind):
        return self.nc.dram_tensor(name, shape, dt, kind=kind).ap()


def alloc_token_local(C):
    C.hT = C.sb("hT", [128, KC_D, NTOK], F32)
    C.hnT = C.sb("hnT", [128, KC_D, NTOK], BF16)
    C.actT = C.sb("actT", [128, WSLOT_KC, NTOK], BF16)
    C.wbuf = [C.sb("wbuf%d" % i, [128, WSLOT_KC, 128], BF16) for i in range(WSLOTS)]
    C.sq = [C.sb("sq%d" % i, [128, NTOK], BF16) for i in range(2)]
    C.rstd = C.sb("rstd", [128, NTOK], F32)
    C.tmpf = [C.sb("tmpf%d" % i, [128, 512], F32) for i in range(3)]
    C.ones_bf = C.sb("ones_bf", [128, 128], BF16)
    C.gvec = C.sb("gvec", [128, 8, KC_D], F32)
    C.n_sq = 0
    C.n_tmpf = 0
    C.epsv = C.sb("epsv", [128, 1], F32)
    C.P.op("dve", lambda e: e.memset(C.ones_bf[:], 1.0), writes=[("ones_bf",)])
    C.P.op("dve", lambda e: e.memset(C.epsv[:], 1e-6), writes=[("epsv",)])


def load_gain(C, idx, g_dram):
    C.P.dma("sp", lambda e: e.dma_start(out=C.gvec[:, idx, :], in_=g_dram), writes=[("gvec", idx)])


def load_h(C, h_dram):
    for kc in range(KC_D):
        C.P.dma("sp", lambda e, kc=kc: e.dma_start(out=C.hT[:, kc, :], in_=h_dram[kc]),
                writes=[("hT", kc)])


def store_h(C, h_dram):
    for kc in range(KC_D):
        C.P.dma("sp", lambda e, kc=kc: e.dma_start(out=h_dram[kc], in_=C.hT[:, kc, :]),
                reads=[("hT", kc)])


def rmsnorm(C, gidx, out_fn=None):
    P = C.P
    banks = [P.bank(), P.bank()]
    for kc in range(KC_D):
        s = C.n_sq % 2
        C.n_sq += 1
        if kc % 2 == 0 or not getattr(C, "fused", False):
            P.op("act", lambda e, kc=kc, s=s: e.activation(out=C.sq[s][:], in_=C.hT[:, kc, :], func=AF.Square),
                 reads=[("hT", kc)], writes=[("sq", s)])
        else:
            P.op("pool", lambda e, kc=kc, s=s: e.tensor_tensor(out=C.sq[s][:], in0=C.hT[:, kc, :], in1=C.hT[:, kc, :], op=ALU.mult),
                 reads=[("hT", kc)], writes=[("sq", s)])
        for th in range(2):
            P.op("pe", lambda e, kc=kc, s=s, th=th: e.matmul(
                C.ps[banks[th]][:], C.ones_bf[:], C.sq[s][:, th * 512:(th + 1) * 512],
                start=(kc == 0), stop=(kc == KC_D - 1)),
                reads=[("sq", s), ("ones_bf",)], writes=[("ps", banks[th])])
    for th in range(2):
        sl = slice(th * 512, (th + 1) * 512)
        P.op("act", lambda e, th=th, sl=sl: e.activation(
            out=C.rstd[:, sl], in_=C.ps[banks[th]][:], func=AF.Ln, bias=C.epsv[:, 0:1], scale=1.0 / D),
            reads=[("ps", banks[th]), ("epsv",)], writes=[("rstd", th)])
        P.op("act", lambda e, sl=sl: e.activation(out=C.rstd[:, sl], in_=C.rstd[:, sl], func=AF.Exp, scale=-0.5),
            reads=[("rstd", th)], writes=[("rstd", th)])
    for kc in range(KC_D):
        if out_fn is None:
            P.op("dve", lambda e, kc=kc: e.scalar_tensor_tensor(
                out=C.hnT[:, kc, :], in0=C.hT[:, kc, :], scalar=C.gvec[:, gidx, kc:kc + 1],
                in1=C.rstd[:], op0=ALU.mult, op1=ALU.mult),
                reads=[("hT", kc), ("rstd", 0), ("rstd", 1), ("gvec", gidx)], writes=[("hnT", kc)])
        else:
            out_fn(kc)


def wload(C, src_ap, kc_n):
    slot = C.n_w % WSLOTS
    C.n_w += 1
    C.P.dma("pool", lambda e: e.dma_start(out=C.wbuf[slot][:, 0:kc_n, :], in_=src_ap),
            writes=[("w", slot)])
    return slot


def mm_fm(C, slot, kc_n, x, xkey, x_kc0, th, bank):
    for kc in range(kc_n):
        C.P.op("pe", lambda e, kc=kc: e.matmul(
            C.ps[bank][:], C.wbuf[slot][:, kc, :], x[:, x_kc0 + kc, th * 512:(th + 1) * 512],
            start=(kc == 0), stop=(kc == kc_n - 1)),
            reads=[("w", slot), (xkey, x_kc0 + kc)], writes=[("ps", bank)])


def ffn(C, gidx, wup, wdown):
    P = C.P
    rmsnorm(C, gidx)
    for half in range(2):
        for cl in range(WSLOT_KC):
            c = half * WSLOT_KC + cl
            sg_ = wload(C, wup[c], KC_D)
            su_ = wload(C, wup[NFF + c], KC_D)
            for th in range(2):
                bg, bu = P.bank(), P.bank()
                mm_fm(C, sg_, KC_D, C.hnT, "hnT", 0, th, bg)
                mm_fm(C, su_, KC_D, C.hnT, "hnT", 0, th, bu)
                t = C.n_tmpf % len(C.tmpf)
                C.n_tmpf += 1
                P.op("act", lambda e, t=t, bg=bg: e.activation(out=C.tmpf[t][:], in_=C.ps[bg][:], func=AF.Silu),
                     reads=[("ps", bg)], writes=[("tmpf", t)])
                P.op("dve", lambda e, t=t, bu=bu, cl=cl, th=th: e.tensor_tensor(
                    out=C.actT[:, cl, th * 512:(th + 1) * 512], in0=C.ps[bu][:], in1=C.tmpf[t][:], op=ALU.mult),
                    reads=[("ps", bu), ("tmpf", t)], writes=[("actT", cl, th)])
        for j in range(KC_D):
            sd = wload(C, wdown[j][:, half * WSLOT_KC:(half + 1) * WSLOT_KC, :], WSLOT_KC)
            for th in range(2):
                b = P.bank()
                for kc in range(WSLOT_KC):
                    P.op("pe", lambda e, kc=kc, th=th, b=b, sd=sd: e.matmul(
                        C.ps[b][:], C.wbuf[sd][:, kc, :], C.actT[:, kc, th * 512:(th + 1) * 512],
                        start=(kc == 0), stop=(kc == WSLOT_KC - 1)),
                        reads=[("w", sd), ("actT", kc, th)], writes=[("ps", b)])
                sl = slice(th * 512, (th + 1) * 512)
                P.op("dve", lambda e, j=j, b=b, sl=sl: e.scalar_tensor_tensor(
                    out=C.hT[:, j, sl], in0=C.ps[b][:], scalar=0.5, in1=C.hT[:, j, sl],
                    op0=ALU.mult, op1=ALU.add),
                    reads=[("ps", b), ("hT", j)], writes=[("hT", j)])


HD_SCALE = 128.0 ** -0.5


def alloc_inproj(C):
    C.stf = [C.sb("stf%d" % i, [128, 512], F32) for i in range(4)]
    C.stb = [C.sb("stb%d" % i, [128, 512], BF16) for i in range(4)]
    C.n_stf = 0
    C.n_stb = 0
    C.kms = C.sb("kms_sb", [128, 16], F32)
    C.wg = C.sb("wg", [128, KC_D, 8], BF16)
    C.gst = C.sb("gst", [128, 8, 8], F32)
    C.vst = [C.sb("vst%d" % i, [128, 128], BF16) for i in range(4)]
    C.n_vst = 0


def _stage_out(C, kind, b, dst_ap, func=None, scale=1.0, eng="act", grp=None):
    P = C.P
    if kind == "f":
        i = C.n_stf % len(C.stf)
        C.n_stf += 1
        buf, key = C.stf[i], ("stf", i)
    else:
        i = C.n_stb % 4
        C.n_stb += 1
        buf, key = C.stb[i], ("stb", i)
    if eng == "act":
        P.op("act", lambda e: e.activation(out=buf[:], in_=C.ps[b][:], func=func or AF.Copy, scale=scale),
             reads=[("ps", b)], writes=[key])
    else:
        P.op("dve", lambda e: e.tensor_copy(out=buf[:], in_=C.ps[b][:]), reads=[("ps", b)], writes=[key])
    P.dma("sp", lambda e: e.dma_start(out=dst_ap, in_=buf[:]), reads=[key], writes=_gk(C, grp))


def _gk(C, grp):
    if grp is None:
        return []
    lst = C.grp_keys.setdefault(grp, [])
    k = ("xw", grp, len(lst), C.grp_epoch)
    lst.append(k)
    return [k]


def inproj(C, gidx, win, win_g, o, group_cb=None):
    P = C.P
    C.grp_keys = {}
    C.grp_epoch = getattr(C, "grp_epoch", 0) + 1

    def done(g):
        if group_cb is not None:
            group_cb(g, C.grp_keys.get(g, []))
    rmsnorm(C, gidx)
    P.dma("pool", lambda e: e.dma_start(out=C.wg[:], in_=win_g), writes=[("wg",)])

    def fm_chunks(lst):
        for (wj, kind, j) in lst:
            slot = wload(C, win[wj], KC_D)
            for th in range(2):
                b = P.bank()
                sl = slice(th * 512, (th + 1) * 512)
                mm_fm(C, slot, KC_D, C.hnT, "hnT", 0, th, b)
                if kind == "qa":
                    _stage_out(C, "b", b, o["qa_bf"][j][:, sl], scale=HD_SCALE)
                    _stage_out(C, "f", b, o["qa_f"][j][:, sl], eng="dve")
                elif kind == "ka":
                    _stage_out(C, "b", b, o["ka_bf"][j][:, sl], grp="kk")
                    P.op("dve", lambda e, j=j, th=th, b=b: e.tensor_reduce(
                        out=C.kms[:, j * 4 + th * 2:j * 4 + th * 2 + 2],
                        in_=C.ps[b][:].rearrange("p (a c) -> p a c", a=2), axis=AX.X, op=ALU.add),
                        reads=[("ps", b)], writes=[("kms", j, th)])
                elif kind == "qc":
                    _stage_out(C, "b", b, o["qc_bf"][j][:, sl], scale=HD_SCALE)
                elif kind == "kc":
                    _stage_out(C, "b", b, o["kc_bf"][j][:, sl], grp="kk")
                elif kind == "uq":
                    _stage_out(C, "f", b, o["uq"][j][:, sl], grp="uq")
                elif kind == "uk":
                    _stage_out(C, "f", b, o["uk"][j][:, sl], grp="uk")
                elif kind == "om":
                    _stage_out(C, "f", b, o["om"][j][:, sl], func=AF.Sigmoid)
    fm_chunks([(4 + j, "ka", j) for j in range(4)] + [(16 + j, "kc", j) for j in range(4)])
    done("kk")
    vt = [(8 + j, j) for j in range(4)] + [(20 + j, 4 + j) for j in range(4)] + [(32 + j, 8 + j) for j in range(8)]
    for (wj, cg) in vt:
        slot = wload(C, win[wj], KC_D)
        for tt in range(8):
            b = P.bank()
            for kc in range(KC_D):
                P.op("pe", lambda e, kc=kc, tt=tt, b=b, slot=slot: e.matmul(
                    C.ps[b][:, 0:128], C.hnT[:, kc, tt * 128:(tt + 1) * 128], C.wbuf[slot][:, kc, :],
                    start=(kc == 0), stop=(kc == KC_D - 1)),
                    reads=[("w", slot), ("hnT", kc)], writes=[("ps", b)])
            i = C.n_vst % 4
            C.n_vst += 1
            P.op("act" if tt % 2 else "dve",
                 (lambda e, i=i, b=b: e.activation(out=C.vst[i][:], in_=C.ps[b][:, 0:128], func=AF.Copy)) if tt % 2 else
                 (lambda e, i=i, b=b: e.tensor_copy(out=C.vst[i][:], in_=C.ps[b][:, 0:128])),
                 reads=[("ps", b)], writes=[("vst", i)])
            P.dma("sp", lambda e, i=i, tt=tt, cg=cg: e.dma_start(
                out=o["v_tm"][tt][:, cg * 128:(cg + 1) * 128], in_=C.vst[i][:]), reads=[("vst", i)],
                writes=_gk(C, "v0" if tt < 4 else "v1"))
    done("v0")
    done("v1")
    fm_chunks([(28 + j, "uk", j) for j in range(4)])
    done("uk")
    fm_chunks([(24 + j, "uq", j) for j in range(4)])
    done("uq")
    for tt in range(8):
        b = P.bank()
        for kc in range(KC_D):
            P.op("pe", lambda e, kc=kc, tt=tt, b=b: e.matmul(
                C.ps[b][:, 0:8], C.hnT[:, kc, tt * 128:(tt + 1) * 128], C.wg[:, kc, :],
                start=(kc == 0), stop=(kc == KC_D - 1)),
                reads=[("wg",), ("hnT", kc)], writes=[("ps", b)])
        P.op("dve", lambda e, tt=tt, b=b: e.tensor_copy(out=C.gst[:, tt, :], in_=C.ps[b][:, 0:8]),
             reads=[("ps", b)], writes=[("gst", tt)])
    P.dma("sp", lambda e: e.dma_start(out=o["gates"].rearrange("t p c -> p t c"), in_=C.gst[:]),
          reads=[("gst", tt) for tt in range(8)], writes=_gk(C, "gk"))
    P.dma("sp", lambda e: e.dma_start(out=o["kms"], in_=C.kms[:]),
          reads=[("kms", j, th) for j in range(4) for th in range(2)], writes=_gk(C, "gk"))
    done("gk")
    fm_chunks([(j, "qa", j) for j in range(4)] + [(12 + j, "qc", j) for j in range(4)] + [(40 + j, "om", j) for j in range(8)])


def outproj(C, mixT_dram, wout):
    P = C.P
    for kc in range(KC_D):
        P.dma("sp", lambda e, kc=kc: e.dma_start(out=C.hnT[:, kc, :], in_=mixT_dram[kc]), writes=[("hnT", kc)])
    for j in range(KC_D):
        slot = wload(C, wout[j], KC_D)
        for th in range(2):
            b = P.bank()
            sl = slice(th * 512, (th + 1) * 512)
            mm_fm(C, slot, KC_D, C.hnT, "hnT", 0, th, b)
            P.op("dve", lambda e, j=j, b=b, sl=sl: e.tensor_tensor(
                out=C.hT[:, j, sl], in0=C.ps[b][:], in1=C.hT[:, j, sl], op=ALU.add),
                reads=[("ps", b), ("hT", j)], writes=[("hT", j)])


def ple(C, gidx, wpg, wpp, pT_dram):
    P = C.P
    rmsnorm(C, gidx)
    for kc in range(2):
        P.dma("pool", lambda e, kc=kc: e.dma_start(out=C.actT[:, kc, :], in_=pT_dram[kc]), writes=[("actT", kc, 0), ("actT", kc, 1)])
    for j in range(KC_D):
        sg = wload(C, wpg[j], KC_D)
        sp_ = wload(C, wpp[j], 2)
        for th in range(2):
            bg, bp = P.bank(), P.bank()
            sl = slice(th * 512, (th + 1) * 512)
            mm_fm(C, sg, KC_D, C.hnT, "hnT", 0, th, bg)
            for kc in range(2):
                P.op("pe", lambda e, kc=kc, th=th, bp=bp, sp_=sp_: e.matmul(
                    C.ps[bp][:], C.wbuf[sp_][:, kc, :], C.actT[:, kc, th * 512:(th + 1) * 512],
                    start=(kc == 0), stop=(kc == 1)),
                    reads=[("w", sp_), ("actT", kc, th)], writes=[("ps", bp)])
            t = C.n_tmpf % len(C.tmpf)
            C.n_tmpf += 1
            P.op("act", lambda e, t=t, bg=bg: e.activation(out=C.tmpf[t][:], in_=C.ps[bg][:], func=AF.Sigmoid),
                 reads=[("ps", bg)], writes=[("tmpf", t)])
            P.op("dve", lambda e, t=t, bp=bp: e.tensor_tensor(
                out=C.tmpf[t][:], in0=C.ps[bp][:], in1=C.tmpf[t][:], op=ALU.mult),
                reads=[("ps", bp), ("tmpf", t)], writes=[("tmpf", t)])
            P.op("dve", lambda e, t=t, j=j, sl=sl: e.tensor_tensor(
                out=C.hT[:, j, sl], in0=C.hT[:, j, sl], in1=C.tmpf[t][:], op=ALU.add),
                reads=[("hT", j), ("tmpf", t)], writes=[("hT", j)])


def final_norm(C, gidx, out_dram):
    P = C.P

    def out_fn(kc):
        for th in range(2):
            sl = slice(th * 512, (th + 1) * 512)
            i = C.n_stf % len(C.stf)
            C.n_stf += 1
            P.op("dve", lambda e, kc=kc, sl=sl, i=i: e.scalar_tensor_tensor(
                out=C.stf[i][:], in0=C.hT[:, kc, sl], scalar=C.gvec[:, gidx, kc:kc + 1],
                in1=C.rstd[:, sl], op0=ALU.mult, op1=ALU.mult),
                reads=[("hT", kc), ("rstd", 0), ("rstd", 1), ("gvec", gidx)], writes=[("stf", i)])
            P.dma("sp", lambda e, kc=kc, sl=sl, i=i: e.dma_start(out=out_dram[kc][:, sl], in_=C.stf[i][:]),
                  reads=[("stf", i)])
    rmsnorm(C, gidx, out_fn=out_fn)


NEG = -30000.0
SLOPES_MOBA = [2.0 ** -1, 2.0 ** -3, 2.0 ** -5, 2.0 ** -7]
SLOPES_DIL = [2.0 ** -2, 2.0 ** -4, 2.0 ** -6, 2.0 ** -8]


def mixer_consts(is_b):
    kk = np.arange(128)[:, None]
    qq = np.arange(512)[None, :]

    def lnmult(dist):
        m = ((dist >= 0) & (dist <= 128)).astype(np.float64)
        m += ((dist >= 0) & (dist % 4 == 0) & (dist <= 512))
        m += ((dist >= 0) & (dist % 16 == 0) & (dist <= 2048))
        return np.where(m > 0, np.log(np.maximum(m, 1)), NEG).astype(np.float32)

    dil_loc = np.stack([lnmult(128 * d + qq - kk) for d in range(-3, 5)], axis=1)
    dil_rem = np.stack([lnmult(128 * d + qq - kk) for d in range(1, 6)], axis=1)
    if not is_b:
        dil_rem = np.full_like(dil_rem, NEG)
    cau = np.stack([np.where(128 * d + qq - kk >= 0, 0.0, NEG) for d in range(-3, 1)], axis=1).astype(np.float32)
    step = (cau == 0).astype(np.float32)
    pk = np.arange(2048)
    posK = np.stack([128.0 * (pk // 128), pk % 128, np.ones(2048), np.ones(2048)]).astype(np.float32)
    pq = 1024 + np.arange(1024)
    posQ = np.zeros((4, 8, 1024), np.float32)
    for i, s in enumerate(SLOPES_MOBA + SLOPES_DIL):
        posQ[0, i] = s
        posQ[1, i] = s
        posQ[2, i] = -s * 128.0 * (pq // 128)
        posQ[3, i] = -s * (pq % 128)
    E8 = np.zeros((8, 8 * 128), np.float32)
    for n in range(8):
        E8[n, n * 128:(n + 1) * 128] = 1.0
    G = np.zeros((1024, 8), np.float32)
    t = np.arange(1024)
    own = 4 + t // 256
    for n in range(8):
        G[:, n] = np.where(n == own, 1e30, np.where(n < own, 0.0, -1e30))
    if not is_b:
        G[:, 0:4] = -1e30
    G = np.ascontiguousarray(G.reshape(8, 128, 8).transpose(1, 0, 2))
    ident = np.eye(128, dtype=np.float32)
    U = np.triu(np.ones((128, 128), np.float32))
    rbias = np.full((128, 1), 0.0 if is_b else NEG, np.float32)
    return dict(dil_loc=dil_loc, dil_rem=dil_rem, cau=cau, step=step, posK=posK, posQ=posQ, E8=E8, G=G,
                ident=ident, U=U, rbias=rbias)


CONST_SHAPES = dict(dil_loc=[128, 8, 512], dil_rem=[128, 5, 512], cau=[128, 4, 512], step=[128, 4, 512],
                    posK=[4, 2048], posQ=[4, 8, 1024], E8=[8, 1024], G=[128, 8, 8], ident=[128, 128],
                    U=[128, 128], rbias=[128, 1])


def alloc_mixer(C):
    sb = C.sb
    C.m_dil_loc = sb("m_dil_loc", [128, 8, 512], F32)
    C.m_dil_rem = sb("m_dil_rem", [128, 5, 512], F32)
    C.m_cau = sb("m_cau", [128, 4, 512], F32)
    C.m_step = sb("m_step", [128, 4, 512], F32)
    C.m_posK = sb("m_posK", [4, 2048], BF16)
    C.m_posQ = sb("m_posQ", [4, 1024], BF16)
    C.m_E8 = sb("m_E8", [8, 1024], BF16)
    C.m_G = sb("m_G", [128, 8, 8], F32)
    C.m_ident = sb("m_ident", [128, 128], F32)
    C.m_U = sb("m_U", [128, 128], F32)
    C.m_rbias = sb("m_rbias", [128, 1], F32)
    C.m_onesf = sb("m_onesf", [128, 512], F32)
    C.m_cw = sb("m_cw", [128, 8, 4], F32)
    C.m_cb = sb("m_cb", [128, 8], F32)
    C.m_gb = sb("m_gb", [128, 8], F32)
    C.m_gh = sb("m_gh", [128, 8], F32)
    C.m_eps = sb("m_eps", [128, 1], F32)
    C.m_qT = sb("m_qT", [128, 1024], BF16)
    C.m_kT = sb("m_kT", [128, 2048], BF16)
    C.m_V = sb("m_V", [128, 16, 256], BF16)
    C.m_qf = sb("m_qf", [128, 1024], F32)
    C.m_kms = sb("m_kms", [128, 8], F32)
    C.m_gs = sb("m_gs", [128, 8], F32)
    C.m_m8 = sb("m_m8", [128, 8], F32)
    C.m_sel = sb("m_sel", [128, 8], F32)
    C.m_val = sb("m_val", [128, 8], F32)
    C.m_gs4 = [C.m_gs] + [sb("m_gs_%d" % i, [128, 8], F32) for i in range(3)]
    C.m_m84 = [C.m_m8] + [sb("m_m8_%d" % i, [128, 8], F32) for i in range(3)]
    C.m_sel4 = [C.m_sel] + [sb("m_sel_%d" % i, [128, 8], F32) for i in range(3)]
    C.m_val4 = [C.m_val] + [sb("m_val_%d" % i, [128, 8], F32) for i in range(3)]
    C.m_selbT = sb("m_selbT", [8, 1024], BF16)
    C.m_pT = [sb("m_pT%d" % i, [128, 512], BF16) for i in range(3)]
    C.m_tmp = [sb("m_tmp%d" % i, [128, 512], F32) for i in range(3)]
    C.m_rl = sb("m_rl", [128, 512], F32)
    C.m_ost = [sb("m_ost%d" % i, [128, 512], BF16) for i in range(2)]
    C.m_gl = sb("m_gl", [128, 16, 8], F32)
    C.m_lf = sb("m_lf", [128, 16, 4], F32)
    C.m_a = sb("m_a", [128, 16, 4], F32)
    C.m_Rrem = sb("m_Rrem", [128, 1], F32)
    C.m_lfrep = sb("m_lfrep", [128, 16, 128], F32)
    C.m_Fbc = sb("m_Fbc", [128, 1024], F32)
    C.m_uqb = sb("m_uqb", [128, 1027], F32)
    C.m_ukb = sb("m_ukb", [128, 2051], F32)
    C.m_acc = sb("m_acc", [128, 2048], F32)
    C.m_hm = [sb("m_hm%d" % i, [128, 512], F32) for i in range(2)]
    C.m_sqb = sb("m_sqb", [128, 512], BF16)
    C.m_omb = [sb("m_omb%d" % i, [128, 512], F32) for i in range(2)]
    C.m_onesb = sb("m_onesb", [128, 128], BF16)
    C.n_pT = 0
    C.n_tmp = 0
    C.n_ost = 0
    C.n_omb = 0
    C.m_tmpx = C.m_tmp
    C.m_pTx = C.m_pT + [sb("m_pT3", [128, 512], BF16)]


def load_mixer_consts(C, cd, conv_w, conv_b, gbias, ghead):
    P = C.P
    C.cd = cd
    for nm, q in (("dil_loc", "sp"), ("dil_rem", "sp"), ("cau", "sp"), ("step", "sp"), ("posK", "pool"),
                  ("E8", "pool"), ("G", "sp"), ("ident", "sp"), ("U", "sp"), ("rbias", "sp")):
        dst = getattr(C, "m_" + nm)
        P.dma(q, lambda e, dst=dst, nm=nm: e.dma_start(out=dst[:], in_=cd[nm]), writes=[("c_" + nm,)])
    P.dma("sp", lambda e: e.dma_start(out=C.m_cw[:], in_=conv_w), writes=[("c_cw",)])
    P.dma("sp", lambda e: e.dma_start(out=C.m_cb[:], in_=conv_b), writes=[("c_cb",)])
    P.dma("sp", lambda e: e.dma_start(out=C.m_gb[:], in_=gbias), writes=[("c_gb",)])
    P.dma("sp", lambda e: e.dma_start(out=C.m_gh[:], in_=ghead), writes=[("c_gh",)])
    P.op("dve", lambda e: e.memset(C.m_onesf[:], 1.0), writes=[("c_onesf",)])
    P.op("dve", lambda e: e.memset(C.m_onesb[:], 1.0), writes=[("c_onesb",)])
    P.op("dve", lambda e: e.memset(C.m_eps[:], 1e-6), writes=[("c_eps",)])
    P.op("dve", lambda e: e.memset(C.m_ukb[:, 0:3], 0.0), writes=[("ukb_pad",)])


def load_V(C, parts, tile0, c0, w, key):
    if not isinstance(parts, (list, tuple)):
        parts = [parts]
    t0 = tile0
    for ap in parts:
        n = ap.shape[0]
        C.P.dma("sp", lambda e, ap=ap, t0=t0, n=n: e.dma_start(
            out=C.m_V[:, t0:t0 + n, 0:w], in_=ap.rearrange("t p c -> p t c")[:, :, c0:c0 + w]), writes=[key])
        t0 += n


def _ktiles(qh):
    lst = [(kt, True, qh * 4 + 8 - kt) for kt in range(8)]
    lst += [(8 + lt, False, qh * 4 - lt) for lt in range(4 * (qh + 1))]
    return lst


def attn_group(C, kind, I, mixT):
    P = C.P
    moba = kind == "moba"
    if getattr(C, "fused", False):
        load_tabs(C, kind)
    qb, kL, kR = (I["qa_bf"], I["ka_bf_L"], I["ka_bf_R"]) if moba else (I["qc_bf"], I["kc_bf_L"], I["kc_bf_R"])
    vcol0 = 0 if moba else 512
    def head(h):
        hh = h if moba else 4 + h
        P.dma("pool", lambda e: e.dma_start(out=C.m_posQ[:], in_=C.cd["posQ"][:, hh, :]), writes=[("c_posQ",)])
        P.dma("sp", lambda e: e.dma_start(out=C.m_qT[:], in_=qb[h]), writes=[("m_qT",)])
        P.dma("sp", lambda e: e.dma_start(out=C.m_kT[:, 0:1024], in_=kR[h]), writes=[("m_kT", 0)])
        P.dma("sp", lambda e: e.dma_start(out=C.m_kT[:, 1024:2048], in_=kL[h]), writes=[("m_kT", 1)])
        c0 = vcol0 + h * 128
        load_V(C, I["v_tm_R"], 0, c0, 128, ("m_V", 0))
        load_V(C, I["v_tm_L"], 8, c0, 128, ("m_V", 1))
        if moba:
            P.dma("sp", lambda e: e.dma_start(out=C.m_qf[:], in_=I["qa_f"][h]), writes=[("m_qf",)])
            P.dma("sp", lambda e: e.dma_start(out=C.m_kms[:, 0:4], in_=I["kms_R"][:, h * 4:h * 4 + 4]), writes=[("m_kms", 0)])
            P.dma("sp", lambda e: e.dma_start(out=C.m_kms[:, 4:8], in_=I["kms_L"][:, h * 4:h * 4 + 4]), writes=[("m_kms", 1)])
            for qt in range(8):
                b = P.bank()
                g_ = qt % 4
                gs, m8, sel, val = C.m_gs4[g_], C.m_m84[g_], C.m_sel4[g_], C.m_val4[g_]
                kg = lambda n, g_=g_: (n, g_)
                P.op("pe", lambda e, qt=qt, b=b: e.matmul(C.ps[b][:, 0:8], C.m_qf[:, qt * 128:(qt + 1) * 128], C.m_kms[:],
                                                          start=True, stop=True),
                     reads=[("m_qf",), ("m_kms", 0), ("m_kms", 1)], writes=[("ps", b)])
                P.op("dve", lambda e, qt=qt, b=b, gs=gs: e.tensor_tensor(out=gs[:], in0=C.ps[b][:, 0:8], in1=C.m_G[:, qt, :], op=ALU.add),
                     reads=[("ps", b), ("c_G",)], writes=[kg("m_gs")])
                P.op("dve", lambda e, gs=gs, m8=m8: e.max(out=m8[:], in_=gs[:]), reads=[kg("m_gs")], writes=[kg("m_m8")])
                P.op("dve", lambda e, gs=gs, m8=m8, sel=sel: e.tensor_scalar(out=sel[:], in0=gs[:], scalar1=m8[:, 3:4], scalar2=None, op0=ALU.is_ge),
                     reads=[kg("m_gs"), kg("m_m8")], writes=[kg("m_sel")])
                P.op("dve", lambda e, gs=gs, val=val: e.tensor_scalar(out=val[:], in0=gs[:], scalar1=-1e29, scalar2=None, op0=ALU.is_gt),
                     reads=[kg("m_gs")], writes=[kg("m_val")])
                P.op("dve", lambda e, sel=sel, val=val: e.tensor_tensor(out=sel[:], in0=sel[:], in1=val[:], op=ALU.mult),
                     reads=[kg("m_sel"), kg("m_val")], writes=[kg("m_sel")])
                P.op("dve", lambda e, sel=sel: e.tensor_scalar(out=sel[:], in0=sel[:], scalar1=1.0, scalar2=-NEG,
                                                               op0=ALU.subtract, op1=ALU.mult),
                     reads=[kg("m_sel")], writes=[kg("m_sel")])
                b2 = P.bank()
                P.op("pe", lambda e, b2=b2, sel=sel: e.matmul(C.ps[b2][0:8, 0:128], sel[:], C.m_ident[:], start=True, stop=True),
                     reads=[kg("m_sel"), ("c_ident",)], writes=[("ps", b2)])
                P.op("act", lambda e, qt=qt, b2=b2: e.activation(out=C.m_selbT[:, qt * 128:(qt + 1) * 128], in_=C.ps[b2][0:8, 0:128], func=AF.Copy),
                     reads=[("ps", b2)], writes=[("m_selbT", qt)])

        def qhalf(qh):
            qsl = slice(qh * 512, (qh + 1) * 512)
            bo = P.bank(0, 4)
            bl = P.bank(0, 4)
            tiles = _ktiles(qh)
            def stage_S(ti):
                kt, rem, dl = tiles[ti]
                bs = P.bank(4, 8)
                ksl = slice(kt * 128, (kt + 1) * 128)
                P.op("pe", lambda e, bs=bs, ksl=ksl: e.matmul(C.ps[bs][:], C.m_kT[:, ksl], C.m_qT[:, qsl], start=True, stop=False),
                     reads=[("m_kT", 0 if rem else 1), ("m_qT",)], writes=[("ps", bs)])
                P.op("pe", lambda e, bs=bs, ksl=ksl: e.matmul(C.ps[bs][:], C.m_posK[:, ksl], C.m_posQ[:, qsl], start=False, stop=(not moba)),
                     reads=[("c_posK",), ("c_posQ",)], writes=[("ps", bs)])
                if moba:
                    n = kt // 2
                    P.op("pe", lambda e, bs=bs, n=n: e.matmul(C.ps[bs][:], C.m_E8[:, n * 128:(n + 1) * 128], C.m_selbT[:, qsl], start=False, stop=True),
                         reads=[("c_E8",)] + [("m_selbT", qh * 4 + i) for i in range(4)], writes=[("ps", bs)])
                tab = None
                if not moba:
                    tab = C.m_dil_rem[:, min(dl, 5) - 1, :] if rem else C.m_dil_loc[:, dl + 3, :]
                elif (not rem) and dl <= 0:
                    tab = C.m_cau[:, dl + 3, :]
                pi = C.n_pT % len(C.m_pT)
                C.n_pT += 1
                if tab is not None:
                    t = C.n_tmp % 3
                    C.n_tmp += 1
                    P.op("dve", lambda e, bs=bs, t=t, tab=tab: e.tensor_tensor(out=C.m_tmp[t][:], in0=C.ps[bs][:], in1=tab, op=ALU.add),
                         reads=[("ps", bs), ("c_dil_loc",), ("c_dil_rem",), ("c_cau",)], writes=[("m_tmp", t)])
                    P.op("act", lambda e, t=t, pi=pi: e.activation(out=C.m_pT[pi][:], in_=C.m_tmp[t][:], func=AF.Exp),
                         reads=[("m_tmp", t)], writes=[("m_pT", pi)])
                else:
                    P.op("act", lambda e, bs=bs, pi=pi: e.activation(out=C.m_pT[pi][:], in_=C.ps[bs][:], func=AF.Exp),
                         reads=[("ps", bs)], writes=[("m_pT", pi)])
                return pi

            def stage_V(ti, pi):
                kt, rem, dl = tiles[ti]
                first, last = ti == 0, ti == len(tiles) - 1
                P.op("pe", lambda e, kt=kt, pi=pi, first=first, last=last: e.matmul(
                    C.ps[bo][:], C.m_V[:, kt, 0:128], C.m_pT[pi][:], start=first, stop=last),
                    reads=[("m_V", 0 if rem else 1), ("m_pT", pi)], writes=[("ps", bo)])
                P.op("pe", lambda e, pi=pi, first=first, last=last: e.matmul(
                    C.ps[bl][:], C.m_onesb[:], C.m_pT[pi][:], start=first, stop=last),
                    reads=[("c_onesb",), ("m_pT", pi)], writes=[("ps", bl)])
            LA = 3 if len(C.m_pT) >= 4 else 2
            pis = {}
            for step in range(len(tiles) + LA):
                if step < len(tiles):
                    pis[step] = stage_S(step)
                if step - LA >= 0:
                    stage_V(step - LA, pis[step - LA])
            P.op("act", lambda e: e.activation(out=C.m_rl[:], in_=C.ps[bl][:], func=AF.Ln), reads=[("ps", bl)], writes=[("m_rl",)])
            P.op("act", lambda e: e.activation(out=C.m_rl[:], in_=C.m_rl[:], func=AF.Exp, scale=-1.0), reads=[("m_rl",)], writes=[("m_rl",)])
            oi = C.n_ost % 2
            C.n_ost += 1
            P.op("dve", lambda e, oi=oi: e.tensor_tensor(out=C.m_ost[oi][:], in0=C.ps[bo][:], in1=C.m_rl[:], op=ALU.mult),
                 reads=[("ps", bo), ("m_rl",)], writes=[("m_ost", oi)])
            ch = h if moba else 12 + h
            P.dma("sp", lambda e, oi=oi, ch=ch: e.dma_start(out=mixT[ch][:, qsl], in_=C.m_ost[oi][:]), reads=[("m_ost", oi)])
        for qh in range(2):
            qhalf(qh)
    for h in range(4):
        head(h)


DK_SCALE = 128.0 ** -0.5


def mlstm_group(C, I, mixT):
    P = C.P
    if getattr(C, "fused", False):
        load_tabs(C, "mlstm")
        l_ = C.m_layer
        cw_, cb_, gb_, gh_ = C.m_cw[:, l_], C.m_cb[:, l_], C.m_gb[:, l_], C.m_gh[:, l_]
    else:
        cw_, cb_, gb_, gh_ = C.m_cw, C.m_cb, C.m_gb, C.m_gh
    P.dma("sp", lambda e: e.dma_start(out=C.m_gl[:, 0:8, :], in_=I["gates_R"].rearrange("t p c -> p t c")), writes=[("m_gl", 0)])
    P.dma("sp", lambda e: e.dma_start(out=C.m_gl[:, 8:16, :], in_=I["gates_L"].rearrange("t p c -> p t c")), writes=[("m_gl", 1)])
    for t in range(16):
        P.op("dve", lambda e, t=t: e.tensor_tensor(out=C.m_gl[:, t, :], in0=C.m_gl[:, t, :], in1=gb_[:], op=ALU.add),
             reads=[("m_gl", 0), ("m_gl", 1), ("c_gb",)], writes=[("m_gl", 0), ("m_gl", 1)])
    P.op("act", lambda e: e.activation(out=C.m_lf[:], in_=C.m_gl[:, :, 4:8], func=AF.Exp, scale=-1.0),
         reads=[("m_gl", 0), ("m_gl", 1)], writes=[("m_lf",)])
    P.op("act", lambda e: e.activation(out=C.m_lf[:], in_=C.m_lf[:], func=AF.Ln, bias=C.m_onesf[:, 0:1], scale=1.0),
         reads=[("m_lf",), ("c_onesf",)], writes=[("m_lf",)])
    P.op("dve", lambda e: e.tensor_scalar(out=C.m_lf[:], in0=C.m_lf[:], scalar1=-1.0, scalar2=None, op0=ALU.mult),
         reads=[("m_lf",)], writes=[("m_lf",)])
    P.op("dve", lambda e: e.tensor_scalar(out=C.m_gl[:, 0:8, 0:4], in0=C.m_gl[:, 0:8, 0:4], scalar1=C.m_rbias[:, 0:1],
                                          scalar2=None, op0=ALU.add),
         reads=[("m_gl", 0), ("c_rbias",)], writes=[("m_gl", 0)])
    for i in range(16):
        b = P.bank()
        for r in range(i + 1):
            lhs = C.m_U if r == i else C.m_onesf
            P.op("pe", lambda e, r=r, b=b, lhs=lhs, i=i: e.matmul(C.ps[b][:, 0:4], lhs[:, 0:128], C.m_lf[:, r, :],
                                                             start=(r == 0), stop=(r == i)),
                 reads=[("m_lf",), ("c_U",), ("c_onesf",)], writes=[("ps", b)])
        P.op("dve", lambda e, i=i, b=b: e.tensor_tensor(out=C.m_a[:, i, :], in0=C.m_gl[:, i, 0:4], in1=C.ps[b][:, 0:4], op=ALU.subtract),
             reads=[("ps", b), ("m_gl", 0), ("m_gl", 1)], writes=[("m_a",)])
    def head(h):
        for t in range(8, 16):
            P.op("dve", lambda e, t=t: e.tensor_scalar(out=C.m_lfrep[:, t, :], in0=C.m_onesf[:, 0:128], scalar1=C.m_lf[:, t, h:h + 1],
                                                       scalar2=None, op0=ALU.mult),
                 reads=[("m_lf",), ("c_onesf",)], writes=[("m_lfrep",)])
        bR = P.bank()
        for t in range(8):
            P.op("pe", lambda e, t=t, bR=bR: e.matmul(C.ps[bR][:, 0:1], C.m_onesf[:, 0:128], C.m_lf[:, t, h:h + 1],
                                                      start=(t == 0), stop=(t == 7)),
                 reads=[("m_lf",), ("c_onesf",)], writes=[("ps", bR)])
        P.op("dve", lambda e, bR=bR: e.tensor_copy(out=C.m_Rrem[:], in_=C.ps[bR][:, 0:1]), reads=[("ps", bR)], writes=[("m_Rrem",)])
        for qh in range(2):
            b = P.bank()
            tl = [x for x in _ktiles(qh) if not x[1]]
            for ti, (kt, rem, dl) in enumerate(tl):
                rhs = C.m_onesf[:, :] if dl >= 1 else C.m_step[:, dl + 3, :]
                P.op("pe", lambda e, kt=kt, rhs=rhs, ti=ti, b=b, n=len(tl): e.matmul(
                    C.ps[b][:], C.m_lfrep[:, kt, :], rhs, start=(ti == 0), stop=(ti == n - 1)),
                    reads=[("m_lfrep",), ("c_onesf",), ("c_step",)], writes=[("ps", b)])
            P.op("dve", lambda e, b=b, qh=qh: e.tensor_scalar(out=C.m_Fbc[:, qh * 512:(qh + 1) * 512], in0=C.ps[b][:],
                                                              scalar1=C.m_Rrem[:, 0:1], scalar2=None, op0=ALU.add),
                 reads=[("ps", b), ("m_Rrem",)], writes=[("m_Fbc", qh)])
        P.dma("sp", lambda e: e.dma_start(out=C.m_uqb[:, 0:3], in_=I["uq_R"][h][:, 1021:1024]), writes=[("m_uqb", 0)])
        P.dma("sp", lambda e: e.dma_start(out=C.m_uqb[:, 3:1027], in_=I["uq_L"][h]), writes=[("m_uqb", 1)])
        P.dma("sp", lambda e: e.dma_start(out=C.m_ukb[:, 3:1027], in_=I["uk_R"][h]), writes=[("m_ukb", 0)])
        P.dma("sp", lambda e: e.dma_start(out=C.m_ukb[:, 1027:2051], in_=I["uk_L"][h]), writes=[("m_ukb", 1)])
        if getattr(C, "fused", False):
            P.op("dve", lambda e: e.tensor_scalar(out=C.m_uqb[:, 0:3], in0=C.m_uqb[:, 0:3], scalar1=C.m_flagB[:, 0:1], scalar2=None, op0=ALU.mult),
                 reads=[("m_uqb", 0), ("c_flagB",)], writes=[("m_uqb", 0)])
            P.op("dve", lambda e: e.tensor_scalar(out=C.m_ukb[:, 1024:1027], in0=C.m_ukb[:, 1024:1027], scalar1=C.m_flagB[:, 0:1], scalar2=None, op0=ALU.mult),
                 reads=[("m_ukb", 0), ("c_flagB",)], writes=[("m_ukb", 0)])
        for (src, skeys, n, c, dst, dkey, scale) in ((C.m_uqb, [("m_uqb", 0), ("m_uqb", 1)], 1024, h, C.m_qT, ("m_qT",), DK_SCALE),
                                                     (C.m_ukb, [("m_ukb", 0), ("m_ukb", 1), ("ukb_pad",)], 2048, 4 + h, C.m_kT, None, None)):
            P.op("dve", lambda e, src=src, n=n, c=c: e.tensor_scalar(out=C.m_acc[:, 0:n], in0=src[:, 3:3 + n], scalar1=cw_[:, c, 3:4],
                                                                 scalar2=None, op0=ALU.mult),
                 reads=skeys + [("c_cw",)], writes=[("m_acc",)])
            for j in (2, 1, 0):
                P.op("dve", lambda e, src=src, n=n, c=c, j=j: e.scalar_tensor_tensor(
                    out=C.m_acc[:, 0:n], in0=src[:, j:j + n], scalar=cw_[:, c, j:j + 1], in1=C.m_acc[:, 0:n],
                    op0=ALU.mult, op1=ALU.add),
                    reads=skeys + [("c_cw",), ("m_acc",)], writes=[("m_acc",)])
            if scale is None:
                P.op("act", lambda e, n=n, c=c, dst=dst: e.activation(out=dst[:, 0:n], in_=C.m_acc[:, 0:n], func=AF.Silu, bias=cb_[:, c:c + 1]),
                     reads=[("m_acc",), ("c_cb",)], writes=[("m_kT", 0), ("m_kT", 1)])
            else:
                P.op("act", lambda e, n=n, c=c: e.activation(out=C.m_acc[:, 0:n], in_=C.m_acc[:, 0:n], func=AF.Silu, bias=cb_[:, c:c + 1]),
                     reads=[("m_acc",), ("c_cb",)], writes=[("m_acc",)])
                P.op("dve", lambda e, n=n, dst=dst, scale=scale: e.tensor_scalar(out=dst[:, 0:n], in0=C.m_acc[:, 0:n], scalar1=scale, scalar2=None, op0=ALU.mult),
                     reads=[("m_acc",)], writes=[dkey])
        c0 = 1024 + h * 256
        load_V(C, I["v_tm_R"], 0, c0, 256, ("m_V", 0))
        load_V(C, I["v_tm_L"], 8, c0, 256, ("m_V", 1))

        def qhalf(qh):
            qsl = slice(qh * 512, (qh + 1) * 512)
            bn = [0, 1]
            bd = 2
            tiles = _ktiles(qh)
            def stage_S(ti):
                kt, rem, dl = tiles[ti]
                bs = P.bank(4, 8)
                ksl = slice(kt * 128, (kt + 1) * 128)
                P.op("pe", lambda e, bs=bs, ksl=ksl: e.matmul(C.ps[bs][:], C.m_kT[:, ksl], C.m_qT[:, qsl], start=True, stop=True),
                     reads=[("m_kT", 0 if rem else 1), ("m_qT",)], writes=[("ps", bs)])
                NT = len(C.m_tmpx)
                t = C.n_tmp % NT
                C.n_tmp += 1
                if (not rem) and dl <= 0:
                    t2 = C.n_tmp % NT
                    C.n_tmp += 1
                    P.op("dve", lambda e, t2=t2, dl=dl: e.tensor_tensor(out=C.m_tmpx[t2][:], in0=C.m_Fbc[:, qsl], in1=C.m_cau[:, dl + 3, :], op=ALU.add),
                         reads=[("m_Fbc", qh), ("c_cau",)], writes=[("m_tmp", t2)])
                    P.op("act", lambda e, t=t, t2=t2, kt=kt: e.activation(out=C.m_tmpx[t][:], in_=C.m_tmpx[t2][:], func=AF.Exp, bias=C.m_a[:, kt, h:h + 1]),
                         reads=[("m_tmp", t2), ("m_a",)], writes=[("m_tmp", t)])
                else:
                    P.op("act", lambda e, t=t, kt=kt: e.activation(out=C.m_tmpx[t][:], in_=C.m_Fbc[:, qsl], func=AF.Exp, bias=C.m_a[:, kt, h:h + 1]),
                         reads=[("m_Fbc", qh), ("m_a",)], writes=[("m_tmp", t)])
                pi = C.n_pT % len(C.m_pTx)
                C.n_pT += 1
                P.op("dve", lambda e, bs=bs, t=t, pi=pi: e.tensor_tensor(out=C.m_pTx[pi][:], in0=C.ps[bs][:], in1=C.m_tmpx[t][:], op=ALU.mult),
                     reads=[("ps", bs), ("m_tmp", t)], writes=[("m_pT", pi)])
                return pi

            def stage_V(ti, pi):
                kt, rem, dl = tiles[ti]
                first, last = ti == 0, ti == len(tiles) - 1
                for dvc in range(2):
                    P.op("pe", lambda e, kt=kt, pi=pi, dvc=dvc, first=first, last=last: e.matmul(
                        C.ps[bn[dvc]][:], C.m_V[:, kt, dvc * 128:(dvc + 1) * 128], C.m_pTx[pi][:], start=first, stop=last),
                        reads=[("m_V", 0 if rem else 1), ("m_pT", pi)], writes=[("ps", bn[dvc])])
                P.op("pe", lambda e, pi=pi, first=first, last=last: e.matmul(C.ps[bd][:], C.m_onesb[:], C.m_pTx[pi][:], start=first, stop=last),
                     reads=[("c_onesb",), ("m_pT", pi)], writes=[("ps", bd)])
            LA = 3
            pis = {}
            for step in range(len(tiles) + LA):
                if step < len(tiles):
                    pis[step] = stage_S(step)
                if step - LA >= 0:
                    stage_V(step - LA, pis[step - LA])
            P.op("act", lambda e: e.activation(out=C.m_rl[:], in_=C.ps[bd][:], func=AF.Abs),
                 reads=[("ps", bd)], writes=[("m_rl",)])
            P.op("dve", lambda e: e.tensor_scalar(out=C.m_rl[:], in0=C.m_rl[:], scalar1=1.0, scalar2=None, op0=ALU.max),
                 reads=[("m_rl",)], writes=[("m_rl",)])
            P.op("act", lambda e: e.activation(out=C.m_rl[:], in_=C.m_rl[:], func=AF.Ln), reads=[("m_rl",)], writes=[("m_rl",)])
            P.op("act", lambda e: e.activation(out=C.m_rl[:], in_=C.m_rl[:], func=AF.Exp, scale=-1.0), reads=[("m_rl",)], writes=[("m_rl",)])
            bq = 3
            for dvc in range(2):
                P.op("dve", lambda e, dvc=dvc: e.tensor_tensor(out=C.m_hm[dvc][:], in0=C.ps[bn[dvc]][:], in1=C.m_rl[:], op=ALU.mult),
                     reads=[("ps", bn[dvc]), ("m_rl",)], writes=[("m_hm", dvc)])
                P.op("act", lambda e, dvc=dvc: e.activation(out=C.m_sqb[:], in_=C.m_hm[dvc][:], func=AF.Square),
                     reads=[("m_hm", dvc)], writes=[("m_sqb",)])
                P.op("pe", lambda e, dvc=dvc: e.matmul(C.ps[bq][:], C.m_onesb[:], C.m_sqb[:], start=(dvc == 0), stop=(dvc == 1)),
                     reads=[("m_sqb",), ("c_onesb",)], writes=[("ps", bq)])
            P.op("act", lambda e: e.activation(out=C.m_rl[:], in_=C.ps[bq][:], func=AF.Ln, bias=C.m_eps[:, 0:1], scale=1.0 / 256),
                 reads=[("ps", bq), ("c_eps",), ("m_hm", 0), ("m_hm", 1)], writes=[("m_rl",)])
            P.op("act", lambda e: e.activation(out=C.m_rl[:], in_=C.m_rl[:], func=AF.Exp, scale=-0.5), reads=[("m_rl",)], writes=[("m_rl",)])
            for dvc in range(2):
                cc = 2 * h + dvc
                oi = C.n_omb % 2
                C.n_omb += 1
                P.dma("sp", lambda e, cc=cc, oi=oi: e.dma_start(out=C.m_omb[oi][:], in_=I["om"][cc][:, qsl]), writes=[("m_omb", oi)])
                P.op("dve", lambda e, dvc=dvc, cc=cc: e.scalar_tensor_tensor(
                    out=C.m_hm[dvc][:], in0=C.m_hm[dvc][:], scalar=gh_[:, cc:cc + 1], in1=C.m_rl[:], op0=ALU.mult, op1=ALU.mult),
                    reads=[("m_hm", dvc), ("c_gh",), ("m_rl",)], writes=[("m_hm", dvc)])
                si = C.n_ost % 2
                C.n_ost += 1
                P.op("dve", lambda e, dvc=dvc, oi=oi, si=si: e.tensor_tensor(out=C.m_ost[si][:], in0=C.m_hm[dvc][:], in1=C.m_omb[oi][:], op=ALU.mult),
                     reads=[("m_hm", dvc), ("m_omb", oi)], writes=[("m_ost", si)])
                P.dma("sp", lambda e, si=si, cc=cc: e.dma_start(out=mixT[4 + cc][:, qsl], in_=C.m_ost[si][:]), reads=[("m_ost", si)])
        for qh in range(2):
            qhalf(qh)
    for h in range(4):
        head(h)


from contextlib import ExitStack as _ES

Z_OUT = dict(qa_bf=([4, 128, 1024], BF16), qa_f=([4, 128, 1024], F32), ka_bf=([4, 128, 1024], BF16),
             kms=([128, 16], F32), qc_bf=([4, 128, 1024], BF16), kc_bf=([4, 128, 1024], BF16),
             uq=([4, 128, 1024], F32), uk=([4, 128, 1024], F32), om=([8, 128, 1024], F32),
             v_tm=([8, 128, 2048], BF16), gates=([8, 128, 8], F32))


def build_A():
    nc = bass.Bass("TRN2", target_bir_lowering=False)
    with _ES() as es:
        C = Ctx(nc, es)
        h_in = C.dram("h_in", [16, 128, 1024], F32, "ExternalInput")
        g1 = C.dram("g1", [128, 16], F32, "ExternalInput")
        gm = C.dram("gm", [128, 16], F32, "ExternalInput")
        wup = C.dram("wup", [88, 128, 16, 128], F32, "ExternalInput")
        wdown = C.dram("wdown", [16, 128, 44, 128], F32, "ExternalInput")
        win = C.dram("win", [48, 128, 16, 128], F32, "ExternalInput")
        win_g = C.dram("win_g", [128, 16, 8], F32, "ExternalInput")
        h_out = C.dram("h_out", [16, 128, 1024], F32, "ExternalOutput")
        o = {k: C.dram(k, sh, dt, "ExternalOutput") for k, (sh, dt) in Z_OUT.items()}
        alloc_token_local(C)
        alloc_inproj(C)
        load_gain(C, 0, g1)
        load_gain(C, 1, gm)
        load_h(C, h_in)
        ffn(C, 0, wup, wdown)
        store_h(C, h_out)
        inproj(C, 1, win, win_g, o)
        C.P.build(es)
    return nc


M_IN = dict(qa_bf=([4, 128, 1024], BF16), qa_f=([4, 128, 1024], F32), ka_bf_L=([4, 128, 1024], BF16),
            ka_bf_R=([4, 128, 1024], BF16), kms_L=([128, 16], F32), kms_R=([128, 16], F32),
            qc_bf=([4, 128, 1024], BF16), kc_bf_L=([4, 128, 1024], BF16), kc_bf_R=([4, 128, 1024], BF16),
            uq_L=([4, 128, 1024], F32), uq_R=([4, 128, 1024], F32), uk_L=([4, 128, 1024], F32),
            uk_R=([4, 128, 1024], F32), om=([8, 128, 1024], F32), v_tm_L=([8, 128, 2048], BF16),
            v_tm_R=([8, 128, 2048], BF16), gates_L=([8, 128, 8], F32), gates_R=([8, 128, 8], F32))


def build_M():
    nc = bass.Bass("TRN2", target_bir_lowering=False)
    with _ES() as es:
        C = Ctx(nc, es)
        I = {k: C.dram(k, sh, dt, "ExternalInput") for k, (sh, dt) in M_IN.items()}
        cd = {k: C.dram("c_" + k, sh, F32, "ExternalInput") for k, sh in CONST_SHAPES.items()}
        conv_w = C.dram("conv_w", [128, 8, 4], F32, "ExternalInput")
        conv_b = C.dram("conv_b", [128, 8], F32, "ExternalInput")
        gbias = C.dram("gbias", [128, 8], F32, "ExternalInput")
        ghead = C.dram("ghead", [128, 8], F32, "ExternalInput")
        mixT = C.dram("mixT", [16, 128, 1024], BF16, "ExternalOutput")
        alloc_mixer(C)
        load_mixer_consts(C, cd, conv_w, conv_b, gbias, ghead)
        attn_group(C, "moba", I, mixT)
        attn_group(C, "dil", I, mixT)
        mlstm_group(C, I, mixT)
        C.P.build(es)
    return nc


def build_C(final):
    nc = bass.Bass("TRN2", target_bir_lowering=False)
    with _ES() as es:
        C = Ctx(nc, es)
        h_in = C.dram("h_in", [16, 128, 1024], F32, "ExternalInput")
        mixT = C.dram("mixT", [16, 128, 1024], BF16, "ExternalInput")
        wout = C.dram("wout", [16, 128, 16, 128], F32, "ExternalInput")
        g2 = C.dram("g2", [128, 16], F32, "ExternalInput")
        gp = C.dram("gp", [128, 16], F32, "ExternalInput")
        gf = C.dram("gf", [128, 16], F32, "ExternalInput")
        wup = C.dram("wup", [88, 128, 16, 128], F32, "ExternalInput")
        wdown = C.dram("wdown", [16, 128, 44, 128], F32, "ExternalInput")
        wpg = C.dram("wpg", [16, 128, 16, 128], F32, "ExternalInput")
        wpp = C.dram("wpp", [16, 128, 2, 128], F32, "ExternalInput")
        pT = C.dram("pT", [2, 128, 1024], F32, "ExternalInput")
        h_out = C.dram("h_out", [16, 128, 1024], F32, "ExternalOutput")
        alloc_token_local(C)
        alloc_inproj(C)
        load_gain(C, 0, g2)
        load_gain(C, 1, gp)
        load_gain(C, 2, gf)
        load_h(C, h_in)
        outproj(C, mixT, wout)
        ffn(C, 0, wup, wdown)
        ple(C, 1, wpg, wpp, pT)
        if final:
            final_norm(C, 2, h_out)
        else:
            store_h(C, h_out)
        C.P.build(es)
    return nc


def tiles_fm(W):
    Kd, M = W.shape
    return np.ascontiguousarray(W.reshape(Kd // 128, 128, M // 128, 128).transpose(2, 1, 0, 3))


def vec_fm(g):
    return np.ascontiguousarray(np.asarray(g, np.float32).reshape(-1, 128).T)


def _run(nc, in_maps):
    res = run_bass_kernel_spmd(nc, in_maps, core_ids=list(range(8)))
    return res.results


def kernel_unfused(x, p, g_ffn1, w_up1, w_down1, g_mix, w_in, conv_w, conv_b, b_igate, b_fgate,
           g_head, w_out, g_ffn2, w_up2, w_down2, g_ple, w_ple_gate, w_ple_proj, g_final):
    f = lambda a: np.asarray(a, dtype=np.float32)
    x, p = f(x), f(p)
    ncA, ncM, ncC, ncF = build_A(), build_M(), build_C(False), build_C(True)
    consts = [mixer_consts(c % 2 == 1) for c in range(8)]
    hs = []
    for c in range(8):
        b, s = c // 2, c % 2
        hs.append(np.ascontiguousarray(x[b, s * 1024:(s + 1) * 1024, :].T.reshape(16, 128, 1024)))
    out = None
    for l in range(4):
        wi = f(w_in[l])
        a_in = dict(g1=vec_fm(g_ffn1[l]), gm=vec_fm(g_mix[l]), wup=tiles_fm(f(w_up1[l])), wdown=tiles_fm(f(w_down1[l])),
                    win=tiles_fm(wi[:, :6144]),
                    win_g=np.ascontiguousarray(wi[:, 6144:6152].reshape(16, 128, 8).transpose(1, 0, 2)))
        ra = _run(ncA, [dict(a_in, h_in=hs[c]) for c in range(8)])
        hs = [ra[c]["h_out"] for c in range(8)]
        cw = np.ascontiguousarray(f(conv_w[l]).reshape(4, 8, 128).transpose(2, 1, 0))
        m_common = dict(conv_w=cw, conv_b=vec_fm(conv_b[l]),
                        gbias=np.ascontiguousarray(np.broadcast_to(np.concatenate([f(b_igate[l]), f(b_fgate[l])])[None, :], (128, 8))),
                        ghead=vec_fm(g_head[l]))
        m_in = []
        for c in range(8):
            d = dict(m_common)
            for k, v in consts[c].items():
                d["c_" + k] = v
            me, other = ra[c], ra[c - 1] if c % 2 == 1 else None
            for k in ("qa_bf", "qa_f", "qc_bf", "om"):
                d[k] = me[k]
            for k in ("ka_bf", "kms", "kc_bf", "uq", "uk", "v_tm", "gates"):
                d[k + "_L"] = me[k]
                d[k + "_R"] = other[k] if other is not None else np.zeros_like(me[k])
            m_in.append(d)
        rm = _run(ncM, m_in)
        c_common = dict(wout=tiles_fm(f(w_out[l])), g2=vec_fm(g_ffn2[l]), gp=vec_fm(g_ple[l]), gf=vec_fm(g_final),
                        wup=tiles_fm(f(w_up2[l])), wdown=tiles_fm(f(w_down2[l])), wpg=tiles_fm(f(w_ple_gate[l])),
                        wpp=tiles_fm(f(w_ple_proj[l])))
        c_in = []
        for c in range(8):
            b, s = c // 2, c % 2
            pT = np.ascontiguousarray(p[l, b, s * 1024:(s + 1) * 1024, :].T.reshape(2, 128, 1024))
            c_in.append(dict(c_common, h_in=hs[c], mixT=rm[c]["mixT"], pT=pT))
        rc = _run(ncF if l == 3 else ncC, c_in)
        hs = [rc[c]["h_out"] for c in range(8)]
    out = np.empty((4, 2048, 2048), np.float32)
    for c in range(8):
        b, s = c // 2, c % 2
        out[b, s * 1024:(s + 1) * 1024, :] = hs[c].reshape(2048, 1024).T
    return out


ARENA_BYTES = 65536 + 106496
NL = 4


def alloc_fused(C):
    sb = C.sb
    C.fused = True
    arena = sb("arena", [128, ARENA_BYTES // 4], F32)

    def view(off, shape, dt, parts=128):
        n = int(np.prod(shape[1:]))
        nb = n * (4 if dt == F32 else 2)
        assert off % 4 == 0 and off + nb <= ARENA_BYTES, (off, nb)
        ap = arena[0:parts, off // 4:(off + nb + 3) // 4]
        if dt != F32:
            ap = ap.bitcast(dt)
        if len(shape) == 3:
            ap = ap.rearrange("p (a b) -> p a b", b=shape[2])
        return ap

    K = 1024
    C.hT = view(0, [128, KC_D, NTOK], F32)
    base = 65536
    C.hnT = view(base, [128, KC_D, NTOK], BF16)
    C.actT = view(base + 32 * K, [128, WSLOT_KC, NTOK], BF16)
    C.wbuf = [view(base + 76 * K + i * 5632, [128, WSLOT_KC, 128], BF16) for i in range(WSLOTS)]
    o = base
    C.m_tab = view(o, [128, 13, 512], F32); o += 26 * K
    C.m_dil_loc = C.m_tab[:, 0:8, :]
    C.m_dil_rem = C.m_tab[:, 8:13, :]
    C.m_cau = C.m_tab[:, 0:4, :]
    C.m_step = C.m_tab[:, 4:8, :]
    C.m_posQ = view(o, [4, 1024], BF16, parts=4); o += 2 * K
    C.m_qT = view(o, [128, 1024], BF16); o += 2 * K
    C.m_kT = view(o, [128, 2048], BF16); o += 4 * K
    C.m_V = view(o, [128, 16, 256], BF16); o += 8 * K
    C.m_qf = view(o, [128, 1024], F32); o += 4 * K
    C.m_selbT = view(o, [8, 1024], BF16, parts=8); o += 2 * K
    C.m_pT = []
    for i in range(3):
        C.m_pT.append(view(o, [128, 512], BF16)); o += K
    C.m_tmp = []
    for i in range(3):
        C.m_tmp.append(view(o, [128, 512], F32)); o += 2 * K
    C.m_tmpx = C.m_tmp + [C.m_tab[:, 8 + i, :] for i in range(4)]
    C.m_pT = C.m_pT + [sb("m_pT3", [128, 512], BF16)]
    C.m_pTx = C.m_pT
    C.m_rl = view(o, [128, 512], F32); o += 2 * K
    C.m_ost = []
    for i in range(2):
        C.m_ost.append(view(o, [128, 512], BF16)); o += K
    C.m_lfrep = view(o, [128, 16, 128], F32); o += 8 * K
    C.m_Fbc = view(o, [128, 1024], F32); o += 4 * K
    C.m_uqb = view(o, [128, 1027], F32); o += 5 * K
    C.m_ukb = view(o, [128, 2051], F32); o += 9 * K
    C.m_acc = view(o, [128, 2048], F32); o += 8 * K
    C.m_hm = []
    for i in range(2):
        C.m_hm.append(view(o, [128, 512], F32)); o += 2 * K
    C.m_sqb = view(o, [128, 512], BF16); o += K
    C.m_omb = []
    for i in range(2):
        C.m_omb.append(view(o, [128, 512], F32)); o += 2 * K
    assert o <= ARENA_BYTES, o
    C.sq = [sb("sq%d" % i, [128, NTOK], BF16) for i in range(2)]
    C.rstd = sb("rstd", [128, NTOK], F32)
    C.tmpf = [sb("tmpf%d" % i, [128, 512], F32) for i in range(2)]
    C.ones_bf = sb("ones_bf", [128, 128], BF16)
    C.epsv = sb("epsv", [128, 1], F32)
    C.gvec = sb("gvec", [128, 4 * NL + 1, KC_D], F32)
    C.stf = [sb("stf%d" % i, [128, 512], F32) for i in range(3)]
    C.stb = [sb("stb%d" % i, [128, 512], BF16) for i in range(4)]
    C.vst = [sb("vst%d" % i, [128, 128], BF16) for i in range(4)]
    C.kms = sb("kms_sb", [128, 16], F32)
    C.wg = sb("wg", [128, KC_D, 8], BF16)
    C.gst = sb("gst", [128, 8, 8], F32)
    C.m_posK = sb("m_posK", [4, 2048], BF16)
    C.m_E8 = sb("m_E8", [8, 1024], BF16)
    C.m_G = sb("m_G", [128, 8, 8], F32)
    C.m_ident = sb("m_ident", [128, 128], F32)
    C.m_U = sb("m_U", [128, 128], F32)
    C.m_rbias = sb("m_rbias", [128, 1], F32)
    C.m_flagB = sb("m_flagB", [128, 1], F32)
    C.m_onesf = sb("m_onesf", [128, 512], F32)
    C.m_cw = sb("m_cw", [128, NL, 8, 4], F32)
    C.m_cb = sb("m_cb", [128, NL, 8], F32)
    C.m_gb = sb("m_gb", [128, NL, 8], F32)
    C.m_gh = sb("m_gh", [128, NL, 8], F32)
    C.m_eps = C.epsv
    C.m_onesb = C.ones_bf
    C.m_kms = sb("m_kms", [128, 8], F32)
    C.m_gs = sb("m_gs", [128, 8], F32)
    C.m_m8 = sb("m_m8", [128, 8], F32)
    C.m_sel = sb("m_sel", [128, 8], F32)
    C.m_val = sb("m_val", [128, 8], F32)
    C.m_gs4 = [C.m_gs] + [sb("m_gs_%d" % i, [128, 8], F32) for i in range(3)]
    C.m_m84 = [C.m_m8] + [sb("m_m8_%d" % i, [128, 8], F32) for i in range(3)]
    C.m_sel4 = [C.m_sel] + [sb("m_sel_%d" % i, [128, 8], F32) for i in range(3)]
    C.m_val4 = [C.m_val] + [sb("m_val_%d" % i, [128, 8], F32) for i in range(3)]
    C.m_gl = sb("m_gl", [128, 16, 8], F32)
    C.m_lf = sb("m_lf", [128, 16, 4], F32)
    C.m_a = sb("m_a", [128, 16, 4], F32)
    C.m_Rrem = sb("m_Rrem", [128, 1], F32)
    for nm in ("n_sq", "n_tmpf", "n_stf", "n_stb", "n_vst", "n_pT", "n_tmp", "n_ost", "n_omb"):
        setattr(C, nm, 0)
    P = C.P
    P.op("dve", lambda e: e.memset(C.ones_bf[:], 1.0), writes=[("ones_bf",), ("c_onesb",)])
    P.op("dve", lambda e: e.memset(C.epsv[:], 1e-6), writes=[("epsv",), ("c_eps",)])
    P.op("dve", lambda e: e.memset(C.m_onesf[:], 1.0), writes=[("c_onesf",)])


def load_small_consts(C, cd, D_):
    P = C.P
    C.cd = cd
    for nm, q in (("posK", "pool"), ("E8", "pool"), ("G", "sp"), ("ident", "sp"), ("U", "sp"), ("rbias", "sp"), ("flagB", "sp")):
        dst = getattr(C, "m_" + nm)
        P.dma(q, lambda e, dst=dst, nm=nm: e.dma_start(out=dst[:], in_=cd[nm]), writes=[("c_" + nm,)])
    P.dma("sp", lambda e: e.dma_start(out=C.m_cw[:], in_=D_["conv_w"]), writes=[("c_cw",)])
    P.dma("sp", lambda e: e.dma_start(out=C.m_cb[:], in_=D_["conv_b"]), writes=[("c_cb",)])
    P.dma("sp", lambda e: e.dma_start(out=C.m_gb[:], in_=D_["gbias"]), writes=[("c_gb",)])
    P.dma("sp", lambda e: e.dma_start(out=C.m_gh[:], in_=D_["ghead"]), writes=[("c_gh",)])
    P.dma("sp", lambda e: e.dma_start(out=C.gvec[:], in_=D_["gains"]), writes=[("gvec", i) for i in range(4 * NL + 1)])


def load_tabs(C, kind):
    P = C.P
    if kind == "mlstm":
        P.op("dve", lambda e: e.memset(C.m_ukb[:, 0:3], 0.0), writes=[("ukb_pad",)])
        return
    P.barrier()
    if kind == "dil":
        P.dma("sp", lambda e: e.dma_start(out=C.m_tab[:, 0:8, :], in_=C.cd["dil_loc"]), writes=[("c_dil_loc",)])
        P.dma("sp", lambda e: e.dma_start(out=C.m_tab[:, 8:13, :], in_=C.cd["dil_rem"]), writes=[("c_dil_rem",)])
    else:
        P.dma("sp", lambda e: e.dma_start(out=C.m_tab[:, 0:4, :], in_=C.cd["cau"]), writes=[("c_cau",)])
        P.dma("sp", lambda e: e.dma_start(out=C.m_tab[:, 4:8, :], in_=C.cd["step"]), writes=[("c_step",)])


XB_ROWS = 3072
XF_ROWS = 1034
PAIRS = [[0, 1], [2, 3], [4, 5], [6, 7]]


def build_fused(nl=NL, phases="AXMC"):
    nc = bass.Bass("TRN2", target_bir_lowering=False)
    with _ES() as es:
        C = Ctx(nc, es)
        P = C.P
        ext = lambda n, sh: C.dram(n, sh, F32, "ExternalInput")
        x_in = ext("x_in", [16, 128, 1024])
        pT = ext("pT", [NL, 2, 128, 1024])
        W = dict(wup1=ext("wup1", [NL, 88, 128, 16, 128]), wdown1=ext("wdown1", [NL, 16, 128, 44, 128]),
                 win=ext("win", [NL, 48, 128, 16, 128]), win_g=ext("win_g", [NL, 128, 16, 8]),
                 wout=ext("wout", [NL, 16, 128, 16, 128]),
                 wup2=ext("wup2", [NL, 88, 128, 16, 128]), wdown2=ext("wdown2", [NL, 16, 128, 44, 128]),
                 wpg=ext("wpg", [NL, 16, 128, 16, 128]), wpp=ext("wpp", [NL, 16, 128, 2, 128]))
        D_ = dict(conv_w=ext("conv_w", [128, NL, 8, 4]), conv_b=ext("conv_b", [128, NL, 8]),
                  gbias=ext("gbias", [128, NL, 8]), ghead=ext("ghead", [128, NL, 8]),
                  gains=ext("gains", [128, 4 * NL + 1, 16]))
        shapes = dict(CONST_SHAPES, flagB=[128, 1])
        cd = {k: ext("c_" + k, sh) for k, sh in shapes.items()}
        out = C.dram("out", [16, 128, 1024], F32, "ExternalOutput")
        I32 = lambda n, sh: nc.dram_tensor(n, sh, F32, kind="Internal").ap()
        I16 = lambda n, sh: nc.dram_tensor(n, sh, BF16, kind="Internal").ap()
        exch = []
        exd = {}

        def mk(n, rows, mkfn):
            a, g = mkfn("x_" + n, [rows, 1024]), mkfn("g_" + n, [2 * rows, 1024])
            exch.append((a, g))
            exd[n] = (a, g)
            return a, g[0:rows, :]
        loc = dict(qa_bf=I16("z_qa_bf", [4, 128, 1024]), qa_f=I32("z_qa_f", [4, 128, 1024]),
                   qc_bf=I16("z_qc_bf", [4, 128, 1024]), om=I32("z_om", [8, 128, 1024]))
        mixT = I16("z_mixT", [16, 128, 1024])
        kk = mk("kk", 1024, I16)
        v0 = mk("v0", 1024, I16)
        v1 = mk("v1", 1024, I16)
        uk = mk("uk", 512, I32)
        uq = mk("uq", 512, I32)
        gk = mk("gk", 128, I32)
        ch = lambda t: t.rearrange("(j p) t -> j p t", p=128)
        vt = lambda t: t.rearrange("(t p a) b -> t p (a b)", t=4, p=128, a=2)
        Lv, Rv = {}, {}
        for i, dst in ((0, Lv), (1, Rv)):
            dst["ka_bf"] = ch(kk[i][0:512, :])
            dst["kc_bf"] = ch(kk[i][512:1024, :])
            dst["v_tm"] = [vt(v0[i]), vt(v1[i])]
            dst["uk"] = ch(uk[i])
            dst["uq"] = ch(uq[i])
            dst["gates"] = gk[i][0:8, :].rearrange("t (p c) -> t p c", c=8)
            dst["kms"] = gk[i][8:10, :].rearrange("a (p c) -> (a p) c", c=16)
        o_A = dict(loc, **Lv)
        v8 = [Lv["v_tm"][0][t] for t in range(4)] + [Lv["v_tm"][1][t] for t in range(4)]
        o_A["v_tm"] = v8
        I_M = dict(loc)
        for k in ("ka_bf", "kms", "kc_bf", "uq", "uk", "v_tm", "gates"):
            I_M[k + "_L"] = Lv[k]
            I_M[k + "_R"] = Rv[k]

        alloc_fused(C)
        load_small_consts(C, cd, D_)
        load_h(C, x_in)
        for l in range(nl):
            if "A" in phases:
                ffn(C, 4 * l + 0, W["wup1"][l], W["wdown1"][l])
                def group_cb(g, keys):
                    if g != "gk" or "X" not in phases:
                        return
                    P.barrier()
                    for (a_, g_) in exch:
                        P.cc(lambda e, a_=a_, g_=g_: e.collective_compute("AllGather", ALU.bypass, replica_groups=PAIRS,
                                                                          ins=[a_.opt()], outs=[g_.opt()]))
                inproj(C, 4 * l + 1, W["win"][l], W["win_g"][l], o_A, group_cb)
            P.barrier()
            C.m_layer = l
            if "M" in phases:
                attn_group(C, "moba", I_M, mixT)
                mlstm_group(C, I_M, mixT)
                attn_group(C, "dil", I_M, mixT)
            P.barrier()
            if "C" in phases:
                outproj(C, mixT, W["wout"][l])
                ffn(C, 4 * l + 2, W["wup2"][l], W["wdown2"][l])
                ple(C, 4 * l + 3, W["wpg"][l], W["wpp"][l], pT[l])
            P.bar = []
        final_norm(C, 4 * NL, out)
        C.P.build(es)
    return nc


def kernel(x, p, g_ffn1, w_up1, w_down1, g_mix, w_in, conv_w, conv_b, b_igate, b_fgate,
           g_head, w_out, g_ffn2, w_up2, w_down2, g_ple, w_ple_gate, w_ple_proj, g_final):
    f = lambda a: np.asarray(a, dtype=np.float32)
    x, p = f(x), f(p)
    nc = build_fused()
    st = lambda w, fn=tiles_fm: np.stack([fn(f(w[l])) for l in range(NL)])
    wi = f(w_in)
    gains = np.stack([vec_fm(g[l]) for l in range(NL) for g in (g_ffn1, g_mix, g_ffn2, g_ple)] + [vec_fm(g_final)], axis=1)
    common = dict(
        wup1=st(w_up1), wdown1=st(w_down1), win=np.stack([tiles_fm(wi[l][:, :6144]) for l in range(NL)]),
        win_g=np.stack([np.ascontiguousarray(wi[l][:, 6144:6152].reshape(16, 128, 8).transpose(1, 0, 2)) for l in range(NL)]),
        wout=st(w_out), wup2=st(w_up2), wdown2=st(w_down2), wpg=st(w_ple_gate), wpp=st(w_ple_proj),
        conv_w=np.ascontiguousarray(np.stack([f(conv_w[l]).reshape(4, 8, 128).transpose(2, 1, 0) for l in range(NL)], axis=1)),
        conv_b=np.ascontiguousarray(np.stack([vec_fm(conv_b[l]) for l in range(NL)], axis=1)),
        gbias=np.ascontiguousarray(np.stack([np.broadcast_to(np.concatenate([f(b_igate[l]), f(b_fgate[l])])[None, :], (128, 8))
                                             for l in range(NL)], axis=1)),
        ghead=np.ascontiguousarray(np.stack([vec_fm(g_head[l]) for l in range(NL)], axis=1)),
        gains=np.ascontiguousarray(gains))
    in_maps = []
    for c in range(8):
        b, s_ = c // 2, c % 2
        d = dict(common)
        d["x_in"] = np.ascontiguousarray(x[b, s_ * 1024:(s_ + 1) * 1024, :].T.reshape(16, 128, 1024))
        d["pT"] = np.ascontiguousarray(np.stack([p[l, b, s_ * 1024:(s_ + 1) * 1024, :].T.reshape(2, 128, 1024) for l in range(NL)]))
        for k, v in mixer_consts(s_ == 1).items():
            d["c_" + k] = v
        d["c_flagB"] = np.full((128, 1), float(s_), np.float32)
        in_maps.append(d)
    res = run_bass_kernel_spmd(nc, in_maps, core_ids=list(range(8))).results
    out = np.empty((4, 2048, 2048), np.float32)
    for c in range(8):
        b, s_ = c // 2, c % 2
        out[b, s_ * 1024:(s_ + 1) * 1024, :] = res[c]["out"].reshape(2048, 1024).T
    return out
```

```python
from contextlib import ExitStack

COMPUTE = ("pe", "act", "dve", "pool")
EPOCH = 30000
NDMA_SLOTS = 24


class Prog:
    def __init__(self, nc):
        self.nc = nc
        self.ops = []
        self.last_w = {}
        self.readers = {}
        self.cnt = {e: 0 for e in COMPUTE}
        self.epoch = {e: 0 for e in COMPUTE}
        self.dma_n = {"sp": 0, "pool": 0, "act": 0}
        self.dma_val = {}
        self.sem_ids = set()
        self._bank = 0

    def barrier(self):
        fin = {}
        for (_e, _f, _d, tok, _i) in self.ops:
            sid = tok[0]
            if sid[0] in COMPUTE:
                k = sid[0]
                if k not in fin or (fin[k][0][1], fin[k][1]) < (sid[1], tok[1]):
                    fin[k] = tok
            else:
                if sid not in fin or fin[sid][1] < tok[1]:
                    fin[sid] = tok
        self.bar = list(fin.values())

    def _deps(self, reads, writes):
        deps = list(getattr(self, "bar", ()))
        for r in reads:
            t = self.last_w.get(r)
            if t is not None:
                deps.append(t)
            if r[0] == "ps":
                deps.extend(self.readers.get(r, ()))
        for w in writes:
            t = self.last_w.get(w)
            if t is not None:
                deps.append(t)
            deps.extend(self.readers.get(w, ()))
        return deps

    def _commit(self, tok, reads, writes):
        for r in reads:
            self.readers.setdefault(r, []).append(tok)
        for w in writes:
            self.last_w[w] = tok
            self.readers[w] = []

    def op(self, eng, fn, reads=(), writes=()):
        deps = self._deps(reads, writes)
        if self.cnt[eng] >= EPOCH:
            self.epoch[eng] += 1
            self.cnt[eng] = 0
        self.cnt[eng] += 1
        sid = (eng, self.epoch[eng])
        self.sem_ids.add(sid)
        tok = (sid, self.cnt[eng], eng)
        self.ops.append((eng, fn, deps, tok, 1))
        self._commit(tok, reads, writes)
        return tok

    def dma(self, q, fn, reads=(), writes=()):
        deps = self._deps(reads, writes)
        slot = self.dma_n[q] % NDMA_SLOTS
        self.dma_n[q] += 1
        sid = ("dma", q, slot)
        self.sem_ids.add(sid)
        prev = self.dma_val.get(sid, 0)
        if prev:
            deps.append((sid, prev, "dma"))
        val = prev + 16
        self.dma_val[sid] = val
        tok = (sid, val, "dma")
        self.ops.append((q, fn, deps, tok, 16))
        self._commit(tok, reads, writes)
        return tok

    def cc(self, fn, reads=(), writes=()):
        deps = self._deps(reads, writes)
        n = self.__dict__.setdefault("_ncc", 0)
        self._ncc = n + 1
        sid = ("cc", n % 8)
        self.sem_ids.add(sid)
        prev = self.dma_val.get(sid, 0)
        if prev:
            deps.append((sid, prev, "dma"))
        val = prev + 1
        self.dma_val[sid] = val
        tok = (sid, val, "dma")
        self.ops.append(("pool", fn, deps, tok, 1))
        self._commit(tok, reads, writes)
        return tok

    def bank(self, lo=0, hi=8):
        if (lo, hi) == (0, 8):
            b = self._bank
            self._bank = (b + 1) % 8
            return b
        st = self.__dict__.setdefault("_rb", {})
        i = st.get((lo, hi), 0)
        st[(lo, hi)] = i + 1
        return lo + i % (hi - lo)

    def build(self, es: ExitStack):
        nc = self.nc
        sems = {}
        for sid in sorted(self.sem_ids, key=str):
            nm = "s_" + "_".join(str(x) for x in sid)
            sems[sid] = es.enter_context(nc.semaphore(nm))
        final = {}
        for (_e, _f, _d, tok, _i) in self.ops:
            final[tok[0]] = max(final.get(tok[0], 0), tok[1])
        ops = self.ops
        block = es.enter_context(nc.Block())

        def emit(eng_name, e):
            waited = {}
            for (en, fn, deps, tok, inc) in ops:
                if en != eng_name:
                    continue
                need = {}
                for (sid, val, deng) in deps:
                    if deng == "pe" and eng_name == "pe":
                        continue
                    if waited.get(sid, 0) >= val:
                        continue
                    if need.get(sid, 0) < val:
                        need[sid] = val
                for sid, val in need.items():
                    e.wait_ge(sems[sid], val)
                    waited[sid] = val
                ins = fn(e)
                ins.then_inc(sems[tok[0]], inc)
            if eng_name == "sp":
                for sid, val in final.items():
                    if waited.get(sid, 0) < val:
                        e.wait_ge(sems[sid], val)

        @block.tensor
        def _(e):
            emit("pe", e)

        @block.scalar
        def _(e):
            emit("act", e)

        @block.vector
        def _(e):
            emit("dve", e)

        @block.gpsimd
        def _(e):
            emit("pool", e)

        @block.sync
        def _(e):
            emit("sp", e)


import numpy as np
import concourse.bass as bass
import concourse.mybir as mybir
from concourse.bass_utils import run_bass_kernel_spmd

F32 = mybir.dt.float32
BF16 = mybir.dt.bfloat16
AF = mybir.ActivationFunctionType
ALU = mybir.AluOpType
AX = mybir.AxisListType

NTOK = 1024
D = 2048
KC_D = 16
DFF = 5632
NFF = 44
WSLOTS = 4
WSLOT_KC = 22


class Ctx:
    def __init__(self, nc, es):
        self.nc = nc
        self.es = es
        self.P = Prog(nc)
        self.n_w = 0
        self.ps = [es.enter_context(nc.psum_tensor("ps%d" % i, [128, 512], F32)) for i in range(8)]

    def sb(self, name, shape, dt):
        return self.es.enter_context(self.nc.sbuf_tensor(name, shape, dt))

    def dram(self, name, shape, dt, kind):
        return self.nc.dram_tensor(name, shape, dt, kind=kind).ap()


def alloc_token_local(C):
    C.hT = C.sb("hT", [128, KC_D, NTOK], F32)
    C.hnT = C.sb("hnT", [128, KC_D, NTOK], BF16)
    C.actT = C.sb("actT", [128, WSLOT_KC, NTOK], BF16)
    C.wbuf = [C.sb("wbuf%d" % i, [128, WSLOT_KC, 128], BF16) for i in range(WSLOTS)]
    C.sq = [C.sb("sq%d" % i, [128, NTOK], BF16) for i in range(2)]
    C.rstd = C.sb("rstd", [128, NTOK], F32)
    C.tmpf = [C.sb("tmpf%d" % i, [128, 512], F32) for i in range(3)]
    C.ones_bf = C.sb("ones_bf", [128, 128], BF16)
    C.gvec = C.sb("gvec", [128, 8, KC_D], F32)
    C.n_sq = 0
    C.n_tmpf = 0
    C.epsv = C.sb("epsv", [128, 1], F32)
    C.P.op("dve", lambda e: e.memset(C.ones_bf[:], 1.0), writes=[("ones_bf",)])
    C.P.op("dve", lambda e: e.memset(C.epsv[:], 1e-6), writes=[("epsv",)])


def load_gain(C, idx, g_dram):
    C.P.dma("sp", lambda e: e.dma_start(out=C.gvec[:, idx, :], in_=g_dram), writes=[("gvec", idx)])


def load_h(C, h_dram):
    for kc in range(KC_D):
        C.P.dma("sp", lambda e, kc=kc: e.dma_start(out=C.hT[:, kc, :], in_=h_dram[kc]),
                writes=[("hT", kc)])


def store_h(C, h_dram):
    for kc in range(KC_D):
        C.P.dma("sp", lambda e, kc=kc: e.dma_start(out=h_dram[kc], in_=C.hT[:, kc, :]),
                reads=[("hT", kc)])


def rmsnorm(C, gidx, out_fn=None):
    P = C.P
    banks = [P.bank(), P.bank()]
    for kc in range(KC_D):
        s = C.n_sq % 2
        C.n_sq += 1
        P.op("act", lambda e, kc=kc, s=s: e.activation(out=C.sq[s][:], in_=C.hT[:, kc, :], func=AF.Square),
             reads=[("hT", kc)], writes=[("sq", s)])
        for th in range(2):
            P.op("pe", lambda e, kc=kc, s=s, th=th: e.matmul(
                C.ps[banks[th]][:], C.ones_bf[:], C.sq[s][:, th * 512:(th + 1) * 512],
                start=(kc == 0), stop=(kc == KC_D - 1)),
                reads=[("sq", s), ("ones_bf",)], writes=[("ps", banks[th])])
    for th in range(2):
        sl = slice(th * 512, (th + 1) * 512)
        P.op("act", lambda e, th=th, sl=sl: e.activation(
            out=C.rstd[:, sl], in_=C.ps[banks[th]][:], func=AF.Ln, bias=C.epsv[:, 0:1], scale=1.0 / D),
            reads=[("ps", banks[th]), ("epsv",)], writes=[("rstd", th)])
        P.op("act", lambda e, sl=sl: e.activation(out=C.rstd[:, sl], in_=C.rstd[:, sl], func=AF.Exp, scale=-0.5),
            reads=[("rstd", th)], writes=[("rstd", th)])
    if out_fn is None:
        for th in range(2):
            sl = slice(th * 512, (th + 1) * 512)
            for kc in range(KC_D):
                P.op("dve", lambda e, kc=kc, sl=sl: e.scalar_tensor_tensor(
                    out=C.hnT[:, kc, sl], in0=C.hT[:, kc, sl], scalar=C.gvec[:, gidx, kc:kc + 1],
                    in1=C.rstd[:, sl], op0=ALU.mult, op1=ALU.mult),
                    reads=[("hT", kc), ("rstd", th), ("gvec", gidx)], writes=[("hnT", kc, th)])
    else:
        for kc in range(KC_D):
            out_fn(kc)


def wload(C, src_ap, kc_n):
    slot = C.n_w % WSLOTS
    C.n_w += 1
    C.P.dma("pool", lambda e: e.dma_start(out=C.wbuf[slot][:, 0:kc_n, :], in_=src_ap),
            writes=[("w", slot)])
    return slot


def mm_fm(C, slot, kc_n, x, xkey, x_kc0, th, bank):
    for kc in range(kc_n):
        C.P.op("pe", lambda e, kc=kc: e.matmul(
            C.ps[bank][:], C.wbuf[slot][:, kc, :], x[:, x_kc0 + kc, th * 512:(th + 1) * 512],
            start=(kc == 0), stop=(kc == kc_n - 1)),
            reads=[("w", slot), (xkey, x_kc0 + kc, th)], writes=[("ps", bank)])


def ffn(C, gidx, wup, wdown):
    P = C.P
    rmsnorm(C, gidx)
    for half in range(2):
        for cl in range(WSLOT_KC):
            c = half * WSLOT_KC + cl
            sg_ = wload(C, wup[c], KC_D)
            su_ = wload(C, wup[NFF + c], KC_D)
            for th in range(2):
                bg, bu = P.bank(), P.bank()
                mm_fm(C, sg_, KC_D, C.hnT, "hnT", 0, th, bg)
                mm_fm(C, su_, KC_D, C.hnT, "hnT", 0, th, bu)
                t = C.n_tmpf % len(C.tmpf)
                C.n_tmpf += 1
                P.op("act", lambda e, t=t, bg=bg: e.activation(out=C.tmpf[t][:], in_=C.ps[bg][:], func=AF.Silu),
                     reads=[("ps", bg)], writes=[("tmpf", t)])
                P.op("dve", lambda e, t=t, bu=bu, cl=cl, th=th: e.tensor_tensor(
                    out=C.actT[:, cl, th * 512:(th + 1) * 512], in0=C.ps[bu][:], in1=C.tmpf[t][:], op=ALU.mult),
                    reads=[("ps", bu), ("tmpf", t)], writes=[("actT", cl, th)])
        for j in range(KC_D):
            sd = wload(C, wdown[j][:, half * WSLOT_KC:(half + 1) * WSLOT_KC, :], WSLOT_KC)
            for th in range(2):
                b = P.bank()
                for kc in range(WSLOT_KC):
                    P.op("pe", lambda e, kc=kc, th=th, b=b, sd=sd: e.matmul(
                        C.ps[b][:], C.wbuf[sd][:, kc, :], C.actT[:, kc, th * 512:(th + 1) * 512],
                        start=(kc == 0), stop=(kc == WSLOT_KC - 1)),
                        reads=[("w", sd), ("actT", kc, th)], writes=[("ps", b)])
                sl = slice(th * 512, (th + 1) * 512)
                P.op("dve", lambda e, j=j, b=b, sl=sl: e.scalar_tensor_tensor(
                    out=C.hT[:, j, sl], in0=C.ps[b][:], scalar=0.5, in1=C.hT[:, j, sl],
                    op0=ALU.mult, op1=ALU.add),
                    reads=[("ps", b), ("hT", j)], writes=[("hT", j)])


HD_SCALE = 128.0 ** -0.5


def alloc_inproj(C):
    C.stf = [C.sb("stf%d" % i, [128, 512], F32) for i in range(4)]
    C.stb = [C.sb("stb%d" % i, [128, 512], BF16) for i in range(4)]
    C.n_stf = 0
    C.n_stb = 0
    C.kms = C.sb("kms_sb", [128, 16], F32)
    C.wg = C.sb("wg", [128, KC_D, 8], BF16)
    C.gst = C.sb("gst", [128, 8, 8], F32)
    C.vst = [C.sb("vst%d" % i, [128, 128], BF16) for i in range(4)]
    C.n_vst = 0


def _stage_out(C, kind, b, dst_ap, func=None, scale=1.0, eng="act", grp=None):
    P = C.P
    if kind == "f":
        i = C.n_stf % len(C.stf)
        C.n_stf += 1
        buf, key = C.stf[i], ("stf", i)
    else:
        i = C.n_stb % 4
        C.n_stb += 1
        buf, key = C.stb[i], ("stb", i)
    if eng == "act":
        P.op("act", lambda e: e.activation(out=buf[:], in_=C.ps[b][:], func=func or AF.Copy, scale=scale),
             reads=[("ps", b)], writes=[key])
    else:
        P.op("dve", lambda e: e.tensor_copy(out=buf[:], in_=C.ps[b][:]), reads=[("ps", b)], writes=[key])
    P.dma("sp", lambda e: e.dma_start(out=dst_ap, in_=buf[:]), reads=[key], writes=_gk(C, grp))


def _gk(C, grp):
    if grp is None:
        return []
    lst = C.grp_keys.setdefault(grp, [])
    k = ("xw", grp, len(lst), C.grp_epoch)
    lst.append(k)
    return [k]


def inproj(C, gidx, win, win_g, o, group_cb=None):
    P = C.P
    C.grp_keys = {}
    C.grp_epoch = getattr(C, "grp_epoch", 0) + 1

    def done(g):
        if group_cb is not None:
            group_cb(g, C.grp_keys.get(g, []))
    rmsnorm(C, gidx)
    P.dma("pool", lambda e: e.dma_start(out=C.wg[:], in_=win_g), writes=[("wg",)])

    def fm_chunks(lst):
        for (wj, kind, j) in lst:
            slot = wload(C, win[wj], KC_D)
            for th in range(2):
                b = P.bank()
                sl = slice(th * 512, (th + 1) * 512)
                mm_fm(C, slot, KC_D, C.hnT, "hnT", 0, th, b)
                if kind == "qa":
                    _stage_out(C, "b", b, o["qa_bf"][j][:, sl], scale=HD_SCALE)
                    _stage_out(C, "f", b, o["qa_f"][j][:, sl], eng="dve")
                elif kind == "ka":
                    _stage_out(C, "b", b, o["ka_bf"][j][:, sl], grp="kk")
                    P.op("dve", lambda e, j=j, th=th, b=b: e.tensor_reduce(
                        out=C.kms[:, j * 4 + th * 2:j * 4 + th * 2 + 2],
                        in_=C.ps[b][:].rearrange("p (a c) -> p a c", a=2), axis=AX.X, op=ALU.add),
                        reads=[("ps", b)], writes=[("kms", j, th)])
                elif kind == "qc":
                    _stage_out(C, "b", b, o["qc_bf"][j][:, sl], scale=HD_SCALE)
                elif kind == "kc":
                    _stage_out(C, "b", b, o["kc_bf"][j][:, sl], grp="kk")
                elif kind == "uq":
                    _stage_out(C, "f", b, o["uq"][j][:, sl], grp="uq")
                elif kind == "uk":
                    _stage_out(C, "f", b, o["uk"][j][:, sl], grp="uk")
                elif kind == "om":
                    _stage_out(C, "f", b, o["om"][j][:, sl], func=AF.Sigmoid)
    fm_chunks([(4 + j, "ka", j) for j in range(4)] + [(16 + j, "kc", j) for j in range(4)])
    done("kk")
    vt = [(8 + j, j) for j in range(4)] + [(20 + j, 4 + j) for j in range(4)] + [(32 + j, 8 + j) for j in range(8)]
    for (wj, cg) in vt:
        slot = wload(C, win[wj], KC_D)
        for tt in range(8):
            b = P.bank()
            for kc in range(KC_D):
                P.op("pe", lambda e, kc=kc, tt=tt, b=b, slot=slot: e.matmul(
                    C.ps[b][:, 0:128], C.hnT[:, kc, tt * 128:(tt + 1) * 128], C.wbuf[slot][:, kc, :],
                    start=(kc == 0), stop=(kc == KC_D - 1)),
                    reads=[("w", slot), ("hnT", kc, tt // 4)], writes=[("ps", b)])
            i = C.n_vst % 4
            C.n_vst += 1
            P.op("act" if tt % 2 else "dve",
                 (lambda e, i=i, b=b: e.activation(out=C.vst[i][:], in_=C.ps[b][:, 0:128], func=AF.Copy)) if tt % 2 else
                 (lambda e, i=i, b=b: e.tensor_copy(out=C.vst[i][:], in_=C.ps[b][:, 0:128])),
                 reads=[("ps", b)], writes=[("vst", i)])
            P.dma("sp", lambda e, i=i, tt=tt, cg=cg: e.dma_start(
                out=o["v_tm"][tt][:, cg * 128:(cg + 1) * 128], in_=C.vst[i][:]), reads=[("vst", i)],
                writes=_gk(C, "v0" if tt < 4 else "v1"))
    done("v0")
    done("v1")
    fm_chunks([(28 + j, "uk", j) for j in range(4)])
    done("uk")
    fm_chunks([(24 + j, "uq", j) for j in range(4)])
    done("uq")
    for tt in range(8):
        b = P.bank()
        for kc in range(KC_D):
            P.op("pe", lambda e, kc=kc, tt=tt, b=b: e.matmul(
                C.ps[b][:, 0:8], C.hnT[:, kc, tt * 128:(tt + 1) * 128], C.wg[:, kc, :],
                start=(kc == 0), stop=(kc == KC_D - 1)),
                reads=[("wg",), ("hnT", kc, tt // 4)], writes=[("ps", b)])
        P.op("dve", lambda e, tt=tt, b=b: e.tensor_copy(out=C.gst[:, tt, :], in_=C.ps[b][:, 0:8]),
             reads=[("ps", b)], writes=[("gst", tt)])
    P.dma("sp", lambda e: e.dma_start(out=o["gates"].rearrange("t p c -> p t c"), in_=C.gst[:]),
          reads=[("gst", tt) for tt in range(8)], writes=_gk(C, "gk"))
    P.dma("sp", lambda e: e.dma_start(out=o["kms"], in_=C.kms[:]),
          reads=[("kms", j, th) for j in range(4) for th in range(2)], writes=_gk(C, "gk"))
    done("gk")
    fm_chunks([(j, "qa", j) for j in range(4)] + [(12 + j, "qc", j) for j in range(4)] + [(40 + j, "om", j) for j in range(8)])


def outproj(C, mixT_dram, wout):
    P = C.P
    for kc in range(KC_D):
        P.dma("sp", lambda e, kc=kc: e.dma_start(out=C.hnT[:, kc, :], in_=mixT_dram[kc]), writes=[("hnT", kc, 0), ("hnT", kc, 1)])
    for j in range(KC_D):
        slot = wload(C, wout[j], KC_D)
        for th in range(2):
            b = P.bank()
            sl = slice(th * 512, (th + 1) * 512)
            mm_fm(C, slot, KC_D, C.hnT, "hnT", 0, th, b)
            P.op("dve", lambda e, j=j, b=b, sl=sl: e.tensor_tensor(
                out=C.hT[:, j, sl], in0=C.ps[b][:], in1=C.hT[:, j, sl], op=ALU.add),
                reads=[("ps", b), ("hT", j)], writes=[("hT", j)])


def ple(C, gidx, wpg, wpp, pT_dram):
    P = C.P
    rmsnorm(C, gidx)
    for kc in range(2):
        P.dma("pool", lambda e, kc=kc: e.dma_start(out=C.actT[:, kc, :], in_=pT_dram[kc]), writes=[("actT", kc, 0), ("actT", kc, 1)])
    for j in range(KC_D):
        sg = wload(C, wpg[j], KC_D)
        sp_ = wload(C, wpp[j], 2)
        for th in range(2):
            bg, bp = P.bank(), P.bank()
            sl = slice(th * 512, (th + 1) * 512)
            mm_fm(C, sg, KC_D, C.hnT, "hnT", 0, th, bg)
            for kc in range(2):
                P.op("pe", lambda e, kc=kc, th=th, bp=bp, sp_=sp_: e.matmul(
                    C.ps[bp][:], C.wbuf[sp_][:, kc, :], C.actT[:, kc, th * 512:(th + 1) * 512],
                    start=(kc == 0), stop=(kc == 1)),
                    reads=[("w", sp_), ("actT", kc, th)], writes=[("ps", bp)])
            t = C.n_tmpf % len(C.tmpf)
            C.n_tmpf += 1
            P.op("act", lambda e, t=t, bg=bg: e.activation(out=C.tmpf[t][:], in_=C.ps[bg][:], func=AF.Sigmoid),
                 reads=[("ps", bg)], writes=[("tmpf", t)])
            P.op("dve", lambda e, t=t, bp=bp: e.tensor_tensor(
                out=C.tmpf[t][:], in0=C.ps[bp][:], in1=C.tmpf[t][:], op=ALU.mult),
                reads=[("ps", bp), ("tmpf", t)], writes=[("tmpf", t)])
            P.op("dve", lambda e, t=t, j=j, sl=sl: e.tensor_tensor(
                out=C.hT[:, j, sl], in0=C.hT[:, j, sl], in1=C.tmpf[t][:], op=ALU.add),
                reads=[("hT", j), ("tmpf", t)], writes=[("hT", j)])


def final_norm(C, gidx, out_dram):
    P = C.P

    def out_fn(kc):
        for th in range(2):
            sl = slice(th * 512, (th + 1) * 512)
            i = C.n_stf % len(C.stf)
            C.n_stf += 1
            P.op("dve", lambda e, kc=kc, sl=sl, i=i: e.scalar_tensor_tensor(
                out=C.stf[i][:], in0=C.hT[:, kc, sl], scalar=C.gvec[:, gidx, kc:kc + 1],
                in1=C.rstd[:, sl], op0=ALU.mult, op1=ALU.mult),
                reads=[("hT", kc), ("rstd", 0), ("rstd", 1), ("gvec", gidx)], writes=[("stf", i)])
            P.dma("sp", lambda e, kc=kc, sl=sl, i=i: e.dma_start(out=out_dram[kc][:, sl], in_=C.stf[i][:]),
                  reads=[("stf", i)])
    rmsnorm(C, gidx, out_fn=out_fn)


NEG = -30000.0
SLOPES_MOBA = [2.0 ** -1, 2.0 ** -3, 2.0 ** -5, 2.0 ** -7]
SLOPES_DIL = [2.0 ** -2, 2.0 ** -4, 2.0 ** -6, 2.0 ** -8]


def mixer_consts(is_b):
    kk = np.arange(128)[:, None]
    qq = np.arange(512)[None, :]

    def lnmult(dist):
        m = ((dist >= 0) & (dist <= 128)).astype(np.float64)
        m += ((dist >= 0) & (dist % 4 == 0) & (dist <= 512))
        m += ((dist >= 0) & (dist % 16 == 0) & (dist <= 2048))
        return np.where(m > 0, np.log(np.maximum(m, 1)), NEG).astype(np.float32)

    dil_loc = np.stack([lnmult(128 * d + qq - kk) for d in range(-3, 5)], axis=1)
    dil_rem = np.stack([lnmult(128 * d + qq - kk) for d in range(1, 6)], axis=1)
    if not is_b:
        dil_rem = np.full_like(dil_rem, NEG)
    cau = np.stack([np.where(128 * d + qq - kk >= 0, 0.0, NEG) for d in range(-3, 1)], axis=1).astype(np.float32)
    step = (cau == 0).astype(np.float32)
    pk = np.arange(2048)
    posK = np.stack([128.0 * (pk // 128), pk % 128, np.ones(2048), np.ones(2048)]).astype(np.float32)
    pq = 1024 + np.arange(1024)
    posQ = np.zeros((4, 8, 1024), np.float32)
    for i, s in enumerate(SLOPES_MOBA + SLOPES_DIL):
        posQ[0, i] = s
        posQ[1, i] = s
        posQ[2, i] = -s * 128.0 * (pq // 128)
        posQ[3, i] = -s * (pq % 128)
    E8 = np.zeros((8, 8 * 128), np.float32)
    for n in range(8):
        E8[n, n * 128:(n + 1) * 128] = 1.0
    G = np.zeros((1024, 8), np.float32)
    t = np.arange(1024)
    own = 4 + t // 256
    for n in range(8):
        G[:, n] = np.where(n == own, 1e30, np.where(n < own, 0.0, -1e30))
    if not is_b:
        G[:, 0:4] = -1e30
    G = np.ascontiguousarray(G.reshape(8, 128, 8).transpose(1, 0, 2))
    ident = np.eye(128, dtype=np.float32)
    U = np.triu(np.ones((128, 128), np.float32))
    rbias = np.full((128, 1), 0.0 if is_b else NEG, np.float32)
    return dict(dil_loc=dil_loc, dil_rem=dil_rem, cau=cau, step=step, posK=posK, posQ=posQ, E8=E8, G=G,
                ident=ident, U=U, rbias=rbias)


CONST_SHAPES = dict(dil_loc=[128, 8, 512], dil_rem=[128, 5, 512], cau=[128, 4, 512], step=[128, 4, 512],
                    posK=[4, 2048], posQ=[4, 8, 1024], E8=[8, 1024], G=[128, 8, 8], ident=[128, 128],
                    U=[128, 128], rbias=[128, 1])


def alloc_mixer(C):
    sb = C.sb
    C.m_dil_loc = sb("m_dil_loc", [128, 8, 512], F32)
    C.m_dil_rem = sb("m_dil_rem", [128, 5, 512], F32)
    C.m_cau = sb("m_cau", [128, 4, 512], F32)
    C.m_step = sb("m_step", [128, 4, 512], F32)
    C.m_posK = sb("m_posK", [4, 2048], BF16)
    C.m_posQ = sb("m_posQ", [4, 1024], BF16)
    C.m_E8 = sb("m_E8", [8, 1024], BF16)
    C.m_G = sb("m_G", [128, 8, 8], F32)
    C.m_ident = sb("m_ident", [128, 128], F32)
    C.m_U = sb("m_U", [128, 128], F32)
    C.m_rbias = sb("m_rbias", [128, 1], F32)
    C.m_onesf = sb("m_onesf", [128, 512], F32)
    C.m_cw = sb("m_cw", [128, 8, 4], F32)
    C.m_cb = sb("m_cb", [128, 8], F32)
    C.m_gb = sb("m_gb", [128, 8], F32)
    C.m_gh = sb("m_gh", [128, 8], F32)
    C.m_eps = sb("m_eps", [128, 1], F32)
    C.m_qT = sb("m_qT", [128, 1024], BF16)
    C.m_kT = sb("m_kT", [128, 2048], BF16)
    C.m_V = sb("m_V", [128, 16, 256], BF16)
    C.m_qf = sb("m_qf", [128, 1024], F32)
    C.m_kms = sb("m_kms", [128, 8], F32)
    C.m_gs = sb("m_gs", [128, 8], F32)
    C.m_m8 = sb("m_m8", [128, 8], F32)
    C.m_sel = sb("m_sel", [128, 8], F32)
    C.m_val = sb("m_val", [128, 8], F32)
    C.m_gs4 = [C.m_gs] + [sb("m_gs_%d" % i, [128, 8], F32) for i in range(3)]
    C.m_m84 = [C.m_m8] + [sb("m_m8_%d" % i, [128, 8], F32) for i in range(3)]
    C.m_sel4 = [C.m_sel] + [sb("m_sel_%d" % i, [128, 8], F32) for i in range(3)]
    C.m_val4 = [C.m_val] + [sb("m_val_%d" % i, [128, 8], F32) for i in range(3)]
    C.m_selbT = sb("m_selbT", [8, 1024], BF16)
    C.m_pT = [sb("m_pT%d" % i, [128, 512], BF16) for i in range(3)]
    C.m_tmp = [sb("m_tmp%d" % i, [128, 512], F32) for i in range(3)]
    C.m_rl = sb("m_rl", [128, 512], F32)
    C.m_ost = [sb("m_ost%d" % i, [128, 512], BF16) for i in range(2)]
    C.m_gl = sb("m_gl", [128, 16, 8], F32)
    C.m_lf = sb("m_lf", [128, 16, 4], F32)
    C.m_a = sb("m_a", [128, 16, 4], F32)
    C.m_Rrem = sb("m_Rrem", [128, 1], F32)
    C.m_lfrep = sb("m_lfrep", [128, 16, 128], F32)
    C.m_Fbc = sb("m_Fbc", [128, 1024], F32)
    C.m_uqb = sb("m_uqb", [128, 1027], F32)
    C.m_ukb = sb("m_ukb", [128, 2051], F32)
    C.m_acc = sb("m_acc", [128, 2048], F32)
    C.m_hm = [sb("m_hm%d" % i, [128, 512], F32) for i in range(2)]
    C.m_sqb = sb("m_sqb", [128, 512], BF16)
    C.m_omb = [sb("m_omb%d" % i, [128, 512], F32) for i in range(2)]
    C.m_onesb = sb("m_onesb", [128, 128], BF16)
    C.n_pT = 0
    C.n_tmp = 0
    C.n_ost = 0
    C.n_omb = 0
    C.m_tmpx = C.m_tmp
    C.m_pTx = C.m_pT + [sb("m_pT3", [128, 512], BF16)]


def load_mixer_consts(C, cd, conv_w, conv_b, gbias, ghead):
    P = C.P
    C.cd = cd
    for nm, q in (("dil_loc", "sp"), ("dil_rem", "sp"), ("cau", "sp"), ("step", "sp"), ("posK", "pool"),
                  ("E8", "pool"), ("G", "sp"), ("ident", "sp"), ("U", "sp"), ("rbias", "sp")):
        dst = getattr(C, "m_" + nm)
        P.dma(q, lambda e, dst=dst, nm=nm: e.dma_start(out=dst[:], in_=cd[nm]), writes=[("c_" + nm,)])
    P.dma("sp", lambda e: e.dma_start(out=C.m_cw[:], in_=conv_w), writes=[("c_cw",)])
    P.dma("sp", lambda e: e.dma_start(out=C.m_cb[:], in_=conv_b), writes=[("c_cb",)])
    P.dma("sp", lambda e: e.dma_start(out=C.m_gb[:], in_=gbias), writes=[("c_gb",)])
    P.dma("sp", lambda e: e.dma_start(out=C.m_gh[:], in_=ghead), writes=[("c_gh",)])
    P.op("dve", lambda e: e.memset(C.m_onesf[:], 1.0), writes=[("c_onesf",)])
    P.op("dve", lambda e: e.memset(C.m_onesb[:], 1.0), writes=[("c_onesb",)])
    P.op("dve", lambda e: e.memset(C.m_eps[:], 1e-6), writes=[("c_eps",)])
    P.op("dve", lambda e: e.memset(C.m_ukb[:, 0:3], 0.0), writes=[("ukb_pad",)])


def load_V(C, parts, tile0, c0, w, key):
    if not isinstance(parts, (list, tuple)):
        parts = [parts]
    t0 = tile0
    for ap in parts:
        n = ap.shape[0]
        C.P.dma("sp", lambda e, ap=ap, t0=t0, n=n: e.dma_start(
            out=C.m_V[:, t0:t0 + n, 0:w], in_=ap.rearrange("t p c -> p t c")[:, :, c0:c0 + w]), writes=[key])
        t0 += n


def _ktiles(qh):
    lst = [(kt, True, qh * 4 + 8 - kt) for kt in range(8)]
    lst += [(8 + lt, False, qh * 4 - lt) for lt in range(4 * (qh + 1))]
    return lst


def attn_group(C, kind, I, mixT):
    P = C.P
    moba = kind == "moba"
    if getattr(C, "fused", False):
        load_tabs(C, kind)
    qb, kL, kR = (I["qa_bf"], I["ka_bf_L"], I["ka_bf_R"]) if moba else (I["qc_bf"], I["kc_bf_L"], I["kc_bf_R"])
    vcol0 = 0 if moba else 512
    def head(h):
        hh = h if moba else 4 + h
        P.dma("pool", lambda e: e.dma_start(out=C.m_posQ[:], in_=C.cd["posQ"][:, hh, :]), writes=[("c_posQ",)])
        P.dma("sp", lambda e: e.dma_start(out=C.m_qT[:], in_=qb[h]), writes=[("m_qT",)])
        P.dma("sp", lambda e: e.dma_start(out=C.m_kT[:, 0:1024], in_=kR[h]), writes=[("m_kT", 0)])
        P.dma("sp", lambda e: e.dma_start(out=C.m_kT[:, 1024:2048], in_=kL[h]), writes=[("m_kT", 1)])
        c0 = vcol0 + h * 128
        load_V(C, I["v_tm_R"], 0, c0, 128, ("m_V", 0))
        load_V(C, I["v_tm_L"], 8, c0, 128, ("m_V", 1))
        if moba:
            P.dma("sp", lambda e: e.dma_start(out=C.m_qf[:], in_=I["qa_f"][h]), writes=[("m_qf",)])
            P.dma("sp", lambda e: e.dma_start(out=C.m_kms[:, 0:4], in_=I["kms_R"][:, h * 4:h * 4 + 4]), writes=[("m_kms", 0)])
            P.dma("sp", lambda e: e.dma_start(out=C.m_kms[:, 4:8], in_=I["kms_L"][:, h * 4:h * 4 + 4]), writes=[("m_kms", 1)])
            for qt in range(8):
                b = P.bank()
                g_ = qt % 4
                gs, m8, sel, val = C.m_gs4[g_], C.m_m84[g_], C.m_sel4[g_], C.m_val4[g_]
                kg = lambda n, g_=g_: (n, g_)
                P.op("pe", lambda e, qt=qt, b=b: e.matmul(C.ps[b][:, 0:8], C.m_qf[:, qt * 128:(qt + 1) * 128], C.m_kms[:],
                                                          start=True, stop=True),
                     reads=[("m_qf",), ("m_kms", 0), ("m_kms", 1)], writes=[("ps", b)])
                P.op("dve", lambda e, qt=qt, b=b, gs=gs: e.tensor_tensor(out=gs[:], in0=C.ps[b][:, 0:8], in1=C.m_G[:, qt, :], op=ALU.add),
                     reads=[("ps", b), ("c_G",)], writes=[kg("m_gs")])
                P.op("dve", lambda e, gs=gs, m8=m8: e.max(out=m8[:], in_=gs[:]), reads=[kg("m_gs")], writes=[kg("m_m8")])
                P.op("dve", lambda e, gs=gs, m8=m8, sel=sel: e.tensor_scalar(out=sel[:], in0=gs[:], scalar1=m8[:, 3:4], scalar2=None, op0=ALU.is_ge),
                     reads=[kg("m_gs"), kg("m_m8")], writes=[kg("m_sel")])
                P.op("dve", lambda e, gs=gs, val=val: e.tensor_scalar(out=val[:], in0=gs[:], scalar1=-1e29, scalar2=None, op0=ALU.is_gt),
                     reads=[kg("m_gs")], writes=[kg("m_val")])
                P.op("dve", lambda e, sel=sel, val=val: e.tensor_tensor(out=sel[:], in0=sel[:], in1=val[:], op=ALU.mult),
                     reads=[kg("m_sel"), kg("m_val")], writes=[kg("m_sel")])
                P.op("dve", lambda e, sel=sel: e.tensor_scalar(out=sel[:], in0=sel[:], scalar1=1.0, scalar2=-NEG,
                                                               op0=ALU.subtract, op1=ALU.mult),
                     reads=[kg("m_sel")], writes=[kg("m_sel")])
                b2 = P.bank()
                P.op("pe", lambda e, b2=b2, sel=sel: e.matmul(C.ps[b2][0:8, 0:128], sel[:], C.m_ident[:], start=True, stop=True),
                     reads=[kg("m_sel"), ("c_ident",)], writes=[("ps", b2)])
                P.op("act", lambda e, qt=qt, b2=b2: e.activation(out=C.m_selbT[:, qt * 128:(qt + 1) * 128], in_=C.ps[b2][0:8, 0:128], func=AF.Copy),
                     reads=[("ps", b2)], writes=[("m_selbT", qt)])

        def qhalf(qh):
            qsl = slice(qh * 512, (qh + 1) * 512)
            bo = P.bank(0, 4)
            bl = P.bank(0, 4)
            tiles = _ktiles(qh)
            def stage_S(ti):
                kt, rem, dl = tiles[ti]
                bs = P.bank(4, 8)
                ksl = slice(kt * 128, (kt + 1) * 128)
                P.op("pe", lambda e, bs=bs, ksl=ksl: e.matmul(C.ps[bs][:], C.m_kT[:, ksl], C.m_qT[:, qsl], start=True, stop=False),
                     reads=[("m_kT", 0 if rem else 1), ("m_qT",)], writes=[("ps", bs)])
                P.op("pe", lambda e, bs=bs, ksl=ksl: e.matmul(C.ps[bs][:], C.m_posK[:, ksl], C.m_posQ[:, qsl], start=False, stop=(not moba)),
                     reads=[("c_posK",), ("c_posQ",)], writes=[("ps", bs)])
                if moba:
                    n = kt // 2
                    P.op("pe", lambda e, bs=bs, n=n: e.matmul(C.ps[bs][:], C.m_E8[:, n * 128:(n + 1) * 128], C.m_selbT[:, qsl], start=False, stop=True),
                         reads=[("c_E8",)] + [("m_selbT", qh * 4 + i) for i in range(4)], writes=[("ps", bs)])
                tab = None
                if not moba:
                    tab = C.m_dil_rem[:, min(dl, 5) - 1, :] if rem else C.m_dil_loc[:, dl + 3, :]
                elif (not rem) and dl <= 0:
                    tab = C.m_cau[:, dl + 3, :]
                pi = C.n_pT % len(C.m_pT)
                C.n_pT += 1
                if tab is not None:
                    t = C.n_tmp % 3
                    C.n_tmp += 1
                    P.op("dve", lambda e, bs=bs, t=t, tab=tab: e.tensor_tensor(out=C.m_tmp[t][:], in0=C.ps[bs][:], in1=tab, op=ALU.add),
                         reads=[("ps", bs), ("c_dil_loc",), ("c_dil_rem",), ("c_cau",)], writes=[("m_tmp", t)])
                    P.op("act", lambda e, t=t, pi=pi: e.activation(out=C.m_pT[pi][:], in_=C.m_tmp[t][:], func=AF.Exp),
                         reads=[("m_tmp", t)], writes=[("m_pT", pi)])
                else:
                    P.op("act", lambda e, bs=bs, pi=pi: e.activation(out=C.m_pT[pi][:], in_=C.ps[bs][:], func=AF.Exp),
                         reads=[("ps", bs)], writes=[("m_pT", pi)])
                return pi

            def stage_V(ti, pi):
                kt, rem, dl = tiles[ti]
                first, last = ti == 0, ti == len(tiles) - 1
                P.op("pe", lambda e, kt=kt, pi=pi, first=first, last=last: e.matmul(
                    C.ps[bo][:], C.m_V[:, kt, 0:128], C.m_pT[pi][:], start=first, stop=last),
                    reads=[("m_V", 0 if rem else 1), ("m_pT", pi)], writes=[("ps", bo)])
                P.op("pe", lambda e, pi=pi, first=first, last=last: e.matmul(
                    C.ps[bl][:], C.m_onesb[:], C.m_pT[pi][:], start=first, stop=last),
                    reads=[("c_onesb",), ("m_pT", pi)], writes=[("ps", bl)])
            LA = 3 if len(C.m_pT) >= 4 else 2
            pis = {}
            for step in range(len(tiles) + LA):
                if step < len(tiles):
                    pis[step] = stage_S(step)
                if step - LA >= 0:
                    stage_V(step - LA, pis[step - LA])
            P.op("act", lambda e: e.activation(out=C.m_rl[:], in_=C.ps[bl][:], func=AF.Ln), reads=[("ps", bl)], writes=[("m_rl",)])
            P.op("act", lambda e: e.activation(out=C.m_rl[:], in_=C.m_rl[:], func=AF.Exp, scale=-1.0), reads=[("m_rl",)], writes=[("m_rl",)])
            oi = C.n_ost % 2
            C.n_ost += 1
            P.op("dve", lambda e, oi=oi: e.tensor_tensor(out=C.m_ost[oi][:], in0=C.ps[bo][:], in1=C.m_rl[:], op=ALU.mult),
                 reads=[("ps", bo), ("m_rl",)], writes=[("m_ost", oi)])
            ch = h if moba else 12 + h
            P.dma("sp", lambda e, oi=oi, ch=ch: e.dma_start(out=mixT[ch][:, qsl], in_=C.m_ost[oi][:]), reads=[("m_ost", oi)])
        for qh in range(2):
            qhalf(qh)
    for h in range(4):
        head(h)


DK_SCALE = 128.0 ** -0.5


def mlstm_group(C, I, mixT):
    P = C.P
    if getattr(C, "fused", False):
        load_tabs(C, "mlstm")
        l_ = C.m_layer
        cw_, cb_, gb_, gh_ = C.m_cw[:, l_], C.m_cb[:, l_], C.m_gb[:, l_], C.m_gh[:, l_]
    else:
        cw_, cb_, gb_, gh_ = C.m_cw, C.m_cb, C.m_gb, C.m_gh
    P.dma("sp", lambda e: e.dma_start(out=C.m_gl[:, 0:8, :], in_=I["gates_R"].rearrange("t p c -> p t c")), writes=[("m_gl", 0)])
    P.dma("sp", lambda e: e.dma_start(out=C.m_gl[:, 8:16, :], in_=I["gates_L"].rearrange("t p c -> p t c")), writes=[("m_gl", 1)])
    for t in range(16):
        P.op("dve", lambda e, t=t: e.tensor_tensor(out=C.m_gl[:, t, :], in0=C.m_gl[:, t, :], in1=gb_[:], op=ALU.add),
             reads=[("m_gl", 0), ("m_gl", 1), ("c_gb",)], writes=[("m_gl", 0), ("m_gl", 1)])
    P.op("act", lambda e: e.activation(out=C.m_lf[:], in_=C.m_gl[:, :, 4:8], func=AF.Exp, scale=-1.0),
         reads=[("m_gl", 0), ("m_gl", 1)], writes=[("m_lf",)])
    P.op("act", lambda e: e.activation(out=C.m_lf[:], in_=C.m_lf[:], func=AF.Ln, bias=C.m_onesf[:, 0:1], scale=1.0),
         reads=[("m_lf",), ("c_onesf",)], writes=[("m_lf",)])
    P.op("dve", lambda e: e.tensor_scalar(out=C.m_lf[:], in0=C.m_lf[:], scalar1=-1.0, scalar2=None, op0=ALU.mult),
         reads=[("m_lf",)], writes=[("m_lf",)])
    P.op("dve", lambda e: e.tensor_scalar(out=C.m_gl[:, 0:8, 0:4], in0=C.m_gl[:, 0:8, 0:4], scalar1=C.m_rbias[:, 0:1],
                                          scalar2=None, op0=ALU.add),
         reads=[("m_gl", 0), ("c_rbias",)], writes=[("m_gl", 0)])
    for i in range(16):
        b = P.bank()
        for r in range(i + 1):
            lhs = C.m_U if r == i else C.m_onesf
            P.op("pe", lambda e, r=r, b=b, lhs=lhs, i=i: e.matmul(C.ps[b][:, 0:4], lhs[:, 0:128], C.m_lf[:, r, :],
                                                             start=(r == 0), stop=(r == i)),
                 reads=[("m_lf",), ("c_U",), ("c_onesf",)], writes=[("ps", b)])
        P.op("dve", lambda e, i=i, b=b: e.tensor_tensor(out=C.m_a[:, i, :], in0=C.m_gl[:, i, 0:4], in1=C.ps[b][:, 0:4], op=ALU.subtract),
             reads=[("ps", b), ("m_gl", 0), ("m_gl", 1)], writes=[("m_a",)])
    def head(h):
        for t in range(8, 16):
            P.op("dve", lambda e, t=t: e.tensor_scalar(out=C.m_lfrep[:, t, :], in0=C.m_onesf[:, 0:128], scalar1=C.m_lf[:, t, h:h + 1],
                                                       scalar2=None, op0=ALU.mult),
                 reads=[("m_lf",), ("c_onesf",)], writes=[("m_lfrep",)])
        bR = P.bank()
        for t in range(8):
            P.op("pe", lambda e, t=t, bR=bR: e.matmul(C.ps[bR][:, 0:1], C.m_onesf[:, 0:128], C.m_lf[:, t, h:h + 1],
                                                      start=(t == 0), stop=(t == 7)),
                 reads=[("m_lf",), ("c_onesf",)], writes=[("ps", bR)])
        P.op("dve", lambda e, bR=bR: e.tensor_copy(out=C.m_Rrem[:], in_=C.ps[bR][:, 0:1]), reads=[("ps", bR)], writes=[("m_Rrem",)])
        for qh in range(2):
            b = P.bank()
            tl = [x for x in _ktiles(qh) if not x[1]]
            for ti, (kt, rem, dl) in enumerate(tl):
                rhs = C.m_onesf[:, :] if dl >= 1 else C.m_step[:, dl + 3, :]
                P.op("pe", lambda e, kt=kt, rhs=rhs, ti=ti, b=b, n=len(tl): e.matmul(
                    C.ps[b][:], C.m_lfrep[:, kt, :], rhs, start=(ti == 0), stop=(ti == n - 1)),
                    reads=[("m_lfrep",), ("c_onesf",), ("c_step",)], writes=[("ps", b)])
            P.op("dve", lambda e, b=b, qh=qh: e.tensor_scalar(out=C.m_Fbc[:, qh * 512:(qh + 1) * 512], in0=C.ps[b][:],
                                                              scalar1=C.m_Rrem[:, 0:1], scalar2=None, op0=ALU.add),
                 reads=[("ps", b), ("m_Rrem",)], writes=[("m_Fbc", qh)])
        P.dma("sp", lambda e: e.dma_start(out=C.m_uqb[:, 0:3], in_=I["uq_R"][h][:, 1021:1024]), writes=[("m_uqb", 0)])
        P.dma("sp", lambda e: e.dma_start(out=C.m_uqb[:, 3:1027], in_=I["uq_L"][h]), writes=[("m_uqb", 1)])
        P.dma("sp", lambda e: e.dma_start(out=C.m_ukb[:, 3:1027], in_=I["uk_R"][h]), writes=[("m_ukb", 0)])
        P.dma("sp", lambda e: e.dma_start(out=C.m_ukb[:, 1027:2051], in_=I["uk_L"][h]), writes=[("m_ukb", 1)])
        if getattr(C, "fused", False):
            P.op("dve", lambda e: e.tensor_scalar(out=C.m_uqb[:, 0:3], in0=C.m_uqb[:, 0:3], scalar1=C.m_flagB[:, 0:1], scalar2=None, op0=ALU.mult),
                 reads=[("m_uqb", 0), ("c_flagB",)], writes=[("m_uqb", 0)])
            P.op("dve", lambda e: e.tensor_scalar(out=C.m_ukb[:, 1024:1027], in0=C.m_ukb[:, 1024:1027], scalar1=C.m_flagB[:, 0:1], scalar2=None, op0=ALU.mult),
                 reads=[("m_ukb", 0), ("c_flagB",)], writes=[("m_ukb", 0)])
        for (src, skeys, n, c, dst, dkey, scale) in ((C.m_uqb, [("m_uqb", 0), ("m_uqb", 1)], 1024, h, C.m_qT, ("m_qT",), DK_SCALE),
                                                     (C.m_ukb, [("m_ukb", 0), ("m_ukb", 1), ("ukb_pad",)], 2048, 4 + h, C.m_kT, None, None)):
            P.op("dve", lambda e, src=src, n=n, c=c: e.tensor_scalar(out=C.m_acc[:, 0:n], in0=src[:, 3:3 + n], scalar1=cw_[:, c, 3:4],
                                                                 scalar2=None, op0=ALU.mult),
                 reads=skeys + [("c_cw",)], writes=[("m_acc",)])
            for j in (2, 1, 0):
                P.op("dve", lambda e, src=src, n=n, c=c, j=j: e.scalar_tensor_tensor(
                    out=C.m_acc[:, 0:n], in0=src[:, j:j + n], scalar=cw_[:, c, j:j + 1], in1=C.m_acc[:, 0:n],
                    op0=ALU.mult, op1=ALU.add),
                    reads=skeys + [("c_cw",), ("m_acc",)], writes=[("m_acc",)])
            if scale is None:
                P.op("act", lambda e, n=n, c=c, dst=dst: e.activation(out=dst[:, 0:n], in_=C.m_acc[:, 0:n], func=AF.Silu, bias=cb_[:, c:c + 1]),
                     reads=[("m_acc",), ("c_cb",)], writes=[("m_kT", 0), ("m_kT", 1)])
            else:
                P.op("act", lambda e, n=n, c=c: e.activation(out=C.m_acc[:, 0:n], in_=C.m_acc[:, 0:n], func=AF.Silu, bias=cb_[:, c:c + 1]),
                     reads=[("m_acc",), ("c_cb",)], writes=[("m_acc",)])
                P.op("dve", lambda e, n=n, dst=dst, scale=scale: e.tensor_scalar(out=dst[:, 0:n], in0=C.m_acc[:, 0:n], scalar1=scale, scalar2=None, op0=ALU.mult),
                     reads=[("m_acc",)], writes=[dkey])
        c0 = 1024 + h * 256
        load_V(C, I["v_tm_R"], 0, c0, 256, ("m_V", 0))
        load_V(C, I["v_tm_L"], 8, c0, 256, ("m_V", 1))

        def qhalf(qh):
            qsl = slice(qh * 512, (qh + 1) * 512)
            bn = [0, 1]
            bd = 2
            tiles = _ktiles(qh)
            def stage_S(ti):
                kt, rem, dl = tiles[ti]
                bs = P.bank(4, 8)
                ksl = slice(kt * 128, (kt + 1) * 128)
                P.op("pe", lambda e, bs=bs, ksl=ksl: e.matmul(C.ps[bs][:], C.m_kT[:, ksl], C.m_qT[:, qsl], start=True, stop=True),
                     reads=[("m_kT", 0 if rem else 1), ("m_qT",)], writes=[("ps", bs)])
                NT = len(C.m_tmpx)
                t = C.n_tmp % NT
                C.n_tmp += 1
                if (not rem) and dl <= 0:
                    t2 = C.n_tmp % NT
                    C.n_tmp += 1
                    P.op("dve", lambda e, t2=t2, dl=dl: e.tensor_tensor(out=C.m_tmpx[t2][:], in0=C.m_Fbc[:, qsl], in1=C.m_cau[:, dl + 3, :], op=ALU.add),
                         reads=[("m_Fbc", qh), ("c_cau",)], writes=[("m_tmp", t2)])
                    P.op("act", lambda e, t=t, t2=t2, kt=kt: e.activation(out=C.m_tmpx[t][:], in_=C.m_tmpx[t2][:], func=AF.Exp, bias=C.m_a[:, kt, h:h + 1]),
                         reads=[("m_tmp", t2), ("m_a",)], writes=[("m_tmp", t)])
                else:
                    P.op("act", lambda e, t=t, kt=kt: e.activation(out=C.m_tmpx[t][:], in_=C.m_Fbc[:, qsl], func=AF.Exp, bias=C.m_a[:, kt, h:h + 1]),
                         reads=[("m_Fbc", qh), ("m_a",)], writes=[("m_tmp", t)])
                pi = C.n_pT % len(C.m_pTx)
                C.n_pT += 1
                P.op("dve", lambda e, bs=bs, t=t, pi=pi: e.tensor_tensor(out=C.m_pTx[pi][:], in0=C.ps[bs][:], in1=C.m_tmpx[t][:], op=ALU.mult),
                     reads=[("ps", bs), ("m_tmp", t)], writes=[("m_pT", pi)])
                return pi

            def stage_V(ti, pi):
                kt, rem, dl = tiles[ti]
                first, last = ti == 0, ti == len(tiles) - 1
                for dvc in range(2):
                    P.op("pe", lambda e, kt=kt, pi=pi, dvc=dvc, first=first, last=last: e.matmul(
                        C.ps[bn[dvc]][:], C.m_V[:, kt, dvc * 128:(dvc + 1) * 128], C.m_pTx[pi][:], start=first, stop=last),
                        reads=[("m_V", 0 if rem else 1), ("m_pT", pi)], writes=[("ps", bn[dvc])])
                P.op("pe", lambda e, pi=pi, first=first, last=last: e.matmul(C.ps[bd][:], C.m_onesb[:], C.m_pTx[pi][:], start=first, stop=last),
                     reads=[("c_onesb",), ("m_pT", pi)], writes=[("ps", bd)])
            LA = 3
            pis = {}
            for step in range(len(tiles) + LA):
                if step < len(tiles):
                    pis[step] = stage_S(step)
                if step - LA >= 0:
                    stage_V(step - LA, pis[step - LA])
            P.op("act", lambda e: e.activation(out=C.m_rl[:], in_=C.ps[bd][:], func=AF.Abs),
                 reads=[("ps", bd)], writes=[("m_rl",)])
            P.op("dve", lambda e: e.tensor_scalar(out=C.m_rl[:], in0=C.m_rl[:], scalar1=1.0, scalar2=None, op0=ALU.max),
                 reads=[("m_rl",)], writes=[("m_rl",)])
            P.op("act", lambda e: e.activation(out=C.m_rl[:], in_=C.m_rl[:], func=AF.Ln), reads=[("m_rl",)], writes=[("m_rl",)])
            P.op("act", lambda e: e.activation(out=C.m_rl[:], in_=C.m_rl[:], func=AF.Exp, scale=-1.0), reads=[("m_rl",)], writes=[("m_rl",)])
            bq = 3
            for dvc in range(2):
                P.op("dve", lambda e, dvc=dvc: e.tensor_tensor(out=C.m_hm[dvc][:], in0=C.ps[bn[dvc]][:], in1=C.m_rl[:], op=ALU.mult),
                     reads=[("ps", bn[dvc]), ("m_rl",)], writes=[("m_hm", dvc)])
                P.op("act", lambda e, dvc=dvc: e.activation(out=C.m_sqb[:], in_=C.m_hm[dvc][:], func=AF.Square),
                     reads=[("m_hm", dvc)], writes=[("m_sqb",)])
                P.op("pe", lambda e, dvc=dvc: e.matmul(C.ps[bq][:], C.m_onesb[:], C.m_sqb[:], start=(dvc == 0), stop=(dvc == 1)),
                     reads=[("m_sqb",), ("c_onesb",)], writes=[("ps", bq)])
            P.op("act", lambda e: e.activation(out=C.m_rl[:], in_=C.ps[bq][:], func=AF.Ln, bias=C.m_eps[:, 0:1], scale=1.0 / 256),
                 reads=[("ps", bq), ("c_eps",), ("m_hm", 0), ("m_hm", 1)], writes=[("m_rl",)])
            P.op("act", lambda e: e.activation(out=C.m_rl[:], in_=C.m_rl[:], func=AF.Exp, scale=-0.5), reads=[("m_rl",)], writes=[("m_rl",)])
            for dvc in range(2):
                cc = 2 * h + dvc
                oi = C.n_omb % 2
                C.n_omb += 1
                P.dma("sp", lambda e, cc=cc, oi=oi: e.dma_start(out=C.m_omb[oi][:], in_=I["om"][cc][:, qsl]), writes=[("m_omb", oi)])
                P.op("dve", lambda e, dvc=dvc, cc=cc: e.scalar_tensor_tensor(
                    out=C.m_hm[dvc][:], in0=C.m_hm[dvc][:], scalar=gh_[:, cc:cc + 1], in1=C.m_rl[:], op0=ALU.mult, op1=ALU.mult),
                    reads=[("m_hm", dvc), ("c_gh",), ("m_rl",)], writes=[("m_hm", dvc)])
                si = C.n_ost % 2
                C.n_ost += 1
                P.op("dve", lambda e, dvc=dvc, oi=oi, si=si: e.tensor_tensor(out=C.m_ost[si][:], in0=C.m_hm[dvc][:], in1=C.m_omb[oi][:], op=ALU.mult),
                     reads=[("m_hm", dvc), ("m_omb", oi)], writes=[("m_ost", si)])
                P.dma("sp", lambda e, si=si, cc=cc: e.dma_start(out=mixT[4 + cc][:, qsl], in_=C.m_ost[si][:]), reads=[("m_ost", si)])
        for qh in range(2):
            qhalf(qh)
    for h in range(4):
        head(h)


from contextlib import ExitStack as _ES

Z_OUT = dict(qa_bf=([4, 128, 1024], BF16), qa_f=([4, 128, 1024], F32), ka_bf=([4, 128, 1024], BF16),
             kms=([128, 16], F32), qc_bf=([4, 128, 1024], BF16), kc_bf=([4, 128, 1024], BF16),
             uq=([4, 128, 1024], F32), uk=([4, 128, 1024], F32), om=([8, 128, 1024], F32),
             v_tm=([8, 128, 2048], BF16), gates=([8, 128, 8], F32))


def build_A():
    nc = bass.Bass("TRN2", target_bir_lowering=False)
    with _ES() as es:
        C = Ctx(nc, es)
        h_in = C.dram("h_in", [16, 128, 1024], F32, "ExternalInput")
        g1 = C.dram("g1", [128, 16], F32, "ExternalInput")
        gm = C.dram("gm", [128, 16], F32, "ExternalInput")
        wup = C.dram("wup", [88, 128, 16, 128], F32, "ExternalInput")
        wdown = C.dram("wdown", [16, 128, 44, 128], F32, "ExternalInput")
        win = C.dram("win", [48, 128, 16, 128], F32, "ExternalInput")
        win_g = C.dram("win_g", [128, 16, 8], F32, "ExternalInput")
        h_out = C.dram("h_out", [16, 128, 1024], F32, "ExternalOutput")
        o = {k: C.dram(k, sh, dt, "ExternalOutput") for k, (sh, dt) in Z_OUT.items()}
        alloc_token_local(C)
        alloc_inproj(C)
        load_gain(C, 0, g1)
        load_gain(C, 1, gm)
        load_h(C, h_in)
        ffn(C, 0, wup, wdown)
        store_h(C, h_out)
        inproj(C, 1, win, win_g, o)
        C.P.build(es)
    return nc


M_IN = dict(qa_bf=([4, 128, 1024], BF16), qa_f=([4, 128, 1024], F32), ka_bf_L=([4, 128, 1024], BF16),
            ka_bf_R=([4, 128, 1024], BF16), kms_L=([128, 16], F32), kms_R=([128, 16], F32),
            qc_bf=([4, 128, 1024], BF16), kc_bf_L=([4, 128, 1024], BF16), kc_bf_R=([4, 128, 1024], BF16),
            uq_L=([4, 128, 1024], F32), uq_R=([4, 128, 1024], F32), uk_L=([4, 128, 1024], F32),
            uk_R=([4, 128, 1024], F32), om=([8, 128, 1024], F32), v_tm_L=([8, 128, 2048], BF16),
            v_tm_R=([8, 128, 2048], BF16), gates_L=([8, 128, 8], F32), gates_R=([8, 128, 8], F32))


def build_M():
    nc = bass.Bass("TRN2", target_bir_lowering=False)
    with _ES() as es:
        C = Ctx(nc, es)
        I = {k: C.dram(k, sh, dt, "ExternalInput") for k, (sh, dt) in M_IN.items()}
        cd = {k: C.dram("c_" + k, sh, F32, "ExternalInput") for k, sh in CONST_SHAPES.items()}
        conv_w = C.dram("conv_w", [128, 8, 4], F32, "ExternalInput")
        conv_b = C.dram("conv_b", [128, 8], F32, "ExternalInput")
        gbias = C.dram("gbias", [128, 8], F32, "ExternalInput")
        ghead = C.dram("ghead", [128, 8], F32, "ExternalInput")
        mixT = C.dram("mixT", [16, 128, 1024], BF16, "ExternalOutput")
        alloc_mixer(C)
        load_mixer_consts(C, cd, conv_w, conv_b, gbias, ghead)
        attn_group(C, "moba", I, mixT)
        attn_group(C, "dil", I, mixT)
        mlstm_group(C, I, mixT)
        C.P.build(es)
    return nc


def build_C(final):
    nc = bass.Bass("TRN2", target_bir_lowering=False)
    with _ES() as es:
        C = Ctx(nc, es)
        h_in = C.dram("h_in", [16, 128, 1024], F32, "ExternalInput")
        mixT = C.dram("mixT", [16, 128, 1024], BF16, "ExternalInput")
        wout = C.dram("wout", [16, 128, 16, 128], F32, "ExternalInput")
        g2 = C.dram("g2", [128, 16], F32, "ExternalInput")
        gp = C.dram("gp", [128, 16], F32, "ExternalInput")
        gf = C.dram("gf", [128, 16], F32, "ExternalInput")
        wup = C.dram("wup", [88, 128, 16, 128], F32, "ExternalInput")
        wdown = C.dram("wdown", [16, 128, 44, 128], F32, "ExternalInput")
        wpg = C.dram("wpg", [16, 128, 16, 128], F32, "ExternalInput")
        wpp = C.dram("wpp", [16, 128, 2, 128], F32, "ExternalInput")
        pT = C.dram("pT", [2, 128, 1024], F32, "ExternalInput")
        h_out = C.dram("h_out", [16, 128, 1024], F32, "ExternalOutput")
        alloc_token_local(C)
        alloc_inproj(C)
        load_gain(C, 0, g2)
        load_gain(C, 1, gp)
        load_gain(C, 2, gf)
        load_h(C, h_in)
        outproj(C, mixT, wout)
        ffn(C, 0, wup, wdown)
        ple(C, 1, wpg, wpp, pT)
        if final:
            final_norm(C, 2, h_out)
        else:
            store_h(C, h_out)
        C.P.build(es)
    return nc


def tiles_fm(W):
    Kd, M = W.shape
    return np.ascontiguousarray(W.reshape(Kd // 128, 128, M // 128, 128).transpose(2, 1, 0, 3))


def vec_fm(g):
    return np.ascontiguousarray(np.asarray(g, np.float32).reshape(-1, 128).T)


def _run(nc, in_maps):
    res = run_bass_kernel_spmd(nc, in_maps, core_ids=list(range(8)))
    return res.results


def kernel_unfused(x, p, g_ffn1, w_up1, w_down1, g_mix, w_in, conv_w, conv_b, b_igate, b_fgate,
           g_head, w_out, g_ffn2, w_up2, w_down2, g_ple, w_ple_gate, w_ple_proj, g_final):
    f = lambda a: np.asarray(a, dtype=np.float32)
    x, p = f(x), f(p)
    ncA, ncM, ncC, ncF = build_A(), build_M(), build_C(False), build_C(True)
    consts = [mixer_consts(c % 2 == 1) for c in range(8)]
    hs = []
    for c in range(8):
        b, s = c // 2, c % 2
        hs.append(np.ascontiguousarray(x[b, s * 1024:(s + 1) * 1024, :].T.reshape(16, 128, 1024)))
    out = None
    for l in range(4):
        wi = f(w_in[l])
        a_in = dict(g1=vec_fm(g_ffn1[l]), gm=vec_fm(g_mix[l]), wup=tiles_fm(f(w_up1[l])), wdown=tiles_fm(f(w_down1[l])),
                    win=tiles_fm(wi[:, :6144]),
                    win_g=np.ascontiguousarray(wi[:, 6144:6152].reshape(16, 128, 8).transpose(1, 0, 2)))
        ra = _run(ncA, [dict(a_in, h_in=hs[c]) for c in range(8)])
        hs = [ra[c]["h_out"] for c in range(8)]
        cw = np.ascontiguousarray(f(conv_w[l]).reshape(4, 8, 128).transpose(2, 1, 0))
        m_common = dict(conv_w=cw, conv_b=vec_fm(conv_b[l]),
                        gbias=np.ascontiguousarray(np.broadcast_to(np.concatenate([f(b_igate[l]), f(b_fgate[l])])[None, :], (128, 8))),
                        ghead=vec_fm(g_head[l]))
        m_in = []
        for c in range(8):
            d = dict(m_common)
            for k, v in consts[c].items():
                d["c_" + k] = v
            me, other = ra[c], ra[c - 1] if c % 2 == 1 else None
            for k in ("qa_bf", "qa_f", "qc_bf", "om"):
                d[k] = me[k]
            for k in ("ka_bf", "kms", "kc_bf", "uq", "uk", "v_tm", "gates"):
                d[k + "_L"] = me[k]
                d[k + "_R"] = other[k] if other is not None else np.zeros_like(me[k])
            m_in.append(d)
        rm = _run(ncM, m_in)
        c_common = dict(wout=tiles_fm(f(w_out[l])), g2=vec_fm(g_ffn2[l]), gp=vec_fm(g_ple[l]), gf=vec_fm(g_final),
                        wup=tiles_fm(f(w_up2[l])), wdown=tiles_fm(f(w_down2[l])), wpg=tiles_fm(f(w_ple_gate[l])),
                        wpp=tiles_fm(f(w_ple_proj[l])))
        c_in = []
        for c in range(8):
            b, s = c // 2, c % 2
            pT = np.ascontiguousarray(p[l, b, s * 1024:(s + 1) * 1024, :].T.reshape(2, 128, 1024))
            c_in.append(dict(c_common, h_in=hs[c], mixT=rm[c]["mixT"], pT=pT))
        rc = _run(ncF if l == 3 else ncC, c_in)
        hs = [rc[c]["h_out"] for c in range(8)]
    out = np.empty((4, 2048, 2048), np.float32)
    for c in range(8):
        b, s = c // 2, c % 2
        out[b, s * 1024:(s + 1) * 1024, :] = hs[c].reshape(2048, 1024).T
    return out


ARENA_BYTES = 65536 + 106496
NL = 4


def alloc_fused(C):
    sb = C.sb
    C.fused = True
    arena = sb("arena", [128, ARENA_BYTES // 4], F32)

    def view(off, shape, dt, parts=128):
        n = int(np.prod(shape[1:]))
        nb = n * (4 if dt == F32 else 2)
        assert off % 4 == 0 and off + nb <= ARENA_BYTES, (off, nb)
        ap = arena[0:parts, off // 4:(off + nb + 3) // 4]
        if dt != F32:
            ap = ap.bitcast(dt)
        if len(shape) == 3:
            ap = ap.rearrange("p (a b) -> p a b", b=shape[2])
        return ap

    K = 1024
    C.hT = view(0, [128, KC_D, NTOK], F32)
    base = 65536
    C.hnT = view(base, [128, KC_D, NTOK], BF16)
    C.actT = view(base + 32 * K, [128, WSLOT_KC, NTOK], BF16)
    C.wbuf = [view(base + 76 * K + i * 5632, [128, WSLOT_KC, 128], BF16) for i in range(WSLOTS)]
    o = base
    C.m_tab = view(o, [128, 13, 512], F32); o += 26 * K
    C.m_dil_loc = C.m_tab[:, 0:8, :]
    C.m_dil_rem = C.m_tab[:, 8:13, :]
    C.m_cau = C.m_tab[:, 0:4, :]
    C.m_step = C.m_tab[:, 4:8, :]
    C.m_posQ = view(o, [4, 1024], BF16, parts=4); o += 2 * K
    C.m_qT = view(o, [128, 1024], BF16); o += 2 * K
    C.m_kT = view(o, [128, 2048], BF16); o += 4 * K
    C.m_V = view(o, [128, 16, 256], BF16); o += 8 * K
    C.m_qf = view(o, [128, 1024], F32); o += 4 * K
    C.m_selbT = view(o, [8, 1024], BF16, parts=8); o += 2 * K
    C.m_pT = []
    for i in range(3):
        C.m_pT.append(view(o, [128, 512], BF16)); o += K
    C.m_tmp = []
    for i in range(3):
        C.m_tmp.append(view(o, [128, 512], F32)); o += 2 * K
    C.m_tmpx = C.m_tmp + [C.m_tab[:, 8 + i, :] for i in range(4)]
    C.m_pT = C.m_pT + [sb("m_pT3", [128, 512], BF16)]
    C.m_pTx = C.m_pT
    C.m_rl = view(o, [128, 512], F32); o += 2 * K
    C.m_ost = []
    for i in range(2):
        C.m_ost.append(view(o, [128, 512], BF16)); o += K
    C.m_lfrep = view(o, [128, 16, 128], F32); o += 8 * K
    C.m_Fbc = view(o, [128, 1024], F32); o += 4 * K
    C.m_uqb = view(o, [128, 1027], F32); o += 5 * K
    C.m_ukb = view(o, [128, 2051], F32); o += 9 * K
    C.m_acc = view(o, [128, 2048], F32); o += 8 * K
    C.m_hm = []
    for i in range(2):
        C.m_hm.append(view(o, [128, 512], F32)); o += 2 * K
    C.m_sqb = view(o, [128, 512], BF16); o += K
    C.m_omb = []
    for i in range(2):
        C.m_omb.append(view(o, [128, 512], F32)); o += 2 * K
    assert o <= ARENA_BYTES, o
    C.sq = [sb("sq%d" % i, [128, NTOK], BF16) for i in range(2)]
    C.rstd = sb("rstd", [128, NTOK], F32)
    C.tmpf = [sb("tmpf%d" % i, [128, 512], F32) for i in range(2)]
    C.ones_bf = sb("ones_bf", [128, 128], BF16)
    C.epsv = sb("epsv", [128, 1], F32)
    C.gvec = sb("gvec", [128, 4 * NL + 1, KC_D], F32)
    C.stf = [sb("stf%d" % i, [128, 512], F32) for i in range(3)]
    C.stb = [sb("stb%d" % i, [128, 512], BF16) for i in range(4)]
    C.vst = [sb("vst%d" % i, [128, 128], BF16) for i in range(4)]
    C.kms = sb("kms_sb", [128, 16], F32)
    C.wg = sb("wg", [128, KC_D, 8], BF16)
    C.gst = sb("gst", [128, 8, 8], F32)
    C.m_posK = sb("m_posK", [4, 2048], BF16)
    C.m_E8 = sb("m_E8", [8, 1024], BF16)
    C.m_G = sb("m_G", [128, 8, 8], F32)
    C.m_ident = sb("m_ident", [128, 128], F32)
    C.m_U = sb("m_U", [128, 128], F32)
    C.m_rbias = sb("m_rbias", [128, 1], F32)
    C.m_flagB = sb("m_flagB", [128, 1], F32)
    C.m_onesf = sb("m_onesf", [128, 512], F32)
    C.m_cw = sb("m_cw", [128, NL, 8, 4], F32)
    C.m_cb = sb("m_cb", [128, NL, 8], F32)
    C.m_gb = sb("m_gb", [128, NL, 8], F32)
    C.m_gh = sb("m_gh", [128, NL, 8], F32)
    C.m_eps = C.epsv
    C.m_onesb = C.ones_bf
    C.m_kms = sb("m_kms", [128, 8], F32)
    C.m_gs = sb("m_gs", [128, 8], F32)
    C.m_m8 = sb("m_m8", [128, 8], F32)
    C.m_sel = sb("m_sel", [128, 8], F32)
    C.m_val = sb("m_val", [128, 8], F32)
    C.m_gs4 = [C.m_gs] + [sb("m_gs_%d" % i, [128, 8], F32) for i in range(3)]
    C.m_m84 = [C.m_m8] + [sb("m_m8_%d" % i, [128, 8], F32) for i in range(3)]
    C.m_sel4 = [C.m_sel] + [sb("m_sel_%d" % i, [128, 8], F32) for i in range(3)]
    C.m_val4 = [C.m_val] + [sb("m_val_%d" % i, [128, 8], F32) for i in range(3)]
    C.m_gl = sb("m_gl", [128, 16, 8], F32)
    C.m_lf = sb("m_lf", [128, 16, 4], F32)
    C.m_a = sb("m_a", [128, 16, 4], F32)
    C.m_Rrem = sb("m_Rrem", [128, 1], F32)
    for nm in ("n_sq", "n_tmpf", "n_stf", "n_stb", "n_vst", "n_pT", "n_tmp", "n_ost", "n_omb"):
        setattr(C, nm, 0)
    P = C.P
    P.op("dve", lambda e: e.memset(C.ones_bf[:], 1.0), writes=[("ones_bf",), ("c_onesb",)])
    P.op("dve", lambda e: e.memset(C.epsv[:], 1e-6), writes=[("epsv",), ("c_eps",)])
    P.op("dve", lambda e: e.memset(C.m_onesf[:], 1.0), writes=[("c_onesf",)])


def load_small_consts(C, cd, D_):
    P = C.P
    C.cd = cd
    for nm, q in (("posK", "pool"), ("E8", "pool"), ("G", "sp"), ("ident", "sp"), ("U", "sp"), ("rbias", "sp"), ("flagB", "sp")):
        dst = getattr(C, "m_" + nm)
        P.dma(q, lambda e, dst=dst, nm=nm: e.dma_start(out=dst[:], in_=cd[nm]), writes=[("c_" + nm,)])
    P.dma("sp", lambda e: e.dma_start(out=C.m_cw[:], in_=D_["conv_w"]), writes=[("c_cw",)])
    P.dma("sp", lambda e: e.dma_start(out=C.m_cb[:], in_=D_["conv_b"]), writes=[("c_cb",)])
    P.dma("sp", lambda e: e.dma_start(out=C.m_gb[:], in_=D_["gbias"]), writes=[("c_gb",)])
    P.dma("sp", lambda e: e.dma_start(out=C.m_gh[:], in_=D_["ghead"]), writes=[("c_gh",)])
    P.dma("sp", lambda e: e.dma_start(out=C.gvec[:], in_=D_["gains"]), writes=[("gvec", i) for i in range(4 * NL + 1)])


def load_tabs(C, kind):
    P = C.P
    if kind == "mlstm":
        P.op("dve", lambda e: e.memset(C.m_ukb[:, 0:3], 0.0), writes=[("ukb_pad",)])
        return
    P.barrier()
    if kind == "dil":
        P.dma("sp", lambda e: e.dma_start(out=C.m_tab[:, 0:8, :], in_=C.cd["dil_loc"]), writes=[("c_dil_loc",)])
        P.dma("sp", lambda e: e.dma_start(out=C.m_tab[:, 8:13, :], in_=C.cd["dil_rem"]), writes=[("c_dil_rem",)])
    else:
        P.dma("sp", lambda e: e.dma_start(out=C.m_tab[:, 0:4, :], in_=C.cd["cau"]), writes=[("c_cau",)])
        P.dma("sp", lambda e: e.dma_start(out=C.m_tab[:, 4:8, :], in_=C.cd["step"]), writes=[("c_step",)])


XB_ROWS = 3072
XF_ROWS = 1034
PAIRS = [[0, 1], [2, 3], [4, 5], [6, 7]]


def build_fused(nl=NL, phases="AXMC"):
    nc = bass.Bass("TRN2", target_bir_lowering=False)
    with _ES() as es:
        C = Ctx(nc, es)
        P = C.P
        ext = lambda n, sh: C.dram(n, sh, F32, "ExternalInput")
        x_in = ext("x_in", [16, 128, 1024])
        pT = ext("pT", [NL, 2, 128, 1024])
        W = dict(wup1=ext("wup1", [NL, 88, 128, 16, 128]), wdown1=ext("wdown1", [NL, 16, 128, 44, 128]),
                 win=ext("win", [NL, 48, 128, 16, 128]), win_g=ext("win_g", [NL, 128, 16, 8]),
                 wout=ext("wout", [NL, 16, 128, 16, 128]),
                 wup2=ext("wup2", [NL, 88, 128, 16, 128]), wdown2=ext("wdown2", [NL, 16, 128, 44, 128]),
                 wpg=ext("wpg", [NL, 16, 128, 16, 128]), wpp=ext("wpp", [NL, 16, 128, 2, 128]))
        D_ = dict(conv_w=ext("conv_w", [128, NL, 8, 4]), conv_b=ext("conv_b", [128, NL, 8]),
                  gbias=ext("gbias", [128, NL, 8]), ghead=ext("ghead", [128, NL, 8]),
                  gains=ext("gains", [128, 4 * NL + 1, 16]))
        shapes = dict(CONST_SHAPES, flagB=[128, 1])
        cd = {k: ext("c_" + k, sh) for k, sh in shapes.items()}
        out = C.dram("out", [16, 128, 1024], F32, "ExternalOutput")
        I32 = lambda n, sh: nc.dram_tensor(n, sh, F32, kind="Internal").ap()
        I16 = lambda n, sh: nc.dram_tensor(n, sh, BF16, kind="Internal").ap()
        exch = []
        exd = {}

        def mk(n, rows, mkfn):
            a, g = mkfn("x_" + n, [rows, 1024]), mkfn("g_" + n, [2 * rows, 1024])
            exch.append((a, g))
            exd[n] = (a, g)
            return a, g[0:rows, :]
        loc = dict(qa_bf=I16("z_qa_bf", [4, 128, 1024]), qa_f=I32("z_qa_f", [4, 128, 1024]),
                   qc_bf=I16("z_qc_bf", [4, 128, 1024]), om=I32("z_om", [8, 128, 1024]))
        mixT = I16("z_mixT", [16, 128, 1024])
        kk = mk("kk", 1024, I16)
        v0 = mk("v0", 1024, I16)
        v1 = mk("v1", 1024, I16)
        uk = mk("uk", 512, I32)
        uq = mk("uq", 512, I32)
        gk = mk("gk", 128, I32)
        ch = lambda t: t.rearrange("(j p) t -> j p t", p=128)
        vt = lambda t: t.rearrange("(t p a) b -> t p (a b)", t=4, p=128, a=2)
        Lv, Rv = {}, {}
        for i, dst in ((0, Lv), (1, Rv)):
            dst["ka_bf"] = ch(kk[i][0:512, :])
            dst["kc_bf"] = ch(kk[i][512:1024, :])
            dst["v_tm"] = [vt(v0[i]), vt(v1[i])]
            dst["uk"] = ch(uk[i])
            dst["uq"] = ch(uq[i])
            dst["gates"] = gk[i][0:8, :].rearrange("t (p c) -> t p c", c=8)
            dst["kms"] = gk[i][8:10, :].rearrange("a (p c) -> (a p) c", c=16)
        o_A = dict(loc, **Lv)
        v8 = [Lv["v_tm"][0][t] for t in range(4)] + [Lv["v_tm"][1][t] for t in range(4)]
        o_A["v_tm"] = v8
        I_M = dict(loc)
        for k in ("ka_bf", "kms", "kc_bf", "uq", "uk", "v_tm", "gates"):
            I_M[k + "_L"] = Lv[k]
            I_M[k + "_R"] = Rv[k]

        alloc_fused(C)
        load_small_consts(C, cd, D_)
        load_h(C, x_in)
        for l in range(nl):
            if "A" in phases:
                ffn(C, 4 * l + 0, W["wup1"][l], W["wdown1"][l])
                def group_cb(g, keys):
                    if g != "gk" or "X" not in phases:
                        return
                    P.barrier()
                    for (a_, g_) in exch:
                        P.cc(lambda e, a_=a_, g_=g_: e.collective_compute("AllGather", ALU.bypass, replica_groups=PAIRS,
                                                                          ins=[a_.opt()], outs=[g_.opt()]))
                inproj(C, 4 * l + 1, W["win"][l], W["win_g"][l], o_A, group_cb)
            P.barrier()
            C.m_layer = l
            if "M" in phases:
                attn_group(C, "moba", I_M, mixT)
                mlstm_group(C, I_M, mixT)
                attn_group(C, "dil", I_M, mixT)
            P.barrier()
            if "C" in phases:
                outproj(C, mixT, W["wout"][l])
                ffn(C, 4 * l + 2, W["wup2"][l], W["wdown2"][l])
                ple(C, 4 * l + 3, W["wpg"][l], W["wpp"][l], pT[l])
            P.bar = []
        final_norm(C, 4 * NL, out)
        C.P.build(es)
    return nc


def kernel(x, p, g_ffn1, w_up1, w_down1, g_mix, w_in, conv_w, conv_b, b_igate, b_fgate,
           g_head, w_out, g_ffn2, w_up2, w_down2, g_ple, w_ple_gate, w_ple_proj, g_final):
    f = lambda a: np.asarray(a, dtype=np.float32)
    x, p = f(x), f(p)
    nc = build_fused()
    st = lambda w, fn=tiles_fm: np.stack([fn(f(w[l])) for l in range(NL)])
    wi = f(w_in)
    gains = np.stack([vec_fm(g[l]) for l in range(NL) for g in (g_ffn1, g_mix, g_ffn2, g_ple)] + [vec_fm(g_final)], axis=1)
    common = dict(
        wup1=st(w_up1), wdown1=st(w_down1), win=np.stack([tiles_fm(wi[l][:, :6144]) for l in range(NL)]),
        win_g=np.stack([np.ascontiguousarray(wi[l][:, 6144:6152].reshape(16, 128, 8).transpose(1, 0, 2)) for l in range(NL)]),
        wout=st(w_out), wup2=st(w_up2), wdown2=st(w_down2), wpg=st(w_ple_gate), wpp=st(w_ple_proj),
        conv_w=np.ascontiguousarray(np.stack([f(conv_w[l]).reshape(4, 8, 128).transpose(2, 1, 0) for l in range(NL)], axis=1)),
        conv_b=np.ascontiguousarray(np.stack([vec_fm(conv_b[l]) for l in range(NL)], axis=1)),
        gbias=np.ascontiguousarray(np.stack([np.broadcast_to(np.concatenate([f(b_igate[l]), f(b_fgate[l])])[None, :], (128, 8))
                                             for l in range(NL)], axis=1)),
        ghead=np.ascontiguousarray(np.stack([vec_fm(g_head[l]) for l in range(NL)], axis=1)),
        gains=np.ascontiguousarray(gains))
    in_maps = []
    for c in range(8):
        b, s_ = c // 2, c % 2
        d = dict(common)
        d["x_in"] = np.ascontiguousarray(x[b, s_ * 1024:(s_ + 1) * 1024, :].T.reshape(16, 128, 1024))
        d["pT"] = np.ascontiguousarray(np.stack([p[l, b, s_ * 1024:(s_ + 1) * 1024, :].T.reshape(2, 128, 1024) for l in range(NL)]))
        for k, v in mixer_consts(s_ == 1).items():
            d["c_" + k] = v
        d["c_flagB"] = np.full((128, 1), float(s_), np.float32)
        in_maps.append(d)
    res = run_bass_kernel_spmd(nc, in_maps, core_ids=list(range(8))).results
    out = np.empty((4, 2048, 2048), np.float32)
    for c in range(8):
        b, s_ = c // 2, c % 2
        out[b, s_ * 1024:(s_ + 1) * 1024, :] = res[c]["out"].reshape(2048, 1024).T
    return out
```
